# Optimizing a Trainium2 kernel written in Bass

```python
import math
import jax, jax.numpy as jnp
from jax import lax
import numpy as np

D_MODEL = 1024
BATCH = 8
SEQ = 4096
DEPTH = 4

HEAD_DIM = 64
GMLP_WIDTH = D_MODEL // 4
GMLP_GROUPS = GMLP_WIDTH // HEAD_DIM
GMLP_CHUNK = 128
DSA_WIDTH = 3 * D_MODEL // 8
DSA_HEADS = DSA_WIDTH // HEAD_DIM
DSA_PATTERNS = ((128, 1), (512, 4), (2048, 16))
DSA_BLOCK = 64
GDN_WIDTH = D_MODEL - GMLP_WIDTH - DSA_WIDTH
GDN_HEADS = GDN_WIDTH // HEAD_DIM
GDN_CONV = 5
GDN_CHUNK = 64
ROT_DIM = HEAD_DIM // 4
ROPE_THETA = 500000.0
N_EXPERTS = 16
D_EXPERT = D_MODEL
EC_CAPACITY = 2
PLE_DIM = 256
NORM_EPS = 1e-6
MASK_VALUE = -1e30
IN_SPLITS = (GMLP_WIDTH, GMLP_WIDTH, DSA_WIDTH, DSA_WIDTH, DSA_WIDTH,
             3 * GDN_WIDTH, GDN_WIDTH, 2 * GDN_HEADS, 2 * GDN_HEADS)
IN_WIDTH = sum(IN_SPLITS)
MIX_WIDTH = GMLP_WIDTH + DSA_WIDTH + GDN_WIDTH

kernel_name = 'hybrid_bidir_encoder_block'

F32 = jnp.float32


def rmsnorm(x, g):
    xf = x.astype(F32)
    y = xf * lax.rsqrt(jnp.mean(xf * xf, axis=-1, keepdims=True) + NORM_EPS) * g.astype(F32)
    return y.astype(x.dtype)


def l2norm(x):
    return x * lax.rsqrt(jnp.sum(x * x, axis=-1, keepdims=True) + NORM_EPS)


def partial_rotary(t, positions):
    half = ROT_DIM // 2
    inv_freq = ROPE_THETA ** (-jnp.arange(half, dtype=F32) * 2.0 / ROT_DIM)
    ang = positions.astype(F32)[..., None] * inv_freq
    cos = jnp.cos(ang)[:, :, None, :]
    sin = jnp.sin(ang)[:, :, None, :]
    tf = t.astype(F32)
    x1, x2, rest = tf[..., :half], tf[..., half:ROT_DIM], tf[..., ROT_DIM:]
    out = jnp.concatenate([x1 * cos - x2 * sin, x2 * cos + x1 * sin, rest], axis=-1)
    return out.astype(t.dtype)


def gmlp_spatial_gating(u, v, ln_g, ln_b, w_s, b_s):
    B_, S, _ = u.shape
    u = jax.nn.gelu(u).reshape(B_, S, GMLP_GROUPS, HEAD_DIM)
    vf = jax.nn.gelu(v).astype(F32).reshape(B_, S, GMLP_GROUPS, HEAD_DIM)
    mu = jnp.mean(vf, axis=-1, keepdims=True)
    var = jnp.mean(jnp.square(vf - mu), axis=-1, keepdims=True)
    vn = ((vf - mu) * lax.rsqrt(var + NORM_EPS) * ln_g.astype(F32) + ln_b.astype(F32)).astype(u.dtype)
    vc = vn.reshape(B_, S // GMLP_CHUNK, GMLP_CHUNK, GMLP_GROUPS, HEAD_DIM)
    mixed = jnp.einsum('gij,bcjgd->bcigd', w_s, vc) + b_s.T[None, None, :, :, None]
    return (u * mixed.reshape(B_, S, GMLP_GROUPS, HEAD_DIM)).reshape(B_, S, GMLP_WIDTH)


def dilated_branch(q, k, v, window, dil):
    B_, S, H, Dh = q.shape
    steps = window // (2 * dil)
    L = S // dil
    nb = -(-L // DSA_BLOCK)
    Lp = nb * DSA_BLOCK

    def to_sub(t):
        return t.reshape(B_, L, dil, H, Dh).transpose(0, 2, 3, 1, 4)

    def neighbours(t):
        tp = jnp.pad(t, ((0, 0), (0, 0), (0, 0), (DSA_BLOCK, Lp - L + DSA_BLOCK), (0, 0)))
        tp = tp.reshape(B_, dil, H, nb + 2, DSA_BLOCK, Dh)
        return jnp.concatenate([tp[:, :, :, :-2], tp[:, :, :, 1:-1], tp[:, :, :, 2:]], axis=4)

    qs = to_sub(q)
    qb = jnp.pad(qs, ((0, 0), (0, 0), (0, 0), (0, Lp - L), (0, 0))).reshape(B_, dil, H, nb, DSA_BLOCK, Dh)
    kb, vb = neighbours(to_sub(k)), neighbours(to_sub(v))
    qi = jnp.arange(Lp).reshape(nb, DSA_BLOCK)
    kj = jnp.arange(nb)[:, None] * DSA_BLOCK - DSA_BLOCK + jnp.arange(3 * DSA_BLOCK)[None, :]
    rel = kj[:, None, :] - qi[:, :, None]
    mask = (jnp.abs(rel) <= steps) & (kj[:, None, :] >= 0) & (kj[:, None, :] < L)
    s = jnp.einsum('bxhntc,bxhnsc->bxhnts', qb, kb).astype(F32) * (Dh ** -0.5)
    s = jnp.where(mask, s, MASK_VALUE)
    lse = jax.nn.logsumexp(s, axis=-1)
    pr = jnp.exp(s - lse[..., None]).astype(v.dtype)
    o = jnp.einsum('bxhnts,bxhnsc->bxhntc', pr, vb)
    o = o.reshape(B_, dil, H, Lp, Dh)[:, :, :, :L].transpose(0, 3, 1, 2, 4).reshape(B_, S, H, Dh)
    lse = lse.reshape(B_, dil, H, Lp)[..., :L].transpose(0, 3, 1, 2).reshape(B_, S, H)
    return o, lse


def dilated_attention(q, k, v, positions, q_norm_g, k_norm_g):
    B_, S, _ = q.shape
    heads = lambda t: t.reshape(B_, S, DSA_HEADS, HEAD_DIM)
    q = partial_rotary(rmsnorm(heads(q), q_norm_g), positions)
    k = partial_rotary(rmsnorm(heads(k), k_norm_g), positions)
    v = heads(v)
    outs, lses = [], []
    for window, dil in DSA_PATTERNS:
        o, l = dilated_branch(q, k, v, window, dil)
        outs.append(o)
        lses.append(l)
    wts = jax.nn.softmax(jnp.stack(lses), axis=0).astype(v.dtype)
    o = jnp.einsum('pbsh,pbshd->bshd', wts, jnp.stack(outs))
    return o.reshape(B_, S, DSA_WIDTH)


def centred_depthwise_conv(x, w):
    K = w.shape[0]
    return lax.conv_general_dilated(x, w[:, None, :].astype(x.dtype), window_strides=(1,),
                                    padding=[(K // 2, K // 2)],
                                    dimension_numbers=('NWC', 'WIO', 'NWC'),
                                    feature_group_count=x.shape[-1])


def gated_delta_chunked(q, k, v, g, beta):
    B_, H, S, Dk = k.shape
    Dv = v.shape[-1]
    C = GDN_CHUNK
    N = S // C
    chunk = lambda t: t.reshape((B_, H, N, C) + t.shape[3:])
    qc, kc, vc = chunk(q * (Dk ** -0.5)), chunk(k), chunk(v)
    gc = jnp.cumsum(chunk(g), axis=-1)
    bc = chunk(beta)
    incl = jnp.tril(jnp.ones((C, C), bool))
    strict = jnp.tril(jnp.ones((C, C), bool), -1)
    decay = jnp.exp(jnp.where(incl, gc[..., :, None] - gc[..., None, :], -jnp.inf))
    k_beta = kc * bc[..., None]
    lower = jnp.where(strict, jnp.einsum('bhntd,bhnsd->bhnts', k_beta, kc) * decay, 0.0)
    eye = jnp.eye(C, dtype=F32)
    t_inv = lax.linalg.triangular_solve(lower + eye, jnp.broadcast_to(eye, lower.shape),
                                        left_side=True, lower=True, unit_diagonal=True)
    u = t_inv @ (vc * bc[..., None])
    w = t_inv @ (k_beta * jnp.exp(gc)[..., None])
    qk = jnp.where(incl, jnp.einsum('bhntd,bhnsd->bhnts', qc, kc) * decay, 0.0)

    def step(state, inp):
        q_n, k_n, u_n, w_n, g_n, qk_n = inp
        v_new = u_n - w_n @ state
        o_n = (q_n * jnp.exp(g_n)[..., None]) @ state + qk_n @ v_new
        g_last = g_n[..., -1:]
        state = state * jnp.exp(g_last)[..., None] + jnp.einsum(
            'bhtd,bhte->bhde', k_n * jnp.exp(g_last - g_n)[..., None], v_new)
        return state, o_n

    xs = tuple(jnp.moveaxis(t, 2, 0) for t in (qc, kc, u, w, gc, qk))
    _, o = lax.scan(step, jnp.zeros((B_, H, Dk, Dv), F32), xs)
    return jnp.moveaxis(o, 0, 2).reshape(B_, H, S, Dv)


def gated_deltanet(qkv, gate, a, b, conv_w, a_log, dt_bias, o_norm_g):
    B_, S, _ = qkv.shape
    out_dtype = qkv.dtype
    qkv = jax.nn.silu(centred_depthwise_conv(qkv, conv_w))
    q, k, v = jnp.split(qkv, 3, axis=-1)
    heads = lambda t: t.reshape(B_, S, GDN_HEADS, HEAD_DIM).transpose(0, 2, 1, 3).astype(F32)
    q, k, v = l2norm(heads(q)), l2norm(heads(k)), heads(v)
    a = a.astype(F32).reshape(B_, S, 2, GDN_HEADS).transpose(2, 0, 3, 1)
    b = b.astype(F32).reshape(B_, S, 2, GDN_HEADS).transpose(2, 0, 3, 1)
    g = -jnp.exp(a_log.astype(F32))[:, None, :, None] * jax.nn.softplus(a + dt_bias.astype(F32)[:, None, :, None])
    beta = jax.nn.sigmoid(b)
    o_f = gated_delta_chunked(q, k, v, g[0], beta[0])
    flip = lambda t: jnp.flip(t, axis=2)
    o_b = flip(gated_delta_chunked(flip(q), flip(k), flip(v), flip(g[1]), flip(beta[1])))
    o = (o_f + o_b).transpose(0, 2, 1, 3)
    o = rmsnorm(o, o_norm_g) * jax.nn.silu(gate.astype(F32).reshape(B_, S, GDN_HEADS, HEAD_DIM))
    return o.reshape(B_, S, GDN_WIDTH).astype(out_dtype)


def expert_choice_ffn(h, w_router, w_gate, w_up, w_down):
    B_, S, D = h.shape
    cap = EC_CAPACITY * S // N_EXPERTS
    logits = jnp.einsum('bsd,de->bse', h, w_router).astype(F32)
    aff = jax.nn.softmax(logits, axis=-1)
    gate, idx = lax.top_k(aff.transpose(0, 2, 1), cap)
    xe = jax.vmap(lambda hb, ib: hb[ib])(h, idx)
    hid = jax.nn.silu(jnp.einsum('becd,edf->becf', xe, w_gate)) * jnp.einsum('becd,edf->becf', xe, w_up)
    ye = jnp.einsum('becf,efd->becd', hid, w_down) * gate[..., None].astype(h.dtype)
    flat = (idx + (jnp.arange(B_, dtype=jnp.int32) * S)[:, None, None]).reshape(-1)
    y = jnp.zeros((B_ * S, D), h.dtype).at[flat].add(ye.reshape(-1, D))
    return y.reshape(B_, S, D)


def setup_inputs(seed: int = 0) -> dict:
    key = jax.random.key(seed)
    ks = jax.random.split(key, 26)

    def nrm(k, shape, fan_in):
        return jax.random.normal(k, shape, F32) * (fan_in ** -0.5)

    def gain(k, shape):
        return 1.0 + 0.02 * jax.random.normal(k, shape, F32)

    x = jax.random.normal(ks[0], (BATCH, SEQ, D_MODEL), F32)
    p = jax.random.normal(ks[1], (DEPTH, BATCH, SEQ, PLE_DIM), F32)
    positions = jnp.arange(SEQ, dtype=jnp.int32)[None, :] + jax.random.randint(ks[2], (BATCH, 1), 0, 1024, dtype=jnp.int32)
    dt = jnp.exp(jax.random.uniform(ks[13], (DEPTH, 2, GDN_HEADS), F32, math.log(1e-3), math.log(1e-1)))
    return {
        'x': x,
        'p': p,
        'positions': positions.astype(jnp.int32),
        'g_mix': gain(ks[3], (DEPTH, D_MODEL)),
        'w_in': nrm(ks[4], (DEPTH, D_MODEL, IN_WIDTH), D_MODEL),
        'ln_v_g': gain(ks[5], (DEPTH, GMLP_GROUPS, HEAD_DIM)),
        'ln_v_b': 0.02 * jax.random.normal(ks[6], (DEPTH, GMLP_GROUPS, HEAD_DIM), F32),
        'w_s': nrm(ks[7], (DEPTH, GMLP_GROUPS, GMLP_CHUNK, GMLP_CHUNK), GMLP_CHUNK),
        'b_s': 1.0 + 0.01 * jax.random.normal(ks[8], (DEPTH, GMLP_GROUPS, GMLP_CHUNK), F32),
        'q_norm_g': gain(ks[9], (DEPTH, HEAD_DIM)),
        'k_norm_g': gain(ks[10], (DEPTH, HEAD_DIM)),
        'conv_w': nrm(ks[11], (DEPTH, GDN_CONV, 3 * GDN_WIDTH), GDN_CONV),
        'a_log': jnp.log(jax.random.uniform(ks[12], (DEPTH, 2, GDN_HEADS), F32, 1.0, 16.0)),
        'dt_bias': dt + jnp.log(-jnp.expm1(-dt)),
        'o_norm_g': gain(ks[14], (DEPTH, HEAD_DIM)),
        'w_out': nrm(ks[15], (DEPTH, MIX_WIDTH, D_MODEL), MIX_WIDTH),
        'g_ffn': gain(ks[16], (DEPTH, D_MODEL)),
        'w_router': nrm(ks[17], (DEPTH, D_MODEL, N_EXPERTS), D_MODEL),
        'w_e_gate': nrm(ks[18], (DEPTH, N_EXPERTS, D_MODEL, D_EXPERT), D_MODEL),
        'w_e_up': nrm(ks[19], (DEPTH, N_EXPERTS, D_MODEL, D_EXPERT), D_MODEL),
        'w_e_down': nrm(ks[20], (DEPTH, N_EXPERTS, D_EXPERT, D_MODEL), D_EXPERT),
        'w_ple': nrm(ks[21], (DEPTH, PLE_DIM, D_MODEL), PLE_DIM),
        'g_ple': gain(ks[22], (DEPTH, D_MODEL)),
        'g_ple_gate': gain(ks[23], (DEPTH, D_MODEL)),
        'w_ple_gate': nrm(ks[24], (DEPTH, D_MODEL, D_MODEL), D_MODEL),
    }


def reference(x, p, positions, g_mix, w_in, ln_v_g, ln_v_b, w_s, b_s, q_norm_g, k_norm_g,
              conv_w, a_log, dt_bias, o_norm_g, w_out, g_ffn, w_router, w_e_gate, w_e_up,
              w_e_down, w_ple, g_ple, g_ple_gate, w_ple_gate):
    split_points = np.cumsum(IN_SPLITS)[:-1].tolist()
    for i in range(DEPTH):
        xn = rmsnorm(x, g_mix[i])
        proj = jnp.einsum('bsd,dn->bsn', xn, w_in[i])
        a_u, a_v, b_q, b_k, b_v, c_qkv, c_gate, c_a, c_b = jnp.split(proj, split_points, axis=-1)
        y_a = gmlp_spatial_gating(a_u, a_v, ln_v_g[i], ln_v_b[i], w_s[i], b_s[i])
        y_b = dilated_attention(b_q, b_k, b_v, positions, q_norm_g[i], k_norm_g[i])
        y_c = gated_deltanet(c_qkv, c_gate, c_a, c_b, conv_w[i], a_log[i], dt_bias[i], o_norm_g[i])
        x = x + jnp.einsum('bsm,md->bsd', jnp.concatenate([y_a, y_b, y_c], axis=-1), w_out[i])
        x = x + expert_choice_ffn(rmsnorm(x, g_ffn[i]), w_router[i], w_e_gate[i], w_e_up[i], w_e_down[i])
        e = rmsnorm(jnp.einsum('bsq,qd->bsd', p[i], w_ple[i]), g_ple[i])
        gate = jax.nn.sigmoid(jnp.einsum('bsd,de->bse', rmsnorm(x, g_ple_gate[i]), w_ple_gate[i]))
        x = x + e * gate
    return x
```

```python
import numpy as np
from contextlib import ExitStack
import concourse.bass as bass
import concourse.mybir as mybir
from concourse.bass_utils import run_bass_kernel_spmd

F32 = mybir.dt.float32
BF16 = mybir.dt.bfloat16
I32 = mybir.dt.int32
ALU = mybir.AluOpType
AF = mybir.ActivationFunctionType
AX = mybir.AxisListType

ENGS = ['pe', 'dve', 'act', 'pool', 'sp']
DMA_ENGS = ['sp', 'pool', 'act']
NDS = 8
EPS = 1e-6
S = 4096
NT = 32


class Prog:
    def __init__(self):
        self.nc = bass.Bass("TRN2", target_bir_lowering=False)
        self.stack = ExitStack()
        self.sems = {}
        for e in ENGS:
            self.sems[e] = self.stack.enter_context(self.nc.semaphore('s_' + e))
        self.dcount = {}
        for e in DMA_ENGS:
            for i in range(NDS):
                k = 'd_%s_%d' % (e, i)
                self.sems[k] = self.stack.enter_context(self.nc.semaphore(k))
                self.dcount[k] = 0
        self.dnext = {e: 0 for e in DMA_ENGS}
        self.cnt = {e: 0 for e in ENGS}
        self.waited = {e: {} for e in ENGS}
        self.q = {e: [] for e in ENGS}
        self.lastw = {}
        self.readers = {}
        self.nops = 0
        self.xkeys = set(['pT', 'pv', 'pq', 'pk', 'pbv', 'pg', 'pf', 'ptr', 'pm', 'pss', 'ppv', 'pd', 'ptb', 'po', 'pbig', 'pl', 'pu', 'py', 'pe', 'pA', 'pB', 'pC', 'pD'])

    def _deps(self, eng, reads, writes):
        deps = {}

        def add(m):
            if m is None:
                return
            k, v = m
            if eng == 'pe' and k == 'pe':
                return
            if deps.get(k, 0) < v:
                deps[k] = v
        for r in reads:
            add(self.lastw.get(r))
        for w in writes:
            add(self.lastw.get(w))
            for m in self.readers.get(w, ()):
                add(m)
        out = []
        wd = self.waited[eng]
        for k, v in deps.items():
            if wd.get(k, 0) < v:
                wd[k] = v
                out.append((k, v))
        return out

    def _mark(self, mark, reads, writes):
        for w in writes:
            self.lastw[w] = mark
            self.readers[w] = []
        for r in reads:
            if r in writes:
                continue
            self.readers.setdefault(r, []).append(mark)

    cut = None
    pc = 0

    def isx(self, k):
        n = k[0] if isinstance(k, tuple) else k
        return isinstance(n, str) and n in self.xkeys

    def op(self, eng, fn, reads=(), writes=()):
        self.pc += 1
        if self.cut is not None and self.pc > self.cut:
            return
        xr = [r for r in reads if self.isx(r) and r not in writes]
        if xr:
            writes = list(writes) + xr
        waits = self._deps(eng, reads, writes)
        self.cnt[eng] += 1
        mark = (eng, self.cnt[eng])
        self.q[eng].append((waits, fn, (eng, 1)))
        self._mark(mark, reads, writes)
        self.nops += 1

    def dma(self, eng, out, in_, reads=(), writes=(), **kw):
        self.pc += 1
        if self.cut is not None and self.pc > self.cut:
            return
        waits = self._deps(eng, reads, writes)
        i = self.dnext[eng]
        self.dnext[eng] = (i + 1) % NDS
        k = 'd_%s_%d' % (eng, i)
        c = self.dcount[k]
        wd = self.waited[eng]
        if c > 0 and wd.get(k, 0) < 16 * c:
            wd[k] = 16 * c
            waits.append((k, 16 * c))
        self.dcount[k] = c + 1
        mark = (k, 16 * (c + 1))
        self.q[eng].append((waits, (lambda e: e.dma_start(out=out, in_=in_, **kw)), (k, 16)))
        self._mark(mark, reads, writes)
        self.nops += 1

    _breg = None

    def breg(self, e):
        if self._breg is None:
            self._breg = e.to_reg(S - 1)
        return self._breg

    def idma(self, fn, reads=(), writes=()):
        eng = 'pool'
        self.pc += 1
        waits = self._deps(eng, reads, writes)
        i = self.dnext[eng]
        self.dnext[eng] = (i + 1) % NDS
        k = 'd_%s_%d' % (eng, i)
        c = self.dcount[k]
        wd = self.waited[eng]
        if c > 0 and wd.get(k, 0) < 16 * c:
            wd[k] = 16 * c
            waits.append((k, 16 * c))
        self.dcount[k] = c + 1
        mark = (k, 16 * (c + 1))
        self.q[eng].append((waits, fn, (k, 16)))
        self._mark(mark, reads, writes)
        self.nops += 1

    def barrier(self):
        for e in ENGS:
            waits = []
            wd = self.waited[e]
            for o in ENGS:
                if o != e and self.cnt[o] > wd.get(o, 0):
                    wd[o] = self.cnt[o]
                    waits.append((o, self.cnt[o]))
            for k, c in self.dcount.items():
                if 16 * c > wd.get(k, 0):
                    wd[k] = 16 * c
                    waits.append((k, 16 * c))
            if waits:
                self.q[e].append((waits, None, None))
        self.lastw = {}
        self.readers = {}

    def emit(self):
        self.barrier()
        nc = self.nc
        sems = self.sems
        q = self.q

        def replay(name, e):
            for waits, fn, inc in q[name]:
                for k, v in waits:
                    e.wait_ge(sems[k], v)
                if fn is not None:
                    ins = fn(e)
                    ins.then_inc(sems[inc[0]], inc[1])

        with nc.Block() as block:
            @block.tensor
            def _(e):
                replay('pe', e)

            @block.vector
            def _(e):
                replay('dve', e)

            @block.scalar
            def _(e):
                replay('act', e)

            @block.gpsimd
            def _(e):
                replay('pool', e)

            @block.sync
            def _(e):
                replay('sp', e)
        self.q = {e: [] for e in ENGS}

    uid = 0

    def sb(self, stack, name, shape, dt):
        self.uid += 1
        return stack.enter_context(self.nc.sbuf_tensor('%s_%d' % (name, self.uid), list(shape), dt))

    def ps(self, stack, name, shape, dt=F32, keys=()):
        for k in keys:
            self.xkeys.add(k)
        self.uid += 1
        return stack.enter_context(self.nc.psum_tensor('%s_%d' % (name, self.uid), list(shape), dt))

    def dram(self, name, shape, dt, kind="Internal"):
        return self.nc.dram_tensor(name, list(shape), dt, kind=kind).ap()


def ssl(a, n, d):
    return slice(a, a + (n - 1) * d + 1, d)


def bc(ap, shape):
    return ap.to_broadcast(list(shape))


def phase_rope(P, D):
    with ExitStack() as s:
        pi_ = P.sb(s, 'r_pi', [128, NT], I32)
        pf = P.sb(s, 'r_pf', [128, NT], F32)
        invf = P.sb(s, 'r_invf', [128, 8], F32)
        ang = P.sb(s, 'r_ang', [128, 2, NT, 8], F32)
        kk = P.sb(s, 'r_kk', [128, 2, NT, 8], F32)
        ki = P.sb(s, 'r_ki', [128, 2, NT, 8], I32)
        cs = P.sb(s, 'r_cs', [128, NT, 16], F32)
        P.dma('sp', pi_[:], D['positions'], writes=['pi'])
        P.dma('sp', invf[:], D['invf'].partition_broadcast(128), writes=['invf'])
        P.op('dve', lambda e: e.tensor_copy(pf[:], pi_[:]), reads=['pi'], writes=['pf'])
        P.op('dve', lambda e: e.tensor_tensor(ang[:, 1], bc(pf[:].unsqueeze(2), [128, NT, 8]), bc(invf[:].unsqueeze(1), [128, NT, 8]), ALU.mult),
             reads=['pf', 'invf'], writes=['ang1'])
        P.op('dve', lambda e: e.tensor_scalar(ang[:, 0], ang[:, 1], float(np.pi / 2), None, ALU.add), reads=['ang1'], writes=['ang0'])
        A = ang[:].rearrange('p a t c -> p (a t c)')
        K = kk[:].rearrange('p a t c -> p (a t c)')
        KI = ki[:].rearrange('p a t c -> p (a t c)')
        P.op('dve', lambda e: e.tensor_scalar(K, A, float(1.0 / (2 * np.pi)), None, ALU.mult), reads=['ang0', 'ang1'], writes=['kk'])
        P.op('dve', lambda e: e.tensor_copy(KI, K), reads=['kk'], writes=['ki'])
        P.op('dve', lambda e: e.tensor_copy(K, KI), reads=['ki'], writes=['kk'])
        C1 = 6.28125
        C2 = float(2 * np.pi - 6.28125)
        P.op('dve', lambda e: e.scalar_tensor_tensor(A, K, -C1, A, ALU.mult, ALU.add), reads=['kk', 'ang0', 'ang1'], writes=['ang'])
        P.op('dve', lambda e: e.scalar_tensor_tensor(A, K, -C2, A, ALU.mult, ALU.add), reads=['kk', 'ang'], writes=['ang'])
        P.op('dve', lambda e: e.tensor_scalar(A, A, 3.1415925, -3.1415925, ALU.min, ALU.max), reads=['ang'], writes=['ang'])
        P.op('act', lambda e: e.activation(cs[:, :, 0:8], ang[:, 0], AF.Sin), reads=['ang'], writes=['cs0'])
        P.op('act', lambda e: e.activation(cs[:, :, 8:16], ang[:, 1], AF.Sin), reads=['ang'], writes=['cs1'])
        P.dma('sp', D['cs'].rearrange('(t p) c -> p t c', p=128), cs[:], reads=['cs0', 'cs1'], writes=['d_cs'])
        P.emit()


def phase_A(P, l, D, first):
    xsrc = D['x'] if first else D['xw']
    with ExitStack() as s:
        wbf = P.sb(s, 'a_wbf', [128, 8, 3224], BF16)
        gmix = P.sb(s, 'a_gmix', [128, 1024], F32)
        lng = P.sb(s, 'a_lng', [128, 256], F32)
        lnb = P.sb(s, 'a_lnb', [128, 256], F32)
        qkg = P.sb(s, 'a_qkg', [128, 2, 64], F32)
        cs = P.sb(s, 'a_cs', [128, NT, 16], F32)
        idb = P.sb(s, 'a_idb', [128, 128], BF16)
        xts = [P.sb(s, 'a_xt%d' % i, [128, 1024], F32) for i in range(2)]
        junk = P.sb(s, 'a_junk', [128, 1024], BF16)
        ss = P.sb(s, 'a_ss', [128, 2], F32)
        xn = P.sb(s, 'a_xn', [128, 1024], BF16)
        xnT = [P.sb(s, 'a_xnT%d' % i, [128, 8, 512], BF16) for i in range(2)]
        ge = P.sb(s, 'a_ge', [128, 4, 64], F32)
        cen = P.sb(s, 'a_cen', [128, 4, 64], F32)
        sq = P.sb(s, 'a_sq', [128, 4, 64], F32)
        m4 = P.sb(s, 'a_m4', [128, 8], F32)
        vnb = [P.sb(s, 'a_vnb%d' % i, [128, 256], BF16) for i in range(2)]
        sqq = P.sb(s, 'a_sqq', [128, 12, 64], F32)
        ss12 = P.sb(s, 'a_ss12', [128, 12], F32)
        qk32 = P.sb(s, 'a_qk32', [128, 12, 64], F32)
        rt = P.sb(s, 'a_rt', [128, 4, 12, 8], F32)
        qkb = P.sb(s, 'a_qkb', [128, 12, 64], BF16)
        qkTs = [P.sb(s, 'a_qkTs%d' % i, [128, 6, 128], BF16) for i in range(2)]
        vaug = [P.sb(s, 'a_vaug%d' % i, [128, 6, 65], BF16) for i in range(2)]
        gs = [P.sb(s, 'a_gs%d' % i, [128, 408], F32) for i in range(2)]
        fo = [P.sb(s, 'a_fo%d' % i, [128, 512], F32) for i in range(2)]
        pT = P.ps(s, 'a_pT', [128, 1024], BF16)
        pv = P.ps(s, 'a_pv', [128, 512])
        pq = P.ps(s, 'a_pq', [128, 512])
        pk = P.ps(s, 'a_pk', [128, 512])
        pbv = P.ps(s, 'a_pbv', [128, 512])
        pg = P.ps(s, 'a_pg', [128, 512])
        pf = [P.ps(s, 'a_pf%d' % i, [128, 512]) for i in range(2)]

        for k in range(8):
            P.dma('pool', wbf[:, k, :], D['w_in'][l, k * 128:(k + 1) * 128, :], writes=[('wbf', k)])
        P.dma('sp', gmix[:], D['g_mix'][l].partition_broadcast(128), writes=['gmix'])
        P.dma('sp', lng[:], D['ln_v_g'][l].rearrange('g d -> (g d)').partition_broadcast(128), writes=['lng'])
        P.dma('sp', lnb[:], D['ln_v_b'][l].rearrange('g d -> (g d)').partition_broadcast(128), writes=['lnb'])
        P.dma('sp', qkg[:, 0, :], D['q_norm_g'][l].partition_broadcast(128), writes=['qkg0'])
        P.dma('sp', qkg[:, 1, :], D['k_norm_g'][l].partition_broadcast(128), writes=['qkg1'])
        P.dma('sp', cs[:], D['cs'].rearrange('(t p) c -> p t c', p=128), reads=['d_cs'], writes=['cs'])
        P.dma('pool', idb[:], D['ident'], writes=['idb'])
        for i in range(2):
            P.op('pool', lambda e, i=i: e.memset(vaug[i][:, :, 64:65], 1.0), writes=[('vaug', i)])
        WB = [('wbf', k) for k in range(8)]
        fcount = 0
        for g in range(8):
            XT = xnT[g % 2]
            kxt = ('xnT', g % 2)
            for j in range(4):
                t = g * 4 + j
                b = t % 2
                xt = xts[b]
                P.dma('sp', xt[:], xsrc[t * 128:(t + 1) * 128, :], writes=[('xt', b)])
                P.op('dve', lambda e, b=b: e.memset(ss[:, b:b + 1], 0.0), writes=[('ss', b)])
                P.op('act', lambda e, xt=xt, b=b: e.activation(junk[:], xt[:], AF.Square, accum_out=ss[:, b:b + 1]),
                     reads=[('xt', b), ('ss', b)], writes=['junk', ('ss', b)])
                P.op('act', lambda e, b=b: e.activation(ss[:, b:b + 1], ss[:, b:b + 1], AF.Sqrt, bias=EPS, scale=1.0 / 1024),
                     reads=[('ss', b)], writes=[('ss', b)])
                P.op('dve', lambda e, b=b: e.reciprocal(ss[:, b:b + 1], ss[:, b:b + 1]), reads=[('ss', b)], writes=[('ss', b)])
                P.op('dve', lambda e, xt=xt, b=b: e.scalar_tensor_tensor(xn[:], xt[:], ss[:, b:b + 1], gmix[:], ALU.mult, ALU.mult),
                     reads=[('xt', b), ('ss', b), 'gmix'], writes=['xn'])
                for k in range(8):
                    P.op('pe', lambda e, k=k: e.transpose(pT[:, k * 128:(k + 1) * 128], xn[:, k * 128:(k + 1) * 128], idb[:]),
                         reads=['xn', 'idb'], writes=['pT'])
                P.op('act', lambda e, XT=XT, j=j: e.copy(XT[:, :, j * 128:(j + 1) * 128], pT[:].rearrange('p (k t) -> p k t', t=128)),
                     reads=['pT'], writes=[kxt + (j,)])
                for (pp, nm, c0, c1) in ((pv, 'pv', 256, 512), (pq, 'pq', 512, 896), (pk, 'pk', 896, 1280), (pbv, 'pbv', 1280, 1664), (pg, 'pg', 2816, 3224)):
                    for k in range(8):
                        P.op('pe', lambda e, pp=pp, k=k, c0=c0, c1=c1, XT=XT, j=j: e.matmul(pp[:, 0:c1 - c0], XT[:, k, j * 128:(j + 1) * 128], wbf[:, k, c0:c1], start=(k == 0), stop=(k == 7)),
                             reads=[kxt + (j,), ('wbf', k)], writes=[nm])
                GE = ge[:].rearrange('p a b -> p (a b)')
                P.op('act', lambda e: e.activation(GE, pv[:, 0:256], AF.Gelu_apprx_tanh), reads=['pv'], writes=['ge'])
                P.op('dve', lambda e: e.tensor_reduce(m4[:, 0:4], ge[:], AX.X, ALU.add), reads=['ge'], writes=['m4a'])
                P.op('dve', lambda e: e.tensor_scalar(m4[:, 0:4], m4[:, 0:4], 1.0 / 64, None, ALU.mult), reads=['m4a'], writes=['m4a'])
                P.op('dve', lambda e: e.tensor_tensor(cen[:], ge[:], bc(m4[:, 0:4].unsqueeze(2), [128, 4, 64]), ALU.subtract), reads=['ge', 'm4a'], writes=['cen'])
                P.op('act', lambda e: e.activation(sq[:], cen[:], AF.Square), reads=['cen'], writes=['sq'])
                P.op('dve', lambda e: e.tensor_reduce(m4[:, 4:8], sq[:], AX.X, ALU.add), reads=['sq'], writes=['m4b'])
                P.op('act', lambda e: e.activation(m4[:, 4:8], m4[:, 4:8], AF.Sqrt, bias=EPS, scale=1.0 / 64), reads=['m4b'], writes=['m4b'])
                P.op('dve', lambda e: e.reciprocal(m4[:, 4:8], m4[:, 4:8]), reads=['m4b'], writes=['m4b'])
                P.op('dve', lambda e: e.tensor_tensor(cen[:], cen[:], bc(m4[:, 4:8].unsqueeze(2), [128, 4, 64]), ALU.mult), reads=['cen', 'm4b'], writes=['cen'])
                CEN = cen[:].rearrange('p a b -> p (a b)')
                P.op('pool', lambda e: e.tensor_tensor(CEN, CEN, lng[:], ALU.mult), reads=['cen', 'lng'], writes=['cen'])
                P.op('pool', lambda e, b=b: e.tensor_tensor(vnb[b][:], CEN, lnb[:], ALU.add), reads=['cen', 'lnb'], writes=[('vnb', b)])
                P.dma('sp', D['vn'][t * 128:(t + 1) * 128, :], vnb[b][:], reads=[('vnb', b)], writes=['d_vn'])
                P.op('act', lambda e: e.activation(sqq[:, 0:6, :].rearrange('p a b -> p (a b)'), pq[:, 0:384], AF.Square), reads=['pq'], writes=['sqq0'])
                P.op('act', lambda e: e.activation(sqq[:, 6:12, :].rearrange('p a b -> p (a b)'), pk[:, 0:384], AF.Square), reads=['pk'], writes=['sqq1'])
                P.op('dve', lambda e: e.tensor_reduce(ss12[:], sqq[:], AX.X, ALU.add), reads=['sqq0', 'sqq1'], writes=['ss12'])
                P.op('act', lambda e: e.activation(ss12[:], ss12[:], AF.Sqrt, bias=EPS, scale=1.0 / 64), reads=['ss12'], writes=['ss12'])
                P.op('dve', lambda e: e.reciprocal(ss12[:], ss12[:]), reads=['ss12'], writes=['ss12'])
                P.op('dve', lambda e: e.tensor_tensor(qk32[:, 0:6, :], pq[:, 0:384].rearrange('p (a b) -> p a b', b=64), bc(ss12[:, 0:6].unsqueeze(2), [128, 6, 64]), ALU.mult),
                     reads=['pq', 'ss12'], writes=['qk32a'])
                P.op('dve', lambda e: e.tensor_tensor(qk32[:, 6:12, :], pk[:, 0:384].rearrange('p (a b) -> p a b', b=64), bc(ss12[:, 6:12].unsqueeze(2), [128, 6, 64]), ALU.mult),
                     reads=['pk', 'ss12'], writes=['qk32b'])
                P.op('pool', lambda e: e.tensor_tensor(qk32[:, 0:6, :], qk32[:, 0:6, :], bc(qkg[:, 0:1, :], [128, 6, 64]), ALU.mult), reads=['qk32a', 'qkg0'], writes=['qk32a'])
                P.op('pool', lambda e: e.tensor_tensor(qk32[:, 6:12, :], qk32[:, 6:12, :], bc(qkg[:, 1:2, :], [128, 6, 64]), ALU.mult), reads=['qk32b', 'qkg1'], writes=['qk32b'])
                cosb = bc(cs[:, t:t + 1, 0:8], [128, 12, 8])
                sinb = bc(cs[:, t:t + 1, 8:16], [128, 12, 8])
                x1 = qk32[:, :, 0:8]
                x2 = qk32[:, :, 8:16]
                P.op('pool', lambda e, cosb=cosb: e.tensor_tensor(rt[:, 0], x1, cosb, ALU.mult), reads=['qk32a', 'qk32b', 'cs'], writes=['rt0'])
                P.op('pool', lambda e, sinb=sinb: e.tensor_tensor(rt[:, 1], x2, sinb, ALU.mult), reads=['qk32a', 'qk32b', 'cs'], writes=['rt1'])
                P.op('dve', lambda e, cosb=cosb: e.tensor_tensor(rt[:, 2], x2, cosb, ALU.mult), reads=['qk32a', 'qk32b', 'cs'], writes=['rt2'])
                P.op('dve', lambda e, sinb=sinb: e.tensor_tensor(rt[:, 3], x1, sinb, ALU.mult), reads=['qk32a', 'qk32b', 'cs'], writes=['rt3'])
                P.op('act', lambda e: e.copy(qkb[:], qk32[:]), reads=['qk32a', 'qk32b'], writes=['qkb'])
                P.op('dve', lambda e: e.tensor_tensor(qkb[:, :, 0:8], rt[:, 0], rt[:, 1], ALU.subtract), reads=['rt0', 'rt1', 'qkb'], writes=['qkb'])
                P.op('dve', lambda e: e.tensor_tensor(qkb[:, :, 8:16], rt[:, 2], rt[:, 3], ALU.add), reads=['rt2', 'rt3', 'qkb'], writes=['qkb'])
                for i in range(6):
                    P.op('pe', lambda e, i=i: e.transpose(pT[:, i * 128:(i + 1) * 128], qkb[:, 2 * i:2 * i + 2, :].rearrange('p a b -> p (a b)'), idb[:]),
                         reads=['qkb', 'idb'], writes=['pT'])
                P.op('act', lambda e, b=b: e.copy(qkTs[b][:], pT[:, 0:768].rearrange('p (k t) -> p k t', t=128)), reads=['pT'], writes=[('qkTs', b)])
                P.dma('sp', D['qkT'][:, :, t * 128:(t + 1) * 128].rearrange('i p t -> p i t'), qkTs[b][:], reads=[('qkTs', b)], writes=['d_qkT'])
                P.op('act', lambda e, b=b: e.copy(vaug[b][:, :, 0:64], pbv[:, 0:384].rearrange('p (a b) -> p a b', b=64)), reads=['pbv', ('vaug', b)], writes=[('vaug', b)])
                P.dma('sp', D['vaug'][t * 128:(t + 1) * 128, :], vaug[b][:].rearrange('p a b -> p (a b)'), reads=[('vaug', b)], writes=['d_vaug'])
                P.op('act', lambda e, b=b: e.activation(gs[b][:, 0:384], pg[:, 0:384], AF.Silu), reads=['pg'], writes=[('gs', b)])
                P.op('dve', lambda e, b=b: e.tensor_copy(gs[b][:, 384:408], pg[:, 384:408]), reads=['pg', ('gs', b)], writes=[('gs', b)])
                P.dma('sp', D['gate_s'][t * 128:(t + 1) * 128, :], gs[b][:, 0:384], reads=[('gs', b)], writes=['d_gate'])
                P.dma('sp', D['ab'][t * 128:(t + 1) * 128, :], gs[b][:, 384:408], reads=[('gs', b)], writes=['d_ab'])
            allx = [kxt + (j,) for j in range(4)]
            for ci in range(11):
                c0 = ci * 128 if ci < 2 else 1664 + (ci - 2) * 128
                fb = fcount % 2
                fcount += 1
                for k in range(8):
                    P.op('pe', lambda e, fb=fb, k=k, c0=c0, XT=XT: e.matmul(pf[fb][:], wbf[:, k, c0:c0 + 128], XT[:, k, :], start=(k == 0), stop=(k == 7)),
                         reads=allx + [('wbf', k)], writes=[('pf', fb)])
                if ci < 2:
                    P.op('act', lambda e, fb=fb: e.activation(fo[fb][:], pf[fb][:], AF.Gelu_apprx_tanh), reads=[('pf', fb)], writes=[('fo', fb)])
                    P.dma('sp', D['uT'][ci * 128:(ci + 1) * 128, g * 512:(g + 1) * 512], fo[fb][:], reads=[('fo', fb)], writes=['d_uT'])
                else:
                    P.op('dve', lambda e, fb=fb: e.tensor_copy(fo[fb][:], pf[fb][:]), reads=[('pf', fb)], writes=[('fo', fb)])
                    P.dma('sp', D['cT'][(ci - 2) * 128:(ci - 1) * 128, g * 512:(g + 1) * 512], fo[fb][:], reads=[('fo', fb)], writes=['d_cT'])
        P.emit()


def phase_B(P, l, D):
    with ExitStack() as s:
        ws32 = P.sb(s, 'b_ws32', [128, 4, 128], F32)
        idf = P.sb(s, 'b_idf', [128, 128], F32)
        wsT = P.sb(s, 'b_wsT', [128, 4, 128], BF16)
        bias = P.sb(s, 'b_bias', [64, 4, 128], F32)
        vn = [P.sb(s, 'b_vn%d' % i, [128, 4, 256], BF16) for i in range(2)]
        ut = [P.sb(s, 'b_ut%d' % i, [64, 4, 512], F32) for i in range(2)]
        mx = P.sb(s, 'b_mx', [64, 4, 128], F32)
        yb = [P.sb(s, 'b_yb%d' % i, [64, 4, 512], BF16) for i in range(2)]
        ptr = P.ps(s, 'b_ptr', [128, 512])
        pm = [P.ps(s, 'b_pm%d' % i, [64, 512]) for i in range(4)]
        P.dma('sp', ws32[:], D['w_s'][l].rearrange('g i j -> i g j'), writes=['ws32'])
        P.dma('sp', idf[:], D['ident'], writes=['idf'])
        P.dma('sp', bias[:].rearrange('p g i -> p (g i)'), D['b_s'][l].rearrange('g i -> (g i)').partition_broadcast(64), writes=['bias'])
        for g in range(4):
            P.op('pe', lambda e, g=g: e.transpose(ptr[:, g * 128:(g + 1) * 128], ws32[:, g, :], idf[:]), reads=['ws32', 'idf'], writes=['ptr'])
        P.op('dve', lambda e: e.tensor_copy(wsT[:].rearrange('p g i -> p (g i)'), ptr[:]), reads=['ptr'], writes=['wsT'])
        for it in range(8):
            b = it % 2
            P.dma('sp', vn[b][:], D['vn'][it * 512:(it + 1) * 512, :].rearrange('(c p) n -> p c n', p=128), reads=['d_vn'], writes=[('vn', b)])
            P.dma('sp', ut[b][:], D['uT'][:, it * 512:(it + 1) * 512].rearrange('(g d) t -> d g t', d=64), reads=['d_uT'], writes=[('ut', b)])
            for g in range(4):
                for c in range(4):
                    P.op('pe', lambda e, g=g, c=c, b=b: e.matmul(pm[g][:, c * 128:(c + 1) * 128], vn[b][:, c, g * 64:(g + 1) * 64], wsT[:, g, :], start=True, stop=True),
                         reads=[('vn', b), 'wsT'], writes=[('pm', g)])
                P.op('dve', lambda e, g=g: e.tensor_tensor(mx[:], pm[g][:].rearrange('p (c i) -> p c i', i=128), bc(bias[:, g:g + 1, :], [64, 4, 128]), ALU.add),
                     reads=[('pm', g), 'bias'], writes=['mx'])
                P.op('dve', lambda e, g=g, b=b: e.tensor_tensor(yb[b][:, g, :], mx[:].rearrange('p c i -> p (c i)'), ut[b][:, g, :], ALU.mult),
                     reads=['mx', ('ut', b)], writes=[('yb', b)])
            P.dma('sp', D['yT'][0:256, it * 512:(it + 1) * 512].rearrange('(g d) t -> d g t', d=64), yb[b][:], reads=[('yb', b)], writes=['d_yT'])
        P.emit()


PATS = (1, 4, 16)
KPAD = 1024


def phase_C(P, l, D):
    with ExitStack() as s:
        vs = {}
        for d in PATS:
            nt = d * (S // d // 128 + 1)
            vs[d] = P.sb(s, 'c_vs%d' % d, [128, nt, 390], BF16)
        mab = P.sb(s, 'c_mab', [128, 512], BF16)
        sel = P.sb(s, 'c_sel', [65, 64], F32)
        qh = [P.sb(s, 'c_qh%d' % i, [64, S], BF16) for i in range(2)]
        kh = [P.sb(s, 'c_kh%d' % i, [64, S + 2 * KPAD], BF16) for i in range(2)]
        pex = [P.sb(s, 'c_pex%d' % i, [128, 512], BF16) for i in range(2)]
        acc = P.sb(s, 'c_acc', [65, S], F32)
        rd = P.sb(s, 'c_rd', [64, 512], F32)
        yb = [P.sb(s, 'c_yb%d' % i, [64, 512], BF16) for i in range(2)]
        pss = [P.ps(s, 'c_ps%d' % i, [128, 512]) for i in range(2)]
        ppv = [P.ps(s, 'c_pv%d' % i, [65, 512]) for i in range(2)]
        pd = P.ps(s, 'c_pd', [64, 512])
        P.dma('pool', mab[:], D['mab'], writes=['mab'])
        P.dma('sp', sel[:], D['sel65'], writes=['sel'])
        for i in range(2):
            P.op('pool', lambda e, i=i: e.memset(kh[i][:, 0:KPAD], 0.0), writes=[('kh', i)])
            P.op('pool', lambda e, i=i: e.memset(kh[i][:, KPAD + S:], 0.0), writes=[('kh', i)])
        for d in PATS:
            L = S // d
            nqb = L // 128
            P.op('pool', lambda e, d=d: e.memset(vs[d][:].rearrange('p a b -> p (a b)'), 0.0), writes=[('vs', d)])
            vsrc = D['vaug'].rearrange('(j r) c -> r j c', r=d)
            for r in range(d):
                tb = r * (nqb + 1)
                if nqb > 1:
                    P.dma('sp', vs[d][:, tb + 1:tb + nqb, :], vsrc[r, 64:64 + (nqb - 1) * 128, :].rearrange('(k p) c -> p k c', p=128),
                          reads=['d_vaug', ('vs', d)], writes=[('vs', d)])
                P.dma('sp', vs[d][64:128, tb, :], vsrc[r, 0:64, :], reads=['d_vaug', ('vs', d)], writes=[('vs', d)])
                P.dma('sp', vs[d][0:64, tb + nqb, :], vsrc[r, L - 64:L, :], reads=['d_vaug', ('vs', d)], writes=[('vs', d)])
        it = 0
        for h in range(6):
            hb = h % 2
            P.dma('sp', qh[hb][:], D['qkT'][h // 2, (h % 2) * 64:(h % 2) * 64 + 64, :], reads=['d_qkT'], writes=[('qh', hb)])
            P.dma('sp', kh[hb][:, KPAD:KPAD + S], D['qkT'][3 + h // 2, (h % 2) * 64:(h % 2) * 64 + 64, :], reads=['d_qkT'], writes=[('kh', hb)])
            for pi, d in enumerate(PATS):
                L = S // d
                nqb = L // 128
                for r in range(d):
                    tb = r * (nqb + 1)
                    for qb0 in range(0, nqb, 2):
                        ib = it % 2
                        it += 1
                        combos = ((qb0, qb0), (qb0 + 1, qb0), (qb0 + 1, qb0 + 1), (qb0 + 2, qb0 + 1))
                        for ci, (kt, qb) in enumerate(combos):
                            k0 = KPAD + r + d * (kt * 128 - 64)
                            q0 = r + d * (qb * 128)
                            P.op('pe', lambda e, ib=ib, ci=ci, k0=k0, q0=q0, d=d, hb=hb: e.matmul(
                                pss[ib][:, ci * 128:(ci + 1) * 128], kh[hb][:, ssl(k0, 128, d)], qh[hb][:, ssl(q0, 128, d)], start=True, stop=True),
                                reads=[('kh', hb), ('qh', hb)], writes=[('pss', ib)])
                        P.op('act', lambda e, ib=ib: e.activation(pex[ib][:], pss[ib][:], AF.Exp, scale=0.125), reads=[('pss', ib)], writes=[('pex', ib)])
                        P.op('dve', lambda e, ib=ib: e.tensor_tensor(pex[ib][:], pex[ib][:], mab[:], ALU.mult), reads=[('pex', ib), 'mab'], writes=[('pex', ib)])
                        for ci, (kt, qb) in enumerate(combos):
                            qi = qb - qb0
                            P.op('pe', lambda e, ib=ib, ci=ci, kt=kt, qi=qi, d=d, tb=tb, h=h: e.matmul(
                                ppv[ib][:, qi * 128:(qi + 1) * 128], vs[d][:, tb + kt, h * 65:(h + 1) * 65], pex[ib][:, ci * 128:(ci + 1) * 128],
                                start=(ci % 2 == 0), stop=(ci % 2 == 1)), reads=[('vs', d), ('pex', ib)], writes=[('ppv', ib)])
                        a0 = r + d * (qb0 * 128)
                        av = acc[:, ssl(a0, 256, d)]
                        if pi == 0:
                            P.op('dve', lambda e, av=av, ib=ib: e.tensor_copy(av, ppv[ib][:, 0:256]), reads=[('ppv', ib)], writes=['acc'])
                        else:
                            P.op('dve', lambda e, av=av, ib=ib: e.tensor_tensor(av, av, ppv[ib][:, 0:256], ALU.add), reads=[('ppv', ib), 'acc'], writes=['acc'])
            for c4 in range(8):
                yb_ = yb[c4 % 2]
                P.op('pe', lambda e, c4=c4: e.matmul(pd[:], sel[:], acc[:, c4 * 512:(c4 + 1) * 512], start=True, stop=True), reads=['sel', 'acc'], writes=['pd'])
                P.op('dve', lambda e: e.reciprocal(rd[:], pd[:]), reads=['pd'], writes=['rd'])
                P.op('dve', lambda e, c4=c4, yb_=yb_: e.tensor_tensor(yb_[:], acc[0:64, c4 * 512:(c4 + 1) * 512], rd[:], ALU.mult), reads=['acc', 'rd'], writes=[('yb', c4 % 2)])
                P.dma('sp', D['yT'][256 + h * 64:256 + (h + 1) * 64, c4 * 512:(c4 + 1) * 512], yb_[:], reads=[('yb', c4 % 2)], writes=['d_yT'])
        P.emit()


def phase_D1(P, l, D):
    with ExitStack() as s:
        cw = P.sb(s, 'd_cw', [128, 5, 9], F32)
        idf = P.sb(s, 'd_idf', [128, 128], F32)
        raw = [P.sb(s, 'd_raw%d' % i, [128, S + 4], F32) for i in range(2)]
        cv = P.sb(s, 'd_cv', [128, S], F32)
        tm = [P.sb(s, 'd_tm%d' % i, [128, 4, 128], F32) for i in range(2)]
        sq = P.sb(s, 'd_sq', [128, 8, 64], F32)
        r8 = P.sb(s, 'd_r8', [128, 8], F32)
        fT = [P.sb(s, 'd_fT%d' % i, [128, 512], BF16) for i in range(2)]
        abt = P.sb(s, 'd_abt', [128, NT, 24], F32)
        gbt = P.sb(s, 'd_gbt', [128, NT, 24], F32)
        dtb = P.sb(s, 'd_dtb', [128, 12], F32)
        nA = P.sb(s, 'd_nA', [128, 12], F32)
        ptr = [P.ps(s, 'd_ptr%d' % i, [128, 512]) for i in range(2)]
        ptb = [P.ps(s, 'd_ptb%d' % i, [128, 512]) for i in range(2)]
        for k in range(5):
            P.dma('sp', cw[:, k, :], D['conv_w'][l, k].rearrange('(c p) -> p c', p=128), writes=['cw'], allow_slow_non_contiguous=True)
        P.dma('sp', idf[:], D['ident'], writes=['idf'])
        for i in range(2):
            P.op('pool', lambda e, i=i: e.memset(raw[i][:, 0:2], 0.0), writes=[('raw', i)])
            P.op('pool', lambda e, i=i: e.memset(raw[i][:, S + 2:S + 4], 0.0), writes=[('raw', i)])
        P.dma('sp', abt[:], D['ab'].rearrange('(t p) c -> p t c', p=128), reads=['d_ab'], writes=['abt'])
        P.dma('sp', dtb[:], D['dt_bias'][l].rearrange('a h -> (a h)').partition_broadcast(128), writes=['dtb'])
        P.dma('sp', nA[:], D['a_log'][l].rearrange('a h -> (a h)').partition_broadcast(128), writes=['nA'])
        P.op('act', lambda e: e.activation(nA[:], nA[:], AF.Exp), reads=['nA'], writes=['nA'])
        P.op('dve', lambda e: e.tensor_scalar(nA[:], nA[:], -1.0, None, ALU.mult), reads=['nA'], writes=['nA'])
        P.op('dve', lambda e: e.tensor_tensor(gbt[:, :, 0:12], abt[:, :, 0:12], bc(dtb[:].unsqueeze(1), [128, NT, 12]), ALU.add), reads=['abt', 'dtb'], writes=['gbt0'])
        P.op('act', lambda e: e.activation(gbt[:, :, 0:12], gbt[:, :, 0:12], AF.Exp), reads=['gbt0'], writes=['gbt0'])
        P.op('act', lambda e: e.activation(gbt[:, :, 0:12], gbt[:, :, 0:12], AF.Ln, bias=1.0), reads=['gbt0'], writes=['gbt0'])
        P.op('dve', lambda e: e.tensor_tensor(gbt[:, :, 0:12], gbt[:, :, 0:12], bc(nA[:].unsqueeze(1), [128, NT, 12]), ALU.mult), reads=['gbt0', 'nA'], writes=['gbt0'])
        P.op('act', lambda e: e.activation(gbt[:, :, 12:24], abt[:, :, 12:24], AF.Sigmoid), reads=['abt'], writes=['gbt1'])
        P.dma('sp', D['gb'].rearrange('(t p) c -> p t c', p=128), gbt[:], reads=['gbt0', 'gbt1'], writes=['d_gb'])
        n4 = 0
        import os
        CUT = int(os.environ.get('D1CUT', '99'))
        for c in range(int(os.environ.get('D1C0', '0')), int(os.environ.get('D1C1', '9'))):
            rb = c % 2
            R = raw[rb]
            P.dma('sp', R[:, 2:S + 2], D['cT'][c * 128:(c + 1) * 128, :], reads=['d_cT'], writes=[('raw', rb)])
            P.op('dve', lambda e, R=R, c=c: e.tensor_scalar(cv[:], R[:, 0:S], cw[:, 0, c:c + 1], None, ALU.mult), reads=[('raw', rb), 'cw'], writes=['cv'])
            for k in range(1, 5):
                eng = 'dve'
                P.op(eng, lambda e, R=R, c=c, k=k: e.scalar_tensor_tensor(cv[:], R[:, k:k + S], cw[:, k, c:c + 1], cv[:], ALU.mult, ALU.add),
                     reads=[('raw', rb), 'cw', 'cv'], writes=['cv'])
            P.op('act', lambda e: e.activation(cv[:], cv[:], AF.Silu), reads=['cv'], writes=['cv'])
            for t4 in range(8 if CUT >= 2 else 0):
                pb = n4 % 2
                n4 += 1
                for j in range(4):
                    t = t4 * 4 + j
                    P.op('pe', lambda e, pb=pb, j=j, t=t: e.transpose(ptr[pb][:, j * 128:(j + 1) * 128], cv[:, t * 128:(t + 1) * 128], idf[:]),
                         reads=['cv', 'idf'], writes=[('ptr', pb)])
                TM = tm[pb]
                PV = ptr[pb][:].rearrange('p (j h d) -> p (j h) d', h=2, d=64)
                if c >= 6:
                    P.op('act', lambda e, pb=pb, TM=TM: e.copy(TM[:].rearrange('p j c -> p (j c)'), ptr[pb][:]), reads=[('ptr', pb)], writes=[('tm', pb)])
                    P.dma('sp', D['v_tm'][t4 * 512:(t4 + 1) * 512, (c - 6) * 128:(c - 5) * 128].rearrange('(j p) c -> p j c', p=128), TM[:], reads=[('tm', pb)], writes=['d_vtm'])
                else:
                    P.op('act', lambda e, pb=pb: e.activation(sq[:].rearrange('p a b -> p (a b)'), ptr[pb][:], AF.Square), reads=[('ptr', pb)], writes=['sq'])
                    P.op('dve', lambda e: e.tensor_reduce(r8[:], sq[:], AX.X, ALU.add), reads=['sq'], writes=['r8'])
                    P.op('act', lambda e: e.activation(r8[:], r8[:], AF.Sqrt, bias=EPS, scale=1.0), reads=['r8'], writes=['r8'])
                    P.op('dve', lambda e: e.reciprocal(r8[:], r8[:]), reads=['r8'], writes=['r8'])
                    if c < 3:
                        P.op('dve', lambda e: e.tensor_scalar(r8[:], r8[:], 0.125, None, ALU.mult), reads=['r8'], writes=['r8'])
                    P.op('dve', lambda e, TM=TM, PV=PV: e.tensor_tensor(TM[:].rearrange('p j (h d) -> p (j h) d', d=64), PV, bc(r8[:].unsqueeze(2), [128, 8, 64]), ALU.mult),
                         reads=[('ptr', pb), 'r8'], writes=[('tm', pb)])
                    if c >= 3:
                        P.dma('sp', D['k_tm'][t4 * 512:(t4 + 1) * 512, (c - 3) * 128:(c - 2) * 128].rearrange('(j p) c -> p j c', p=128), TM[:], reads=[('tm', pb)], writes=['d_ktm'])
                    for j in range(4):
                        P.op('pe', lambda e, pb=pb, j=j, TM=TM: e.transpose(ptb[pb][:, j * 128:(j + 1) * 128], TM[:, j, :], idf[:]), reads=[('tm', pb), 'idf'], writes=[('ptb', pb)])
                    P.op('act', lambda e, pb=pb: e.copy(fT[pb][:], ptb[pb][:]), reads=[('ptb', pb)], writes=[('fT', pb)])
                    dst = D['qT_g'] if c < 3 else D['kT_g']
                    cc = c if c < 3 else c - 3
                    P.dma('sp', dst[cc * 128:(cc + 1) * 128, t4 * 512:(t4 + 1) * 512], fT[pb][:], reads=[('fT', pb)], writes=['d_qkTg'])
        P.emit()


def phase_D2(P, l, D):
    import os
    C = 64
    NCH = S // C
    NST = int(os.environ.get('D2N', str(NCH)))
    MD = BF16 if os.environ.get('D2BF', '1') == '1' else F32
    with ExitStack() as s:
        def T12(name, dt=F32):
            return P.sb(s, 'e_' + name, [64, 12, 64], dt)
        ones = P.sb(s, 'e_ones', [64, 64], F32)
        idf = P.sb(s, 'e_idf', [64, 64], F32)
        idm = P.sb(s, 'e_idm', [64, 64], MD)
        idbc = T12('idbc')
        triF = P.sb(s, 'e_triF', [64, 64], F32)
        triB = P.sb(s, 'e_triB', [64, 64], F32)
        mW, mWt, mI = T12('mW'), T12('mWt'), T12('mI')
        St, St2, Sm = T12('S'), T12('S2'), T12('Sm', MD)
        ktm = [T12('ktm%d' % i) for i in range(2)]
        vtm = [T12('vtm%d' % i) for i in range(2)]
        kT = [T12('kT%d' % i, MD) for i in range(2)]
        qT = [T12('qT%d' % i, MD) for i in range(2)]
        gbv = [P.sb(s, 'e_gb%d' % i, [64, 24], F32) for i in range(2)]
        gc = P.sb(s, 'e_gc', [64, 12], F32)
        egc = [P.sb(s, 'e_egc%d' % i, [64, 12], F32) for i in range(2)]
        egl = [P.sb(s, 'e_egl%d' % i, [64, 12], F32) for i in range(2)]
        egd = P.sb(s, 'e_egd', [64, 12], F32)
        Dg = P.sb(s, 'e_Dg', [64, 24, 64], F32)
        diff, Ea, Eb = T12('diff'), T12('Ea'), T12('Eb')
        W, Wt = T12('W', MD), T12('Wt', MD)
        A1, A1t, A2, A2t = T12('A1', MD), T12('A1t', MD), T12('A2', MD), T12('A2t', MD)
        nxTI = T12('nxTI', MD)
        Yt = [T12('Yt0', MD), T12('Yt1', MD)]
        Yf = [T12('Yf0', MD), T12('Yf1', MD)]
        QKm = [T12('QKm0', MD), T12('QKm1', MD)]
        kd = [T12('kd0', MD), T12('kd1', MD)]
        Rr, Rm, vnew, o1 = T12('R'), T12('Rm', MD), T12('vnew', MD), T12('o1')
        ob = [T12('ob0'), T12('ob1')]
        pA = P.ps(s, 'e_pA', [64, 1024])
        pB = P.ps(s, 'e_pB', [64, 1024])
        pC = P.ps(s, 'e_pC', [64, 1024])
        pDD = P.ps(s, 'e_pD', [64, 1024])

        def pv(p, h):
            return p[:, h * 512:h * 512 + 384].rearrange('p (j t) -> p j t', t=64)

        def sv(t, h):
            return t[:, h * 6:(h + 1) * 6, :]

        def pcol(p, j):
            c0 = (j // 6) * 512 + (j % 6) * 64
            return p[:, c0:c0 + 64]

        def mm12(pt, pn, lfn, rfn, rfun):
            for j in range(12):
                o_, l_, r_ = pcol(pt, j), lfn(j), rfn(j)
                P.op('pe', lambda e, o_=o_, l_=l_, r_=r_: e.matmul(o_, l_, r_, start=True, stop=True), reads=rfun(j // 6), writes=[(pn, j // 6)])

        def bcol(ap12, h):
            return bc(ap12[:, h * 6:(h + 1) * 6].unsqueeze(2), [64, 6, 64])

        P.dma('sp', ones[:], D['ones64'], writes=['ones'])
        P.dma('sp', idf[:], D['ident'][0:64, 0:64], writes=['idf'])
        P.dma('sp', triF[:], D['triF'], writes=['triF'])
        P.dma('sp', triB[:], D['triB'], writes=['triB'])
        P.dma('sp', mW[:], D['mW'], writes=['mW'])
        P.dma('sp', mWt[:], D['mWt'], writes=['mWt'])
        P.dma('sp', mI[:], D['mI'], writes=['mI'])
        P.op('dve', lambda e: e.tensor_copy(idbc[:], bc(idf[:].unsqueeze(1), [64, 12, 64])), reads=['idf'], writes=['idbc'])
        P.op('dve', lambda e: e.tensor_copy(idm[:], idf[:]), reads=['idf'], writes=['idm'])
        P.op('dve', lambda e: e.memset(St[:].rearrange('p a b -> p (a b)'), 0.0), writes=[('S', 0), ('S', 1)])
        P.op('pool', lambda e: e.memset(Sm[:].rearrange('p a b -> p (a b)'), 0.0), writes=[('Sm', 0), ('Sm', 1)])

        def prep(i):
            b = i % 2
            cf = i
            cb = NCH - 1 - i
            K_, V_, KT_, QT_, GB_ = ktm[b], vtm[b], kT[b], qT[b], gbv[b]
            EGC, EGL, QKM, KD = egc[b], egl[b], QKm[b], kd[b]
            for h, cc in ((0, cf), (1, cb)):
                sl = slice(h * 6, h * 6 + 6)
                tk = slice(cc * C, (cc + 1) * C)
                P.dma('sp', K_[:, sl, :], D['k_tm'][tk, :].rearrange('t (h d) -> t h d', d=64), reads=['d_ktm'], writes=[('ktm', b, h)])
                P.dma('sp', V_[:, sl, :], D['v_tm'][tk, :].rearrange('t (h d) -> t h d', d=64), reads=['d_vtm'], writes=[('vtm', b, h)])
                P.dma('sp', KT_[:, sl, :], D['kT_g'][:, tk].rearrange('(h d) t -> d h t', d=64), reads=['d_qkTg'], writes=[('kT', b, h)])
                P.dma('sp', QT_[:, sl, :], D['qT_g'][:, tk].rearrange('(h d) t -> d h t', d=64), reads=['d_qkTg'], writes=[('qT', b, h)])
                P.dma('sp', GB_[:, h * 6:h * 6 + 6], D['gb'][tk, h * 6:h * 6 + 6], reads=['d_gb'], writes=[('gb', b, h)])
                P.dma('sp', GB_[:, 12 + h * 6:12 + h * 6 + 6], D['gb'][tk, 12 + h * 6:12 + h * 6 + 6], reads=['d_gb'], writes=[('gbb', b, h)])
            beta = GB_[:, 12:24]
            DgF = Dg[:].rearrange('p a b -> p (a b)')
            for h in range(2):
                tri = triF if h == 0 else triB
                trin = 'triF' if h == 0 else 'triB'
                rG, rBt = ('gb', b, h), ('gbb', b, h)
                P.op('pe', lambda e, h=h, tri=tri, GB_=GB_: e.matmul(pA[:, h * 512:h * 512 + 6], tri[:], GB_[:, h * 6:h * 6 + 6], start=True, stop=True), reads=[trin, rG], writes=[('pA', h)])
                P.op('dve', lambda e, h=h: e.tensor_copy(gc[:, h * 6:h * 6 + 6], pA[:, h * 512:h * 512 + 6]), reads=[('pA', h)], writes=[('gc', h)])
                P.op('act', lambda e, h=h, EGC=EGC: e.activation(EGC[:, h * 6:h * 6 + 6], pA[:, h * 512:h * 512 + 6], AF.Exp), reads=[('pA', h)], writes=[('egc', b, h)])
                P.op('dve', lambda e, h=h: e.tensor_tensor(Dg[:, h * 6:h * 6 + 6, :], sv(idbc, 0), bcol(gc, h), ALU.mult), reads=['idbc', ('gc', h)], writes=[('Dg', h)])
                P.op('pool', lambda e, h=h, beta=beta: e.tensor_tensor(Dg[:, 12 + h * 6:12 + h * 6 + 6, :], sv(idbc, 0), bcol(beta, h), ALU.mult), reads=['idbc', rBt], writes=[('Dgb', h)])
                P.op('pe', lambda e, h=h: e.matmul(pB[:, h * 512:h * 512 + 384], ones[:], DgF[:, h * 384:(h + 1) * 384], start=True, stop=True), reads=['ones', ('Dg', h)], writes=[('pB', h)])
                P.op('pe', lambda e, h=h: e.matmul(pC[:, h * 512:h * 512 + 384], ones[:], DgF[:, 768 + h * 384:768 + (h + 1) * 384], start=True, stop=True), reads=['ones', ('Dgb', h)], writes=[('pC', h)])
                P.op('dve', lambda e, h=h: e.tensor_tensor(sv(diff, h), pv(pB, h), bcol(gc, h), ALU.subtract), reads=[('pB', h), ('gc', h)], writes=[('diff', h)])
                lc = h * 512 + (63 if h == 0 else 0)
                lastv = pB[:, lc:lc + 64 * 5 + 1:64]
                P.op('act', lambda e, h=h, lastv=lastv, EGL=EGL: e.activation(EGL[:, h * 6:h * 6 + 6], lastv, AF.Exp), reads=[('pB', h)], writes=[('egl', b, h)])
                P.op('dve', lambda e, h=h, lastv=lastv: e.tensor_tensor(egd[:, h * 6:h * 6 + 6], lastv, gc[:, h * 6:h * 6 + 6], ALU.subtract), reads=[('pB', h), ('gc', h)], writes=[('egd', h)])
                P.op('act', lambda e, h=h: e.activation(egd[:, h * 6:h * 6 + 6], egd[:, h * 6:h * 6 + 6], AF.Exp), reads=[('egd', h)], writes=[('egd', h)])
                P.op('pool', lambda e, h=h, K_=K_, KD=KD: e.tensor_tensor(sv(KD, h), sv(K_, h), bcol(egd, h), ALU.mult), reads=[('ktm', b, h), ('egd', h)], writes=[('kd', b, h)])
                P.op('act', lambda e, h=h: e.activation(sv(Ea, h), sv(diff, h), AF.Exp), reads=[('diff', h)], writes=[('Ea', h)])
                P.op('act', lambda e, h=h: e.activation(sv(Eb, h), sv(diff, h), AF.Exp, scale=-1.0), reads=[('diff', h)], writes=[('Eb', h)])
            mm12(pA, 'pA', lambda j: KT_[:, j, :], lambda j: KT_[:, j, :], lambda h: [('kT', b, h)])
            mm12(pDD, 'pD', lambda j: KT_[:, j, :], lambda j: QT_[:, j, :], lambda h: [('kT', b, h), ('qT', b, h)])
            for h in range(2):
                rBt = ('gbb', b, h)
                P.op('dve', lambda e, h=h: e.scalar_tensor_tensor(sv(Eb, h), sv(Eb, h), 1.0, sv(mWt, h), ALU.min, ALU.mult), reads=[('Eb', h), 'mWt'], writes=[('Eb', h)])
                P.op('dve', lambda e, h=h: e.tensor_tensor(sv(Eb, h), sv(Eb, h), pv(pC, h), ALU.mult), reads=[('Eb', h), ('pC', h)], writes=[('Eb', h)])
                P.op('dve', lambda e, h=h: e.tensor_tensor(sv(Wt, h), sv(Eb, h), pv(pA, h), ALU.mult), reads=[('Eb', h), ('pA', h)], writes=[('Wt', h)])
                P.op('dve', lambda e, h=h: e.scalar_tensor_tensor(sv(diff, h), sv(Ea, h), 1.0, sv(mI, h), ALU.min, ALU.mult), reads=[('Ea', h), 'mI'], writes=[('diff', h)])
                P.op('dve', lambda e, h=h, QKM=QKM: e.tensor_tensor(sv(QKM, h), sv(diff, h), pv(pDD, h), ALU.mult), reads=[('diff', h), ('pD', h)], writes=[('QKm', b, h)])
                P.op('dve', lambda e, h=h: e.scalar_tensor_tensor(sv(Ea, h), sv(Ea, h), 1.0, sv(mW, h), ALU.min, ALU.mult), reads=[('Ea', h), 'mW', ('diff', h)], writes=[('Ea', h)])
                P.op('pool', lambda e, h=h, beta=beta: e.tensor_tensor(sv(Ea, h), sv(Ea, h), bcol(beta, h), ALU.mult), reads=[('Ea', h), rBt], writes=[('Ea', h)])
                P.op('dve', lambda e, h=h: e.tensor_tensor(sv(W, h), sv(Ea, h), pv(pA, h), ALU.mult), reads=[('Ea', h), ('pA', h)], writes=[('W', h)])
                P.op('pool', lambda e, h=h: e.tensor_tensor(sv(Yt[0], h), sv(idbc, h), sv(W, h), ALU.subtract), reads=['idbc', ('W', h)], writes=[('Yt0', h)])
            cur, curT, cn, cnT = W, Wt, 'W', 'Wt'
            yi = 0
            bufs = [(A1, A1t, 'A1', 'A1t'), (A2, A2t, 'A2', 'A2t')]
            for lev in range(5):
                nx, nxT, nn, nnT = bufs[lev % 2]
                mm12(pB, 'pB', lambda j, cur=cur: cur[:, j, :], lambda j, curT=curT: curT[:, j, :], lambda h, cn=cn, cnT=cnT: [(cn, h), (cnT, h)])
                for h in range(2):
                    if lev < 4:
                        P.op('act', lambda e, h=h, nxT=nxT: e.copy(sv(nxT, h), pv(pB, h)), reads=[('pB', h)], writes=[(nnT, h)])
                    P.op('dve', lambda e, h=h: e.tensor_tensor(sv(nxTI, h), pv(pB, h), sv(idbc, h), ALU.add), reads=[('pB', h), 'idbc'], writes=[('nxTI', h)])
                if lev < 4:
                    mm12(pC, 'pC', lambda j, curT=curT: curT[:, j, :], lambda j, cur=cur: cur[:, j, :], lambda h, cn=cn, cnT=cnT: [(cn, h), (cnT, h)])
                    for h in range(2):
                        P.op('dve', lambda e, h=h, nx=nx: e.tensor_copy(sv(nx, h), pv(pC, h)), reads=[('pC', h)], writes=[(nn, h)])
                Yc = Yt[yi]
                yc_ = 'Yt%d' % yi
                if lev < 4:
                    Yn, yn_ = Yt[1 - yi], ('Yt%d' % (1 - yi),)
                else:
                    Yn, yn_ = Yf[b], ('Yf', b)
                for j in range(12):
                    h = j // 6
                    P.op('pe', lambda e, j=j, Yc=Yc: e.matmul(pcol(pDD, j), nxTI[:, j, :], Yc[:, j, :], start=True, stop=True), reads=[('nxTI', h), (yc_, h)], writes=[('pD', h)])
                for h in range(2):
                    P.op('dve', lambda e, h=h, Yn=Yn: e.tensor_copy(sv(Yn, h), pv(pDD, h)), reads=[('pD', h)], writes=[yn_ + (h,)])
                yi = 1 - yi
                cur, curT, cn, cnT = nx, nxT, nn, nnT

        def scan(i):
            b = i % 2
            cf = i
            cb = NCH - 1 - i
            V_, KT_, QT_, GB_ = vtm[b], kT[b], qT[b], gbv[b]
            EGC, EGL, QKM, KD, YF = egc[b], egl[b], QKm[b], kd[b], Yf[b]
            beta = GB_[:, 12:24]
            mm12(pA, 'pA', lambda j: KT_[:, j, :], lambda j: Sm[:, j, :], lambda h: [('kT', b, h), ('Sm', h)])
            mm12(pB, 'pB', lambda j: QT_[:, j, :], lambda j: Sm[:, j, :], lambda h: [('qT', b, h), ('Sm', h)])
            for h in range(2):
                P.op('dve', lambda e, h=h, EGC=EGC: e.tensor_tensor(sv(Rr, h), pv(pA, h), bcol(EGC, h), ALU.mult), reads=[('pA', h), ('egc', b, h)], writes=[('R', h)])
                P.op('dve', lambda e, h=h, V_=V_: e.tensor_tensor(sv(Rm, h), sv(V_, h), sv(Rr, h), ALU.subtract), reads=[('R', h), ('vtm', b, h)], writes=[('Rm', h)])
                P.op('act', lambda e, h=h: e.copy(sv(o1, h), pv(pB, h)), reads=[('pB', h)], writes=[('o1', h)])
                P.op('pool', lambda e, h=h, EGC=EGC: e.tensor_tensor(sv(o1, h), sv(o1, h), bcol(EGC, h), ALU.mult), reads=[('o1', h), ('egc', b, h)], writes=[('o1', h)])
            mm12(pC, 'pC', lambda j: YF[:, j, :], lambda j: Rm[:, j, :], lambda h: [('Yf', b, h), ('Rm', h)])
            for h in range(2):
                P.op('dve', lambda e, h=h, beta=beta: e.tensor_tensor(sv(vnew, h), pv(pC, h), bcol(beta, h), ALU.mult), reads=[('pC', h), ('gbb', b, h)], writes=[('vnew', h)])
            mm12(pDD, 'pD', lambda j: QKM[:, j, :], lambda j: vnew[:, j, :], lambda h: [('QKm', b, h), ('vnew', h)])
            mm12(pA, 'pA', lambda j: KD[:, j, :], lambda j: vnew[:, j, :], lambda h: [('kd', b, h), ('vnew', h)])
            OB = ob[b]
            for h in range(2):
                cc = cf if h == 0 else cb
                P.op('pool', lambda e, h=h, EGL=EGL: e.tensor_tensor(sv(St2, h), sv(St, h), bcol(EGL, h), ALU.mult), reads=[('S', h), ('egl', b, h)], writes=[('S2', h)])
                P.op('dve', lambda e, h=h: e.tensor_tensor(sv(St, h), sv(St2, h), pv(pA, h), ALU.add), reads=[('S2', h), ('pA', h)], writes=[('S', h)])
                P.op('act', lambda e, h=h: e.copy(sv(Sm, h), sv(St, h)), reads=[('S', h)], writes=[('Sm', h)])
                P.op('dve', lambda e, h=h, OB=OB: e.tensor_tensor(sv(OB, h), sv(o1, h), pv(pDD, h), ALU.add), reads=[('o1', h), ('pD', h)], writes=[('ob', b, h)])
                P.dma('sp', D['o_fb'][h, cc * C:(cc + 1) * C, :].rearrange('t (h d) -> t h d', d=64), sv(OB, h), reads=[('ob', b, h)], writes=['d_ofb'])

        prep(0)
        for i in range(NST):
            if i + 1 < NST:
                prep(i + 1)
            scan(i)
        P.emit()


def phase_E(P, l, D, first):
    xsrc = D['x'] if first else D['xw']
    with ExitStack() as s:
        wo = P.sb(s, 'f_wo', [128, 8, 1024], BF16)
        ong = P.sb(s, 'f_ong', [128, 64], F32)
        idb = P.sb(s, 'f_idb', [128, 128], BF16)
        of_ = [P.sb(s, 'f_of%d' % i, [128, 2, 384], F32) for i in range(2)]
        gt = [P.sb(s, 'f_gt%d' % i, [128, 384], F32) for i in range(2)]
        o = P.sb(s, 'f_o', [128, 6, 64], F32)
        sq = P.sb(s, 'f_sq', [128, 6, 64], F32)
        r6 = P.sb(s, 'f_r6', [128, 6], F32)
        ycb = P.sb(s, 'f_ycb', [128, 384], BF16)
        yT = [P.sb(s, 'f_yT%d' % i, [128, 8, 128], BF16) for i in range(2)]
        xt = [P.sb(s, 'f_xt%d' % i, [128, 1024], F32) for i in range(2)]
        pT = P.ps(s, 'f_pT', [128, 512], BF16)
        po = [P.ps(s, 'f_po%d' % i, [128, 512]) for i in range(2)]
        for k in range(8):
            P.dma('pool', wo[:, k, :], D['w_out'][l, k * 128:(k + 1) * 128, :], writes=[('wo', k)])
        P.dma('sp', ong[:], D['o_norm_g'][l].partition_broadcast(128), writes=['ong'])
        P.dma('pool', idb[:], D['ident'], writes=['idb'])
        for t in range(NT):
            b = t % 2
            tk = slice(t * 128, (t + 1) * 128)
            P.dma('sp', of_[b][:], D['o_fb'][:, tk, :].rearrange('a t c -> t a c'), reads=['d_ofb'], writes=[('of', b)])
            P.dma('sp', gt[b][:], D['gate_s'][tk, :], reads=['d_gate'], writes=[('gt', b)])
            P.dma('sp', xt[b][:], xsrc[tk, :], writes=[('xt', b)])
            P.dma('sp', yT[b][:, 0:5, :], D['yT'][0:640, tk].rearrange('(k p) t -> p k t', p=128), reads=['d_yT'], writes=[('yT', b, 0)])
            OF = o[:].rearrange('p a b -> p (a b)')
            P.op('dve', lambda e, b=b: e.tensor_tensor(OF, of_[b][:, 0, :], of_[b][:, 1, :], ALU.add), reads=[('of', b)], writes=['o'])
            P.op('act', lambda e: e.activation(sq[:], o[:], AF.Square), reads=['o'], writes=['sq'])
            P.op('dve', lambda e: e.tensor_reduce(r6[:], sq[:], AX.X, ALU.add), reads=['sq'], writes=['r6'])
            P.op('act', lambda e: e.activation(r6[:], r6[:], AF.Sqrt, bias=EPS, scale=1.0 / 64), reads=['r6'], writes=['r6'])
            P.op('dve', lambda e: e.reciprocal(r6[:], r6[:]), reads=['r6'], writes=['r6'])
            P.op('dve', lambda e: e.tensor_tensor(o[:], o[:], bc(r6[:].unsqueeze(2), [128, 6, 64]), ALU.mult), reads=['o', 'r6'], writes=['o'])
            P.op('pool', lambda e: e.tensor_tensor(o[:], o[:], bc(ong[:].unsqueeze(1), [128, 6, 64]), ALU.mult), reads=['o', 'ong'], writes=['o'])
            P.op('pool', lambda e, b=b: e.tensor_tensor(ycb[:], OF, gt[b][:], ALU.mult), reads=['o', ('gt', b)], writes=['ycb'])
            for k in range(3):
                P.op('pe', lambda e, k=k: e.transpose(pT[:, k * 128:(k + 1) * 128], ycb[:, k * 128:(k + 1) * 128], idb[:]), reads=['ycb', 'idb'], writes=['pT'])
            P.op('act', lambda e, b=b: e.copy(yT[b][:, 5:8, :], pT[:, 0:384].rearrange('p (k t) -> p k t', t=128)), reads=['pT'], writes=[('yT', b, 1)])
            for hf in range(2):
                for k in range(8):
                    P.op('pe', lambda e, hf=hf, k=k, b=b: e.matmul(po[hf][:], yT[b][:, k, :], wo[:, k, hf * 512:(hf + 1) * 512], start=(k == 0), stop=(k == 7)),
                         reads=[('yT', b, 0), ('yT', b, 1), ('wo', k)], writes=[('po', hf)])
                P.op('dve', lambda e, hf=hf, b=b: e.tensor_tensor(xt[b][:, hf * 512:(hf + 1) * 512], xt[b][:, hf * 512:(hf + 1) * 512], po[hf][:], ALU.add),
                     reads=[('po', hf), ('xt', b)], writes=[('xt', b)])
            P.dma('sp', D['xw'][tk, :], xt[b][:], reads=[('xt', b)], writes=['d_xw'])
        P.emit()


def phase_F(P, l, D):
    import os
    NE = int(os.environ.get('FNE', '16'))
    with ExitStack() as s:
        gff = P.sb(s, 'g_gff', [128, 1024], F32)
        idf = P.sb(s, 'g_idf', [128, 128], F32)
        idb = P.sb(s, 'g_idb', [128, 128], BF16)
        wr = P.sb(s, 'g_wr', [128, 8, 16], F32)
        aff = P.sb(s, 'g_aff', [128, NT, 16], F32)
        sel = P.sb(s, 'g_sel', [128, NT, 16], F32)
        rank = P.sb(s, 'g_rank', [128, NT, 16], F32)
        cA = P.sb(s, 'g_cA', [128, NT, 16], F32)
        cB = P.sb(s, 'g_cB', [128, NT, 16], F32)
        selb = P.sb(s, 'g_selb', [128, NT * 16], BF16)
        triS = P.sb(s, 'g_triS', [128, 128], BF16)
        onesb = P.sb(s, 'g_onesb', [128, 128], BF16)
        tg = P.sb(s, 'g_tg', [128, NT, 16, 5], BF16)
        tp = P.sb(s, 'g_tp', [128, NT, 2], F32)
        iota = P.sb(s, 'g_iota', [128, 512], F32)
        Selt = [P.sb(s, 'g_Selt%d' % i, [128, 512], BF16) for i in range(2)]
        idxf = P.sb(s, 'g_idxf', [128, 4, 8], F32)
        row5 = P.sb(s, 'g_row5', [5, 512], F32)
        idxv = P.sb(s, 'g_idxv', [128, 4], F32)
        idxi = [P.sb(s, 'g_idxi%d' % i, [128, 4], I32) for i in range(2)]
        gate = [P.sb(s, 'g_gate%d' % i, [128, 4], F32) for i in range(2)]
        affT2 = P.sb(s, 'g_affT2', [16, S], F32)
        bj = P.sb(s, 'g_bj', [16, S], BF16)
        bs = P.sb(s, 'g_bs', [16, 8], F32)
        ones16 = P.sb(s, 'g_ones16', [16, 128], F32)
        dthr = P.sb(s, 'g_dthr', [16, 16], F32)
        thrb = P.sb(s, 'g_thrb', [128, 16], F32)
        xt = P.sb(s, 'g_xt', [128, 1024], F32)
        junk = P.sb(s, 'g_junk', [128, 1024], BF16)
        ss = P.sb(s, 'g_ss', [128, 4], F32)
        h32 = P.sb(s, 'g_h32', [128, 1024], F32)
        hb16 = P.sb(s, 'g_hb16', [128, 1024], BF16)
        hT32 = P.sb(s, 'g_hT32', [128, 8, 128], F32)
        sm = P.sb(s, 'g_sm', [128, 4], F32)
        ex = P.sb(s, 'g_ex', [128, 16], F32)
        wg = [P.sb(s, 'g_wg%d' % i, [128, 8, 1024], BF16) for i in range(2)]
        wu = [P.sb(s, 'g_wu%d' % i, [128, 8, 1024], BF16) for i in range(2)]
        wd = [P.sb(s, 'g_wd%d' % i, [128, 8, 1024], BF16) for i in range(2)]
        xe = P.sb(s, 'g_xe', [128, 4, 1024], BF16)
        xeT = P.sb(s, 'g_xeT', [128, 8, 512], BF16)
        hid = P.sb(s, 'g_hid', [128, 8, 512], BF16)
        sg = [P.sb(s, 'g_sg%d' % i, [128, 512], BF16) for i in range(2)]
        ye = [P.sb(s, 'g_ye%d' % i, [128, 1024], F32) for i in range(2)]
        pbig = P.ps(s, 'g_pbig', [128, 1024])
        pl = P.ps(s, 'g_pl', [128, 512])
        pT = P.ps(s, 'g_pT', [128, 1024], BF16)
        pg_ = P.ps(s, 'g_pg', [128, 512])
        pu_ = P.ps(s, 'g_pu', [128, 512])
        py = [P.ps(s, 'g_py%d' % i, [128, 512]) for i in range(2)]

        def load_w(ex_):
            eb = ex_ % 2
            for k in range(8):
                P.dma('pool', wg[eb][:, k, :], D['w_e_gate'][l, ex_, k * 128:(k + 1) * 128, :], writes=[('wg', eb, k)])
                P.dma('pool', wu[eb][:, k, :], D['w_e_up'][l, ex_, k * 128:(k + 1) * 128, :], writes=[('wu', eb, k)])
            for k in range(8):
                P.dma('pool', wd[eb][:, k, :], D['w_e_down'][l, ex_, k * 128:(k + 1) * 128, :], writes=[('wd', eb, k)])

        P.dma('sp', gff[:], D['g_ffn'][l].partition_broadcast(128), writes=['gff'])
        P.dma('sp', idf[:], D['ident'], writes=['idf'])
        P.dma('pool', idb[:], D['ident'], writes=['idb'])
        P.dma('pool', triS[:], D['triS'], writes=['triS'])
        P.dma('pool', onesb[:], D['ones128'], writes=['onesb'])
        P.dma('sp', wr[:], D['w_router'][l].rearrange('(k p) e -> p k e', p=128), writes=['wr'])
        P.dma('sp', ones16[:], D['ones128'][0:16, :], writes=['ones16'])
        P.dma('sp', tp[:], D['tp'], writes=['tp'])
        P.dma('sp', iota[:], D['iota512'], writes=['iota'])
        load_w(0)
        for t in range(NT):
            tk = slice(t * 128, (t + 1) * 128)
            P.dma('sp', xt[:], D['xw'][tk, :], reads=['d_xw'], writes=['xt'])
            P.op('dve', lambda e: e.memset(ss[:, 0:1], 0.0), writes=['ss'])
            P.op('act', lambda e: e.activation(junk[:], xt[:], AF.Square, accum_out=ss[:, 0:1]), reads=['xt', 'ss'], writes=['junk', 'ss'])
            P.op('act', lambda e: e.activation(ss[:, 0:1], ss[:, 0:1], AF.Sqrt, bias=EPS, scale=1.0 / 1024), reads=['ss'], writes=['ss'])
            P.op('dve', lambda e: e.reciprocal(ss[:, 0:1], ss[:, 0:1]), reads=['ss'], writes=['ss'])
            P.op('dve', lambda e: e.scalar_tensor_tensor(h32[:], xt[:], ss[:, 0:1], gff[:], ALU.mult, ALU.mult), reads=['xt', 'ss', 'gff'], writes=['h32'])
            P.op('act', lambda e: e.copy(hb16[:], h32[:]), reads=['h32'], writes=['hb16'])
            P.dma('sp', D['hb'][tk, :], hb16[:], reads=['hb16'], writes=['d_hb'])
            for k in range(8):
                P.op('pe', lambda e, k=k: e.transpose(pbig[:, k * 128:(k + 1) * 128], h32[:, k * 128:(k + 1) * 128], idf[:]), reads=['h32', 'idf'], writes=[('pbig', k // 4)])
            P.op('act', lambda e: e.copy(hT32[:, 0:4, :], pbig[:, 0:512].rearrange('p (k t) -> p k t', t=128)), reads=[('pbig', 0)], writes=['hT32a'])
            P.op('dve', lambda e: e.tensor_copy(hT32[:, 4:8, :], pbig[:, 512:1024].rearrange('p (k t) -> p k t', t=128)), reads=[('pbig', 1)], writes=['hT32b'])
            for k in range(8):
                P.op('pe', lambda e, k=k: e.matmul(pl[:, 0:16], hT32[:, k, :], wr[:, k, :], start=(k == 0), stop=(k == 7)), reads=['hT32a', 'hT32b', 'wr'], writes=['pl'])
            P.op('dve', lambda e: e.tensor_reduce(sm[:, 0:1], pl[:, 0:16], AX.X, ALU.max), reads=['pl'], writes=['sm'])
            P.op('dve', lambda e: e.tensor_scalar(sm[:, 1:2], sm[:, 0:1], -1.0, None, ALU.mult), reads=['sm'], writes=['sm'])
            P.op('dve', lambda e: e.memset(sm[:, 2:3], 0.0), reads=['sm'], writes=['sm'])
            P.op('act', lambda e: e.activation(ex[:], pl[:, 0:16], AF.Exp, bias=sm[:, 1:2], accum_out=sm[:, 2:3]), reads=['pl', 'sm'], writes=['ex', 'sm'])
            P.op('dve', lambda e: e.reciprocal(sm[:, 3:4], sm[:, 2:3]), reads=['sm'], writes=['sm'])
            P.op('dve', lambda e, t=t: e.tensor_scalar(aff[:, t, :], ex[:], sm[:, 3:4], None, ALU.mult), reads=['ex', 'sm'], writes=[('aff', t)])
            P.op('pe', lambda e, t=t: e.transpose(pl[0:16, 128:256], aff[:, t, :], idf[:]), reads=[('aff', t), 'idf'], writes=['pl'])
            P.op('act', lambda e, t=t: e.mul(affT2[:, t * 128:(t + 1) * 128], pl[0:16, 128:256], 2.0), reads=['pl'], writes=['affT2'])
        lo, hi, half, mid2, cnt, gef, tt = (bs[:, i:i + 1] for i in range(7))
        P.op('dve', lambda e: e.memset(bs[:], 0.0), writes=['bs'])
        P.op('dve', lambda e: e.memset(hi, 1.0), reads=['bs'], writes=['bs'])
        for itn in range(30):
            P.op('dve', lambda e: e.tensor_tensor(mid2, lo, hi, ALU.add), reads=['bs'], writes=['bs'])
            P.op('dve', lambda e: e.tensor_scalar(half, mid2, 0.5, None, ALU.mult), reads=['bs'], writes=['bs'])
            P.op('dve', lambda e: e.memset(cnt, 0.0), reads=['bs'], writes=['bs'])
            P.op('dve', lambda e: e.tensor_scalar(bj[:], affT2[:], mid2, 0.0, ALU.is_ge, ALU.add, accum_out=cnt), reads=['affT2', 'bs'], writes=['bj', 'bs'])
            P.op('dve', lambda e: e.tensor_scalar(gef, cnt, 511.5, None, ALU.is_ge), reads=['bs'], writes=['bs'])
            P.op('dve', lambda e: e.tensor_tensor(tt, half, lo, ALU.subtract), reads=['bs'], writes=['bs'])
            P.op('dve', lambda e: e.tensor_tensor(tt, tt, gef, ALU.mult), reads=['bs'], writes=['bs'])
            P.op('dve', lambda e: e.tensor_tensor(lo, lo, tt, ALU.add), reads=['bs'], writes=['bs'])
            P.op('dve', lambda e: e.tensor_tensor(tt, hi, half, ALU.subtract), reads=['bs'], writes=['bs'])
            P.op('dve', lambda e: e.tensor_tensor(tt, tt, gef, ALU.mult), reads=['bs'], writes=['bs'])
            P.op('dve', lambda e: e.tensor_tensor(hi, half, tt, ALU.add), reads=['bs'], writes=['bs'])
        P.op('dve', lambda e: e.tensor_scalar(dthr[:], idf[0:16, 0:16], lo, None, ALU.mult), reads=['idf', 'bs'], writes=['dthr'])
        P.op('pe', lambda e: e.matmul(pl[:, 256:272], ones16[:], dthr[:], start=True, stop=True), reads=['ones16', 'dthr'], writes=['pl'])
        P.op('dve', lambda e: e.tensor_copy(thrb[:], pl[:, 256:272]), reads=['pl'], writes=['thrb'])
        AFF = [('aff', t) for t in range(NT)]
        P.op('dve', lambda e: e.tensor_tensor(sel[:], aff[:], bc(thrb[:].unsqueeze(1), [128, NT, 16]), ALU.is_ge), reads=AFF + ['thrb'], writes=['sel'])
        P.op('dve', lambda e: e.tensor_copy(selb[:], sel[:].rearrange('p t e -> p (t e)')), reads=['sel'], writes=['selb'])
        P.op('pe', lambda e: e.matmul(pg_[:], triS[:], selb[:], start=True, stop=True), reads=['triS', 'selb'], writes=['pg'])
        P.op('pe', lambda e: e.matmul(pu_[:], onesb[:], selb[:], start=True, stop=True), reads=['onesb', 'selb'], writes=['pu'])
        P.op('dve', lambda e: e.tensor_copy(cA[:].rearrange('p t e -> p (t e)'), pu_[:]), reads=['pu'], writes=['cA'])
        src, dst, sn, dn = cA, cB, 'cA', 'cB'
        for sft in (1, 2, 4, 8, 16):
            P.op('pool', lambda e, src=src, dst=dst, sft=sft: e.tensor_copy(dst[:, 0:sft, :], src[:, 0:sft, :]), reads=[sn], writes=[dn])
            P.op('dve', lambda e, src=src, dst=dst, sft=sft: e.tensor_tensor(dst[:, sft:NT, :], src[:, sft:NT, :], src[:, 0:NT - sft, :], ALU.add), reads=[sn], writes=[dn])
            src, dst, sn, dn = dst, src, dn, sn
        P.op('dve', lambda e, src=src: e.tensor_tensor(rank[:].rearrange('p t e -> p (t e)'), src[:].rearrange('p t e -> p (t e)'), pu_[:], ALU.subtract), reads=[sn, 'pu'], writes=['rank'])
        P.op('dve', lambda e: e.tensor_tensor(rank[:].rearrange('p t e -> p (t e)'), rank[:].rearrange('p t e -> p (t e)'), pg_[:], ALU.add), reads=['rank', 'pg'], writes=['rank'])
        P.op('dve', lambda e: e.scalar_tensor_tensor(rank[:], rank[:], 1.0, sel[:], ALU.add, ALU.mult), reads=['rank', 'sel'], writes=['rank'])
        P.op('dve', lambda e: e.tensor_scalar(rank[:], rank[:], -1.0, None, ALU.add), reads=['rank'], writes=['rank'])
        P.op('dve', lambda e: e.tensor_copy(tg[:, :, :, 0:2], bc(tp[:].unsqueeze(2), [128, NT, 16, 2])), reads=['tp'], writes=['tg0'])
        P.op('dve', lambda e: e.tensor_copy(tg[:, :, :, 2], aff[:]), reads=AFF, writes=['tg1'])
        P.op('dve', lambda e: e.tensor_tensor(cA[:], aff[:], tg[:, :, :, 2], ALU.subtract), reads=AFF + ['tg1', 'cA', 'cB'], writes=['cA'])
        P.op('dve', lambda e: e.tensor_copy(tg[:, :, :, 3], cA[:]), reads=['cA'], writes=['tg2'])
        P.op('dve', lambda e: e.tensor_tensor(cB[:], cA[:], tg[:, :, :, 3], ALU.subtract), reads=['cA', 'tg2', 'cB'], writes=['cB'])
        P.op('dve', lambda e: e.tensor_copy(tg[:, :, :, 4], cB[:]), reads=['cB'], writes=['tg3'])
        TG = ['tg0', 'tg1', 'tg2', 'tg3']
        nsel = 0
        npy = 0
        nsg = 0
        for ex_ in range(NE):
            eb = ex_ % 2
            if ex_ + 1 < NE:
                load_w(ex_ + 1)
            for t in range(NT):
                sb_ = nsel % 2
                nsel += 1
                P.op('dve', lambda e, sb_=sb_, t=t, ex_=ex_: e.tensor_scalar(Selt[sb_][:], iota[:], rank[:, t, ex_:ex_ + 1], None, ALU.is_equal), reads=['iota', 'rank'], writes=[('Selt', sb_)])
                P.op('pe', lambda e, sb_=sb_, t=t, ex_=ex_: e.matmul(pl[0:5, 0:512], tg[:, t, ex_, :], Selt[sb_][:], start=(t == 0), stop=(t == NT - 1)),
                     reads=[('Selt', sb_)] + TG, writes=['pl'])
            P.op('act', lambda e: e.copy(row5[:], pl[0:5, 0:512]), reads=['pl'], writes=['row5'])
            for g in range(4):
                P.op('pe', lambda e, g=g: e.transpose(pl[:, g * 8:g * 8 + 5], row5[0:5, g * 128:(g + 1) * 128], idf[0:5, 0:5]), reads=['row5', 'idf'], writes=['pl'])
            P.op('dve', lambda e: e.tensor_copy(idxf[:, :, 0:5], pl[:, 0:32].rearrange('p (g c) -> p g c', c=8)[:, :, 0:5]), reads=['pl'], writes=['idxf'])
            P.op('dve', lambda e: e.scalar_tensor_tensor(idxv[:], idxf[:, :, 0], 128.0, idxf[:, :, 1], ALU.mult, ALU.add), reads=['idxf'], writes=['idxv'])
            P.op('dve', lambda e, eb=eb: e.tensor_copy(idxi[eb][:], idxv[:]), reads=['idxv'], writes=[('idxi', eb)])
            P.op('dve', lambda e, eb=eb: e.tensor_tensor(gate[eb][:], idxf[:, :, 2], idxf[:, :, 3], ALU.add), reads=['idxf'], writes=[('gate', eb)])
            P.op('dve', lambda e, eb=eb: e.tensor_tensor(gate[eb][:], gate[eb][:], idxf[:, :, 4], ALU.add), reads=['idxf', ('gate', eb)], writes=[('gate', eb)])
            for g in range(4):
                P.idma(lambda e, g=g, eb=eb: e.indirect_dma_start(out=xe[:, g, :], out_offset=None, in_=D['hb'][:, :],
                                                                   in_offset=bass.IndirectOffsetOnAxis(ap=idxi[eb][:, g:g + 1], axis=0), bounds_check=P.breg(e), oob_is_err=False),
                       reads=['d_hb', ('idxi', eb)], writes=[('xe', g)])
            for g in range(4):
                for k in range(8):
                    P.op('pe', lambda e, g=g, k=k: e.transpose(pT[:, k * 128:(k + 1) * 128], xe[:, g, k * 128:(k + 1) * 128], idb[:]), reads=[('xe', g), 'idb'], writes=['pT'])
                eng = 'act' if g % 2 == 0 else 'dve'
                if eng == 'act':
                    P.op('act', lambda e, g=g: e.copy(xeT[:, :, g * 128:(g + 1) * 128], pT[:].rearrange('p (k t) -> p k t', t=128)), reads=['pT'], writes=[('xeT', g)])
                else:
                    P.op('dve', lambda e, g=g: e.tensor_copy(xeT[:, :, g * 128:(g + 1) * 128], pT[:].rearrange('p (k t) -> p k t', t=128)), reads=['pT'], writes=[('xeT', g)])
            XET = [('xeT', g) for g in range(4)]
            for fc in range(8):
                for k in range(8):
                    P.op('pe', lambda e, fc=fc, k=k, eb=eb: e.matmul(pg_[:], wg[eb][:, k, fc * 128:(fc + 1) * 128], xeT[:, k, :], start=(k == 0), stop=(k == 7)),
                         reads=XET + [('wg', eb, k)], writes=['pg'])
                for k in range(8):
                    P.op('pe', lambda e, fc=fc, k=k, eb=eb: e.matmul(pu_[:], wu[eb][:, k, fc * 128:(fc + 1) * 128], xeT[:, k, :], start=(k == 0), stop=(k == 7)),
                         reads=XET + [('wu', eb, k)], writes=['pu'])
                sb2 = nsg % 2
                nsg += 1
                P.op('act', lambda e, sb2=sb2: e.activation(sg[sb2][:], pg_[:], AF.Silu), reads=['pg'], writes=[('sg', sb2)])
                P.op('dve', lambda e, sb2=sb2, fc=fc: e.tensor_tensor(hid[:, fc, :], sg[sb2][:], pu_[:], ALU.mult), reads=[('sg', sb2), 'pu'], writes=[('hid', fc)])
            HID = [('hid', fc) for fc in range(8)]
            for g in range(4):
                yb = g % 2
                for hf in range(2):
                    pb = npy % 2
                    npy += 1
                    for fc in range(8):
                        P.op('pe', lambda e, pb=pb, fc=fc, g=g, hf=hf, eb=eb: e.matmul(py[pb][:], hid[:, fc, g * 128:(g + 1) * 128], wd[eb][:, fc, hf * 512:(hf + 1) * 512], start=(fc == 0), stop=(fc == 7)),
                             reads=HID + [('wd', eb, fc)], writes=[('py', pb)])
                    if hf == 0:
                        P.op('act', lambda e, pb=pb, yb=yb, g=g, eb=eb: e.activation(ye[yb][:, 0:512], py[pb][:], AF.Copy, scale=gate[eb][:, g:g + 1]), reads=[('py', pb), ('gate', eb)], writes=[('ye', yb, 0)])
                    else:
                        P.op('dve', lambda e, pb=pb, yb=yb, g=g, eb=eb: e.tensor_scalar(ye[yb][:, 512:1024], py[pb][:], gate[eb][:, g:g + 1], None, ALU.mult), reads=[('py', pb), ('gate', eb)], writes=[('ye', yb, 1)])
                P.idma(lambda e, g=g, eb=eb, yb=yb: e.indirect_dma_start(out=D['xw'][:, :], out_offset=bass.IndirectOffsetOnAxis(ap=idxi[eb][:, g:g + 1], axis=0), in_=ye[yb][:],
                                                                          in_offset=None, bounds_check=P.breg(e), oob_is_err=False, compute_op=ALU.add),
                       reads=[('ye', yb, 0), ('ye', yb, 1), ('idxi', eb), 'd_xw'], writes=['d_xw'])
        P.emit()


def phase_G(P, l, D, last):
    xdst = D['out'] if last else D['xw']
    with ExitStack() as s:
        wp = P.sb(s, 'h_wp', [128, 2, 1024], BF16)
        wgt = P.sb(s, 'h_wgt', [128, 8, 1024], BF16)
        gpl = P.sb(s, 'h_gpl', [128, 1024], F32)
        gpg = P.sb(s, 'h_gpg', [128, 1024], F32)
        idb = P.sb(s, 'h_idb', [128, 128], BF16)
        xt = [P.sb(s, 'h_xt%d' % i, [128, 1024], F32) for i in range(2)]
        pb_ = [P.sb(s, 'h_pb%d' % i, [128, 256], BF16) for i in range(2)]
        junk = P.sb(s, 'h_junk', [128, 1024], BF16)
        ss = P.sb(s, 'h_ss', [128, 4], F32)
        xn = P.sb(s, 'h_xn', [128, 1024], BF16)
        xT = P.sb(s, 'h_xT', [128, 10, 128], BF16)
        er = P.sb(s, 'h_er', [128, 1024], F32)
        gt = P.sb(s, 'h_gt', [128, 1024], F32)
        pT = P.ps(s, 'h_pT', [128, 2048], BF16)
        pe_ = P.ps(s, 'h_pe', [128, 1024])
        pg_ = P.ps(s, 'h_pg', [128, 1024])
        for k in range(2):
            P.dma('pool', wp[:, k, :], D['w_ple'][l, k * 128:(k + 1) * 128, :], writes=[('wp', k)])
        for k in range(8):
            P.dma('pool', wgt[:, k, :], D['w_ple_gate'][l, k * 128:(k + 1) * 128, :], writes=[('wgt', k)])
        P.dma('sp', gpl[:], D['g_ple'][l].partition_broadcast(128), writes=['gpl'])
        P.dma('sp', gpg[:], D['g_ple_gate'][l].partition_broadcast(128), writes=['gpg'])
        P.dma('pool', idb[:], D['ident'], writes=['idb'])
        for t in range(NT):
            b = t % 2
            tk = slice(t * 128, (t + 1) * 128)
            P.dma('sp', xt[b][:], D['xw'][tk, :], reads=['d_xw'], writes=[('xt', b)])
            P.dma('pool', pb_[b][:], D['p'][l, tk, :], writes=[('pb', b)])
            P.op('dve', lambda e: e.memset(ss[:, 0:2], 0.0), writes=['ss'])
            P.op('act', lambda e, b=b: e.activation(junk[:], xt[b][:], AF.Square, accum_out=ss[:, 0:1]), reads=[('xt', b), 'ss'], writes=['junk', 'ss'])
            P.op('act', lambda e: e.activation(ss[:, 0:1], ss[:, 0:1], AF.Sqrt, bias=EPS, scale=1.0 / 1024), reads=['ss'], writes=['ss'])
            P.op('dve', lambda e: e.reciprocal(ss[:, 0:1], ss[:, 0:1]), reads=['ss'], writes=['ss'])
            P.op('dve', lambda e, b=b: e.scalar_tensor_tensor(xn[:], xt[b][:], ss[:, 0:1], gpg[:], ALU.mult, ALU.mult), reads=[('xt', b), 'ss', 'gpg'], writes=['xn'])
            for k in range(8):
                P.op('pe', lambda e, k=k: e.transpose(pT[:, k * 128:(k + 1) * 128], xn[:, k * 128:(k + 1) * 128], idb[:]), reads=['xn', 'idb'], writes=[('pT', 0)])
            for k in range(2):
                P.op('pe', lambda e, k=k, b=b: e.transpose(pT[:, (8 + k) * 128:(9 + k) * 128], pb_[b][:, k * 128:(k + 1) * 128], idb[:]), reads=[('pb', b), 'idb'], writes=[('pT', 1)])
            P.op('act', lambda e: e.copy(xT[:, 0:8, :].rearrange('p k t -> p (k t)'), pT[:, 0:1024]), reads=[('pT', 0)], writes=['xTa'])
            P.op('dve', lambda e: e.tensor_copy(xT[:, 8:10, :].rearrange('p k t -> p (k t)'), pT[:, 1024:1280]), reads=[('pT', 1)], writes=['xTb'])
            for hf in range(2):
                for k in range(2):
                    P.op('pe', lambda e, hf=hf, k=k: e.matmul(pe_[:, hf * 512:(hf + 1) * 512], xT[:, 8 + k, :], wp[:, k, hf * 512:(hf + 1) * 512], start=(k == 0), stop=(k == 1)),
                         reads=['xTb', ('wp', k)], writes=[('pe', hf)])
                for k in range(8):
                    P.op('pe', lambda e, hf=hf, k=k: e.matmul(pg_[:, hf * 512:(hf + 1) * 512], xT[:, k, :], wgt[:, k, hf * 512:(hf + 1) * 512], start=(k == 0), stop=(k == 7)),
                         reads=['xTa', ('wgt', k)], writes=[('pg', hf)])
            P.op('dve', lambda e: e.memset(ss[:, 1:3], 0.0), reads=['ss'], writes=['ss'])
            for hf in range(2):
                P.op('act', lambda e, hf=hf: e.activation(junk[:, hf * 512:(hf + 1) * 512], pe_[:, hf * 512:(hf + 1) * 512], AF.Square, accum_out=ss[:, 1 + hf:2 + hf]), reads=[('pe', hf), 'ss'], writes=['junk', 'ss'])
            P.op('dve', lambda e: e.tensor_tensor(ss[:, 1:2], ss[:, 1:2], ss[:, 2:3], ALU.add), reads=['ss'], writes=['ss'])
            P.op('act', lambda e: e.activation(ss[:, 1:2], ss[:, 1:2], AF.Sqrt, bias=EPS, scale=1.0 / 1024), reads=['ss'], writes=['ss'])
            P.op('dve', lambda e: e.reciprocal(ss[:, 1:2], ss[:, 1:2]), reads=['ss'], writes=['ss'])
            for hf in range(2):
                hs = slice(hf * 512, (hf + 1) * 512)
                P.op('dve', lambda e, hs=hs: e.scalar_tensor_tensor(er[:, hs], pe_[:, hs], ss[:, 1:2], gpl[:, hs], ALU.mult, ALU.mult), reads=[('pe', hf), 'ss', 'gpl'], writes=['er'])
                P.op('act', lambda e, hs=hs: e.activation(gt[:, hs], pg_[:, hs], AF.Sigmoid), reads=[('pg', hf)], writes=['gt'])
            P.op('dve', lambda e: e.tensor_tensor(er[:], er[:], gt[:], ALU.mult), reads=['er', 'gt'], writes=['er'])
            P.op('dve', lambda e, b=b: e.tensor_tensor(xt[b][:], xt[b][:], er[:], ALU.add), reads=['er', ('xt', b)], writes=[('xt', b)])
            P.dma('sp', xdst[tk, :], xt[b][:], reads=[('xt', b)], writes=['d_xw'])
        P.emit()


WEIGHTS = [('g_mix', [4, 1024]), ('w_in', [4, 1024, 3224]), ('ln_v_g', [4, 4, 64]), ('ln_v_b', [4, 4, 64]), ('w_s', [4, 4, 128, 128]),
           ('b_s', [4, 4, 128]), ('q_norm_g', [4, 64]), ('k_norm_g', [4, 64]), ('conv_w', [4, 5, 1152]), ('a_log', [4, 2, 6]),
           ('dt_bias', [4, 2, 6]), ('o_norm_g', [4, 64]), ('w_out', [4, 1024, 1024]), ('g_ffn', [4, 1024]), ('w_router', [4, 1024, 16]),
           ('w_e_gate', [4, 16, 1024, 1024]), ('w_e_up', [4, 16, 1024, 1024]), ('w_e_down', [4, 16, 1024, 1024]), ('w_ple', [4, 256, 1024]),
           ('g_ple', [4, 1024]), ('g_ple_gate', [4, 1024]), ('w_ple_gate', [4, 1024, 1024])]


def make_consts():
    c = {}
    c['ident'] = np.eye(128, dtype=np.float32)
    c['ones64'] = np.ones((64, 64), np.float32)
    c['ones128'] = np.ones((128, 128), np.float32)
    half = 8
    c['invf'] = (np.float32(500000.0) ** (-np.arange(half, dtype=np.float32) * np.float32(2.0) / np.float32(16))).astype(np.float32)
    a = np.arange(128)[:, None]
    b = np.arange(128)[None, :]
    mA = (a >= b).astype(np.float32)
    mB = (a <= b).astype(np.float32)
    c['mab'] = np.concatenate([mA, mB, mA, mB], axis=1)
    sel = np.zeros((65, 64), np.float32)
    sel[64, :] = 1.0
    c['sel65'] = sel
    p = np.arange(64)[:, None]
    f = np.arange(64)[None, :]
    c['triF'] = (p <= f).astype(np.float32)
    c['triB'] = (p >= f).astype(np.float32)

    def m12(fw, bw):
        return np.ascontiguousarray(np.stack([fw] * 6 + [bw] * 6, axis=1).astype(np.float32))
    c['mW'] = m12(f > p, f < p)
    c['mWt'] = m12(p > f, p < f)
    c['mI'] = m12(f >= p, f <= p)
    c['triS'] = (a < b).astype(np.float32)
    c['iota512'] = np.ascontiguousarray(np.broadcast_to(np.arange(512, dtype=np.float32)[None, :], (128, 512)))
    tpv = np.zeros((128, NT, 2), np.float32)
    tpv[:, :, 0] = np.arange(NT)[None, :]
    tpv[:, :, 1] = np.arange(128)[:, None]
    c['tp'] = tpv
    return c


SCRATCH = [('cs', [S, 16], F32), ('vn', [S, 256], BF16), ('qkT', [6, 128, S], BF16), ('vaug', [S, 390], BF16), ('gate_s', [S, 384], F32),
           ('ab', [S, 24], F32), ('uT', [256, S], F32), ('cT', [1152, S], F32), ('yT', [1024, S], BF16), ('v_tm', [S, 384], F32),
           ('k_tm', [S, 384], F32), ('qT_g', [384, S], BF16), ('kT_g', [384, S], BF16), ('gb', [S, 24], F32), ('o_fb', [2, S, 384], F32),
           ('xw', [S, 1024], F32), ('hb', [S, 1024], BF16)]


def build(n_layers=4, phases=None, dbg=()):
    P = Prog()
    D = {}
    D['x'] = P.dram('x', [S, 1024], F32, 'ExternalInput')
    D['p'] = P.dram('p', [n_layers, S, 256], F32, 'ExternalInput')
    D['positions'] = P.dram('positions', [128, NT], I32, 'ExternalInput')
    for n, shp in WEIGHTS:
        D[n] = P.dram(n, [n_layers] + list(shp[1:]), F32, 'ExternalInput')
    for n, v in make_consts().items():
        D[n] = P.dram(n, list(v.shape), F32, 'ExternalInput')
    for n, shp, dt in SCRATCH:
        D[n] = P.dram(n, shp, dt, 'ExternalOutput' if n in dbg else 'Internal')
    D['out'] = P.dram('out', [S, 1024], F32, 'ExternalOutput')
    allp = phases is None
    if allp or 'R' in phases:
        phase_rope(P, D)
    for l in range(n_layers):
        first = (l == 0)
        last = (l == n_layers - 1)
        if allp or 'A' in phases:
            phase_A(P, l, D, first)
        if allp or 'B' in phases:
            phase_B(P, l, D)
        if allp or 'C' in phases:
            phase_C(P, l, D)
        if allp or 'D1' in phases:
            phase_D1(P, l, D)
        if allp or 'D2' in phases:
            phase_D2(P, l, D)
        if allp or 'E' in phases:
            phase_E(P, l, D, first)
        if allp or 'F' in phases:
            phase_F(P, l, D)
        if allp or 'G' in phases:
            phase_G(P, l, D, last and allp)
    return P


def kernel(**inputs):
    n = 8
    P = build(4)
    consts = make_consts()
    shared = {k: np.ascontiguousarray(np.asarray(inputs[k], dtype=np.float32)) for k, _ in WEIGHTS}
    shared.update(consts)
    x = np.asarray(inputs['x'], dtype=np.float32)
    p = np.asarray(inputs['p'], dtype=np.float32)
    pos = np.asarray(inputs['positions']).astype(np.int32)
    in_maps = []
    for c in range(n):
        m = dict(shared)
        m['x'] = np.ascontiguousarray(x[c])
        m['p'] = np.ascontiguousarray(p[:, c])
        m['positions'] = np.ascontiguousarray(pos[c].reshape(NT, 128).T)
        in_maps.append(m)
    res = run_bass_kernel_spmd(P.nc, in_maps, core_ids=list(range(n)))
    return np.stack([np.asarray(res.results[c]['out'], dtype=np.float32) for c in range(n)], axis=0)
```

```python
import numpy as np
from contextlib import ExitStack
import concourse.bass as bass
import concourse.mybir as mybir
from concourse.bass_utils import run_bass_kernel_spmd

F32 = mybir.dt.float32
BF16 = mybir.dt.bfloat16
I32 = mybir.dt.int32
ALU = mybir.AluOpType
AF = mybir.ActivationFunctionType
AX = mybir.AxisListType

ENGS = ['pe', 'dve', 'act', 'pool', 'sp']
DMA_ENGS = ['sp', 'pool', 'act']
NDS = 8
EPS = 1e-6
S = 4096
NT = 32


class Prog:
    def __init__(self):
        self.nc = bass.Bass("TRN2", target_bir_lowering=False)
        self.stack = ExitStack()
        self.sems = {}
        for e in ENGS:
            self.sems[e] = self.stack.enter_context(self.nc.semaphore('s_' + e))
        self.dcount = {}
        for e in DMA_ENGS:
            for i in range(NDS):
                k = 'd_%s_%d' % (e, i)
                self.sems[k] = self.stack.enter_context(self.nc.semaphore(k))
                self.dcount[k] = 0
        self.dnext = {e: 0 for e in DMA_ENGS}
        self.cnt = {e: 0 for e in ENGS}
        self.waited = {e: {} for e in ENGS}
        self.q = {e: [] for e in ENGS}
        self.lastw = {}
        self.readers = {}
        self.nops = 0
        self.xkeys = set(['pT', 'pv', 'pq', 'pk', 'pbv', 'pg', 'pf', 'ptr', 'pm', 'pss', 'ppv', 'pd', 'ptb', 'po', 'pbig', 'pl', 'pu', 'py', 'pe', 'pA', 'pB', 'pC', 'pD', 'pS'])

    def _deps(self, eng, reads, writes):
        deps = {}

        def add(m):
            if m is None:
                return
            k, v = m
            if eng == 'pe' and k == 'pe':
                return
            if deps.get(k, 0) < v:
                deps[k] = v
        for r in reads:
            add(self.lastw.get(r))
        for w in writes:
            add(self.lastw.get(w))
            for m in self.readers.get(w, ()):
                add(m)
        out = []
        wd = self.waited[eng]
        for k, v in deps.items():
            if wd.get(k, 0) < v:
                wd[k] = v
                out.append((k, v))
        return out

    def _mark(self, mark, reads, writes):
        for w in writes:
            self.lastw[w] = mark
            self.readers[w] = []
        for r in reads:
            if r in writes:
                continue
            self.readers.setdefault(r, []).append(mark)

    cut = None
    pc = 0

    def isx(self, k):
        n = k[0] if isinstance(k, tuple) else k
        return isinstance(n, str) and n in self.xkeys

    def op(self, eng, fn, reads=(), writes=()):
        self.pc += 1
        if self.cut is not None and self.pc > self.cut:
            return
        xr = [r for r in reads if self.isx(r) and r not in writes]
        if xr:
            writes = list(writes) + xr
        waits = self._deps(eng, reads, writes)
        self.cnt[eng] += 1
        mark = (eng, self.cnt[eng])
        self.q[eng].append((waits, fn, (eng, 1)))
        self._mark(mark, reads, writes)
        self.nops += 1

    def dma(self, eng, out, in_, reads=(), writes=(), **kw):
        self.pc += 1
        if self.cut is not None and self.pc > self.cut:
            return
        waits = self._deps(eng, reads, writes)
        i = self.dnext[eng]
        self.dnext[eng] = (i + 1) % NDS
        k = 'd_%s_%d' % (eng, i)
        c = self.dcount[k]
        wd = self.waited[eng]
        if c > 0 and wd.get(k, 0) < 16 * c:
            wd[k] = 16 * c
            waits.append((k, 16 * c))
        self.dcount[k] = c + 1
        mark = (k, 16 * (c + 1))
        self.q[eng].append((waits, (lambda e: e.dma_start(out=out, in_=in_, **kw)), (k, 16)))
        self._mark(mark, reads, writes)
        self.nops += 1

    _breg = None

    def breg(self, e):
        if self._breg is None:
            self._breg = e.to_reg(S - 1)
        return self._breg

    def idma(self, fn, reads=(), writes=()):
        eng = 'pool'
        self.pc += 1
        waits = self._deps(eng, reads, writes)
        i = self.dnext[eng]
        self.dnext[eng] = (i + 1) % NDS
        k = 'd_%s_%d' % (eng, i)
        c = self.dcount[k]
        wd = self.waited[eng]
        if c > 0 and wd.get(k, 0) < 16 * c:
            wd[k] = 16 * c
            waits.append((k, 16 * c))
        self.dcount[k] = c + 1
        mark = (k, 16 * (c + 1))
        self.q[eng].append((waits, fn, (k, 16)))
        self._mark(mark, reads, writes)
        self.nops += 1

    def barrier(self):
        for e in ENGS:
            waits = []
            wd = self.waited[e]
            for o in ENGS:
                if o != e and self.cnt[o] > wd.get(o, 0):
                    wd[o] = self.cnt[o]
                    waits.append((o, self.cnt[o]))
            for k, c in self.dcount.items():
                if 16 * c > wd.get(k, 0):
                    wd[k] = 16 * c
                    waits.append((k, 16 * c))
            if waits:
                self.q[e].append((waits, None, None))
        self.lastw = {}
        self.readers = {}

    def emit(self):
        self.barrier()
        nc = self.nc
        sems = self.sems
        q = self.q

        def replay(name, e):
            for waits, fn, inc in q[name]:
                for k, v in waits:
                    e.wait_ge(sems[k], v)
                if fn is not None:
                    ins = fn(e)
                    ins.then_inc(sems[inc[0]], inc[1])

        with nc.Block() as block:
            @block.tensor
            def _(e):
                replay('pe', e)

            @block.vector
            def _(e):
                replay('dve', e)

            @block.scalar
            def _(e):
                replay('act', e)

            @block.gpsimd
            def _(e):
                replay('pool', e)

            @block.sync
            def _(e):
                replay('sp', e)
        self.q = {e: [] for e in ENGS}

    uid = 0

    def sb(self, stack, name, shape, dt):
        self.uid += 1
        return stack.enter_context(self.nc.sbuf_tensor('%s_%d' % (name, self.uid), list(shape), dt))

    def ps(self, stack, name, shape, dt=F32, keys=()):
        for k in keys:
            self.xkeys.add(k)
        self.uid += 1
        return stack.enter_context(self.nc.psum_tensor('%s_%d' % (name, self.uid), list(shape), dt))

    def dram(self, name, shape, dt, kind="Internal"):
        return self.nc.dram_tensor(name, list(shape), dt, kind=kind).ap()


def ssl(a, n, d):
    return slice(a, a + (n - 1) * d + 1, d)


def bc(ap, shape):
    return ap.to_broadcast(list(shape))


def phase_rope(P, D):
    with ExitStack() as s:
        pi_ = P.sb(s, 'r_pi', [128, NT], I32)
        pf = P.sb(s, 'r_pf', [128, NT], F32)
        invf = P.sb(s, 'r_invf', [128, 8], F32)
        ang = P.sb(s, 'r_ang', [128, 2, NT, 8], F32)
        kk = P.sb(s, 'r_kk', [128, 2, NT, 8], F32)
        ki = P.sb(s, 'r_ki', [128, 2, NT, 8], I32)
        cs = P.sb(s, 'r_cs', [128, NT, 16], F32)
        P.dma('sp', pi_[:], D['positions'], writes=['pi'])
        P.dma('sp', invf[:], D['invf'].partition_broadcast(128), writes=['invf'])
        P.op('dve', lambda e: e.tensor_copy(pf[:], pi_[:]), reads=['pi'], writes=['pf'])
        P.op('dve', lambda e: e.tensor_tensor(ang[:, 1], bc(pf[:].unsqueeze(2), [128, NT, 8]), bc(invf[:].unsqueeze(1), [128, NT, 8]), ALU.mult),
             reads=['pf', 'invf'], writes=['ang1'])
        P.op('dve', lambda e: e.tensor_scalar(ang[:, 0], ang[:, 1], float(np.pi / 2), None, ALU.add), reads=['ang1'], writes=['ang0'])
        A = ang[:].rearrange('p a t c -> p (a t c)')
        K = kk[:].rearrange('p a t c -> p (a t c)')
        KI = ki[:].rearrange('p a t c -> p (a t c)')
        P.op('dve', lambda e: e.tensor_scalar(K, A, float(1.0 / (2 * np.pi)), None, ALU.mult), reads=['ang0', 'ang1'], writes=['kk'])
        P.op('dve', lambda e: e.tensor_copy(KI, K), reads=['kk'], writes=['ki'])
        P.op('dve', lambda e: e.tensor_copy(K, KI), reads=['ki'], writes=['kk'])
        C1 = 6.28125
        C2 = float(2 * np.pi - 6.28125)
        P.op('dve', lambda e: e.scalar_tensor_tensor(A, K, -C1, A, ALU.mult, ALU.add), reads=['kk', 'ang0', 'ang1'], writes=['ang'])
        P.op('dve', lambda e: e.scalar_tensor_tensor(A, K, -C2, A, ALU.mult, ALU.add), reads=['kk', 'ang'], writes=['ang'])
        P.op('dve', lambda e: e.tensor_scalar(A, A, 3.1415925, -3.1415925, ALU.min, ALU.max), reads=['ang'], writes=['ang'])
        P.op('act', lambda e: e.activation(cs[:, :, 0:8], ang[:, 0], AF.Sin), reads=['ang'], writes=['cs0'])
        P.op('act', lambda e: e.activation(cs[:, :, 8:16], ang[:, 1], AF.Sin), reads=['ang'], writes=['cs1'])
        P.dma('sp', D['cs'].rearrange('(t p) c -> p t c', p=128), cs[:], reads=['cs0', 'cs1'], writes=['d_cs'])
        P.emit()


def phase_A(P, l, D, first):
    xsrc = D['x'] if first else D['xw']
    with ExitStack() as s:
        wbf = P.sb(s, 'a_wbf', [128, 8, 3224], BF16)
        gmix = P.sb(s, 'a_gmix', [128, 1024], F32)
        lng = P.sb(s, 'a_lng', [128, 256], F32)
        lnb = P.sb(s, 'a_lnb', [128, 256], F32)
        qkg = P.sb(s, 'a_qkg', [128, 2, 64], F32)
        cs = P.sb(s, 'a_cs', [128, NT, 16], F32)
        idb = P.sb(s, 'a_idb', [128, 128], BF16)
        xts = [P.sb(s, 'a_xt%d' % i, [128, 1024], F32) for i in range(2)]
        junk = P.sb(s, 'a_junk', [128, 1024], BF16)
        ss = P.sb(s, 'a_ss', [128, 2], F32)
        xn = P.sb(s, 'a_xn', [128, 1024], BF16)
        xnT = [P.sb(s, 'a_xnT%d' % i, [128, 8, 512], BF16) for i in range(2)]
        ge = P.sb(s, 'a_ge', [128, 4, 64], F32)
        cen = P.sb(s, 'a_cen', [128, 4, 64], F32)
        sq = P.sb(s, 'a_sq', [128, 4, 64], F32)
        m4 = P.sb(s, 'a_m4', [128, 8], F32)
        vnb = [P.sb(s, 'a_vnb%d' % i, [128, 256], BF16) for i in range(2)]
        sqq = P.sb(s, 'a_sqq', [128, 12, 64], F32)
        ss12 = P.sb(s, 'a_ss12', [128, 12], F32)
        qk32 = P.sb(s, 'a_qk32', [128, 12, 64], F32)
        rt = P.sb(s, 'a_rt', [128, 4, 12, 8], F32)
        qkb = P.sb(s, 'a_qkb', [128, 12, 64], BF16)
        qkTs = [P.sb(s, 'a_qkTs%d' % i, [128, 6, 128], BF16) for i in range(2)]
        vaug = [P.sb(s, 'a_vaug%d' % i, [128, 6, 65], BF16) for i in range(2)]
        gs = [P.sb(s, 'a_gs%d' % i, [128, 408], F32) for i in range(2)]
        fo = [P.sb(s, 'a_fo%d' % i, [128, 512], F32) for i in range(2)]
        pT = P.ps(s, 'a_pT', [128, 1024], BF16)
        pv = P.ps(s, 'a_pv', [128, 512])
        pq = P.ps(s, 'a_pq', [128, 512])
        pk = P.ps(s, 'a_pk', [128, 512])
        pbv = P.ps(s, 'a_pbv', [128, 512])
        pg = P.ps(s, 'a_pg', [128, 512])
        pf = [P.ps(s, 'a_pf%d' % i, [128, 512]) for i in range(2)]

        for k in range(8):
            P.dma('pool', wbf[:, k, :], D['w_in'][l, k * 128:(k + 1) * 128, :], writes=[('wbf', k)])
        P.dma('sp', gmix[:], D['g_mix'][l].partition_broadcast(128), writes=['gmix'])
        P.dma('sp', lng[:], D['ln_v_g'][l].rearrange('g d -> (g d)').partition_broadcast(128), writes=['lng'])
        P.dma('sp', lnb[:], D['ln_v_b'][l].rearrange('g d -> (g d)').partition_broadcast(128), writes=['lnb'])
        P.dma('sp', qkg[:, 0, :], D['q_norm_g'][l].partition_broadcast(128), writes=['qkg0'])
        P.dma('sp', qkg[:, 1, :], D['k_norm_g'][l].partition_broadcast(128), writes=['qkg1'])
        P.dma('sp', cs[:], D['cs'].rearrange('(t p) c -> p t c', p=128), reads=['d_cs'], writes=['cs'])
        P.dma('pool', idb[:], D['ident'], writes=['idb'])
        for i in range(2):
            P.op('pool', lambda e, i=i: e.memset(vaug[i][:, :, 64:65], 1.0), writes=[('vaug', i)])
        WB = [('wbf', k) for k in range(8)]
        fcount = 0
        for g in range(8):
            XT = xnT[g % 2]
            kxt = ('xnT', g % 2)
            for j in range(4):
                t = g * 4 + j
                b = t % 2
                xt = xts[b]
                P.dma('sp', xt[:], xsrc[t * 128:(t + 1) * 128, :], writes=[('xt', b)])
                P.op('dve', lambda e, b=b: e.memset(ss[:, b:b + 1], 0.0), writes=[('ss', b)])
                P.op('act', lambda e, xt=xt, b=b: e.activation(junk[:], xt[:], AF.Square, accum_out=ss[:, b:b + 1]),
                     reads=[('xt', b), ('ss', b)], writes=['junk', ('ss', b)])
                P.op('act', lambda e, b=b: e.activation(ss[:, b:b + 1], ss[:, b:b + 1], AF.Sqrt, bias=EPS, scale=1.0 / 1024),
                     reads=[('ss', b)], writes=[('ss', b)])
                P.op('dve', lambda e, b=b: e.reciprocal(ss[:, b:b + 1], ss[:, b:b + 1]), reads=[('ss', b)], writes=[('ss', b)])
                P.op('dve', lambda e, xt=xt, b=b: e.scalar_tensor_tensor(xn[:], xt[:], ss[:, b:b + 1], gmix[:], ALU.mult, ALU.mult),
                     reads=[('xt', b), ('ss', b), 'gmix'], writes=['xn'])
                for k in range(8):
                    P.op('pe', lambda e, k=k: e.transpose(pT[:, k * 128:(k + 1) * 128], xn[:, k * 128:(k + 1) * 128], idb[:]),
                         reads=['xn', 'idb'], writes=['pT'])
                P.op('act', lambda e, XT=XT, j=j: e.copy(XT[:, :, j * 128:(j + 1) * 128], pT[:].rearrange('p (k t) -> p k t', t=128)),
                     reads=['pT'], writes=[kxt + (j,)])
                for (pp, nm, c0, c1) in ((pv, 'pv', 256, 512), (pq, 'pq', 512, 896), (pk, 'pk', 896, 1280), (pbv, 'pbv', 1280, 1664), (pg, 'pg', 2816, 3224)):
                    for k in range(8):
                        P.op('pe', lambda e, pp=pp, k=k, c0=c0, c1=c1, XT=XT, j=j: e.matmul(pp[:, 0:c1 - c0], XT[:, k, j * 128:(j + 1) * 128], wbf[:, k, c0:c1], start=(k == 0), stop=(k == 7)),
                             reads=[kxt + (j,), ('wbf', k)], writes=[nm])
                GE = ge[:].rearrange('p a b -> p (a b)')
                P.op('act', lambda e: e.activation(GE, pv[:, 0:256], AF.Gelu_apprx_tanh), reads=['pv'], writes=['ge'])
                P.op('dve', lambda e: e.tensor_reduce(m4[:, 0:4], ge[:], AX.X, ALU.add), reads=['ge'], writes=['m4a'])
                P.op('dve', lambda e: e.tensor_scalar(m4[:, 0:4], m4[:, 0:4], 1.0 / 64, None, ALU.mult), reads=['m4a'], writes=['m4a'])
                P.op('dve', lambda e: e.tensor_tensor(cen[:], ge[:], bc(m4[:, 0:4].unsqueeze(2), [128, 4, 64]), ALU.subtract), reads=['ge', 'm4a'], writes=['cen'])
                P.op('act', lambda e: e.activation(sq[:], cen[:], AF.Square), reads=['cen'], writes=['sq'])
                P.op('dve', lambda e: e.tensor_reduce(m4[:, 4:8], sq[:], AX.X, ALU.add), reads=['sq'], writes=['m4b'])
                P.op('act', lambda e: e.activation(m4[:, 4:8], m4[:, 4:8], AF.Sqrt, bias=EPS, scale=1.0 / 64), reads=['m4b'], writes=['m4b'])
                P.op('dve', lambda e: e.reciprocal(m4[:, 4:8], m4[:, 4:8]), reads=['m4b'], writes=['m4b'])
                P.op('dve', lambda e: e.tensor_tensor(cen[:], cen[:], bc(m4[:, 4:8].unsqueeze(2), [128, 4, 64]), ALU.mult), reads=['cen', 'm4b'], writes=['cen'])
                CEN = cen[:].rearrange('p a b -> p (a b)')
                P.op('pool', lambda e: e.tensor_tensor(CEN, CEN, lng[:], ALU.mult), reads=['cen', 'lng'], writes=['cen'])
                P.op('pool', lambda e, b=b: e.tensor_tensor(vnb[b][:], CEN, lnb[:], ALU.add), reads=['cen', 'lnb'], writes=[('vnb', b)])
                P.dma('sp', D['vn'][t * 128:(t + 1) * 128, :], vnb[b][:], reads=[('vnb', b)], writes=['d_vn'])
                P.op('act', lambda e: e.activation(sqq[:, 0:6, :].rearrange('p a b -> p (a b)'), pq[:, 0:384], AF.Square), reads=['pq'], writes=['sqq0'])
                P.op('act', lambda e: e.activation(sqq[:, 6:12, :].rearrange('p a b -> p (a b)'), pk[:, 0:384], AF.Square), reads=['pk'], writes=['sqq1'])
                P.op('dve', lambda e: e.tensor_reduce(ss12[:], sqq[:], AX.X, ALU.add), reads=['sqq0', 'sqq1'], writes=['ss12'])
                P.op('act', lambda e: e.activation(ss12[:], ss12[:], AF.Sqrt, bias=EPS, scale=1.0 / 64), reads=['ss12'], writes=['ss12'])
                P.op('dve', lambda e: e.reciprocal(ss12[:], ss12[:]), reads=['ss12'], writes=['ss12'])
                P.op('dve', lambda e: e.tensor_tensor(qk32[:, 0:6, :], pq[:, 0:384].rearrange('p (a b) -> p a b', b=64), bc(ss12[:, 0:6].unsqueeze(2), [128, 6, 64]), ALU.mult),
                     reads=['pq', 'ss12'], writes=['qk32a'])
                P.op('dve', lambda e: e.tensor_tensor(qk32[:, 6:12, :], pk[:, 0:384].rearrange('p (a b) -> p a b', b=64), bc(ss12[:, 6:12].unsqueeze(2), [128, 6, 64]), ALU.mult),
                     reads=['pk', 'ss12'], writes=['qk32b'])
                P.op('pool', lambda e: e.tensor_tensor(qk32[:, 0:6, :], qk32[:, 0:6, :], bc(qkg[:, 0:1, :], [128, 6, 64]), ALU.mult), reads=['qk32a', 'qkg0'], writes=['qk32a'])
                P.op('pool', lambda e: e.tensor_tensor(qk32[:, 6:12, :], qk32[:, 6:12, :], bc(qkg[:, 1:2, :], [128, 6, 64]), ALU.mult), reads=['qk32b', 'qkg1'], writes=['qk32b'])
                cosb = bc(cs[:, t:t + 1, 0:8], [128, 12, 8])
                sinb = bc(cs[:, t:t + 1, 8:16], [128, 12, 8])
                x1 = qk32[:, :, 0:8]
                x2 = qk32[:, :, 8:16]
                P.op('pool', lambda e, cosb=cosb: e.tensor_tensor(rt[:, 0], x1, cosb, ALU.mult), reads=['qk32a', 'qk32b', 'cs'], writes=['rt0'])
                P.op('pool', lambda e, sinb=sinb: e.tensor_tensor(rt[:, 1], x2, sinb, ALU.mult), reads=['qk32a', 'qk32b', 'cs'], writes=['rt1'])
                P.op('dve', lambda e, cosb=cosb: e.tensor_tensor(rt[:, 2], x2, cosb, ALU.mult), reads=['qk32a', 'qk32b', 'cs'], writes=['rt2'])
                P.op('dve', lambda e, sinb=sinb: e.tensor_tensor(rt[:, 3], x1, sinb, ALU.mult), reads=['qk32a', 'qk32b', 'cs'], writes=['rt3'])
                P.op('act', lambda e: e.copy(qkb[:], qk32[:]), reads=['qk32a', 'qk32b'], writes=['qkb'])
                P.op('dve', lambda e: e.tensor_tensor(qkb[:, :, 0:8], rt[:, 0], rt[:, 1], ALU.subtract), reads=['rt0', 'rt1', 'qkb'], writes=['qkb'])
                P.op('dve', lambda e: e.tensor_tensor(qkb[:, :, 8:16], rt[:, 2], rt[:, 3], ALU.add), reads=['rt2', 'rt3', 'qkb'], writes=['qkb'])
                for i in range(6):
                    P.op('pe', lambda e, i=i: e.transpose(pT[:, i * 128:(i + 1) * 128], qkb[:, 2 * i:2 * i + 2, :].rearrange('p a b -> p (a b)'), idb[:]),
                         reads=['qkb', 'idb'], writes=['pT'])
                P.op('act', lambda e, b=b: e.copy(qkTs[b][:], pT[:, 0:768].rearrange('p (k t) -> p k t', t=128)), reads=['pT'], writes=[('qkTs', b)])
                P.dma('sp', D['qkT'][:, :, t * 128:(t + 1) * 128].rearrange('i p t -> p i t'), qkTs[b][:], reads=[('qkTs', b)], writes=['d_qkT'])
                P.op('act', lambda e, b=b: e.copy(vaug[b][:, :, 0:64], pbv[:, 0:384].rearrange('p (a b) -> p a b', b=64)), reads=['pbv', ('vaug', b)], writes=[('vaug', b)])
                P.dma('sp', D['vaug'][t * 128:(t + 1) * 128, :], vaug[b][:].rearrange('p a b -> p (a b)'), reads=[('vaug', b)], writes=['d_vaug'])
                P.op('act', lambda e, b=b: e.activation(gs[b][:, 0:384], pg[:, 0:384], AF.Silu), reads=['pg'], writes=[('gs', b)])
                P.op('dve', lambda e, b=b: e.tensor_copy(gs[b][:, 384:408], pg[:, 384:408]), reads=['pg', ('gs', b)], writes=[('gs', b)])
                P.dma('sp', D['gate_s'][t * 128:(t + 1) * 128, :], gs[b][:, 0:384], reads=[('gs', b)], writes=['d_gate'])
                P.dma('sp', D['ab'][t * 128:(t + 1) * 128, :], gs[b][:, 384:408], reads=[('gs', b)], writes=['d_ab'])
            allx = [kxt + (j,) for j in range(4)]
            for ci in range(11):
                c0 = ci * 128 if ci < 2 else 1664 + (ci - 2) * 128
                fb = fcount % 2
                fcount += 1
                for k in range(8):
                    P.op('pe', lambda e, fb=fb, k=k, c0=c0, XT=XT: e.matmul(pf[fb][:], wbf[:, k, c0:c0 + 128], XT[:, k, :], start=(k == 0), stop=(k == 7)),
                         reads=allx + [('wbf', k)], writes=[('pf', fb)])
                if ci < 2:
                    P.op('act', lambda e, fb=fb: e.activation(fo[fb][:], pf[fb][:], AF.Gelu_apprx_tanh), reads=[('pf', fb)], writes=[('fo', fb)])
                    P.dma('sp', D['uT'][ci * 128:(ci + 1) * 128, g * 512:(g + 1) * 512], fo[fb][:], reads=[('fo', fb)], writes=['d_uT'])
                else:
                    P.op('dve', lambda e, fb=fb: e.tensor_copy(fo[fb][:], pf[fb][:]), reads=[('pf', fb)], writes=[('fo', fb)])
                    P.dma('sp', D['cT'][(ci - 2) * 128:(ci - 1) * 128, g * 512:(g + 1) * 512], fo[fb][:], reads=[('fo', fb)], writes=['d_cT'])
        P.emit()


def phase_B(P, l, D):
    with ExitStack() as s:
        ws32 = P.sb(s, 'b_ws32', [128, 4, 128], F32)
        idf = P.sb(s, 'b_idf', [128, 128], F32)
        wsT = P.sb(s, 'b_wsT', [128, 4, 128], BF16)
        bias = P.sb(s, 'b_bias', [64, 4, 128], F32)
        vn = [P.sb(s, 'b_vn%d' % i, [128, 4, 256], BF16) for i in range(2)]
        ut = [P.sb(s, 'b_ut%d' % i, [64, 4, 512], F32) for i in range(2)]
        mx = P.sb(s, 'b_mx', [64, 4, 128], F32)
        yb = [P.sb(s, 'b_yb%d' % i, [64, 4, 512], BF16) for i in range(2)]
        ptr = P.ps(s, 'b_ptr', [128, 512])
        pm = [P.ps(s, 'b_pm%d' % i, [64, 512]) for i in range(4)]
        P.dma('sp', ws32[:], D['w_s'][l].rearrange('g i j -> i g j'), writes=['ws32'])
        P.dma('sp', idf[:], D['ident'], writes=['idf'])
        P.dma('sp', bias[:].rearrange('p g i -> p (g i)'), D['b_s'][l].rearrange('g i -> (g i)').partition_broadcast(64), writes=['bias'])
        for g in range(4):
            P.op('pe', lambda e, g=g: e.transpose(ptr[:, g * 128:(g + 1) * 128], ws32[:, g, :], idf[:]), reads=['ws32', 'idf'], writes=['ptr'])
        P.op('dve', lambda e: e.tensor_copy(wsT[:].rearrange('p g i -> p (g i)'), ptr[:]), reads=['ptr'], writes=['wsT'])
        for it in range(8):
            b = it % 2
            P.dma('sp', vn[b][:], D['vn'][it * 512:(it + 1) * 512, :].rearrange('(c p) n -> p c n', p=128), reads=['d_vn'], writes=[('vn', b)])
            P.dma('sp', ut[b][:], D['uT'][:, it * 512:(it + 1) * 512].rearrange('(g d) t -> d g t', d=64), reads=['d_uT'], writes=[('ut', b)])
            for g in range(4):
                for c in range(4):
                    P.op('pe', lambda e, g=g, c=c, b=b: e.matmul(pm[g][:, c * 128:(c + 1) * 128], vn[b][:, c, g * 64:(g + 1) * 64], wsT[:, g, :], start=True, stop=True),
                         reads=[('vn', b), 'wsT'], writes=[('pm', g)])
                P.op('dve', lambda e, g=g: e.tensor_tensor(mx[:], pm[g][:].rearrange('p (c i) -> p c i', i=128), bc(bias[:, g:g + 1, :], [64, 4, 128]), ALU.add),
                     reads=[('pm', g), 'bias'], writes=['mx'])
                P.op('dve', lambda e, g=g, b=b: e.tensor_tensor(yb[b][:, g, :], mx[:].rearrange('p c i -> p (c i)'), ut[b][:, g, :], ALU.mult),
                     reads=['mx', ('ut', b)], writes=[('yb', b)])
            P.dma('sp', D['yT'][0:256, it * 512:(it + 1) * 512].rearrange('(g d) t -> d g t', d=64), yb[b][:], reads=[('yb', b)], writes=['d_yT'])
        P.emit()


PATS = (1, 4, 16)
KPAD = 1024


def phase_C(P, l, D):
    with ExitStack() as s:
        vs = {}
        for d in PATS:
            nt = d * (S // d // 128 + 1)
            vs[d] = P.sb(s, 'c_vs%d' % d, [128, nt, 390], BF16)
        mab = P.sb(s, 'c_mab', [128, 512], BF16)
        sel = P.sb(s, 'c_sel', [65, 64], F32)
        qh = [P.sb(s, 'c_qh%d' % i, [64, S], BF16) for i in range(2)]
        kh = [P.sb(s, 'c_kh%d' % i, [64, S + 2 * KPAD], BF16) for i in range(2)]
        pex = [P.sb(s, 'c_pex%d' % i, [128, 512], BF16) for i in range(2)]
        acc = P.sb(s, 'c_acc', [65, S], F32)
        rd = P.sb(s, 'c_rd', [64, 512], F32)
        yb = [P.sb(s, 'c_yb%d' % i, [64, 512], BF16) for i in range(2)]
        pss = [P.ps(s, 'c_ps%d' % i, [128, 512]) for i in range(2)]
        ppv = [P.ps(s, 'c_pv%d' % i, [65, 512]) for i in range(2)]
        pd = P.ps(s, 'c_pd', [64, 512])
        P.dma('pool', mab[:], D['mab'], writes=['mab'])
        P.dma('sp', sel[:], D['sel65'], writes=['sel'])
        for i in range(2):
            P.op('pool', lambda e, i=i: e.memset(kh[i][:, 0:KPAD], 0.0), writes=[('kh', i)])
            P.op('pool', lambda e, i=i: e.memset(kh[i][:, KPAD + S:], 0.0), writes=[('kh', i)])
        for d in PATS:
            L = S // d
            nqb = L // 128
            P.op('pool', lambda e, d=d: e.memset(vs[d][:].rearrange('p a b -> p (a b)'), 0.0), writes=[('vs', d)])
            vsrc = D['vaug'].rearrange('(j r) c -> r j c', r=d)
            for r in range(d):
                tb = r * (nqb + 1)
                if nqb > 1:
                    P.dma('sp', vs[d][:, tb + 1:tb + nqb, :], vsrc[r, 64:64 + (nqb - 1) * 128, :].rearrange('(k p) c -> p k c', p=128),
                          reads=['d_vaug', ('vs', d)], writes=[('vs', d)])
                P.dma('sp', vs[d][64:128, tb, :], vsrc[r, 0:64, :], reads=['d_vaug', ('vs', d)], writes=[('vs', d)])
                P.dma('sp', vs[d][0:64, tb + nqb, :], vsrc[r, L - 64:L, :], reads=['d_vaug', ('vs', d)], writes=[('vs', d)])
        it = 0
        for h in range(6):
            hb = h % 2
            P.dma('sp', qh[hb][:], D['qkT'][h // 2, (h % 2) * 64:(h % 2) * 64 + 64, :], reads=['d_qkT'], writes=[('qh', hb)])
            P.dma('sp', kh[hb][:, KPAD:KPAD + S], D['qkT'][3 + h // 2, (h % 2) * 64:(h % 2) * 64 + 64, :], reads=['d_qkT'], writes=[('kh', hb)])
            for pi, d in enumerate(PATS):
                L = S // d
                nqb = L // 128
                for r in range(d):
                    tb = r * (nqb + 1)
                    for qb0 in range(0, nqb, 2):
                        ib = it % 2
                        it += 1
                        combos = ((qb0, qb0), (qb0 + 1, qb0), (qb0 + 1, qb0 + 1), (qb0 + 2, qb0 + 1))
                        for ci, (kt, qb) in enumerate(combos):
                            k0 = KPAD + r + d * (kt * 128 - 64)
                            q0 = r + d * (qb * 128)
                            P.op('pe', lambda e, ib=ib, ci=ci, k0=k0, q0=q0, d=d, hb=hb: e.matmul(
                                pss[ib][:, ci * 128:(ci + 1) * 128], kh[hb][:, ssl(k0, 128, d)], qh[hb][:, ssl(q0, 128, d)], start=True, stop=True),
                                reads=[('kh', hb), ('qh', hb)], writes=[('pss', ib)])
                        P.op('act', lambda e, ib=ib: e.activation(pex[ib][:], pss[ib][:], AF.Exp, scale=0.125), reads=[('pss', ib)], writes=[('pex', ib)])
                        P.op('dve', lambda e, ib=ib: e.tensor_tensor(pex[ib][:], pex[ib][:], mab[:], ALU.mult), reads=[('pex', ib), 'mab'], writes=[('pex', ib)])
                        for ci, (kt, qb) in enumerate(combos):
                            qi = qb - qb0
                            P.op('pe', lambda e, ib=ib, ci=ci, kt=kt, qi=qi, d=d, tb=tb, h=h: e.matmul(
                                ppv[ib][:, qi * 128:(qi + 1) * 128], vs[d][:, tb + kt, h * 65:(h + 1) * 65], pex[ib][:, ci * 128:(ci + 1) * 128],
                                start=(ci % 2 == 0), stop=(ci % 2 == 1)), reads=[('vs', d), ('pex', ib)], writes=[('ppv', ib)])
                        a0 = r + d * (qb0 * 128)
                        av = acc[:, ssl(a0, 256, d)]
                        if pi == 0:
                            P.op('dve', lambda e, av=av, ib=ib: e.tensor_copy(av, ppv[ib][:, 0:256]), reads=[('ppv', ib)], writes=['acc'])
                        else:
                            P.op('dve', lambda e, av=av, ib=ib: e.tensor_tensor(av, av, ppv[ib][:, 0:256], ALU.add), reads=[('ppv', ib), 'acc'], writes=['acc'])
            for c4 in range(8):
                yb_ = yb[c4 % 2]
                P.op('pe', lambda e, c4=c4: e.matmul(pd[:], sel[:], acc[:, c4 * 512:(c4 + 1) * 512], start=True, stop=True), reads=['sel', 'acc'], writes=['pd'])
                P.op('dve', lambda e: e.reciprocal(rd[:], pd[:]), reads=['pd'], writes=['rd'])
                P.op('dve', lambda e, c4=c4, yb_=yb_: e.tensor_tensor(yb_[:], acc[0:64, c4 * 512:(c4 + 1) * 512], rd[:], ALU.mult), reads=['acc', 'rd'], writes=[('yb', c4 % 2)])
                P.dma('sp', D['yT'][256 + h * 64:256 + (h + 1) * 64, c4 * 512:(c4 + 1) * 512], yb_[:], reads=[('yb', c4 % 2)], writes=['d_yT'])
        P.emit()


def phase_D1(P, l, D):
    with ExitStack() as s:
        cw = P.sb(s, 'd_cw', [128, 5, 9], F32)
        idf = P.sb(s, 'd_idf', [128, 128], F32)
        raw = [P.sb(s, 'd_raw%d' % i, [128, S + 4], F32) for i in range(2)]
        cv = P.sb(s, 'd_cv', [128, S], F32)
        tm = [P.sb(s, 'd_tm%d' % i, [128, 4, 128], F32) for i in range(2)]
        sq = P.sb(s, 'd_sq', [128, 8, 64], F32)
        r8 = P.sb(s, 'd_r8', [128, 8], F32)
        fT = [P.sb(s, 'd_fT%d' % i, [128, 512], BF16) for i in range(2)]
        abt = P.sb(s, 'd_abt', [128, NT, 24], F32)
        gbt = P.sb(s, 'd_gbt', [128, NT, 24], F32)
        dtb = P.sb(s, 'd_dtb', [128, 12], F32)
        nA = P.sb(s, 'd_nA', [128, 12], F32)
        ptr = [P.ps(s, 'd_ptr%d' % i, [128, 512]) for i in range(2)]
        ptb = [P.ps(s, 'd_ptb%d' % i, [128, 512]) for i in range(2)]
        for k in range(5):
            P.dma('sp', cw[:, k, :], D['conv_w'][l, k].rearrange('(c p) -> p c', p=128), writes=['cw'], allow_slow_non_contiguous=True)
        P.dma('sp', idf[:], D['ident'], writes=['idf'])
        for i in range(2):
            P.op('pool', lambda e, i=i: e.memset(raw[i][:, 0:2], 0.0), writes=[('raw', i)])
            P.op('pool', lambda e, i=i: e.memset(raw[i][:, S + 2:S + 4], 0.0), writes=[('raw', i)])
        P.dma('sp', abt[:], D['ab'].rearrange('(t p) c -> p t c', p=128), reads=['d_ab'], writes=['abt'])
        P.dma('sp', dtb[:], D['dt_bias'][l].rearrange('a h -> (a h)').partition_broadcast(128), writes=['dtb'])
        P.dma('sp', nA[:], D['a_log'][l].rearrange('a h -> (a h)').partition_broadcast(128), writes=['nA'])
        P.op('act', lambda e: e.activation(nA[:], nA[:], AF.Exp), reads=['nA'], writes=['nA'])
        P.op('dve', lambda e: e.tensor_scalar(nA[:], nA[:], -1.0, None, ALU.mult), reads=['nA'], writes=['nA'])
        P.op('dve', lambda e: e.tensor_tensor(gbt[:, :, 0:12], abt[:, :, 0:12], bc(dtb[:].unsqueeze(1), [128, NT, 12]), ALU.add), reads=['abt', 'dtb'], writes=['gbt0'])
        P.op('act', lambda e: e.activation(gbt[:, :, 0:12], gbt[:, :, 0:12], AF.Exp), reads=['gbt0'], writes=['gbt0'])
        P.op('act', lambda e: e.activation(gbt[:, :, 0:12], gbt[:, :, 0:12], AF.Ln, bias=1.0), reads=['gbt0'], writes=['gbt0'])
        P.op('dve', lambda e: e.tensor_tensor(gbt[:, :, 0:12], gbt[:, :, 0:12], bc(nA[:].unsqueeze(1), [128, NT, 12]), ALU.mult), reads=['gbt0', 'nA'], writes=['gbt0'])
        P.op('act', lambda e: e.activation(gbt[:, :, 12:24], abt[:, :, 12:24], AF.Sigmoid), reads=['abt'], writes=['gbt1'])
        P.dma('sp', D['gb'].rearrange('(t p) c -> p t c', p=128), gbt[:], reads=['gbt0', 'gbt1'], writes=['d_gb'])
        n4 = 0
        import os
        CUT = int(os.environ.get('D1CUT', '99'))
        for c in range(int(os.environ.get('D1C0', '0')), int(os.environ.get('D1C1', '9'))):
            rb = c % 2
            R = raw[rb]
            P.dma('sp', R[:, 2:S + 2], D['cT'][c * 128:(c + 1) * 128, :], reads=['d_cT'], writes=[('raw', rb)])
            P.op('dve', lambda e, R=R, c=c: e.tensor_scalar(cv[:], R[:, 0:S], cw[:, 0, c:c + 1], None, ALU.mult), reads=[('raw', rb), 'cw'], writes=['cv'])
            for k in range(1, 5):
                eng = 'dve'
                P.op(eng, lambda e, R=R, c=c, k=k: e.scalar_tensor_tensor(cv[:], R[:, k:k + S], cw[:, k, c:c + 1], cv[:], ALU.mult, ALU.add),
                     reads=[('raw', rb), 'cw', 'cv'], writes=['cv'])
            P.op('act', lambda e: e.activation(cv[:], cv[:], AF.Silu), reads=['cv'], writes=['cv'])
            for t4 in range(8 if CUT >= 2 else 0):
                pb = n4 % 2
                n4 += 1
                for j in range(4):
                    t = t4 * 4 + j
                    P.op('pe', lambda e, pb=pb, j=j, t=t: e.transpose(ptr[pb][:, j * 128:(j + 1) * 128], cv[:, t * 128:(t + 1) * 128], idf[:]),
                         reads=['cv', 'idf'], writes=[('ptr', pb)])
                TM = tm[pb]
                PV = ptr[pb][:].rearrange('p (j h d) -> p (j h) d', h=2, d=64)
                if c >= 6:
                    P.op('act', lambda e, pb=pb, TM=TM: e.copy(TM[:].rearrange('p j c -> p (j c)'), ptr[pb][:]), reads=[('ptr', pb)], writes=[('tm', pb)])
                    P.dma('sp', D['v_tm'][t4 * 512:(t4 + 1) * 512, (c - 6) * 128:(c - 5) * 128].rearrange('(j p) c -> p j c', p=128), TM[:], reads=[('tm', pb)], writes=['d_vtm'])
                else:
                    P.op('act', lambda e, pb=pb: e.activation(sq[:].rearrange('p a b -> p (a b)'), ptr[pb][:], AF.Square), reads=[('ptr', pb)], writes=['sq'])
                    P.op('dve', lambda e: e.tensor_reduce(r8[:], sq[:], AX.X, ALU.add), reads=['sq'], writes=['r8'])
                    P.op('act', lambda e: e.activation(r8[:], r8[:], AF.Sqrt, bias=EPS, scale=1.0), reads=['r8'], writes=['r8'])
                    P.op('dve', lambda e: e.reciprocal(r8[:], r8[:]), reads=['r8'], writes=['r8'])
                    if c < 3:
                        P.op('dve', lambda e: e.tensor_scalar(r8[:], r8[:], 0.125, None, ALU.mult), reads=['r8'], writes=['r8'])
                    P.op('dve', lambda e, TM=TM, PV=PV: e.tensor_tensor(TM[:].rearrange('p j (h d) -> p (j h) d', d=64), PV, bc(r8[:].unsqueeze(2), [128, 8, 64]), ALU.mult),
                         reads=[('ptr', pb), 'r8'], writes=[('tm', pb)])
                    if c >= 3:
                        P.dma('sp', D['k_tm'][t4 * 512:(t4 + 1) * 512, (c - 3) * 128:(c - 2) * 128].rearrange('(j p) c -> p j c', p=128), TM[:], reads=[('tm', pb)], writes=['d_ktm'])
                    for j in range(4):
                        P.op('pe', lambda e, pb=pb, j=j, TM=TM: e.transpose(ptb[pb][:, j * 128:(j + 1) * 128], TM[:, j, :], idf[:]), reads=[('tm', pb), 'idf'], writes=[('ptb', pb)])
                    P.op('act', lambda e, pb=pb: e.copy(fT[pb][:], ptb[pb][:]), reads=[('ptb', pb)], writes=[('fT', pb)])
                    dst = D['qT_g'] if c < 3 else D['kT_g']
                    cc = c if c < 3 else c - 3
                    P.dma('sp', dst[cc * 128:(cc + 1) * 128, t4 * 512:(t4 + 1) * 512], fT[pb][:], reads=[('fT', pb)], writes=['d_qkTg'])
        P.emit()


def phase_D2(P, l, D):
    import os
    C = 64
    NCH = S // C
    NST = int(os.environ.get('D2N', str(NCH)))
    MD = BF16 if os.environ.get('D2BF', '1') == '1' else F32
    with ExitStack() as s:
        def T12(name, dt=F32):
            return P.sb(s, 'e_' + name, [64, 12, 64], dt)
        ones = P.sb(s, 'e_ones', [64, 64], F32)
        idf = P.sb(s, 'e_idf', [64, 64], F32)
        idm = P.sb(s, 'e_idm', [64, 64], MD)
        idbc = T12('idbc')
        triF = P.sb(s, 'e_triF', [64, 64], F32)
        triB = P.sb(s, 'e_triB', [64, 64], F32)
        mW, mWt, mI = T12('mW'), T12('mWt'), T12('mI')
        St, St2, Sm = T12('S'), T12('S2'), T12('Sm', MD)
        ktm = [T12('ktm%d' % i) for i in range(2)]
        vtm = [T12('vtm%d' % i) for i in range(2)]
        kT = [T12('kT%d' % i, MD) for i in range(2)]
        qT = [T12('qT%d' % i, MD) for i in range(2)]
        gbv = [P.sb(s, 'e_gb%d' % i, [64, 24], F32) for i in range(2)]
        gc = P.sb(s, 'e_gc', [64, 12], F32)
        egc = [P.sb(s, 'e_egc%d' % i, [64, 12], F32) for i in range(2)]
        egl = [P.sb(s, 'e_egl%d' % i, [64, 12], F32) for i in range(2)]
        egd = P.sb(s, 'e_egd', [64, 12], F32)
        Dg = P.sb(s, 'e_Dg', [64, 24, 64], F32)
        diff, Ea, Eb = T12('diff'), T12('Ea'), T12('Eb')
        W, Wt = T12('W', MD), T12('Wt', MD)
        A1, A1t, A2, A2t = T12('A1', MD), T12('A1t', MD), T12('A2', MD), T12('A2t', MD)
        nxTI = T12('nxTI', MD)
        Yt = [T12('Yt0', MD), T12('Yt1', MD)]
        Yf = [T12('Yf0', MD), T12('Yf1', MD)]
        QKm = [T12('QKm0', MD), T12('QKm1', MD)]
        kd = [T12('kd0', MD), T12('kd1', MD)]
        Rr, Rm, vnew, o1 = T12('R'), T12('Rm', MD), T12('vnew', MD), T12('o1')
        ob = [T12('ob0'), T12('ob1')]
        pA = P.ps(s, 'e_pA', [64, 1024])
        pB = P.ps(s, 'e_pB', [64, 1024])
        pC = P.ps(s, 'e_pC', [64, 1024])
        pS = P.ps(s, 'e_pS', [64, 1024])

        def pv(p, h):
            return p[:, h * 512:h * 512 + 384].rearrange('p (j t) -> p j t', t=64)

        def sv(t, h):
            return t[:, h * 6:(h + 1) * 6, :]

        def pcol(p, j):
            c0 = (j // 6) * 512 + (j % 6) * 64
            return p[:, c0:c0 + 64]

        def mm12(pt, pn, lfn, rfn, rfun):
            for j in range(12):
                o_, l_, r_ = pcol(pt, j), lfn(j), rfn(j)
                P.op('pe', lambda e, o_=o_, l_=l_, r_=r_: e.matmul(o_, l_, r_, start=True, stop=True), reads=rfun(j // 6), writes=[(pn, j // 6)])

        def bcol(ap12, h):
            return bc(ap12[:, h * 6:(h + 1) * 6].unsqueeze(2), [64, 6, 64])

        P.dma('sp', ones[:], D['ones64'], writes=['ones'])
        P.dma('sp', idf[:], D['ident'][0:64, 0:64], writes=['idf'])
        P.dma('sp', triF[:], D['triF'], writes=['triF'])
        P.dma('sp', triB[:], D['triB'], writes=['triB'])
        P.dma('sp', mW[:], D['mW'], writes=['mW'])
        P.dma('sp', mWt[:], D['mWt'], writes=['mWt'])
        P.dma('sp', mI[:], D['mI'], writes=['mI'])
        P.op('dve', lambda e: e.tensor_copy(idbc[:], bc(idf[:].unsqueeze(1), [64, 12, 64])), reads=['idf'], writes=['idbc'])
        P.op('dve', lambda e: e.tensor_copy(idm[:], idf[:]), reads=['idf'], writes=['idm'])
        P.op('dve', lambda e: e.memset(St[:].rearrange('p a b -> p (a b)'), 0.0), writes=[('S', 0), ('S', 1)])
        P.op('pool', lambda e: e.memset(Sm[:].rearrange('p a b -> p (a b)'), 0.0), writes=[('Sm', 0), ('Sm', 1)])

        def prep(i):
            b = i % 2
            cf = i
            cb = NCH - 1 - i
            K_, V_, KT_, QT_, GB_ = ktm[b], vtm[b], kT[b], qT[b], gbv[b]
            EGC, EGL, QKM, KD = egc[b], egl[b], QKm[b], kd[b]
            for h, cc in ((0, cf), (1, cb)):
                sl = slice(h * 6, h * 6 + 6)
                tk = slice(cc * C, (cc + 1) * C)
                P.dma('sp', K_[:, sl, :], D['k_tm'][tk, :].rearrange('t (h d) -> t h d', d=64), reads=['d_ktm'], writes=[('ktm', b, h)])
                P.dma('sp', V_[:, sl, :], D['v_tm'][tk, :].rearrange('t (h d) -> t h d', d=64), reads=['d_vtm'], writes=[('vtm', b, h)])
                P.dma('sp', KT_[:, sl, :], D['kT_g'][:, tk].rearrange('(h d) t -> d h t', d=64), reads=['d_qkTg'], writes=[('kT', b, h)])
                P.dma('sp', QT_[:, sl, :], D['qT_g'][:, tk].rearrange('(h d) t -> d h t', d=64), reads=['d_qkTg'], writes=[('qT', b, h)])
                P.dma('sp', GB_[:, h * 6:h * 6 + 6], D['gb'][tk, h * 6:h * 6 + 6], reads=['d_gb'], writes=[('gb', b, h)])
                P.dma('sp', GB_[:, 12 + h * 6:12 + h * 6 + 6], D['gb'][tk, 12 + h * 6:12 + h * 6 + 6], reads=['d_gb'], writes=[('gbb', b, h)])
            beta = GB_[:, 12:24]
            DgF = Dg[:].rearrange('p a b -> p (a b)')
            for h in range(2):
                tri = triF if h == 0 else triB
                trin = 'triF' if h == 0 else 'triB'
                rG, rBt = ('gb', b, h), ('gbb', b, h)
                P.op('pe', lambda e, h=h, tri=tri, GB_=GB_: e.matmul(pA[:, h * 512:h * 512 + 6], tri[:], GB_[:, h * 6:h * 6 + 6], start=True, stop=True), reads=[trin, rG], writes=[('pA', h)])
                P.op('dve', lambda e, h=h: e.tensor_copy(gc[:, h * 6:h * 6 + 6], pA[:, h * 512:h * 512 + 6]), reads=[('pA', h)], writes=[('gc', h)])
                P.op('act', lambda e, h=h, EGC=EGC: e.activation(EGC[:, h * 6:h * 6 + 6], pA[:, h * 512:h * 512 + 6], AF.Exp), reads=[('pA', h)], writes=[('egc', b, h)])
                P.op('dve', lambda e, h=h: e.tensor_tensor(Dg[:, h * 6:h * 6 + 6, :], sv(idbc, 0), bcol(gc, h), ALU.mult), reads=['idbc', ('gc', h)], writes=[('Dg', h)])
                P.op('pool', lambda e, h=h, beta=beta: e.tensor_tensor(Dg[:, 12 + h * 6:12 + h * 6 + 6, :], sv(idbc, 0), bcol(beta, h), ALU.mult), reads=['idbc', rBt], writes=[('Dgb', h)])
                P.op('pe', lambda e, h=h: e.matmul(pB[:, h * 512:h * 512 + 384], ones[:], DgF[:, h * 384:(h + 1) * 384], start=True, stop=True), reads=['ones', ('Dg', h)], writes=[('pB', h)])
                P.op('pe', lambda e, h=h: e.matmul(pC[:, h * 512:h * 512 + 384], ones[:], DgF[:, 768 + h * 384:768 + (h + 1) * 384], start=True, stop=True), reads=['ones', ('Dgb', h)], writes=[('pC', h)])
                P.op('dve', lambda e, h=h: e.tensor_tensor(sv(diff, h), pv(pB, h), bcol(gc, h), ALU.subtract), reads=[('pB', h), ('gc', h)], writes=[('diff', h)])
                lc = h * 512 + (63 if h == 0 else 0)
                lastv = pB[:, lc:lc + 64 * 5 + 1:64]
                P.op('act', lambda e, h=h, lastv=lastv, EGL=EGL: e.activation(EGL[:, h * 6:h * 6 + 6], lastv, AF.Exp), reads=[('pB', h)], writes=[('egl', b, h)])
                P.op('dve', lambda e, h=h, lastv=lastv: e.tensor_tensor(egd[:, h * 6:h * 6 + 6], lastv, gc[:, h * 6:h * 6 + 6], ALU.subtract), reads=[('pB', h), ('gc', h)], writes=[('egd', h)])
                P.op('act', lambda e, h=h: e.activation(egd[:, h * 6:h * 6 + 6], egd[:, h * 6:h * 6 + 6], AF.Exp), reads=[('egd', h)], writes=[('egd', h)])
                P.op('pool', lambda e, h=h, K_=K_, KD=KD: e.tensor_tensor(sv(KD, h), sv(K_, h), bcol(egd, h), ALU.mult), reads=[('ktm', b, h), ('egd', h)], writes=[('kd', b, h)])
                P.op('act', lambda e, h=h: e.activation(sv(Ea, h), sv(diff, h), AF.Exp), reads=[('diff', h)], writes=[('Ea', h)])
                P.op('act', lambda e, h=h: e.activation(sv(Eb, h), sv(diff, h), AF.Exp, scale=-1.0), reads=[('diff', h)], writes=[('Eb', h)])
            yield
            mm12(pA, 'pA', lambda j: KT_[:, j, :], lambda j: KT_[:, j, :], lambda h: [('kT', b, h)])
            for h in range(2):
                rBt = ('gbb', b, h)
                P.op('dve', lambda e, h=h: e.scalar_tensor_tensor(sv(Eb, h), sv(Eb, h), 1.0, sv(mWt, h), ALU.min, ALU.mult), reads=[('Eb', h), 'mWt'], writes=[('Eb', h)])
                P.op('dve', lambda e, h=h: e.tensor_tensor(sv(Eb, h), sv(Eb, h), pv(pC, h), ALU.mult), reads=[('Eb', h), ('pC', h)], writes=[('Eb', h)])
                P.op('dve', lambda e, h=h: e.tensor_tensor(sv(Wt, h), sv(Eb, h), pv(pA, h), ALU.mult), reads=[('Eb', h), ('pA', h)], writes=[('Wt', h)])
            yield
            mm12(pC, 'pC', lambda j: KT_[:, j, :], lambda j: QT_[:, j, :], lambda h: [('kT', b, h), ('qT', b, h)])
            for h in range(2):
                rBt = ('gbb', b, h)
                P.op('dve', lambda e, h=h: e.scalar_tensor_tensor(sv(diff, h), sv(Ea, h), 1.0, sv(mI, h), ALU.min, ALU.mult), reads=[('Ea', h), 'mI'], writes=[('diff', h)])
                P.op('dve', lambda e, h=h, QKM=QKM: e.tensor_tensor(sv(QKM, h), sv(diff, h), pv(pC, h), ALU.mult), reads=[('diff', h), ('pC', h)], writes=[('QKm', b, h)])
                P.op('dve', lambda e, h=h: e.scalar_tensor_tensor(sv(Ea, h), sv(Ea, h), 1.0, sv(mW, h), ALU.min, ALU.mult), reads=[('Ea', h), 'mW', ('diff', h)], writes=[('Ea', h)])
                P.op('pool', lambda e, h=h, beta=beta: e.tensor_tensor(sv(Ea, h), sv(Ea, h), bcol(beta, h), ALU.mult), reads=[('Ea', h), rBt], writes=[('Ea', h)])
                P.op('dve', lambda e, h=h: e.tensor_tensor(sv(W, h), sv(Ea, h), pv(pA, h), ALU.mult), reads=[('Ea', h), ('pA', h)], writes=[('W', h)])
                P.op('pool', lambda e, h=h: e.tensor_tensor(sv(Yt[0], h), sv(idbc, h), sv(W, h), ALU.subtract), reads=['idbc', ('W', h)], writes=[('Yt0', h)])
            yield
            cur, curT, cn, cnT = W, Wt, 'W', 'Wt'
            yi = 0
            bufs = [(A1, A1t, 'A1', 'A1t'), (A2, A2t, 'A2', 'A2t')]
            for lev in range(5):
                nx, nxT, nn, nnT = bufs[lev % 2]
                mm12(pB, 'pB', lambda j, cur=cur: cur[:, j, :], lambda j, curT=curT: curT[:, j, :], lambda h, cn=cn, cnT=cnT: [(cn, h), (cnT, h)])
                for h in range(2):
                    if lev < 4:
                        P.op('act', lambda e, h=h, nxT=nxT: e.copy(sv(nxT, h), pv(pB, h)), reads=[('pB', h)], writes=[(nnT, h)])
                    P.op('dve', lambda e, h=h: e.tensor_tensor(sv(nxTI, h), pv(pB, h), sv(idbc, h), ALU.add), reads=[('pB', h), 'idbc'], writes=[('nxTI', h)])
                if lev < 4:
                    mm12(pC, 'pC', lambda j, curT=curT: curT[:, j, :], lambda j, cur=cur: cur[:, j, :], lambda h, cn=cn, cnT=cnT: [(cn, h), (cnT, h)])
                    for h in range(2):
                        P.op('dve', lambda e, h=h, nx=nx: e.tensor_copy(sv(nx, h), pv(pC, h)), reads=[('pC', h)], writes=[(nn, h)])
                Yc = Yt[yi]
                yc_ = 'Yt%d' % yi
                if lev < 4:
                    Yn, yn_ = Yt[1 - yi], ('Yt%d' % (1 - yi),)
                else:
                    Yn, yn_ = Yf[b], ('Yf', b)
                for j in range(12):
                    h = j // 6
                    P.op('pe', lambda e, j=j, Yc=Yc: e.matmul(pcol(pA, j), nxTI[:, j, :], Yc[:, j, :], start=True, stop=True), reads=[('nxTI', h), (yc_, h)], writes=[('pA', h)])
                for h in range(2):
                    P.op('dve', lambda e, h=h, Yn=Yn: e.tensor_copy(sv(Yn, h), pv(pA, h)), reads=[('pA', h)], writes=[yn_ + (h,)])
                yi = 1 - yi
                cur, curT, cn, cnT = nx, nxT, nn, nnT
                yield

        def scan(i):
            b = i % 2
            cf = i
            cb = NCH - 1 - i
            V_, KT_, QT_, GB_ = vtm[b], kT[b], qT[b], gbv[b]
            EGC, EGL, QKM, KD, YF = egc[b], egl[b], QKm[b], kd[b], Yf[b]
            beta = GB_[:, 12:24]
            mm12(pS, 'pS', lambda j: KT_[:, j, :], lambda j: Sm[:, j, :], lambda h: [('kT', b, h), ('Sm', h)])
            for h in range(2):
                P.op('dve', lambda e, h=h, EGC=EGC: e.tensor_tensor(sv(Rr, h), pv(pS, h), bcol(EGC, h), ALU.mult), reads=[('pS', h), ('egc', b, h)], writes=[('R', h)])
                P.op('dve', lambda e, h=h, V_=V_: e.tensor_tensor(sv(Rm, h), sv(V_, h), sv(Rr, h), ALU.subtract), reads=[('R', h), ('vtm', b, h)], writes=[('Rm', h)])
            yield
            mm12(pS, 'pS', lambda j: QT_[:, j, :], lambda j: Sm[:, j, :], lambda h: [('qT', b, h), ('Sm', h)])
            for h in range(2):
                P.op('act', lambda e, h=h: e.copy(sv(o1, h), pv(pS, h)), reads=[('pS', h)], writes=[('o1', h)])
                P.op('pool', lambda e, h=h, EGC=EGC: e.tensor_tensor(sv(o1, h), sv(o1, h), bcol(EGC, h), ALU.mult), reads=[('o1', h), ('egc', b, h)], writes=[('o1', h)])
            yield
            yield
            mm12(pS, 'pS', lambda j: YF[:, j, :], lambda j: Rm[:, j, :], lambda h: [('Yf', b, h), ('Rm', h)])
            for h in range(2):
                P.op('dve', lambda e, h=h, beta=beta: e.tensor_tensor(sv(vnew, h), pv(pS, h), bcol(beta, h), ALU.mult), reads=[('pS', h), ('gbb', b, h)], writes=[('vnew', h)])
            yield
            mm12(pS, 'pS', lambda j: KD[:, j, :], lambda j: vnew[:, j, :], lambda h: [('kd', b, h), ('vnew', h)])
            OB = ob[b]
            for h in range(2):
                P.op('pool', lambda e, h=h, EGL=EGL: e.tensor_tensor(sv(St2, h), sv(St, h), bcol(EGL, h), ALU.mult), reads=[('S', h), ('egl', b, h)], writes=[('S2', h)])
                P.op('dve', lambda e, h=h: e.tensor_tensor(sv(St, h), sv(St2, h), pv(pS, h), ALU.add), reads=[('S2', h), ('pS', h)], writes=[('S', h)])
                P.op('act', lambda e, h=h: e.copy(sv(Sm, h), sv(St, h)), reads=[('S', h)], writes=[('Sm', h)])
            yield
            mm12(pS, 'pS', lambda j: QKM[:, j, :], lambda j: vnew[:, j, :], lambda h: [('QKm', b, h), ('vnew', h)])
            for h in range(2):
                cc = cf if h == 0 else cb
                P.op('dve', lambda e, h=h, OB=OB: e.tensor_tensor(sv(OB, h), sv(o1, h), pv(pS, h), ALU.add), reads=[('o1', h), ('pS', h)], writes=[('ob', b, h)])
                P.dma('sp', D['o_fb'][h, cc * C:(cc + 1) * C, :].rearrange('t (h d) -> t h d', d=64), sv(OB, h), reads=[('ob', b, h)], writes=['d_ofb'])

        def run(gens):
            gens = [g for g in gens if g is not None]
            while gens:
                for g in list(gens):
                    try:
                        next(g)
                    except StopIteration:
                        gens.remove(g)

        run([prep(0)])
        for i in range(NST):
            run([prep(i + 1) if i + 1 < NST else None, scan(i)])
        P.emit()


def phase_E(P, l, D, first):
    xsrc = D['x'] if first else D['xw']
    with ExitStack() as s:
        wo = P.sb(s, 'f_wo', [128, 8, 1024], BF16)
        ong = P.sb(s, 'f_ong', [128, 64], F32)
        idb = P.sb(s, 'f_idb', [128, 128], BF16)
        of_ = [P.sb(s, 'f_of%d' % i, [128, 2, 384], F32) for i in range(2)]
        gt = [P.sb(s, 'f_gt%d' % i, [128, 384], F32) for i in range(2)]
        o = P.sb(s, 'f_o', [128, 6, 64], F32)
        sq = P.sb(s, 'f_sq', [128, 6, 64], F32)
        r6 = P.sb(s, 'f_r6', [128, 6], F32)
        ycb = P.sb(s, 'f_ycb', [128, 384], BF16)
        yT = [P.sb(s, 'f_yT%d' % i, [128, 8, 128], BF16) for i in range(2)]
        xt = [P.sb(s, 'f_xt%d' % i, [128, 1024], F32) for i in range(2)]
        pT = P.ps(s, 'f_pT', [128, 512], BF16)
        po = [P.ps(s, 'f_po%d' % i, [128, 512]) for i in range(2)]
        for k in range(8):
            P.dma('pool', wo[:, k, :], D['w_out'][l, k * 128:(k + 1) * 128, :], writes=[('wo', k)])
        P.dma('sp', ong[:], D['o_norm_g'][l].partition_broadcast(128), writes=['ong'])
        P.dma('pool', idb[:], D['ident'], writes=['idb'])
        def front(t):
            b = t % 2
            tk = slice(t * 128, (t + 1) * 128)
            P.dma('sp', of_[b][:], D['o_fb'][:, tk, :].rearrange('a t c -> t a c'), reads=['d_ofb'], writes=[('of', b)])
            P.dma('sp', gt[b][:], D['gate_s'][tk, :], reads=['d_gate'], writes=[('gt', b)])
            P.dma('sp', xt[b][:], xsrc[tk, :], writes=[('xt', b)])
            P.dma('sp', yT[b][:, 0:5, :], D['yT'][0:640, tk].rearrange('(k p) t -> p k t', p=128), reads=['d_yT'], writes=[('yT', b, 0)])
            OF = o[:].rearrange('p a b -> p (a b)')
            P.op('dve', lambda e, b=b: e.tensor_tensor(OF, of_[b][:, 0, :], of_[b][:, 1, :], ALU.add), reads=[('of', b)], writes=['o'])
            P.op('act', lambda e: e.activation(sq[:], o[:], AF.Square), reads=['o'], writes=['sq'])
            P.op('dve', lambda e: e.tensor_reduce(r6[:], sq[:], AX.X, ALU.add), reads=['sq'], writes=['r6'])
            P.op('act', lambda e: e.activation(r6[:], r6[:], AF.Sqrt, bias=EPS, scale=1.0 / 64), reads=['r6'], writes=['r6'])
            P.op('dve', lambda e: e.reciprocal(r6[:], r6[:]), reads=['r6'], writes=['r6'])
            P.op('dve', lambda e: e.tensor_tensor(o[:], o[:], bc(r6[:].unsqueeze(2), [128, 6, 64]), ALU.mult), reads=['o', 'r6'], writes=['o'])
            P.op('pool', lambda e: e.tensor_tensor(o[:], o[:], bc(ong[:].unsqueeze(1), [128, 6, 64]), ALU.mult), reads=['o', 'ong'], writes=['o'])
            P.op('pool', lambda e, b=b: e.tensor_tensor(ycb[:], OF, gt[b][:], ALU.mult), reads=['o', ('gt', b)], writes=['ycb'])
            for k in range(3):
                P.op('pe', lambda e, k=k: e.transpose(pT[:, k * 128:(k + 1) * 128], ycb[:, k * 128:(k + 1) * 128], idb[:]), reads=['ycb', 'idb'], writes=['pT'])
            P.op('act', lambda e, b=b: e.copy(yT[b][:, 5:8, :], pT[:, 0:384].rearrange('p (k t) -> p k t', t=128)), reads=['pT'], writes=[('yT', b, 1)])
        def back(t):
            b = t % 2
            tk = slice(t * 128, (t + 1) * 128)
            for hf in range(2):
                for k in range(8):
                    P.op('pe', lambda e, hf=hf, k=k, b=b: e.matmul(po[hf][:], yT[b][:, k, :], wo[:, k, hf * 512:(hf + 1) * 512], start=(k == 0), stop=(k == 7)),
                         reads=[('yT', b, 0), ('yT', b, 1), ('wo', k)], writes=[('po', hf)])
                P.op('dve', lambda e, hf=hf, b=b: e.tensor_tensor(xt[b][:, hf * 512:(hf + 1) * 512], xt[b][:, hf * 512:(hf + 1) * 512], po[hf][:], ALU.add),
                     reads=[('po', hf), ('xt', b)], writes=[('xt', b)])
            P.dma('sp', D['xw'][tk, :], xt[b][:], reads=[('xt', b)], writes=[('d_xw', t)])

        front(0)
        for t in range(NT):
            if t + 1 < NT:
                front(t + 1)
            back(t)
        P.emit()


def phase_F(P, l, D):
    import os
    NE = int(os.environ.get('FNE', '16'))
    with ExitStack() as s:
        gff = P.sb(s, 'g_gff', [128, 1024], F32)
        idf = P.sb(s, 'g_idf', [128, 128], F32)
        idb = P.sb(s, 'g_idb', [128, 128], BF16)
        wr = P.sb(s, 'g_wr', [128, 8, 16], F32)
        aff = P.sb(s, 'g_aff', [128, NT, 16], F32)
        sel = P.sb(s, 'g_sel', [128, NT, 16], F32)
        rank = P.sb(s, 'g_rank', [128, NT, 16], F32)
        cA = P.sb(s, 'g_cA', [128, NT, 16], F32)
        cB = P.sb(s, 'g_cB', [128, NT, 16], F32)
        selb = P.sb(s, 'g_selb', [128, NT * 16], BF16)
        triS = P.sb(s, 'g_triS', [128, 128], BF16)
        onesb = P.sb(s, 'g_onesb', [128, 128], BF16)
        tg = P.sb(s, 'g_tg', [128, NT, 16, 5], BF16)
        tp = P.sb(s, 'g_tp', [128, NT, 2], F32)
        iota = P.sb(s, 'g_iota', [128, 512], F32)
        Selt = [P.sb(s, 'g_Selt%d' % i, [128, 512], BF16) for i in range(2)]
        idxf = P.sb(s, 'g_idxf', [128, 4, 8], F32)
        row5 = P.sb(s, 'g_row5', [5, 512], F32)
        idxv = P.sb(s, 'g_idxv', [128, 4], F32)
        idxi = [P.sb(s, 'g_idxi%d' % i, [128, 4], I32) for i in range(2)]
        gate = [P.sb(s, 'g_gate%d' % i, [128, 4], F32) for i in range(2)]
        affT2 = P.sb(s, 'g_affT2', [16, S], F32)
        bj = P.sb(s, 'g_bj', [16, S], BF16)
        bs = P.sb(s, 'g_bs', [16, 8], F32)
        ones16 = P.sb(s, 'g_ones16', [16, 128], F32)
        dthr = P.sb(s, 'g_dthr', [16, 16], F32)
        thrb = P.sb(s, 'g_thrb', [128, 16], F32)
        xt = P.sb(s, 'g_xt', [128, 1024], F32)
        junk = P.sb(s, 'g_junk', [128, 1024], BF16)
        ss = P.sb(s, 'g_ss', [128, 4], F32)
        h32 = P.sb(s, 'g_h32', [128, 1024], F32)
        hb16 = P.sb(s, 'g_hb16', [128, 1024], BF16)
        hT32 = P.sb(s, 'g_hT32', [128, 8, 128], F32)
        sm = P.sb(s, 'g_sm', [128, 4], F32)
        ex = P.sb(s, 'g_ex', [128, 16], F32)
        wg = [P.sb(s, 'g_wg%d' % i, [128, 8, 1024], BF16) for i in range(2)]
        wu = [P.sb(s, 'g_wu%d' % i, [128, 8, 1024], BF16) for i in range(2)]
        wd = [P.sb(s, 'g_wd%d' % i, [128, 8, 1024], BF16) for i in range(2)]
        xe = P.sb(s, 'g_xe', [128, 4, 1024], BF16)
        xeT = P.sb(s, 'g_xeT', [128, 8, 512], BF16)
        hid = P.sb(s, 'g_hid', [128, 8, 512], BF16)
        sg = [P.sb(s, 'g_sg%d' % i, [128, 512], BF16) for i in range(2)]
        ye = [P.sb(s, 'g_ye%d' % i, [128, 1024], F32) for i in range(2)]
        pbig = P.ps(s, 'g_pbig', [128, 1024])
        pl = P.ps(s, 'g_pl', [128, 512])
        pT = P.ps(s, 'g_pT', [128, 1024], BF16)
        pg_ = P.ps(s, 'g_pg', [128, 512])
        pu_ = P.ps(s, 'g_pu', [128, 512])
        py = [P.ps(s, 'g_py%d' % i, [128, 512]) for i in range(2)]

        def load_w(ex_):
            eb = ex_ % 2
            for k in range(8):
                P.dma('pool', wg[eb][:, k, :], D['w_e_gate'][l, ex_, k * 128:(k + 1) * 128, :], writes=[('wg', eb, k)])
                P.dma('pool', wu[eb][:, k, :], D['w_e_up'][l, ex_, k * 128:(k + 1) * 128, :], writes=[('wu', eb, k)])
            for k in range(8):
                P.dma('pool', wd[eb][:, k, :], D['w_e_down'][l, ex_, k * 128:(k + 1) * 128, :], writes=[('wd', eb, k)])

        P.dma('sp', gff[:], D['g_ffn'][l].partition_broadcast(128), writes=['gff'])
        P.dma('sp', idf[:], D['ident'], writes=['idf'])
        P.dma('pool', idb[:], D['ident'], writes=['idb'])
        P.dma('pool', triS[:], D['triS'], writes=['triS'])
        P.dma('pool', onesb[:], D['ones128'], writes=['onesb'])
        P.dma('sp', wr[:], D['w_router'][l].rearrange('(k p) e -> p k e', p=128), writes=['wr'])
        P.dma('sp', ones16[:], D['ones128'][0:16, :], writes=['ones16'])
        P.dma('sp', tp[:], D['tp'], writes=['tp'])
        P.dma('sp', iota[:], D['iota512'], writes=['iota'])
        load_w(0)
        for t in range(NT):
            tk = slice(t * 128, (t + 1) * 128)
            P.dma('sp', xt[:], D['xw'][tk, :], reads=['d_xw'], writes=['xt'])
            P.op('dve', lambda e: e.memset(ss[:, 0:1], 0.0), writes=['ss'])
            P.op('act', lambda e: e.activation(junk[:], xt[:], AF.Square, accum_out=ss[:, 0:1]), reads=['xt', 'ss'], writes=['junk', 'ss'])
            P.op('act', lambda e: e.activation(ss[:, 0:1], ss[:, 0:1], AF.Sqrt, bias=EPS, scale=1.0 / 1024), reads=['ss'], writes=['ss'])
            P.op('dve', lambda e: e.reciprocal(ss[:, 0:1], ss[:, 0:1]), reads=['ss'], writes=['ss'])
            P.op('dve', lambda e: e.scalar_tensor_tensor(h32[:], xt[:], ss[:, 0:1], gff[:], ALU.mult, ALU.mult), reads=['xt', 'ss', 'gff'], writes=['h32'])
            P.op('act', lambda e: e.copy(hb16[:], h32[:]), reads=['h32'], writes=['hb16'])
            P.dma('sp', D['hb'][tk, :], hb16[:], reads=['hb16'], writes=['d_hb'])
            for k in range(8):
                P.op('pe', lambda e, k=k: e.transpose(pbig[:, k * 128:(k + 1) * 128], h32[:, k * 128:(k + 1) * 128], idf[:]), reads=['h32', 'idf'], writes=[('pbig', k // 4)])
            P.op('act', lambda e: e.copy(hT32[:, 0:4, :], pbig[:, 0:512].rearrange('p (k t) -> p k t', t=128)), reads=[('pbig', 0)], writes=['hT32a'])
            P.op('dve', lambda e: e.tensor_copy(hT32[:, 4:8, :], pbig[:, 512:1024].rearrange('p (k t) -> p k t', t=128)), reads=[('pbig', 1)], writes=['hT32b'])
            for k in range(8):
                P.op('pe', lambda e, k=k: e.matmul(pl[:, 0:16], hT32[:, k, :], wr[:, k, :], start=(k == 0), stop=(k == 7)), reads=['hT32a', 'hT32b', 'wr'], writes=['pl'])
            P.op('dve', lambda e: e.tensor_reduce(sm[:, 0:1], pl[:, 0:16], AX.X, ALU.max), reads=['pl'], writes=['sm'])
            P.op('dve', lambda e: e.tensor_scalar(sm[:, 1:2], sm[:, 0:1], -1.0, None, ALU.mult), reads=['sm'], writes=['sm'])
            P.op('dve', lambda e: e.memset(sm[:, 2:3], 0.0), reads=['sm'], writes=['sm'])
            P.op('act', lambda e: e.activation(ex[:], pl[:, 0:16], AF.Exp, bias=sm[:, 1:2], accum_out=sm[:, 2:3]), reads=['pl', 'sm'], writes=['ex', 'sm'])
            P.op('dve', lambda e: e.reciprocal(sm[:, 3:4], sm[:, 2:3]), reads=['sm'], writes=['sm'])
            P.op('dve', lambda e, t=t: e.tensor_scalar(aff[:, t, :], ex[:], sm[:, 3:4], None, ALU.mult), reads=['ex', 'sm'], writes=[('aff', t)])
            P.op('pe', lambda e, t=t: e.transpose(pl[0:16, 128:256], aff[:, t, :], idf[:]), reads=[('aff', t), 'idf'], writes=['pl'])
            P.op('act', lambda e, t=t: e.mul(affT2[:, t * 128:(t + 1) * 128], pl[0:16, 128:256], 2.0), reads=['pl'], writes=['affT2'])
        lo, hi, half, mid2, cnt, gef, tt = (bs[:, i:i + 1] for i in range(7))
        P.op('dve', lambda e: e.memset(bs[:], 0.0), writes=['bs'])
        P.op('dve', lambda e: e.memset(hi, 1.0), reads=['bs'], writes=['bs'])
        for itn in range(30):
            P.op('dve', lambda e: e.tensor_tensor(mid2, lo, hi, ALU.add), reads=['bs'], writes=['bs'])
            P.op('dve', lambda e: e.tensor_scalar(half, mid2, 0.5, None, ALU.mult), reads=['bs'], writes=['bs'])
            P.op('dve', lambda e: e.memset(cnt, 0.0), reads=['bs'], writes=['bs'])
            P.op('dve', lambda e: e.tensor_scalar(bj[:], affT2[:], mid2, 0.0, ALU.is_ge, ALU.add, accum_out=cnt), reads=['affT2', 'bs'], writes=['bj', 'bs'])
            P.op('dve', lambda e: e.tensor_scalar(gef, cnt, 511.5, None, ALU.is_ge), reads=['bs'], writes=['bs'])
            P.op('dve', lambda e: e.tensor_tensor(tt, half, lo, ALU.subtract), reads=['bs'], writes=['bs'])
            P.op('dve', lambda e: e.tensor_tensor(tt, tt, gef, ALU.mult), reads=['bs'], writes=['bs'])
            P.op('dve', lambda e: e.tensor_tensor(lo, lo, tt, ALU.add), reads=['bs'], writes=['bs'])
            P.op('dve', lambda e: e.tensor_tensor(tt, hi, half, ALU.subtract), reads=['bs'], writes=['bs'])
            P.op('dve', lambda e: e.tensor_tensor(tt, tt, gef, ALU.mult), reads=['bs'], writes=['bs'])
            P.op('dve', lambda e: e.tensor_tensor(hi, half, tt, ALU.add), reads=['bs'], writes=['bs'])
        P.op('dve', lambda e: e.tensor_scalar(dthr[:], idf[0:16, 0:16], lo, None, ALU.mult), reads=['idf', 'bs'], writes=['dthr'])
        P.op('pe', lambda e: e.matmul(pl[:, 256:272], ones16[:], dthr[:], start=True, stop=True), reads=['ones16', 'dthr'], writes=['pl'])
        P.op('dve', lambda e: e.tensor_copy(thrb[:], pl[:, 256:272]), reads=['pl'], writes=['thrb'])
        AFF = [('aff', t) for t in range(NT)]
        P.op('dve', lambda e: e.tensor_tensor(sel[:], aff[:], bc(thrb[:].unsqueeze(1), [128, NT, 16]), ALU.is_ge), reads=AFF + ['thrb'], writes=['sel'])
        P.op('dve', lambda e: e.tensor_copy(selb[:], sel[:].rearrange('p t e -> p (t e)')), reads=['sel'], writes=['selb'])
        P.op('pe', lambda e: e.matmul(pg_[:], triS[:], selb[:], start=True, stop=True), reads=['triS', 'selb'], writes=['pg'])
        P.op('pe', lambda e: e.matmul(pu_[:], onesb[:], selb[:], start=True, stop=True), reads=['onesb', 'selb'], writes=['pu'])
        P.op('dve', lambda e: e.tensor_copy(cA[:].rearrange('p t e -> p (t e)'), pu_[:]), reads=['pu'], writes=['cA'])
        src, dst, sn, dn = cA, cB, 'cA', 'cB'
        for sft in (1, 2, 4, 8, 16):
            P.op('pool', lambda e, src=src, dst=dst, sft=sft: e.tensor_copy(dst[:, 0:sft, :], src[:, 0:sft, :]), reads=[sn], writes=[dn])
            P.op('dve', lambda e, src=src, dst=dst, sft=sft: e.tensor_tensor(dst[:, sft:NT, :], src[:, sft:NT, :], src[:, 0:NT - sft, :], ALU.add), reads=[sn], writes=[dn])
            src, dst, sn, dn = dst, src, dn, sn
        P.op('dve', lambda e, src=src: e.tensor_tensor(rank[:].rearrange('p t e -> p (t e)'), src[:].rearrange('p t e -> p (t e)'), pu_[:], ALU.subtract), reads=[sn, 'pu'], writes=['rank'])
        P.op('dve', lambda e: e.tensor_tensor(rank[:].rearrange('p t e -> p (t e)'), rank[:].rearrange('p t e -> p (t e)'), pg_[:], ALU.add), reads=['rank', 'pg'], writes=['rank'])
        P.op('dve', lambda e: e.scalar_tensor_tensor(rank[:], rank[:], 1.0, sel[:], ALU.add, ALU.mult), reads=['rank', 'sel'], writes=['rank'])
        P.op('dve', lambda e: e.tensor_scalar(rank[:], rank[:], -1.0, None, ALU.add), reads=['rank'], writes=['rank'])
        P.op('dve', lambda e: e.tensor_copy(tg[:, :, :, 0:2], bc(tp[:].unsqueeze(2), [128, NT, 16, 2])), reads=['tp'], writes=['tg0'])
        P.op('dve', lambda e: e.tensor_copy(tg[:, :, :, 2], aff[:]), reads=AFF, writes=['tg1'])
        P.op('dve', lambda e: e.tensor_tensor(cA[:], aff[:], tg[:, :, :, 2], ALU.subtract), reads=AFF + ['tg1', 'cA', 'cB'], writes=['cA'])
        P.op('dve', lambda e: e.tensor_copy(tg[:, :, :, 3], cA[:]), reads=['cA'], writes=['tg2'])
        P.op('dve', lambda e: e.tensor_tensor(cB[:], cA[:], tg[:, :, :, 3], ALU.subtract), reads=['cA', 'tg2', 'cB'], writes=['cB'])
        P.op('dve', lambda e: e.tensor_copy(tg[:, :, :, 4], cB[:]), reads=['cB'], writes=['tg3'])
        TG = ['tg0', 'tg1', 'tg2', 'tg3']
        nsel = 0
        npy = 0
        nsg = 0
        for ex_ in range(NE):
            eb = ex_ % 2
            if ex_ + 1 < NE:
                load_w(ex_ + 1)
            for t in range(NT):
                sb_ = nsel % 2
                nsel += 1
                P.op('dve', lambda e, sb_=sb_, t=t, ex_=ex_: e.tensor_scalar(Selt[sb_][:], iota[:], rank[:, t, ex_:ex_ + 1], None, ALU.is_equal), reads=['iota', 'rank'], writes=[('Selt', sb_)])
                P.op('pe', lambda e, sb_=sb_, t=t, ex_=ex_: e.matmul(pl[0:5, 0:512], tg[:, t, ex_, :], Selt[sb_][:], start=(t == 0), stop=(t == NT - 1)),
                     reads=[('Selt', sb_)] + TG, writes=['pl'])
            P.op('act', lambda e: e.copy(row5[:], pl[0:5, 0:512]), reads=['pl'], writes=['row5'])
            for g in range(4):
                P.op('pe', lambda e, g=g: e.transpose(pl[:, g * 8:g * 8 + 5], row5[0:5, g * 128:(g + 1) * 128], idf[0:5, 0:5]), reads=['row5', 'idf'], writes=['pl'])
            P.op('dve', lambda e: e.tensor_copy(idxf[:, :, 0:5], pl[:, 0:32].rearrange('p (g c) -> p g c', c=8)[:, :, 0:5]), reads=['pl'], writes=['idxf'])
            P.op('dve', lambda e: e.scalar_tensor_tensor(idxv[:], idxf[:, :, 0], 128.0, idxf[:, :, 1], ALU.mult, ALU.add), reads=['idxf'], writes=['idxv'])
            P.op('dve', lambda e, eb=eb: e.tensor_copy(idxi[eb][:], idxv[:]), reads=['idxv'], writes=[('idxi', eb)])
            P.op('dve', lambda e, eb=eb: e.tensor_tensor(gate[eb][:], idxf[:, :, 2], idxf[:, :, 3], ALU.add), reads=['idxf'], writes=[('gate', eb)])
            P.op('dve', lambda e, eb=eb: e.tensor_tensor(gate[eb][:], gate[eb][:], idxf[:, :, 4], ALU.add), reads=['idxf', ('gate', eb)], writes=[('gate', eb)])
            for g in range(4):
                P.idma(lambda e, g=g, eb=eb: e.indirect_dma_start(out=xe[:, g, :], out_offset=None, in_=D['hb'][:, :],
                                                                   in_offset=bass.IndirectOffsetOnAxis(ap=idxi[eb][:, g:g + 1], axis=0), bounds_check=P.breg(e), oob_is_err=False),
                       reads=['d_hb', ('idxi', eb)], writes=[('xe', g)])
            for g in range(4):
                for k in range(8):
                    P.op('pe', lambda e, g=g, k=k: e.transpose(pT[:, k * 128:(k + 1) * 128], xe[:, g, k * 128:(k + 1) * 128], idb[:]), reads=[('xe', g), 'idb'], writes=['pT'])
                eng = 'act' if g % 2 == 0 else 'dve'
                if eng == 'act':
                    P.op('act', lambda e, g=g: e.copy(xeT[:, :, g * 128:(g + 1) * 128], pT[:].rearrange('p (k t) -> p k t', t=128)), reads=['pT'], writes=[('xeT', g)])
                else:
                    P.op('dve', lambda e, g=g: e.tensor_copy(xeT[:, :, g * 128:(g + 1) * 128], pT[:].rearrange('p (k t) -> p k t', t=128)), reads=['pT'], writes=[('xeT', g)])
            XET = [('xeT', g) for g in range(4)]
            for fc in range(8):
                for k in range(8):
                    P.op('pe', lambda e, fc=fc, k=k, eb=eb: e.matmul(pg_[:], wg[eb][:, k, fc * 128:(fc + 1) * 128], xeT[:, k, :], start=(k == 0), stop=(k == 7)),
                         reads=XET + [('wg', eb, k)], writes=['pg'])
                for k in range(8):
                    P.op('pe', lambda e, fc=fc, k=k, eb=eb: e.matmul(pu_[:], wu[eb][:, k, fc * 128:(fc + 1) * 128], xeT[:, k, :], start=(k == 0), stop=(k == 7)),
                         reads=XET + [('wu', eb, k)], writes=['pu'])
                sb2 = nsg % 2
                nsg += 1
                P.op('act', lambda e, sb2=sb2: e.activation(sg[sb2][:], pg_[:], AF.Silu), reads=['pg'], writes=[('sg', sb2)])
                P.op('dve', lambda e, sb2=sb2, fc=fc: e.tensor_tensor(hid[:, fc, :], sg[sb2][:], pu_[:], ALU.mult), reads=[('sg', sb2), 'pu'], writes=[('hid', fc)])
            HID = [('hid', fc) for fc in range(8)]
            for g in range(4):
                yb = g % 2
                for hf in range(2):
                    pb = npy % 2
                    npy += 1
                    for fc in range(8):
                        P.op('pe', lambda e, pb=pb, fc=fc, g=g, hf=hf, eb=eb: e.matmul(py[pb][:], hid[:, fc, g * 128:(g + 1) * 128], wd[eb][:, fc, hf * 512:(hf + 1) * 512], start=(fc == 0), stop=(fc == 7)),
                             reads=HID + [('wd', eb, fc)], writes=[('py', pb)])
                    if hf == 0:
                        P.op('act', lambda e, pb=pb, yb=yb, g=g, eb=eb: e.activation(ye[yb][:, 0:512], py[pb][:], AF.Copy, scale=gate[eb][:, g:g + 1]), reads=[('py', pb), ('gate', eb)], writes=[('ye', yb, 0)])
                    else:
                        P.op('dve', lambda e, pb=pb, yb=yb, g=g, eb=eb: e.tensor_scalar(ye[yb][:, 512:1024], py[pb][:], gate[eb][:, g:g + 1], None, ALU.mult), reads=[('py', pb), ('gate', eb)], writes=[('ye', yb, 1)])
                P.idma(lambda e, g=g, eb=eb, yb=yb: e.indirect_dma_start(out=D['xw'][:, :], out_offset=bass.IndirectOffsetOnAxis(ap=idxi[eb][:, g:g + 1], axis=0), in_=ye[yb][:],
                                                                          in_offset=None, bounds_check=P.breg(e), oob_is_err=False, compute_op=ALU.add),
                       reads=[('ye', yb, 0), ('ye', yb, 1), ('idxi', eb), 'd_xw'], writes=['d_xw'])
        P.emit()


def phase_G(P, l, D, last):
    xdst = D['out'] if last else D['xw']
    with ExitStack() as s:
        wp = P.sb(s, 'h_wp', [128, 2, 1024], BF16)
        wgt = P.sb(s, 'h_wgt', [128, 8, 1024], BF16)
        gpl = P.sb(s, 'h_gpl', [128, 1024], F32)
        gpg = P.sb(s, 'h_gpg', [128, 1024], F32)
        idb = P.sb(s, 'h_idb', [128, 128], BF16)
        xt = [P.sb(s, 'h_xt%d' % i, [128, 1024], F32) for i in range(2)]
        pb_ = [P.sb(s, 'h_pb%d' % i, [128, 256], BF16) for i in range(2)]
        junk = P.sb(s, 'h_junk', [128, 1024], BF16)
        ss = P.sb(s, 'h_ss', [128, 4], F32)
        ssf = P.sb(s, 'h_ssf', [128, 2], F32)
        junkf = P.sb(s, 'h_junkf', [128, 1024], BF16)
        xn = P.sb(s, 'h_xn', [128, 1024], BF16)
        xTs = [P.sb(s, 'h_xT%d' % i, [128, 10, 128], BF16) for i in range(2)]
        er = P.sb(s, 'h_er', [128, 1024], F32)
        gt = P.sb(s, 'h_gt', [128, 1024], F32)
        pT = P.ps(s, 'h_pT', [128, 2048], BF16)
        pe_ = P.ps(s, 'h_pe', [128, 1024])
        pg_ = P.ps(s, 'h_pg', [128, 1024])
        for k in range(2):
            P.dma('pool', wp[:, k, :], D['w_ple'][l, k * 128:(k + 1) * 128, :], writes=[('wp', k)])
        for k in range(8):
            P.dma('pool', wgt[:, k, :], D['w_ple_gate'][l, k * 128:(k + 1) * 128, :], writes=[('wgt', k)])
        P.dma('sp', gpl[:], D['g_ple'][l].partition_broadcast(128), writes=['gpl'])
        P.dma('sp', gpg[:], D['g_ple_gate'][l].partition_broadcast(128), writes=['gpg'])
        P.dma('pool', idb[:], D['ident'], writes=['idb'])
        def front(t):
            b = t % 2
            xT = xTs[b]
            tk = slice(t * 128, (t + 1) * 128)
            P.dma('sp', xt[b][:], D['xw'][tk, :], reads=[('d_xw', t)], writes=[('xt', b)])
            P.dma('pool', pb_[b][:], D['p'][l, tk, :], writes=[('pb', b)])
            P.op('dve', lambda e: e.memset(ssf[:, 0:1], 0.0), writes=['ssf'])
            P.op('act', lambda e, b=b: e.activation(junkf[:], xt[b][:], AF.Square, accum_out=ssf[:, 0:1]), reads=[('xt', b), 'ssf'], writes=['junkf', 'ssf'])
            P.op('act', lambda e: e.activation(ssf[:, 0:1], ssf[:, 0:1], AF.Sqrt, bias=EPS, scale=1.0 / 1024), reads=['ssf'], writes=['ssf'])
            P.op('dve', lambda e: e.reciprocal(ssf[:, 0:1], ssf[:, 0:1]), reads=['ssf'], writes=['ssf'])
            P.op('dve', lambda e, b=b: e.scalar_tensor_tensor(xn[:], xt[b][:], ssf[:, 0:1], gpg[:], ALU.mult, ALU.mult), reads=[('xt', b), 'ssf', 'gpg'], writes=['xn'])
            for k in range(8):
                P.op('pe', lambda e, k=k: e.transpose(pT[:, k * 128:(k + 1) * 128], xn[:, k * 128:(k + 1) * 128], idb[:]), reads=['xn', 'idb'], writes=[('pT', 0)])
            for k in range(2):
                P.op('pe', lambda e, k=k, b=b: e.transpose(pT[:, (8 + k) * 128:(9 + k) * 128], pb_[b][:, k * 128:(k + 1) * 128], idb[:]), reads=[('pb', b), 'idb'], writes=[('pT', 1)])
            P.op('act', lambda e, xT=xT: e.copy(xT[:, 0:8, :].rearrange('p k t -> p (k t)'), pT[:, 0:1024]), reads=[('pT', 0)], writes=[('xTa', b)])
            P.op('dve', lambda e, xT=xT: e.tensor_copy(xT[:, 8:10, :].rearrange('p k t -> p (k t)'), pT[:, 1024:1280]), reads=[('pT', 1)], writes=[('xTb', b)])

        def back(t):
            b = t % 2
            xT = xTs[b]
            tk = slice(t * 128, (t + 1) * 128)
            for hf in range(2):
                for k in range(2):
                    P.op('pe', lambda e, hf=hf, k=k, xT=xT: e.matmul(pe_[:, hf * 512:(hf + 1) * 512], xT[:, 8 + k, :], wp[:, k, hf * 512:(hf + 1) * 512], start=(k == 0), stop=(k == 1)),
                         reads=[('xTb', b), ('wp', k)], writes=[('pe', hf)])
                for k in range(8):
                    P.op('pe', lambda e, hf=hf, k=k, xT=xT: e.matmul(pg_[:, hf * 512:(hf + 1) * 512], xT[:, k, :], wgt[:, k, hf * 512:(hf + 1) * 512], start=(k == 0), stop=(k == 7)),
                         reads=[('xTa', b), ('wgt', k)], writes=[('pg', hf)])
            P.op('dve', lambda e: e.memset(ss[:, 1:3], 0.0), reads=['ss'], writes=['ss'])
            for hf in range(2):
                P.op('act', lambda e, hf=hf: e.activation(junk[:, hf * 512:(hf + 1) * 512], pe_[:, hf * 512:(hf + 1) * 512], AF.Square, accum_out=ss[:, 1 + hf:2 + hf]), reads=[('pe', hf), 'ss'], writes=['junk', 'ss'])
            P.op('dve', lambda e: e.tensor_tensor(ss[:, 1:2], ss[:, 1:2], ss[:, 2:3], ALU.add), reads=['ss'], writes=['ss'])
            P.op('act', lambda e: e.activation(ss[:, 1:2], ss[:, 1:2], AF.Sqrt, bias=EPS, scale=1.0 / 1024), reads=['ss'], writes=['ss'])
            P.op('dve', lambda e: e.reciprocal(ss[:, 1:2], ss[:, 1:2]), reads=['ss'], writes=['ss'])
            for hf in range(2):
                hs = slice(hf * 512, (hf + 1) * 512)
                P.op('dve', lambda e, hs=hs: e.scalar_tensor_tensor(er[:, hs], pe_[:, hs], ss[:, 1:2], gpl[:, hs], ALU.mult, ALU.mult), reads=[('pe', hf), 'ss', 'gpl'], writes=['er'])
                P.op('act', lambda e, hs=hs: e.activation(gt[:, hs], pg_[:, hs], AF.Sigmoid), reads=[('pg', hf)], writes=['gt'])
            P.op('dve', lambda e: e.tensor_tensor(er[:], er[:], gt[:], ALU.mult), reads=['er', 'gt'], writes=['er'])
            P.op('dve', lambda e, b=b: e.tensor_tensor(xt[b][:], xt[b][:], er[:], ALU.add), reads=['er', ('xt', b)], writes=[('xt', b)])
            P.dma('sp', xdst[tk, :], xt[b][:], reads=[('xt', b)], writes=[('d_xw', t)])

        front(0)
        for t in range(NT):
            if t + 1 < NT:
                front(t + 1)
            back(t)
        P.emit()


WEIGHTS = [('g_mix', [4, 1024]), ('w_in', [4, 1024, 3224]), ('ln_v_g', [4, 4, 64]), ('ln_v_b', [4, 4, 64]), ('w_s', [4, 4, 128, 128]),
           ('b_s', [4, 4, 128]), ('q_norm_g', [4, 64]), ('k_norm_g', [4, 64]), ('conv_w', [4, 5, 1152]), ('a_log', [4, 2, 6]),
           ('dt_bias', [4, 2, 6]), ('o_norm_g', [4, 64]), ('w_out', [4, 1024, 1024]), ('g_ffn', [4, 1024]), ('w_router', [4, 1024, 16]),
           ('w_e_gate', [4, 16, 1024, 1024]), ('w_e_up', [4, 16, 1024, 1024]), ('w_e_down', [4, 16, 1024, 1024]), ('w_ple', [4, 256, 1024]),
           ('g_ple', [4, 1024]), ('g_ple_gate', [4, 1024]), ('w_ple_gate', [4, 1024, 1024])]


def make_consts():
    c = {}
    c['ident'] = np.eye(128, dtype=np.float32)
    c['ones64'] = np.ones((64, 64), np.float32)
    c['ones128'] = np.ones((128, 128), np.float32)
    half = 8
    c['invf'] = (np.float32(500000.0) ** (-np.arange(half, dtype=np.float32) * np.float32(2.0) / np.float32(16))).astype(np.float32)
    a = np.arange(128)[:, None]
    b = np.arange(128)[None, :]
    mA = (a >= b).astype(np.float32)
    mB = (a <= b).astype(np.float32)
    c['mab'] = np.concatenate([mA, mB, mA, mB], axis=1)
    sel = np.zeros((65, 64), np.float32)
    sel[64, :] = 1.0
    c['sel65'] = sel
    p = np.arange(64)[:, None]
    f = np.arange(64)[None, :]
    c['triF'] = (p <= f).astype(np.float32)
    c['triB'] = (p >= f).astype(np.float32)

    def m12(fw, bw):
        return np.ascontiguousarray(np.stack([fw] * 6 + [bw] * 6, axis=1).astype(np.float32))
    c['mW'] = m12(f > p, f < p)
    c['mWt'] = m12(p > f, p < f)
    c['mI'] = m12(f >= p, f <= p)
    c['triS'] = (a < b).astype(np.float32)
    c['iota512'] = np.ascontiguousarray(np.broadcast_to(np.arange(512, dtype=np.float32)[None, :], (128, 512)))
    tpv = np.zeros((128, NT, 2), np.float32)
    tpv[:, :, 0] = np.arange(NT)[None, :]
    tpv[:, :, 1] = np.arange(128)[:, None]
    c['tp'] = tpv
    return c


SCRATCH = [('cs', [S, 16], F32), ('vn', [S, 256], BF16), ('qkT', [6, 128, S], BF16), ('vaug', [S, 390], BF16), ('gate_s', [S, 384], F32),
           ('ab', [S, 24], F32), ('uT', [256, S], F32), ('cT', [1152, S], F32), ('yT', [1024, S], BF16), ('v_tm', [S, 384], F32),
           ('k_tm', [S, 384], F32), ('qT_g', [384, S], BF16), ('kT_g', [384, S], BF16), ('gb', [S, 24], F32), ('o_fb', [2, S, 384], F32),
           ('xw', [S, 1024], F32), ('hb', [S, 1024], BF16)]


def build(n_layers=4, phases=None, dbg=()):
    P = Prog()
    D = {}
    D['x'] = P.dram('x', [S, 1024], F32, 'ExternalInput')
    D['p'] = P.dram('p', [n_layers, S, 256], F32, 'ExternalInput')
    D['positions'] = P.dram('positions', [128, NT], I32, 'ExternalInput')
    for n, shp in WEIGHTS:
        D[n] = P.dram(n, [n_layers] + list(shp[1:]), F32, 'ExternalInput')
    for n, v in make_consts().items():
        D[n] = P.dram(n, list(v.shape), F32, 'ExternalInput')
    for n, shp, dt in SCRATCH:
        D[n] = P.dram(n, shp, dt, 'ExternalOutput' if n in dbg else 'Internal')
    D['out'] = P.dram('out', [S, 1024], F32, 'ExternalOutput')
    allp = phases is None
    if allp or 'R' in phases:
        phase_rope(P, D)
    for l in range(n_layers):
        first = (l == 0)
        last = (l == n_layers - 1)
        if allp or 'A' in phases:
            phase_A(P, l, D, first)
        if allp or 'B' in phases:
            phase_B(P, l, D)
        if allp or 'C' in phases:
            phase_C(P, l, D)
        if allp or 'D1' in phases:
            phase_D1(P, l, D)
        if allp or 'D2' in phases:
            phase_D2(P, l, D)
        if allp or 'E' in phases:
            phase_E(P, l, D, first)
        if allp or 'F' in phases:
            phase_F(P, l, D)
        if allp or 'G' in phases:
            phase_G(P, l, D, last and allp)
    return P


def kernel(**inputs):
    n = 8
    P = build(4)
    consts = make_consts()
    shared = {k: np.ascontiguousarray(np.asarray(inputs[k], dtype=np.float32)) for k, _ in WEIGHTS}
    shared.update(consts)
    x = np.asarray(inputs['x'], dtype=np.float32)
    p = np.asarray(inputs['p'], dtype=np.float32)
    pos = np.asarray(inputs['positions']).astype(np.int32)
    in_maps = []
    for c in range(n):
        m = dict(shared)
        m['x'] = np.ascontiguousarray(x[c])
        m['p'] = np.ascontiguousarray(p[:, c])
        m['positions'] = np.ascontiguousarray(pos[c].reshape(NT, 128).T)
        in_maps.append(m)
    res = run_bass_kernel_spmd(P.nc, in_maps, core_ids=list(range(n)))
    return np.stack([np.asarray(res.results[c]['out'], dtype=np.float32) for c in range(n)], axis=0)
```

```python
import numpy as np
from contextlib import ExitStack
import concourse.bass as bass
import concourse.mybir as mybir
from concourse.bass_utils import run_bass_kernel_spmd

F32 = mybir.dt.float32
BF16 = mybir.dt.bfloat16
I32 = mybir.dt.int32
ALU = mybir.AluOpType
AF = mybir.ActivationFunctionType
AX = mybir.AxisListType

ENGS = ['pe', 'dve', 'act', 'pool', 'sp']
DMA_ENGS = ['sp', 'pool', 'act']
NDS = 8
EPS = 1e-6
S = 4096
NT = 32


class Prog:
    def __init__(self):
        self.nc = bass.Bass("TRN2", target_bir_lowering=False)
        self.stack = ExitStack()
        self.sems = {}
        for e in ENGS:
            self.sems[e] = self.stack.enter_context(self.nc.semaphore('s_' + e))
        self.dcount = {}
        for e in DMA_ENGS:
            for i in range(NDS):
                k = 'd_%s_%d' % (e, i)
                self.sems[k] = self.stack.enter_context(self.nc.semaphore(k))
                self.dcount[k] = 0
        self.dnext = {e: 0 for e in DMA_ENGS}
        self.cnt = {e: 0 for e in ENGS}
        self.waited = {e: {} for e in ENGS}
        self.q = {e: [] for e in ENGS}
        self.lastw = {}
        self.readers = {}
        self.nops = 0
        self.xkeys = set(['pT', 'pv', 'pq', 'pk', 'pbv', 'pg', 'pf', 'ptr', 'pm', 'pss', 'ppv', 'pd', 'ptb', 'po', 'pbig', 'pl', 'pu', 'py', 'pe', 'pA', 'pB', 'pC', 'pD', 'pS'])

    def _deps(self, eng, reads, writes):
        deps = {}

        def add(m):
            if m is None:
                return
            k, v = m
            if eng == 'pe' and k == 'pe':
                return
            if deps.get(k, 0) < v:
                deps[k] = v
        for r in reads:
            add(self.lastw.get(r))
        for w in writes:
            add(self.lastw.get(w))
            for m in self.readers.get(w, ()):
                add(m)
        out = []
        wd = self.waited[eng]
        for k, v in deps.items():
            if wd.get(k, 0) < v:
                wd[k] = v
                out.append((k, v))
        return out

    def _mark(self, mark, reads, writes):
        for w in writes:
            self.lastw[w] = mark
            self.readers[w] = []
        for r in reads:
            if r in writes:
                continue
            self.readers.setdefault(r, []).append(mark)

    cut = None
    pc = 0

    def isx(self, k):
        n = k[0] if isinstance(k, tuple) else k
        return isinstance(n, str) and n in self.xkeys

    def op(self, eng, fn, reads=(), writes=()):
        self.pc += 1
        if self.cut is not None and self.pc > self.cut:
            return
        xr = [r for r in reads if self.isx(r) and r not in writes]
        if xr:
            writes = list(writes) + xr
        waits = self._deps(eng, reads, writes)
        self.cnt[eng] += 1
        mark = (eng, self.cnt[eng])
        self.q[eng].append((waits, fn, (eng, 1)))
        self._mark(mark, reads, writes)
        self.nops += 1

    def dma(self, eng, out, in_, reads=(), writes=(), **kw):
        self.pc += 1
        if self.cut is not None and self.pc > self.cut:
            return
        waits = self._deps(eng, reads, writes)
        i = self.dnext[eng]
        self.dnext[eng] = (i + 1) % NDS
        k = 'd_%s_%d' % (eng, i)
        c = self.dcount[k]
        wd = self.waited[eng]
        if c > 0 and wd.get(k, 0) < 16 * c:
            wd[k] = 16 * c
            waits.append((k, 16 * c))
        self.dcount[k] = c + 1
        mark = (k, 16 * (c + 1))
        self.q[eng].append((waits, (lambda e: e.dma_start(out=out, in_=in_, **kw)), (k, 16)))
        self._mark(mark, reads, writes)
        self.nops += 1

    _breg = None

    def breg(self, e):
        if self._breg is None:
            self._breg = e.to_reg(S - 1)
        return self._breg

    def idma(self, fn, reads=(), writes=()):
        eng = 'pool'
        self.pc += 1
        waits = self._deps(eng, reads, writes)
        i = self.dnext[eng]
        self.dnext[eng] = (i + 1) % NDS
        k = 'd_%s_%d' % (eng, i)
        c = self.dcount[k]
        wd = self.waited[eng]
        if c > 0 and wd.get(k, 0) < 16 * c:
            wd[k] = 16 * c
            waits.append((k, 16 * c))
        self.dcount[k] = c + 1
        mark = (k, 16 * (c + 1))
        self.q[eng].append((waits, fn, (k, 16)))
        self._mark(mark, reads, writes)
        self.nops += 1

    def barrier(self):
        for e in ENGS:
            waits = []
            wd = self.waited[e]
            for o in ENGS:
                if o != e and self.cnt[o] > wd.get(o, 0):
                    wd[o] = self.cnt[o]
                    waits.append((o, self.cnt[o]))
            for k, c in self.dcount.items():
                if 16 * c > wd.get(k, 0):
                    wd[k] = 16 * c
                    waits.append((k, 16 * c))
            if waits:
                self.q[e].append((waits, None, None))
        self.lastw = {}
        self.readers = {}

    def emit(self):
        self.barrier()
        nc = self.nc
        sems = self.sems
        q = self.q

        def replay(name, e):
            for waits, fn, inc in q[name]:
                for k, v in waits:
                    e.wait_ge(sems[k], v)
                if fn is not None:
                    ins = fn(e)
                    ins.then_inc(sems[inc[0]], inc[1])

        with nc.Block() as block:
            @block.tensor
            def _(e):
                replay('pe', e)

            @block.vector
            def _(e):
                replay('dve', e)

            @block.scalar
            def _(e):
                replay('act', e)

            @block.gpsimd
            def _(e):
                replay('pool', e)

            @block.sync
            def _(e):
                replay('sp', e)
        self.q = {e: [] for e in ENGS}

    uid = 0

    def sb(self, stack, name, shape, dt):
        self.uid += 1
        return stack.enter_context(self.nc.sbuf_tensor('%s_%d' % (name, self.uid), list(shape), dt))

    def ps(self, stack, name, shape, dt=F32, keys=()):
        for k in keys:
            self.xkeys.add(k)
        self.uid += 1
        return stack.enter_context(self.nc.psum_tensor('%s_%d' % (name, self.uid), list(shape), dt))

    def dram(self, name, shape, dt, kind="Internal"):
        return self.nc.dram_tensor(name, list(shape), dt, kind=kind).ap()


def ssl(a, n, d):
    return slice(a, a + (n - 1) * d + 1, d)


def bc(ap, shape):
    return ap.to_broadcast(list(shape))


def phase_rope(P, D):
    with ExitStack() as s:
        pi_ = P.sb(s, 'r_pi', [128, NT], I32)
        pf = P.sb(s, 'r_pf', [128, NT], F32)
        invf = P.sb(s, 'r_invf', [128, 8], F32)
        ang = P.sb(s, 'r_ang', [128, 2, NT, 8], F32)
        kk = P.sb(s, 'r_kk', [128, 2, NT, 8], F32)
        ki = P.sb(s, 'r_ki', [128, 2, NT, 8], I32)
        cs = P.sb(s, 'r_cs', [128, NT, 16], F32)
        P.dma('sp', pi_[:], D['positions'], writes=['pi'])
        P.dma('sp', invf[:], D['invf'].partition_broadcast(128), writes=['invf'])
        P.op('dve', lambda e: e.tensor_copy(pf[:], pi_[:]), reads=['pi'], writes=['pf'])
        P.op('dve', lambda e: e.tensor_tensor(ang[:, 1], bc(pf[:].unsqueeze(2), [128, NT, 8]), bc(invf[:].unsqueeze(1), [128, NT, 8]), ALU.mult),
             reads=['pf', 'invf'], writes=['ang1'])
        P.op('dve', lambda e: e.tensor_scalar(ang[:, 0], ang[:, 1], float(np.pi / 2), None, ALU.add), reads=['ang1'], writes=['ang0'])
        A = ang[:].rearrange('p a t c -> p (a t c)')
        K = kk[:].rearrange('p a t c -> p (a t c)')
        KI = ki[:].rearrange('p a t c -> p (a t c)')
        P.op('dve', lambda e: e.tensor_scalar(K, A, float(1.0 / (2 * np.pi)), None, ALU.mult), reads=['ang0', 'ang1'], writes=['kk'])
        P.op('dve', lambda e: e.tensor_copy(KI, K), reads=['kk'], writes=['ki'])
        P.op('dve', lambda e: e.tensor_copy(K, KI), reads=['ki'], writes=['kk'])
        C1 = 6.28125
        C2 = float(2 * np.pi - 6.28125)
        P.op('dve', lambda e: e.scalar_tensor_tensor(A, K, -C1, A, ALU.mult, ALU.add), reads=['kk', 'ang0', 'ang1'], writes=['ang'])
        P.op('dve', lambda e: e.scalar_tensor_tensor(A, K, -C2, A, ALU.mult, ALU.add), reads=['kk', 'ang'], writes=['ang'])
        P.op('dve', lambda e: e.tensor_scalar(A, A, 3.1415925, -3.1415925, ALU.min, ALU.max), reads=['ang'], writes=['ang'])
        P.op('act', lambda e: e.activation(cs[:, :, 0:8], ang[:, 0], AF.Sin), reads=['ang'], writes=['cs0'])
        P.op('act', lambda e: e.activation(cs[:, :, 8:16], ang[:, 1], AF.Sin), reads=['ang'], writes=['cs1'])
        P.dma('sp', D['cs'].rearrange('(t p) c -> p t c', p=128), cs[:], reads=['cs0', 'cs1'], writes=['d_cs'])
        P.emit()


def phase_A(P, l, D, first):
    xsrc = D['x'] if first else D['xw']
    with ExitStack() as s:
        wbf = P.sb(s, 'a_wbf', [128, 8, 3224], BF16)
        gmix = P.sb(s, 'a_gmix', [128, 1024], F32)
        lng = P.sb(s, 'a_lng', [128, 256], F32)
        lnb = P.sb(s, 'a_lnb', [128, 256], F32)
        qkg = P.sb(s, 'a_qkg', [128, 2, 64], F32)
        cs = P.sb(s, 'a_cs', [128, NT, 16], F32)
        idb = P.sb(s, 'a_idb', [128, 128], BF16)
        xts = [P.sb(s, 'a_xt%d' % i, [128, 1024], F32) for i in range(2)]
        junk = P.sb(s, 'a_junk', [128, 1024], BF16)
        ss = P.sb(s, 'a_ss', [128, 2], F32)
        xn = P.sb(s, 'a_xn', [128, 1024], BF16)
        xnT = [P.sb(s, 'a_xnT%d' % i, [128, 8, 512], BF16) for i in range(2)]
        ge = P.sb(s, 'a_ge', [128, 4, 64], F32)
        cen = P.sb(s, 'a_cen', [128, 4, 64], F32)
        sq = P.sb(s, 'a_sq', [128, 4, 64], F32)
        m4 = P.sb(s, 'a_m4', [128, 8], F32)
        vnb = [P.sb(s, 'a_vnb%d' % i, [128, 256], BF16) for i in range(2)]
        sqq = P.sb(s, 'a_sqq', [128, 12, 64], F32)
        ss12 = P.sb(s, 'a_ss12', [128, 12], F32)
        qk32 = P.sb(s, 'a_qk32', [128, 12, 64], F32)
        rt = P.sb(s, 'a_rt', [128, 4, 12, 8], F32)
        qkb = P.sb(s, 'a_qkb', [128, 12, 64], BF16)
        qkTs = [P.sb(s, 'a_qkTs%d' % i, [128, 6, 128], BF16) for i in range(2)]
        vaug = [P.sb(s, 'a_vaug%d' % i, [128, 6, 65], BF16) for i in range(2)]
        gs = [P.sb(s, 'a_gs%d' % i, [128, 408], F32) for i in range(2)]
        fo = [P.sb(s, 'a_fo%d' % i, [128, 512], F32) for i in range(2)]
        pT = P.ps(s, 'a_pT', [128, 1024], BF16)
        pv = P.ps(s, 'a_pv', [128, 512])
        pq = P.ps(s, 'a_pq', [128, 512])
        pk = P.ps(s, 'a_pk', [128, 512])
        pbv = P.ps(s, 'a_pbv', [128, 512])
        pg = P.ps(s, 'a_pg', [128, 512])
        pf = [P.ps(s, 'a_pf%d' % i, [128, 512]) for i in range(2)]

        for k in range(8):
            P.dma('pool', wbf[:, k, :], D['w_in'][l, k * 128:(k + 1) * 128, :], writes=[('wbf', k)])
        P.dma('sp', gmix[:], D['g_mix'][l].partition_broadcast(128), writes=['gmix'])
        P.dma('sp', lng[:], D['ln_v_g'][l].rearrange('g d -> (g d)').partition_broadcast(128), writes=['lng'])
        P.dma('sp', lnb[:], D['ln_v_b'][l].rearrange('g d -> (g d)').partition_broadcast(128), writes=['lnb'])
        P.dma('sp', qkg[:, 0, :], D['q_norm_g'][l].partition_broadcast(128), writes=['qkg0'])
        P.dma('sp', qkg[:, 1, :], D['k_norm_g'][l].partition_broadcast(128), writes=['qkg1'])
        P.dma('sp', cs[:], D['cs'].rearrange('(t p) c -> p t c', p=128), reads=['d_cs'], writes=['cs'])
        P.dma('pool', idb[:], D['ident'], writes=['idb'])
        for i in range(2):
            P.op('pool', lambda e, i=i: e.memset(vaug[i][:, :, 64:65], 1.0), writes=[('vaug', i)])
        WB = [('wbf', k) for k in range(8)]
        fcount = [0]

        def front(t):
            if True:
                g, j = t // 4, t % 4
                XT = xnT[g % 2]
                kxt = ('xnT', g % 2)
                b = t % 2
                xt = xts[b]
                P.dma('sp', xt[:], xsrc[t * 128:(t + 1) * 128, :], writes=[('xt', b)])
                P.op('dve', lambda e, b=b: e.memset(ss[:, b:b + 1], 0.0), writes=[('ss', b)])
                P.op('act', lambda e, xt=xt, b=b: e.activation(junk[:], xt[:], AF.Square, accum_out=ss[:, b:b + 1]),
                     reads=[('xt', b), ('ss', b)], writes=['junk', ('ss', b)])
                P.op('act', lambda e, b=b: e.activation(ss[:, b:b + 1], ss[:, b:b + 1], AF.Sqrt, bias=EPS, scale=1.0 / 1024),
                     reads=[('ss', b)], writes=[('ss', b)])
                P.op('dve', lambda e, b=b: e.reciprocal(ss[:, b:b + 1], ss[:, b:b + 1]), reads=[('ss', b)], writes=[('ss', b)])
                P.op('dve', lambda e, xt=xt, b=b: e.scalar_tensor_tensor(xn[:], xt[:], ss[:, b:b + 1], gmix[:], ALU.mult, ALU.mult),
                     reads=[('xt', b), ('ss', b), 'gmix'], writes=['xn'])
                for k in range(8):
                    P.op('pe', lambda e, k=k: e.transpose(pT[:, k * 128:(k + 1) * 128], xn[:, k * 128:(k + 1) * 128], idb[:]),
                         reads=['xn', 'idb'], writes=['pT'])
                P.op('act', lambda e, XT=XT, j=j: e.copy(XT[:, :, j * 128:(j + 1) * 128], pT[:].rearrange('p (k t) -> p k t', t=128)),
                     reads=['pT'], writes=[kxt + (j,)])

        def back(t):
            if True:
                g, j = t // 4, t % 4
                XT = xnT[g % 2]
                kxt = ('xnT', g % 2)
                b = t % 2
                for (pp, nm, c0, c1) in ((pv, 'pv', 256, 512), (pq, 'pq', 512, 896), (pk, 'pk', 896, 1280), (pbv, 'pbv', 1280, 1664), (pg, 'pg', 2816, 3224)):
                    for k in range(8):
                        P.op('pe', lambda e, pp=pp, k=k, c0=c0, c1=c1, XT=XT, j=j: e.matmul(pp[:, 0:c1 - c0], XT[:, k, j * 128:(j + 1) * 128], wbf[:, k, c0:c1], start=(k == 0), stop=(k == 7)),
                             reads=[kxt + (j,), ('wbf', k)], writes=[nm])
                GE = ge[:].rearrange('p a b -> p (a b)')
                P.op('act', lambda e: e.activation(GE, pv[:, 0:256], AF.Gelu_apprx_tanh), reads=['pv'], writes=['ge'])
                P.op('dve', lambda e: e.tensor_reduce(m4[:, 0:4], ge[:], AX.X, ALU.add), reads=['ge'], writes=['m4a'])
                P.op('dve', lambda e: e.tensor_scalar(m4[:, 0:4], m4[:, 0:4], 1.0 / 64, None, ALU.mult), reads=['m4a'], writes=['m4a'])
                P.op('dve', lambda e: e.tensor_tensor(cen[:], ge[:], bc(m4[:, 0:4].unsqueeze(2), [128, 4, 64]), ALU.subtract), reads=['ge', 'm4a'], writes=['cen'])
                P.op('act', lambda e: e.activation(sq[:], cen[:], AF.Square), reads=['cen'], writes=['sq'])
                P.op('dve', lambda e: e.tensor_reduce(m4[:, 4:8], sq[:], AX.X, ALU.add), reads=['sq'], writes=['m4b'])
                P.op('act', lambda e: e.activation(m4[:, 4:8], m4[:, 4:8], AF.Sqrt, bias=EPS, scale=1.0 / 64), reads=['m4b'], writes=['m4b'])
                P.op('dve', lambda e: e.reciprocal(m4[:, 4:8], m4[:, 4:8]), reads=['m4b'], writes=['m4b'])
                P.op('dve', lambda e: e.tensor_tensor(cen[:], cen[:], bc(m4[:, 4:8].unsqueeze(2), [128, 4, 64]), ALU.mult), reads=['cen', 'm4b'], writes=['cen'])
                CEN = cen[:].rearrange('p a b -> p (a b)')
                P.op('pool', lambda e: e.tensor_tensor(CEN, CEN, lng[:], ALU.mult), reads=['cen', 'lng'], writes=['cen'])
                P.op('pool', lambda e, b=b: e.tensor_tensor(vnb[b][:], CEN, lnb[:], ALU.add), reads=['cen', 'lnb'], writes=[('vnb', b)])
                P.dma('sp', D['vn'][t * 128:(t + 1) * 128, :], vnb[b][:], reads=[('vnb', b)], writes=['d_vn'])
                P.op('act', lambda e: e.activation(sqq[:, 0:6, :].rearrange('p a b -> p (a b)'), pq[:, 0:384], AF.Square), reads=['pq'], writes=['sqq0'])
                P.op('act', lambda e: e.activation(sqq[:, 6:12, :].rearrange('p a b -> p (a b)'), pk[:, 0:384], AF.Square), reads=['pk'], writes=['sqq1'])
                P.op('dve', lambda e: e.tensor_reduce(ss12[:], sqq[:], AX.X, ALU.add), reads=['sqq0', 'sqq1'], writes=['ss12'])
                P.op('act', lambda e: e.activation(ss12[:], ss12[:], AF.Sqrt, bias=EPS, scale=1.0 / 64), reads=['ss12'], writes=['ss12'])
                P.op('dve', lambda e: e.reciprocal(ss12[:], ss12[:]), reads=['ss12'], writes=['ss12'])
                P.op('dve', lambda e: e.tensor_tensor(qk32[:, 0:6, :], pq[:, 0:384].rearrange('p (a b) -> p a b', b=64), bc(ss12[:, 0:6].unsqueeze(2), [128, 6, 64]), ALU.mult),
                     reads=['pq', 'ss12'], writes=['qk32a'])
                P.op('dve', lambda e: e.tensor_tensor(qk32[:, 6:12, :], pk[:, 0:384].rearrange('p (a b) -> p a b', b=64), bc(ss12[:, 6:12].unsqueeze(2), [128, 6, 64]), ALU.mult),
                     reads=['pk', 'ss12'], writes=['qk32b'])
                P.op('pool', lambda e: e.tensor_tensor(qk32[:, 0:6, :], qk32[:, 0:6, :], bc(qkg[:, 0:1, :], [128, 6, 64]), ALU.mult), reads=['qk32a', 'qkg0'], writes=['qk32a'])
                P.op('pool', lambda e: e.tensor_tensor(qk32[:, 6:12, :], qk32[:, 6:12, :], bc(qkg[:, 1:2, :], [128, 6, 64]), ALU.mult), reads=['qk32b', 'qkg1'], writes=['qk32b'])
                cosb = bc(cs[:, t:t + 1, 0:8], [128, 12, 8])
                sinb = bc(cs[:, t:t + 1, 8:16], [128, 12, 8])
                x1 = qk32[:, :, 0:8]
                x2 = qk32[:, :, 8:16]
                P.op('pool', lambda e, cosb=cosb: e.tensor_tensor(rt[:, 0], x1, cosb, ALU.mult), reads=['qk32a', 'qk32b', 'cs'], writes=['rt0'])
                P.op('pool', lambda e, sinb=sinb: e.tensor_tensor(rt[:, 1], x2, sinb, ALU.mult), reads=['qk32a', 'qk32b', 'cs'], writes=['rt1'])
                P.op('dve', lambda e, cosb=cosb: e.tensor_tensor(rt[:, 2], x2, cosb, ALU.mult), reads=['qk32a', 'qk32b', 'cs'], writes=['rt2'])
                P.op('dve', lambda e, sinb=sinb: e.tensor_tensor(rt[:, 3], x1, sinb, ALU.mult), reads=['qk32a', 'qk32b', 'cs'], writes=['rt3'])
                P.op('act', lambda e: e.copy(qkb[:], qk32[:]), reads=['qk32a', 'qk32b'], writes=['qkb'])
                P.op('dve', lambda e: e.tensor_tensor(qkb[:, :, 0:8], rt[:, 0], rt[:, 1], ALU.subtract), reads=['rt0', 'rt1', 'qkb'], writes=['qkb'])
                P.op('dve', lambda e: e.tensor_tensor(qkb[:, :, 8:16], rt[:, 2], rt[:, 3], ALU.add), reads=['rt2', 'rt3', 'qkb'], writes=['qkb'])
                for i in range(6):
                    P.op('pe', lambda e, i=i: e.transpose(pT[:, i * 128:(i + 1) * 128], qkb[:, 2 * i:2 * i + 2, :].rearrange('p a b -> p (a b)'), idb[:]),
                         reads=['qkb', 'idb'], writes=['pT'])
                P.op('act', lambda e, b=b: e.copy(qkTs[b][:], pT[:, 0:768].rearrange('p (k t) -> p k t', t=128)), reads=['pT'], writes=[('qkTs', b)])
                P.dma('sp', D['qkT'][:, :, t * 128:(t + 1) * 128].rearrange('i p t -> p i t'), qkTs[b][:], reads=[('qkTs', b)], writes=['d_qkT'])
                P.op('act', lambda e, b=b: e.copy(vaug[b][:, :, 0:64], pbv[:, 0:384].rearrange('p (a b) -> p a b', b=64)), reads=['pbv', ('vaug', b)], writes=[('vaug', b)])
                P.dma('sp', D['vaug'][t * 128:(t + 1) * 128, :], vaug[b][:].rearrange('p a b -> p (a b)'), reads=[('vaug', b)], writes=['d_vaug'])
                P.op('act', lambda e, b=b: e.activation(gs[b][:, 0:384], pg[:, 0:384], AF.Silu), reads=['pg'], writes=[('gs', b)])
                P.op('dve', lambda e, b=b: e.tensor_copy(gs[b][:, 384:408], pg[:, 384:408]), reads=['pg', ('gs', b)], writes=[('gs', b)])
                P.dma('sp', D['gate_s'][t * 128:(t + 1) * 128, :], gs[b][:, 0:384], reads=[('gs', b)], writes=['d_gate'])
                P.dma('sp', D['ab'][t * 128:(t + 1) * 128, :], gs[b][:, 384:408], reads=[('gs', b)], writes=['d_ab'])

        def fmaj(g):
            XT = xnT[g % 2]
            kxt = ('xnT', g % 2)
            allx = [kxt + (j,) for j in range(4)]
            for ci in range(11):
                c0 = ci * 128 if ci < 2 else 1664 + (ci - 2) * 128
                fb = fcount[0] % 2
                fcount[0] += 1
                for k in range(8):
                    P.op('pe', lambda e, fb=fb, k=k, c0=c0, XT=XT: e.matmul(pf[fb][:], wbf[:, k, c0:c0 + 128], XT[:, k, :], start=(k == 0), stop=(k == 7)),
                         reads=allx + [('wbf', k)], writes=[('pf', fb)])
                if ci < 2:
                    P.op('act', lambda e, fb=fb: e.activation(fo[fb][:], pf[fb][:], AF.Gelu_apprx_tanh), reads=[('pf', fb)], writes=[('fo', fb)])
                    P.dma('sp', D['uT'][ci * 128:(ci + 1) * 128, g * 512:(g + 1) * 512], fo[fb][:], reads=[('fo', fb)], writes=['d_uT'])
                else:
                    P.op('dve', lambda e, fb=fb: e.tensor_copy(fo[fb][:], pf[fb][:]), reads=[('pf', fb)], writes=[('fo', fb)])
                    P.dma('sp', D['cT'][(ci - 2) * 128:(ci - 1) * 128, g * 512:(g + 1) * 512], fo[fb][:], reads=[('fo', fb)], writes=['d_cT'])

        front(0)
        for t in range(NT):
            if t + 1 < NT:
                front(t + 1)
            back(t)
            if t % 4 == 3:
                fmaj(t // 4)
        P.emit()


def phase_B(P, l, D):
    with ExitStack() as s:
        ws32 = P.sb(s, 'b_ws32', [128, 4, 128], F32)
        idf = P.sb(s, 'b_idf', [128, 128], F32)
        wsT = P.sb(s, 'b_wsT', [128, 4, 128], BF16)
        bias = P.sb(s, 'b_bias', [64, 4, 128], F32)
        vn = [P.sb(s, 'b_vn%d' % i, [128, 4, 256], BF16) for i in range(2)]
        ut = [P.sb(s, 'b_ut%d' % i, [64, 4, 512], F32) for i in range(2)]
        mx = P.sb(s, 'b_mx', [64, 4, 128], F32)
        yb = [P.sb(s, 'b_yb%d' % i, [64, 4, 512], BF16) for i in range(2)]
        ptr = P.ps(s, 'b_ptr', [128, 512])
        pm = [P.ps(s, 'b_pm%d' % i, [64, 512]) for i in range(4)]
        P.dma('sp', ws32[:], D['w_s'][l].rearrange('g i j -> i g j'), writes=['ws32'])
        P.dma('sp', idf[:], D['ident'], writes=['idf'])
        P.dma('sp', bias[:].rearrange('p g i -> p (g i)'), D['b_s'][l].rearrange('g i -> (g i)').partition_broadcast(64), writes=['bias'])
        for g in range(4):
            P.op('pe', lambda e, g=g: e.transpose(ptr[:, g * 128:(g + 1) * 128], ws32[:, g, :], idf[:]), reads=['ws32', 'idf'], writes=['ptr'])
        P.op('dve', lambda e: e.tensor_copy(wsT[:].rearrange('p g i -> p (g i)'), ptr[:]), reads=['ptr'], writes=['wsT'])
        for it in range(8):
            b = it % 2
            P.dma('sp', vn[b][:], D['vn'][it * 512:(it + 1) * 512, :].rearrange('(c p) n -> p c n', p=128), reads=['d_vn'], writes=[('vn', b)])
            P.dma('sp', ut[b][:], D['uT'][:, it * 512:(it + 1) * 512].rearrange('(g d) t -> d g t', d=64), reads=['d_uT'], writes=[('ut', b)])
            for g in range(4):
                for c in range(4):
                    P.op('pe', lambda e, g=g, c=c, b=b: e.matmul(pm[g][:, c * 128:(c + 1) * 128], vn[b][:, c, g * 64:(g + 1) * 64], wsT[:, g, :], start=True, stop=True),
                         reads=[('vn', b), 'wsT'], writes=[('pm', g)])
                P.op('dve', lambda e, g=g: e.tensor_tensor(mx[:], pm[g][:].rearrange('p (c i) -> p c i', i=128), bc(bias[:, g:g + 1, :], [64, 4, 128]), ALU.add),
                     reads=[('pm', g), 'bias'], writes=['mx'])
                P.op('dve', lambda e, g=g, b=b: e.tensor_tensor(yb[b][:, g, :], mx[:].rearrange('p c i -> p (c i)'), ut[b][:, g, :], ALU.mult),
                     reads=['mx', ('ut', b)], writes=[('yb', b)])
            P.dma('sp', D['yT'][0:256, it * 512:(it + 1) * 512].rearrange('(g d) t -> d g t', d=64), yb[b][:], reads=[('yb', b)], writes=['d_yT'])
        P.emit()


PATS = (1, 4, 16)
KPAD = 1024


def phase_C(P, l, D):
    with ExitStack() as s:
        vs = {}
        for d in PATS:
            nt = d * (S // d // 128 + 1)
            vs[d] = P.sb(s, 'c_vs%d' % d, [128, nt, 390], BF16)
        mab = P.sb(s, 'c_mab', [128, 512], BF16)
        sel = P.sb(s, 'c_sel', [65, 64], F32)
        qh = [P.sb(s, 'c_qh%d' % i, [64, S], BF16) for i in range(2)]
        kh = [P.sb(s, 'c_kh%d' % i, [64, S + 2 * KPAD], BF16) for i in range(2)]
        pex = [P.sb(s, 'c_pex%d' % i, [128, 512], BF16) for i in range(3)]
        acc = P.sb(s, 'c_acc', [65, S], F32)
        rd = P.sb(s, 'c_rd', [64, 512], F32)
        yb = [P.sb(s, 'c_yb%d' % i, [64, 512], BF16) for i in range(2)]
        pss = [P.ps(s, 'c_ps%d' % i, [128, 512]) for i in range(3)]
        ppv = [P.ps(s, 'c_pv%d' % i, [65, 512]) for i in range(3)]
        pd = P.ps(s, 'c_pd', [64, 512])
        P.dma('pool', mab[:], D['mab'], writes=['mab'])
        P.dma('sp', sel[:], D['sel65'], writes=['sel'])
        for i in range(2):
            P.op('pool', lambda e, i=i: e.memset(kh[i][:, 0:KPAD], 0.0), writes=[('kh', i)])
            P.op('pool', lambda e, i=i: e.memset(kh[i][:, KPAD + S:], 0.0), writes=[('kh', i)])
        for d in PATS:
            L = S // d
            nqb = L // 128
            P.op('pool', lambda e, d=d: e.memset(vs[d][:].rearrange('p a b -> p (a b)'), 0.0), writes=[('vs', d)])
            vsrc = D['vaug'].rearrange('(j r) c -> r j c', r=d)
            for r in range(d):
                tb = r * (nqb + 1)
                if nqb > 1:
                    P.dma('sp', vs[d][:, tb + 1:tb + nqb, :], vsrc[r, 64:64 + (nqb - 1) * 128, :].rearrange('(k p) c -> p k c', p=128),
                          reads=['d_vaug', ('vs', d)], writes=[('vs', d)])
                P.dma('sp', vs[d][64:128, tb, :], vsrc[r, 0:64, :], reads=['d_vaug', ('vs', d)], writes=[('vs', d)])
                P.dma('sp', vs[d][0:64, tb + nqb, :], vsrc[r, L - 64:L, :], reads=['d_vaug', ('vs', d)], writes=[('vs', d)])
        for h in range(6):
            hb = h % 2
            P.dma('sp', qh[hb][:], D['qkT'][h // 2, (h % 2) * 64:(h % 2) * 64 + 64, :], reads=['d_qkT'], writes=[('qh', hb)])
            P.dma('sp', kh[hb][:, KPAD:KPAD + S], D['qkT'][3 + h // 2, (h % 2) * 64:(h % 2) * 64 + 64, :], reads=['d_qkT'], writes=[('kh', hb)])
            its = []
            for pi, d in enumerate(PATS):
                L = S // d
                nqb = L // 128
                for r in range(d):
                    tb = r * (nqb + 1)
                    for qb0 in range(0, nqb, 2):
                        its.append((pi, d, r, tb, qb0))

            def stage1(n):
                pi, d, r, tb, qb0 = its[n]
                ib = n % 3
                combos = ((qb0, qb0), (qb0 + 1, qb0), (qb0 + 1, qb0 + 1), (qb0 + 2, qb0 + 1))
                for ci, (kt, qb) in enumerate(combos):
                    k0 = KPAD + r + d * (kt * 128 - 64)
                    q0 = r + d * (qb * 128)
                    P.op('pe', lambda e, ib=ib, ci=ci, k0=k0, q0=q0, d=d, hb=hb: e.matmul(
                        pss[ib][:, ci * 128:(ci + 1) * 128], kh[hb][:, ssl(k0, 128, d)], qh[hb][:, ssl(q0, 128, d)], start=True, stop=True),
                        reads=[('kh', hb), ('qh', hb)], writes=[('pss', ib)])
                P.op('act', lambda e, ib=ib: e.activation(pex[ib][:], pss[ib][:], AF.Exp, scale=0.125), reads=[('pss', ib)], writes=[('pex', ib)])
                P.op('dve', lambda e, ib=ib: e.tensor_tensor(pex[ib][:], pex[ib][:], mab[:], ALU.mult), reads=[('pex', ib), 'mab'], writes=[('pex', ib)])

            def stage2(n):
                pi, d, r, tb, qb0 = its[n]
                ib = n % 3
                combos = ((qb0, qb0), (qb0 + 1, qb0), (qb0 + 1, qb0 + 1), (qb0 + 2, qb0 + 1))
                for ci, (kt, qb) in enumerate(combos):
                    qi = qb - qb0
                    P.op('pe', lambda e, ib=ib, ci=ci, kt=kt, qi=qi, d=d, tb=tb, h=h: e.matmul(
                        ppv[ib][:, qi * 128:(qi + 1) * 128], vs[d][:, tb + kt, h * 65:(h + 1) * 65], pex[ib][:, ci * 128:(ci + 1) * 128],
                        start=(ci % 2 == 0), stop=(ci % 2 == 1)), reads=[('vs', d), ('pex', ib)], writes=[('ppv', ib)])
                a0 = r + d * (qb0 * 128)
                av = acc[:, ssl(a0, 256, d)]
                if pi == 0:
                    P.op('dve', lambda e, av=av, ib=ib: e.tensor_copy(av, ppv[ib][:, 0:256]), reads=[('ppv', ib)], writes=['acc'])
                else:
                    P.op('dve', lambda e, av=av, ib=ib: e.tensor_tensor(av, av, ppv[ib][:, 0:256], ALU.add), reads=[('ppv', ib), 'acc'], writes=['acc'])

            stage1(0)
            for n in range(len(its)):
                if n + 1 < len(its):
                    stage1(n + 1)
                stage2(n)
            for c4 in range(8):
                yb_ = yb[c4 % 2]
                P.op('pe', lambda e, c4=c4: e.matmul(pd[:], sel[:], acc[:, c4 * 512:(c4 + 1) * 512], start=True, stop=True), reads=['sel', 'acc'], writes=['pd'])
                P.op('dve', lambda e: e.reciprocal(rd[:], pd[:]), reads=['pd'], writes=['rd'])
                P.op('dve', lambda e, c4=c4, yb_=yb_: e.tensor_tensor(yb_[:], acc[0:64, c4 * 512:(c4 + 1) * 512], rd[:], ALU.mult), reads=['acc', 'rd'], writes=[('yb', c4 % 2)])
                P.dma('sp', D['yT'][256 + h * 64:256 + (h + 1) * 64, c4 * 512:(c4 + 1) * 512], yb_[:], reads=[('yb', c4 % 2)], writes=['d_yT'])
        P.emit()


def phase_D1(P, l, D):
    with ExitStack() as s:
        cw = P.sb(s, 'd_cw', [128, 5, 9], F32)
        idf = P.sb(s, 'd_idf', [128, 128], F32)
        raw = [P.sb(s, 'd_raw%d' % i, [128, S + 4], F32) for i in range(2)]
        cv = P.sb(s, 'd_cv', [128, S], F32)
        tm = [P.sb(s, 'd_tm%d' % i, [128, 4, 128], F32) for i in range(2)]
        sq = P.sb(s, 'd_sq', [128, 8, 64], F32)
        r8 = P.sb(s, 'd_r8', [128, 8], F32)
        fT = [P.sb(s, 'd_fT%d' % i, [128, 512], BF16) for i in range(2)]
        abt = P.sb(s, 'd_abt', [128, NT, 24], F32)
        gbt = P.sb(s, 'd_gbt', [128, NT, 24], F32)
        dtb = P.sb(s, 'd_dtb', [128, 12], F32)
        nA = P.sb(s, 'd_nA', [128, 12], F32)
        ptr = [P.ps(s, 'd_ptr%d' % i, [128, 512]) for i in range(2)]
        ptb = [P.ps(s, 'd_ptb%d' % i, [128, 512]) for i in range(2)]
        for k in range(5):
            P.dma('sp', cw[:, k, :], D['conv_w'][l, k].rearrange('(c p) -> p c', p=128), writes=['cw'], allow_slow_non_contiguous=True)
        P.dma('sp', idf[:], D['ident'], writes=['idf'])
        for i in range(2):
            P.op('pool', lambda e, i=i: e.memset(raw[i][:, 0:2], 0.0), writes=[('raw', i)])
            P.op('pool', lambda e, i=i: e.memset(raw[i][:, S + 2:S + 4], 0.0), writes=[('raw', i)])
        P.dma('sp', abt[:], D['ab'].rearrange('(t p) c -> p t c', p=128), reads=['d_ab'], writes=['abt'])
        P.dma('sp', dtb[:], D['dt_bias'][l].rearrange('a h -> (a h)').partition_broadcast(128), writes=['dtb'])
        P.dma('sp', nA[:], D['a_log'][l].rearrange('a h -> (a h)').partition_broadcast(128), writes=['nA'])
        P.op('act', lambda e: e.activation(nA[:], nA[:], AF.Exp), reads=['nA'], writes=['nA'])
        P.op('dve', lambda e: e.tensor_scalar(nA[:], nA[:], -1.0, None, ALU.mult), reads=['nA'], writes=['nA'])
        P.op('dve', lambda e: e.tensor_tensor(gbt[:, :, 0:12], abt[:, :, 0:12], bc(dtb[:].unsqueeze(1), [128, NT, 12]), ALU.add), reads=['abt', 'dtb'], writes=['gbt0'])
        P.op('act', lambda e: e.activation(gbt[:, :, 0:12], gbt[:, :, 0:12], AF.Exp), reads=['gbt0'], writes=['gbt0'])
        P.op('act', lambda e: e.activation(gbt[:, :, 0:12], gbt[:, :, 0:12], AF.Ln, bias=1.0), reads=['gbt0'], writes=['gbt0'])
        P.op('dve', lambda e: e.tensor_tensor(gbt[:, :, 0:12], gbt[:, :, 0:12], bc(nA[:].unsqueeze(1), [128, NT, 12]), ALU.mult), reads=['gbt0', 'nA'], writes=['gbt0'])
        P.op('act', lambda e: e.activation(gbt[:, :, 12:24], abt[:, :, 12:24], AF.Sigmoid), reads=['abt'], writes=['gbt1'])
        P.dma('sp', D['gb'].rearrange('(t p) c -> p t c', p=128), gbt[:], reads=['gbt0', 'gbt1'], writes=['d_gb'])
        n4 = 0
        import os
        CUT = int(os.environ.get('D1CUT', '99'))
        for c in range(int(os.environ.get('D1C0', '0')), int(os.environ.get('D1C1', '9'))):
            rb = c % 2
            R = raw[rb]
            P.dma('sp', R[:, 2:S + 2], D['cT'][c * 128:(c + 1) * 128, :], reads=['d_cT'], writes=[('raw', rb)])
            P.op('dve', lambda e, R=R, c=c: e.tensor_scalar(cv[:], R[:, 0:S], cw[:, 0, c:c + 1], None, ALU.mult), reads=[('raw', rb), 'cw'], writes=['cv'])
            for k in range(1, 5):
                eng = 'dve'
                P.op(eng, lambda e, R=R, c=c, k=k: e.scalar_tensor_tensor(cv[:], R[:, k:k + S], cw[:, k, c:c + 1], cv[:], ALU.mult, ALU.add),
                     reads=[('raw', rb), 'cw', 'cv'], writes=['cv'])
            P.op('act', lambda e: e.activation(cv[:], cv[:], AF.Silu), reads=['cv'], writes=['cv'])
            for t4 in range(8 if CUT >= 2 else 0):
                pb = n4 % 2
                n4 += 1
                for j in range(4):
                    t = t4 * 4 + j
                    P.op('pe', lambda e, pb=pb, j=j, t=t: e.transpose(ptr[pb][:, j * 128:(j + 1) * 128], cv[:, t * 128:(t + 1) * 128], idf[:]),
                         reads=['cv', 'idf'], writes=[('ptr', pb)])
                TM = tm[pb]
                PV = ptr[pb][:].rearrange('p (j h d) -> p (j h) d', h=2, d=64)
                if c >= 6:
                    P.op('act', lambda e, pb=pb, TM=TM: e.copy(TM[:].rearrange('p j c -> p (j c)'), ptr[pb][:]), reads=[('ptr', pb)], writes=[('tm', pb)])
                    P.dma('sp', D['v_tm'][t4 * 512:(t4 + 1) * 512, (c - 6) * 128:(c - 5) * 128].rearrange('(j p) c -> p j c', p=128), TM[:], reads=[('tm', pb)], writes=['d_vtm'])
                else:
                    P.op('act', lambda e, pb=pb: e.activation(sq[:].rearrange('p a b -> p (a b)'), ptr[pb][:], AF.Square), reads=[('ptr', pb)], writes=['sq'])
                    P.op('dve', lambda e: e.tensor_reduce(r8[:], sq[:], AX.X, ALU.add), reads=['sq'], writes=['r8'])
                    P.op('act', lambda e: e.activation(r8[:], r8[:], AF.Sqrt, bias=EPS, scale=1.0), reads=['r8'], writes=['r8'])
                    P.op('dve', lambda e: e.reciprocal(r8[:], r8[:]), reads=['r8'], writes=['r8'])
                    if c < 3:
                        P.op('dve', lambda e: e.tensor_scalar(r8[:], r8[:], 0.125, None, ALU.mult), reads=['r8'], writes=['r8'])
                    P.op('dve', lambda e, TM=TM, PV=PV: e.tensor_tensor(TM[:].rearrange('p j (h d) -> p (j h) d', d=64), PV, bc(r8[:].unsqueeze(2), [128, 8, 64]), ALU.mult),
                         reads=[('ptr', pb), 'r8'], writes=[('tm', pb)])
                    if c >= 3:
                        P.dma('sp', D['k_tm'][t4 * 512:(t4 + 1) * 512, (c - 3) * 128:(c - 2) * 128].rearrange('(j p) c -> p j c', p=128), TM[:], reads=[('tm', pb)], writes=['d_ktm'])
                    for j in range(4):
                        P.op('pe', lambda e, pb=pb, j=j, TM=TM: e.transpose(ptb[pb][:, j * 128:(j + 1) * 128], TM[:, j, :], idf[:]), reads=[('tm', pb), 'idf'], writes=[('ptb', pb)])
                    P.op('act', lambda e, pb=pb: e.copy(fT[pb][:], ptb[pb][:]), reads=[('ptb', pb)], writes=[('fT', pb)])
                    dst = D['qT_g'] if c < 3 else D['kT_g']
                    cc = c if c < 3 else c - 3
                    P.dma('sp', dst[cc * 128:(cc + 1) * 128, t4 * 512:(t4 + 1) * 512], fT[pb][:], reads=[('fT', pb)], writes=['d_qkTg'])
        P.emit()


def phase_D2(P, l, D):
    import os
    C = 64
    NCH = S // C
    NST = int(os.environ.get('D2N', str(NCH)))
    MD = BF16 if os.environ.get('D2BF', '1') == '1' else F32
    with ExitStack() as s:
        def T12(name, dt=F32):
            return P.sb(s, 'e_' + name, [64, 12, 64], dt)
        ones = P.sb(s, 'e_ones', [64, 64], F32)
        idf = P.sb(s, 'e_idf', [64, 64], F32)
        idm = P.sb(s, 'e_idm', [64, 64], MD)
        idbc = T12('idbc')
        triF = P.sb(s, 'e_triF', [64, 64], F32)
        triB = P.sb(s, 'e_triB', [64, 64], F32)
        mW, mWt, mI = T12('mW'), T12('mWt'), T12('mI')
        St, St2, Sm = T12('S'), T12('S2'), T12('Sm', MD)
        ktm = [T12('ktm%d' % i) for i in range(2)]
        vtm = [T12('vtm%d' % i) for i in range(2)]
        kT = [T12('kT%d' % i, MD) for i in range(2)]
        qT = [T12('qT%d' % i, MD) for i in range(2)]
        gbv = [P.sb(s, 'e_gb%d' % i, [64, 24], F32) for i in range(2)]
        gc = P.sb(s, 'e_gc', [64, 12], F32)
        egc = [P.sb(s, 'e_egc%d' % i, [64, 12], F32) for i in range(2)]
        egl = [P.sb(s, 'e_egl%d' % i, [64, 12], F32) for i in range(2)]
        egd = P.sb(s, 'e_egd', [64, 12], F32)
        Dg = P.sb(s, 'e_Dg', [64, 24, 64], F32)
        diff, Ea, Eb = T12('diff'), T12('Ea'), T12('Eb')
        W, Wt = T12('W', MD), T12('Wt', MD)
        A1, A1t, A2, A2t = T12('A1', MD), T12('A1t', MD), T12('A2', MD), T12('A2t', MD)
        nxTI = T12('nxTI', MD)
        Yt = [T12('Yt0', MD), T12('Yt1', MD)]
        Yf = [T12('Yf0', MD), T12('Yf1', MD)]
        QKm = [T12('QKm0', MD), T12('QKm1', MD)]
        kd = [T12('kd0', MD), T12('kd1', MD)]
        Rr, Rm, vnew, o1 = T12('R'), T12('Rm', MD), T12('vnew', MD), T12('o1')
        ob = [T12('ob0'), T12('ob1')]
        pA = P.ps(s, 'e_pA', [64, 1024])
        pB = P.ps(s, 'e_pB', [64, 1024])
        pC = P.ps(s, 'e_pC', [64, 1024])
        pS = P.ps(s, 'e_pS', [64, 1024])

        def pv(p, h):
            return p[:, h * 512:h * 512 + 384].rearrange('p (j t) -> p j t', t=64)

        def sv(t, h):
            return t[:, h * 6:(h + 1) * 6, :]

        def pcol(p, j):
            c0 = (j // 6) * 512 + (j % 6) * 64
            return p[:, c0:c0 + 64]

        def mm12(pt, pn, lfn, rfn, rfun):
            for j in range(12):
                o_, l_, r_ = pcol(pt, j), lfn(j), rfn(j)
                P.op('pe', lambda e, o_=o_, l_=l_, r_=r_: e.matmul(o_, l_, r_, start=True, stop=True), reads=rfun(j // 6), writes=[(pn, j // 6)])

        def bcol(ap12, h):
            return bc(ap12[:, h * 6:(h + 1) * 6].unsqueeze(2), [64, 6, 64])

        P.dma('sp', ones[:], D['ones64'], writes=['ones'])
        P.dma('sp', idf[:], D['ident'][0:64, 0:64], writes=['idf'])
        P.dma('sp', triF[:], D['triF'], writes=['triF'])
        P.dma('sp', triB[:], D['triB'], writes=['triB'])
        P.dma('sp', mW[:], D['mW'], writes=['mW'])
        P.dma('sp', mWt[:], D['mWt'], writes=['mWt'])
        P.dma('sp', mI[:], D['mI'], writes=['mI'])
        P.op('dve', lambda e: e.tensor_copy(idbc[:], bc(idf[:].unsqueeze(1), [64, 12, 64])), reads=['idf'], writes=['idbc'])
        P.op('dve', lambda e: e.tensor_copy(idm[:], idf[:]), reads=['idf'], writes=['idm'])
        P.op('dve', lambda e: e.memset(St[:].rearrange('p a b -> p (a b)'), 0.0), writes=[('S', 0), ('S', 1)])
        P.op('pool', lambda e: e.memset(Sm[:].rearrange('p a b -> p (a b)'), 0.0), writes=[('Sm', 0), ('Sm', 1)])

        def prep(i):
            b = i % 2
            cf = i
            cb = NCH - 1 - i
            K_, V_, KT_, QT_, GB_ = ktm[b], vtm[b], kT[b], qT[b], gbv[b]
            EGC, EGL, QKM, KD = egc[b], egl[b], QKm[b], kd[b]
            for h, cc in ((0, cf), (1, cb)):
                sl = slice(h * 6, h * 6 + 6)
                tk = slice(cc * C, (cc + 1) * C)
                P.dma('sp', K_[:, sl, :], D['k_tm'][tk, :].rearrange('t (h d) -> t h d', d=64), reads=['d_ktm'], writes=[('ktm', b, h)])
                P.dma('sp', V_[:, sl, :], D['v_tm'][tk, :].rearrange('t (h d) -> t h d', d=64), reads=['d_vtm'], writes=[('vtm', b, h)])
                P.dma('sp', KT_[:, sl, :], D['kT_g'][:, tk].rearrange('(h d) t -> d h t', d=64), reads=['d_qkTg'], writes=[('kT', b, h)])
                P.dma('sp', QT_[:, sl, :], D['qT_g'][:, tk].rearrange('(h d) t -> d h t', d=64), reads=['d_qkTg'], writes=[('qT', b, h)])
                P.dma('sp', GB_[:, h * 6:h * 6 + 6], D['gb'][tk, h * 6:h * 6 + 6], reads=['d_gb'], writes=[('gb', b, h)])
                P.dma('sp', GB_[:, 12 + h * 6:12 + h * 6 + 6], D['gb'][tk, 12 + h * 6:12 + h * 6 + 6], reads=['d_gb'], writes=[('gbb', b, h)])
            beta = GB_[:, 12:24]
            DgF = Dg[:].rearrange('p a b -> p (a b)')
            for h in range(2):
                tri = triF if h == 0 else triB
                trin = 'triF' if h == 0 else 'triB'
                rG, rBt = ('gb', b, h), ('gbb', b, h)
                P.op('pe', lambda e, h=h, tri=tri, GB_=GB_: e.matmul(pA[:, h * 512:h * 512 + 6], tri[:], GB_[:, h * 6:h * 6 + 6], start=True, stop=True), reads=[trin, rG], writes=[('pA', h)])
                P.op('dve', lambda e, h=h: e.tensor_copy(gc[:, h * 6:h * 6 + 6], pA[:, h * 512:h * 512 + 6]), reads=[('pA', h)], writes=[('gc', h)])
                P.op('act', lambda e, h=h, EGC=EGC: e.activation(EGC[:, h * 6:h * 6 + 6], pA[:, h * 512:h * 512 + 6], AF.Exp), reads=[('pA', h)], writes=[('egc', b, h)])
                P.op('dve', lambda e, h=h: e.tensor_tensor(Dg[:, h * 6:h * 6 + 6, :], sv(idbc, 0), bcol(gc, h), ALU.mult), reads=['idbc', ('gc', h)], writes=[('Dg', h)])
                P.op('pool', lambda e, h=h, beta=beta: e.tensor_tensor(Dg[:, 12 + h * 6:12 + h * 6 + 6, :], sv(idbc, 0), bcol(beta, h), ALU.mult), reads=['idbc', rBt], writes=[('Dgb', h)])
                P.op('pe', lambda e, h=h: e.matmul(pB[:, h * 512:h * 512 + 384], ones[:], DgF[:, h * 384:(h + 1) * 384], start=True, stop=True), reads=['ones', ('Dg', h)], writes=[('pB', h)])
                P.op('pe', lambda e, h=h: e.matmul(pC[:, h * 512:h * 512 + 384], ones[:], DgF[:, 768 + h * 384:768 + (h + 1) * 384], start=True, stop=True), reads=['ones', ('Dgb', h)], writes=[('pC', h)])
                P.op('dve', lambda e, h=h: e.tensor_tensor(sv(diff, h), pv(pB, h), bcol(gc, h), ALU.subtract), reads=[('pB', h), ('gc', h)], writes=[('diff', h)])
                lc = h * 512 + (63 if h == 0 else 0)
                lastv = pB[:, lc:lc + 64 * 5 + 1:64]
                P.op('act', lambda e, h=h, lastv=lastv, EGL=EGL: e.activation(EGL[:, h * 6:h * 6 + 6], lastv, AF.Exp), reads=[('pB', h)], writes=[('egl', b, h)])
                P.op('dve', lambda e, h=h, lastv=lastv: e.tensor_tensor(egd[:, h * 6:h * 6 + 6], lastv, gc[:, h * 6:h * 6 + 6], ALU.subtract), reads=[('pB', h), ('gc', h)], writes=[('egd', h)])
                P.op('act', lambda e, h=h: e.activation(egd[:, h * 6:h * 6 + 6], egd[:, h * 6:h * 6 + 6], AF.Exp), reads=[('egd', h)], writes=[('egd', h)])
                P.op('pool', lambda e, h=h, K_=K_, KD=KD: e.tensor_tensor(sv(KD, h), sv(K_, h), bcol(egd, h), ALU.mult), reads=[('ktm', b, h), ('egd', h)], writes=[('kd', b, h)])
                P.op('act', lambda e, h=h: e.activation(sv(Ea, h), sv(diff, h), AF.Exp), reads=[('diff', h)], writes=[('Ea', h)])
                P.op('act', lambda e, h=h: e.activation(sv(Eb, h), sv(diff, h), AF.Exp, scale=-1.0), reads=[('diff', h)], writes=[('Eb', h)])
            yield
            mm12(pA, 'pA', lambda j: KT_[:, j, :], lambda j: KT_[:, j, :], lambda h: [('kT', b, h)])
            for h in range(2):
                rBt = ('gbb', b, h)
                P.op('dve', lambda e, h=h: e.scalar_tensor_tensor(sv(Eb, h), sv(Eb, h), 1.0, sv(mWt, h), ALU.min, ALU.mult), reads=[('Eb', h), 'mWt'], writes=[('Eb', h)])
                P.op('dve', lambda e, h=h: e.tensor_tensor(sv(Eb, h), sv(Eb, h), pv(pC, h), ALU.mult), reads=[('Eb', h), ('pC', h)], writes=[('Eb', h)])
                P.op('dve', lambda e, h=h: e.tensor_tensor(sv(Wt, h), sv(Eb, h), pv(pA, h), ALU.mult), reads=[('Eb', h), ('pA', h)], writes=[('Wt', h)])
            yield
            mm12(pC, 'pC', lambda j: KT_[:, j, :], lambda j: QT_[:, j, :], lambda h: [('kT', b, h), ('qT', b, h)])
            for h in range(2):
                rBt = ('gbb', b, h)
                P.op('dve', lambda e, h=h: e.scalar_tensor_tensor(sv(diff, h), sv(Ea, h), 1.0, sv(mI, h), ALU.min, ALU.mult), reads=[('Ea', h), 'mI'], writes=[('diff', h)])
                P.op('dve', lambda e, h=h, QKM=QKM: e.tensor_tensor(sv(QKM, h), sv(diff, h), pv(pC, h), ALU.mult), reads=[('diff', h), ('pC', h)], writes=[('QKm', b, h)])
                P.op('dve', lambda e, h=h: e.scalar_tensor_tensor(sv(Ea, h), sv(Ea, h), 1.0, sv(mW, h), ALU.min, ALU.mult), reads=[('Ea', h), 'mW', ('diff', h)], writes=[('Ea', h)])
                P.op('pool', lambda e, h=h, beta=beta: e.tensor_tensor(sv(Ea, h), sv(Ea, h), bcol(beta, h), ALU.mult), reads=[('Ea', h), rBt], writes=[('Ea', h)])
                P.op('dve', lambda e, h=h: e.tensor_tensor(sv(W, h), sv(Ea, h), pv(pA, h), ALU.mult), reads=[('Ea', h), ('pA', h)], writes=[('W', h)])
                P.op('pool', lambda e, h=h: e.tensor_tensor(sv(Yt[0], h), sv(idbc, h), sv(W, h), ALU.subtract), reads=['idbc', ('W', h)], writes=[('Yt0', h)])
            yield
            cur, curT, cn, cnT = W, Wt, 'W', 'Wt'
            yi = 0
            bufs = [(A1, A1t, 'A1', 'A1t'), (A2, A2t, 'A2', 'A2t')]
            for lev in range(5):
                nx, nxT, nn, nnT = bufs[lev % 2]
                mm12(pB, 'pB', lambda j, cur=cur: cur[:, j, :], lambda j, curT=curT: curT[:, j, :], lambda h, cn=cn, cnT=cnT: [(cn, h), (cnT, h)])
                for h in range(2):
                    if lev < 4:
                        P.op('act', lambda e, h=h, nxT=nxT: e.copy(sv(nxT, h), pv(pB, h)), reads=[('pB', h)], writes=[(nnT, h)])
                    P.op('dve', lambda e, h=h: e.tensor_tensor(sv(nxTI, h), pv(pB, h), sv(idbc, h), ALU.add), reads=[('pB', h), 'idbc'], writes=[('nxTI', h)])
                if lev < 4:
                    mm12(pC, 'pC', lambda j, curT=curT: curT[:, j, :], lambda j, cur=cur: cur[:, j, :], lambda h, cn=cn, cnT=cnT: [(cn, h), (cnT, h)])
                    for h in range(2):
                        P.op('dve', lambda e, h=h, nx=nx: e.tensor_copy(sv(nx, h), pv(pC, h)), reads=[('pC', h)], writes=[(nn, h)])
                Yc = Yt[yi]
                yc_ = 'Yt%d' % yi
                if lev < 4:
                    Yn, yn_ = Yt[1 - yi], ('Yt%d' % (1 - yi),)
                else:
                    Yn, yn_ = Yf[b], ('Yf', b)
                for j in range(12):
                    h = j // 6
                    P.op('pe', lambda e, j=j, Yc=Yc: e.matmul(pcol(pA, j), nxTI[:, j, :], Yc[:, j, :], start=True, stop=True), reads=[('nxTI', h), (yc_, h)], writes=[('pA', h)])
                for h in range(2):
                    P.op('dve', lambda e, h=h, Yn=Yn: e.tensor_copy(sv(Yn, h), pv(pA, h)), reads=[('pA', h)], writes=[yn_ + (h,)])
                yi = 1 - yi
                cur, curT, cn, cnT = nx, nxT, nn, nnT
                yield

        def scan(i):
            b = i % 2
            cf = i
            cb = NCH - 1 - i
            V_, KT_, QT_, GB_ = vtm[b], kT[b], qT[b], gbv[b]
            EGC, EGL, QKM, KD, YF = egc[b], egl[b], QKm[b], kd[b], Yf[b]
            beta = GB_[:, 12:24]
            mm12(pS, 'pS', lambda j: KT_[:, j, :], lambda j: Sm[:, j, :], lambda h: [('kT', b, h), ('Sm', h)])
            for h in range(2):
                P.op('dve', lambda e, h=h, EGC=EGC: e.tensor_tensor(sv(Rr, h), pv(pS, h), bcol(EGC, h), ALU.mult), reads=[('pS', h), ('egc', b, h)], writes=[('R', h)])
                P.op('dve', lambda e, h=h, V_=V_: e.tensor_tensor(sv(Rm, h), sv(V_, h), sv(Rr, h), ALU.subtract), reads=[('R', h), ('vtm', b, h)], writes=[('Rm', h)])
            yield
            mm12(pS, 'pS', lambda j: QT_[:, j, :], lambda j: Sm[:, j, :], lambda h: [('qT', b, h), ('Sm', h)])
            for h in range(2):
                P.op('act', lambda e, h=h: e.copy(sv(o1, h), pv(pS, h)), reads=[('pS', h)], writes=[('o1', h)])
                P.op('pool', lambda e, h=h, EGC=EGC: e.tensor_tensor(sv(o1, h), sv(o1, h), bcol(EGC, h), ALU.mult), reads=[('o1', h), ('egc', b, h)], writes=[('o1', h)])
            yield
            yield
            mm12(pS, 'pS', lambda j: YF[:, j, :], lambda j: Rm[:, j, :], lambda h: [('Yf', b, h), ('Rm', h)])
            for h in range(2):
                P.op('dve', lambda e, h=h, beta=beta: e.tensor_tensor(sv(vnew, h), pv(pS, h), bcol(beta, h), ALU.mult), reads=[('pS', h), ('gbb', b, h)], writes=[('vnew', h)])
            yield
            mm12(pS, 'pS', lambda j: KD[:, j, :], lambda j: vnew[:, j, :], lambda h: [('kd', b, h), ('vnew', h)])
            OB = ob[b]
            for h in range(2):
                P.op('pool', lambda e, h=h, EGL=EGL: e.tensor_tensor(sv(St2, h), sv(St, h), bcol(EGL, h), ALU.mult), reads=[('S', h), ('egl', b, h)], writes=[('S2', h)])
                P.op('dve', lambda e, h=h: e.tensor_tensor(sv(St, h), sv(St2, h), pv(pS, h), ALU.add), reads=[('S2', h), ('pS', h)], writes=[('S', h)])
                P.op('act', lambda e, h=h: e.copy(sv(Sm, h), sv(St, h)), reads=[('S', h)], writes=[('Sm', h)])
            yield
            mm12(pS, 'pS', lambda j: QKM[:, j, :], lambda j: vnew[:, j, :], lambda h: [('QKm', b, h), ('vnew', h)])
            for h in range(2):
                cc = cf if h == 0 else cb
                P.op('dve', lambda e, h=h, OB=OB: e.tensor_tensor(sv(OB, h), sv(o1, h), pv(pS, h), ALU.add), reads=[('o1', h), ('pS', h)], writes=[('ob', b, h)])
                P.dma('sp', D['o_fb'][h, cc * C:(cc + 1) * C, :].rearrange('t (h d) -> t h d', d=64), sv(OB, h), reads=[('ob', b, h)], writes=['d_ofb'])

        def run(gens):
            gens = [g for g in gens if g is not None]
            while gens:
                for g in list(gens):
                    try:
                        next(g)
                    except StopIteration:
                        gens.remove(g)

        run([prep(0)])
        for i in range(NST):
            run([prep(i + 1) if i + 1 < NST else None, scan(i)])
        P.emit()


def phase_E(P, l, D, first):
    xsrc = D['x'] if first else D['xw']
    with ExitStack() as s:
        wo = P.sb(s, 'f_wo', [128, 8, 1024], BF16)
        ong = P.sb(s, 'f_ong', [128, 64], F32)
        idb = P.sb(s, 'f_idb', [128, 128], BF16)
        of_ = [P.sb(s, 'f_of%d' % i, [128, 2, 384], F32) for i in range(2)]
        gt = [P.sb(s, 'f_gt%d' % i, [128, 384], F32) for i in range(2)]
        o = P.sb(s, 'f_o', [128, 6, 64], F32)
        sq = P.sb(s, 'f_sq', [128, 6, 64], F32)
        r6 = P.sb(s, 'f_r6', [128, 6], F32)
        ycb = P.sb(s, 'f_ycb', [128, 384], BF16)
        yT = [P.sb(s, 'f_yT%d' % i, [128, 8, 128], BF16) for i in range(2)]
        xt = [P.sb(s, 'f_xt%d' % i, [128, 1024], F32) for i in range(2)]
        pT = P.ps(s, 'f_pT', [128, 512], BF16)
        po = [P.ps(s, 'f_po%d' % i, [128, 512]) for i in range(2)]
        for k in range(8):
            P.dma('pool', wo[:, k, :], D['w_out'][l, k * 128:(k + 1) * 128, :], writes=[('wo', k)])
        P.dma('sp', ong[:], D['o_norm_g'][l].partition_broadcast(128), writes=['ong'])
        P.dma('pool', idb[:], D['ident'], writes=['idb'])
        def front(t):
            b = t % 2
            tk = slice(t * 128, (t + 1) * 128)
            P.dma('sp', of_[b][:], D['o_fb'][:, tk, :].rearrange('a t c -> t a c'), reads=['d_ofb'], writes=[('of', b)])
            P.dma('sp', gt[b][:], D['gate_s'][tk, :], reads=['d_gate'], writes=[('gt', b)])
            P.dma('sp', xt[b][:], xsrc[tk, :], writes=[('xt', b)])
            P.dma('sp', yT[b][:, 0:5, :], D['yT'][0:640, tk].rearrange('(k p) t -> p k t', p=128), reads=['d_yT'], writes=[('yT', b, 0)])
            OF = o[:].rearrange('p a b -> p (a b)')
            P.op('dve', lambda e, b=b: e.tensor_tensor(OF, of_[b][:, 0, :], of_[b][:, 1, :], ALU.add), reads=[('of', b)], writes=['o'])
            P.op('act', lambda e: e.activation(sq[:], o[:], AF.Square), reads=['o'], writes=['sq'])
            P.op('dve', lambda e: e.tensor_reduce(r6[:], sq[:], AX.X, ALU.add), reads=['sq'], writes=['r6'])
            P.op('act', lambda e: e.activation(r6[:], r6[:], AF.Sqrt, bias=EPS, scale=1.0 / 64), reads=['r6'], writes=['r6'])
            P.op('dve', lambda e: e.reciprocal(r6[:], r6[:]), reads=['r6'], writes=['r6'])
            P.op('dve', lambda e: e.tensor_tensor(o[:], o[:], bc(r6[:].unsqueeze(2), [128, 6, 64]), ALU.mult), reads=['o', 'r6'], writes=['o'])
            P.op('pool', lambda e: e.tensor_tensor(o[:], o[:], bc(ong[:].unsqueeze(1), [128, 6, 64]), ALU.mult), reads=['o', 'ong'], writes=['o'])
            P.op('pool', lambda e, b=b: e.tensor_tensor(ycb[:], OF, gt[b][:], ALU.mult), reads=['o', ('gt', b)], writes=['ycb'])
            for k in range(3):
                P.op('pe', lambda e, k=k: e.transpose(pT[:, k * 128:(k + 1) * 128], ycb[:, k * 128:(k + 1) * 128], idb[:]), reads=['ycb', 'idb'], writes=['pT'])
            P.op('act', lambda e, b=b: e.copy(yT[b][:, 5:8, :], pT[:, 0:384].rearrange('p (k t) -> p k t', t=128)), reads=['pT'], writes=[('yT', b, 1)])
        def back(t):
            b = t % 2
            tk = slice(t * 128, (t + 1) * 128)
            for hf in range(2):
                for k in range(8):
                    P.op('pe', lambda e, hf=hf, k=k, b=b: e.matmul(po[hf][:], yT[b][:, k, :], wo[:, k, hf * 512:(hf + 1) * 512], start=(k == 0), stop=(k == 7)),
                         reads=[('yT', b, 0), ('yT', b, 1), ('wo', k)], writes=[('po', hf)])
                P.op('dve', lambda e, hf=hf, b=b: e.tensor_tensor(xt[b][:, hf * 512:(hf + 1) * 512], xt[b][:, hf * 512:(hf + 1) * 512], po[hf][:], ALU.add),
                     reads=[('po', hf), ('xt', b)], writes=[('xt', b)])
            P.dma('sp', D['xw'][tk, :], xt[b][:], reads=[('xt', b)], writes=[('d_xw', t)])

        front(0)
        for t in range(NT):
            if t + 1 < NT:
                front(t + 1)
            back(t)
        P.emit()


def phase_F(P, l, D):
    import os
    NE = int(os.environ.get('FNE', '16'))
    with ExitStack() as s:
        gff = P.sb(s, 'g_gff', [128, 1024], F32)
        idf = P.sb(s, 'g_idf', [128, 128], F32)
        idb = P.sb(s, 'g_idb', [128, 128], BF16)
        wr = P.sb(s, 'g_wr', [128, 8, 16], F32)
        aff = P.sb(s, 'g_aff', [128, NT, 16], F32)
        sel = P.sb(s, 'g_sel', [128, NT, 16], F32)
        rank = P.sb(s, 'g_rank', [128, NT, 16], F32)
        cA = P.sb(s, 'g_cA', [128, NT, 16], F32)
        cB = P.sb(s, 'g_cB', [128, NT, 16], F32)
        selb = P.sb(s, 'g_selb', [128, NT * 16], BF16)
        triS = P.sb(s, 'g_triS', [128, 128], BF16)
        onesb = P.sb(s, 'g_onesb', [128, 128], BF16)
        tg = P.sb(s, 'g_tg', [128, NT, 16, 5], BF16)
        tp = P.sb(s, 'g_tp', [128, NT, 2], F32)
        iota = P.sb(s, 'g_iota', [128, 512], F32)
        Selt = [P.sb(s, 'g_Selt%d' % i, [128, 512], BF16) for i in range(2)]
        idxf = P.sb(s, 'g_idxf', [128, 4, 8], F32)
        row5 = P.sb(s, 'g_row5', [5, 512], F32)
        idxv = P.sb(s, 'g_idxv', [128, 4], F32)
        idxi = [P.sb(s, 'g_idxi%d' % i, [128, 4], I32) for i in range(2)]
        gate = [P.sb(s, 'g_gate%d' % i, [128, 4], F32) for i in range(2)]
        affT2 = P.sb(s, 'g_affT2', [16, S], F32)
        bj = P.sb(s, 'g_bj', [16, S], BF16)
        bs = P.sb(s, 'g_bs', [16, 8], F32)
        ones16 = P.sb(s, 'g_ones16', [16, 128], F32)
        dthr = P.sb(s, 'g_dthr', [16, 16], F32)
        thrb = P.sb(s, 'g_thrb', [128, 16], F32)
        xt = P.sb(s, 'g_xt', [128, 1024], F32)
        junk = P.sb(s, 'g_junk', [128, 1024], BF16)
        ss = P.sb(s, 'g_ss', [128, 4], F32)
        h32 = P.sb(s, 'g_h32', [128, 1024], F32)
        hb16 = P.sb(s, 'g_hb16', [128, 1024], BF16)
        hT32 = P.sb(s, 'g_hT32', [128, 8, 128], F32)
        sm = P.sb(s, 'g_sm', [128, 4], F32)
        ex = P.sb(s, 'g_ex', [128, 16], F32)
        wg = [P.sb(s, 'g_wg%d' % i, [128, 8, 1024], BF16) for i in range(2)]
        wu = [P.sb(s, 'g_wu%d' % i, [128, 8, 1024], BF16) for i in range(2)]
        wd = [P.sb(s, 'g_wd%d' % i, [128, 8, 1024], BF16) for i in range(2)]
        xe = P.sb(s, 'g_xe', [128, 4, 1024], BF16)
        xeTs = [P.sb(s, 'g_xeT%d' % i, [128, 8, 512], BF16) for i in range(2)]
        hid = P.sb(s, 'g_hid', [128, 8, 512], BF16)
        sg = [P.sb(s, 'g_sg%d' % i, [128, 512], BF16) for i in range(2)]
        ye = [P.sb(s, 'g_ye%d' % i, [128, 1024], F32) for i in range(2)]
        pbig = P.ps(s, 'g_pbig', [128, 1024])
        pl = P.ps(s, 'g_pl', [128, 512])
        pT = P.ps(s, 'g_pT', [128, 1024], BF16)
        pg_ = P.ps(s, 'g_pg', [128, 512])
        pu_ = P.ps(s, 'g_pu', [128, 512])
        py = [P.ps(s, 'g_py%d' % i, [128, 512]) for i in range(2)]

        def load_w(ex_):
            eb = ex_ % 2
            for k in range(8):
                P.dma('pool', wg[eb][:, k, :], D['w_e_gate'][l, ex_, k * 128:(k + 1) * 128, :], writes=[('wg', eb, k)])
                P.dma('pool', wu[eb][:, k, :], D['w_e_up'][l, ex_, k * 128:(k + 1) * 128, :], writes=[('wu', eb, k)])
            for k in range(8):
                P.dma('pool', wd[eb][:, k, :], D['w_e_down'][l, ex_, k * 128:(k + 1) * 128, :], writes=[('wd', eb, k)])

        P.dma('sp', gff[:], D['g_ffn'][l].partition_broadcast(128), writes=['gff'])
        P.dma('sp', idf[:], D['ident'], writes=['idf'])
        P.dma('pool', idb[:], D['ident'], writes=['idb'])
        P.dma('pool', triS[:], D['triS'], writes=['triS'])
        P.dma('pool', onesb[:], D['ones128'], writes=['onesb'])
        P.dma('sp', wr[:], D['w_router'][l].rearrange('(k p) e -> p k e', p=128), writes=['wr'])
        P.dma('sp', ones16[:], D['ones128'][0:16, :], writes=['ones16'])
        P.dma('sp', tp[:], D['tp'], writes=['tp'])
        P.dma('sp', iota[:], D['iota512'], writes=['iota'])
        load_w(0)
        for t in range(NT):
            tk = slice(t * 128, (t + 1) * 128)
            P.dma('sp', xt[:], D['xw'][tk, :], reads=['d_xw'], writes=['xt'])
            P.op('dve', lambda e: e.memset(ss[:, 0:1], 0.0), writes=['ss'])
            P.op('act', lambda e: e.activation(junk[:], xt[:], AF.Square, accum_out=ss[:, 0:1]), reads=['xt', 'ss'], writes=['junk', 'ss'])
            P.op('act', lambda e: e.activation(ss[:, 0:1], ss[:, 0:1], AF.Sqrt, bias=EPS, scale=1.0 / 1024), reads=['ss'], writes=['ss'])
            P.op('dve', lambda e: e.reciprocal(ss[:, 0:1], ss[:, 0:1]), reads=['ss'], writes=['ss'])
            P.op('dve', lambda e: e.scalar_tensor_tensor(h32[:], xt[:], ss[:, 0:1], gff[:], ALU.mult, ALU.mult), reads=['xt', 'ss', 'gff'], writes=['h32'])
            P.op('act', lambda e: e.copy(hb16[:], h32[:]), reads=['h32'], writes=['hb16'])
            P.dma('sp', D['hb'][tk, :], hb16[:], reads=['hb16'], writes=['d_hb'])
            for k in range(8):
                P.op('pe', lambda e, k=k: e.transpose(pbig[:, k * 128:(k + 1) * 128], h32[:, k * 128:(k + 1) * 128], idf[:]), reads=['h32', 'idf'], writes=[('pbig', k // 4)])
            P.op('act', lambda e: e.copy(hT32[:, 0:4, :], pbig[:, 0:512].rearrange('p (k t) -> p k t', t=128)), reads=[('pbig', 0)], writes=['hT32a'])
            P.op('dve', lambda e: e.tensor_copy(hT32[:, 4:8, :], pbig[:, 512:1024].rearrange('p (k t) -> p k t', t=128)), reads=[('pbig', 1)], writes=['hT32b'])
            for k in range(8):
                P.op('pe', lambda e, k=k: e.matmul(pl[:, 0:16], hT32[:, k, :], wr[:, k, :], start=(k == 0), stop=(k == 7)), reads=['hT32a', 'hT32b', 'wr'], writes=['pl'])
            P.op('dve', lambda e: e.tensor_reduce(sm[:, 0:1], pl[:, 0:16], AX.X, ALU.max), reads=['pl'], writes=['sm'])
            P.op('dve', lambda e: e.tensor_scalar(sm[:, 1:2], sm[:, 0:1], -1.0, None, ALU.mult), reads=['sm'], writes=['sm'])
            P.op('dve', lambda e: e.memset(sm[:, 2:3], 0.0), reads=['sm'], writes=['sm'])
            P.op('act', lambda e: e.activation(ex[:], pl[:, 0:16], AF.Exp, bias=sm[:, 1:2], accum_out=sm[:, 2:3]), reads=['pl', 'sm'], writes=['ex', 'sm'])
            P.op('dve', lambda e: e.reciprocal(sm[:, 3:4], sm[:, 2:3]), reads=['sm'], writes=['sm'])
            P.op('dve', lambda e, t=t: e.tensor_scalar(aff[:, t, :], ex[:], sm[:, 3:4], None, ALU.mult), reads=['ex', 'sm'], writes=[('aff', t)])
            P.op('pe', lambda e, t=t: e.transpose(pl[0:16, 128:256], aff[:, t, :], idf[:]), reads=[('aff', t), 'idf'], writes=['pl'])
            P.op('act', lambda e, t=t: e.mul(affT2[:, t * 128:(t + 1) * 128], pl[0:16, 128:256], 2.0), reads=['pl'], writes=['affT2'])
        lo, hi, half, mid2, cnt, gef, tt = (bs[:, i:i + 1] for i in range(7))
        P.op('dve', lambda e: e.memset(bs[:], 0.0), writes=['bs'])
        P.op('dve', lambda e: e.memset(hi, 1.0), reads=['bs'], writes=['bs'])
        for itn in range(30):
            P.op('dve', lambda e: e.tensor_tensor(mid2, lo, hi, ALU.add), reads=['bs'], writes=['bs'])
            P.op('dve', lambda e: e.tensor_scalar(half, mid2, 0.5, None, ALU.mult), reads=['bs'], writes=['bs'])
            P.op('dve', lambda e: e.memset(cnt, 0.0), reads=['bs'], writes=['bs'])
            P.op('dve', lambda e: e.tensor_scalar(bj[:], affT2[:], mid2, 0.0, ALU.is_ge, ALU.add, accum_out=cnt), reads=['affT2', 'bs'], writes=['bj', 'bs'])
            P.op('dve', lambda e: e.tensor_scalar(gef, cnt, 511.5, None, ALU.is_ge), reads=['bs'], writes=['bs'])
            P.op('dve', lambda e: e.tensor_tensor(tt, half, lo, ALU.subtract), reads=['bs'], writes=['bs'])
            P.op('dve', lambda e: e.tensor_tensor(tt, tt, gef, ALU.mult), reads=['bs'], writes=['bs'])
            P.op('dve', lambda e: e.tensor_tensor(lo, lo, tt, ALU.add), reads=['bs'], writes=['bs'])
            P.op('dve', lambda e: e.tensor_tensor(tt, hi, half, ALU.subtract), reads=['bs'], writes=['bs'])
            P.op('dve', lambda e: e.tensor_tensor(tt, tt, gef, ALU.mult), reads=['bs'], writes=['bs'])
            P.op('dve', lambda e: e.tensor_tensor(hi, half, tt, ALU.add), reads=['bs'], writes=['bs'])
        P.op('dve', lambda e: e.tensor_scalar(dthr[:], idf[0:16, 0:16], lo, None, ALU.mult), reads=['idf', 'bs'], writes=['dthr'])
        P.op('pe', lambda e: e.matmul(pl[:, 256:272], ones16[:], dthr[:], start=True, stop=True), reads=['ones16', 'dthr'], writes=['pl'])
        P.op('dve', lambda e: e.tensor_copy(thrb[:], pl[:, 256:272]), reads=['pl'], writes=['thrb'])
        AFF = [('aff', t) for t in range(NT)]
        P.op('dve', lambda e: e.tensor_tensor(sel[:], aff[:], bc(thrb[:].unsqueeze(1), [128, NT, 16]), ALU.is_ge), reads=AFF + ['thrb'], writes=['sel'])
        P.op('dve', lambda e: e.tensor_copy(selb[:], sel[:].rearrange('p t e -> p (t e)')), reads=['sel'], writes=['selb'])
        P.op('pe', lambda e: e.matmul(pg_[:], triS[:], selb[:], start=True, stop=True), reads=['triS', 'selb'], writes=['pg'])
        P.op('pe', lambda e: e.matmul(pu_[:], onesb[:], selb[:], start=True, stop=True), reads=['onesb', 'selb'], writes=['pu'])
        P.op('dve', lambda e: e.tensor_copy(cA[:].rearrange('p t e -> p (t e)'), pu_[:]), reads=['pu'], writes=['cA'])
        src, dst, sn, dn = cA, cB, 'cA', 'cB'
        for sft in (1, 2, 4, 8, 16):
            P.op('pool', lambda e, src=src, dst=dst, sft=sft: e.tensor_copy(dst[:, 0:sft, :], src[:, 0:sft, :]), reads=[sn], writes=[dn])
            P.op('dve', lambda e, src=src, dst=dst, sft=sft: e.tensor_tensor(dst[:, sft:NT, :], src[:, sft:NT, :], src[:, 0:NT - sft, :], ALU.add), reads=[sn], writes=[dn])
            src, dst, sn, dn = dst, src, dn, sn
        P.op('dve', lambda e, src=src: e.tensor_tensor(rank[:].rearrange('p t e -> p (t e)'), src[:].rearrange('p t e -> p (t e)'), pu_[:], ALU.subtract), reads=[sn, 'pu'], writes=['rank'])
        P.op('dve', lambda e: e.tensor_tensor(rank[:].rearrange('p t e -> p (t e)'), rank[:].rearrange('p t e -> p (t e)'), pg_[:], ALU.add), reads=['rank', 'pg'], writes=['rank'])
        P.op('dve', lambda e: e.scalar_tensor_tensor(rank[:], rank[:], 1.0, sel[:], ALU.add, ALU.mult), reads=['rank', 'sel'], writes=['rank'])
        P.op('dve', lambda e: e.tensor_scalar(rank[:], rank[:], -1.0, None, ALU.add), reads=['rank'], writes=['rank'])
        P.op('dve', lambda e: e.tensor_copy(tg[:, :, :, 0:2], bc(tp[:].unsqueeze(2), [128, NT, 16, 2])), reads=['tp'], writes=['tg0'])
        P.op('dve', lambda e: e.tensor_copy(tg[:, :, :, 2], aff[:]), reads=AFF, writes=['tg1'])
        P.op('dve', lambda e: e.tensor_tensor(cA[:], aff[:], tg[:, :, :, 2], ALU.subtract), reads=AFF + ['tg1', 'cA', 'cB'], writes=['cA'])
        P.op('dve', lambda e: e.tensor_copy(tg[:, :, :, 3], cA[:]), reads=['cA'], writes=['tg2'])
        P.op('dve', lambda e: e.tensor_tensor(cB[:], cA[:], tg[:, :, :, 3], ALU.subtract), reads=['cA', 'tg2', 'cB'], writes=['cB'])
        P.op('dve', lambda e: e.tensor_copy(tg[:, :, :, 4], cB[:]), reads=['cB'], writes=['tg3'])
        TG = ['tg0', 'tg1', 'tg2', 'tg3']
        nsel = 0
        npy = 0
        nsg = 0
        def stage1(ex_):
            nonlocal nsel
            eb = ex_ % 2
            xeT = xeTs[eb]
            for t in range(NT):
                sb_ = nsel % 2
                nsel += 1
                P.op('dve', lambda e, sb_=sb_, t=t, ex_=ex_: e.tensor_scalar(Selt[sb_][:], iota[:], rank[:, t, ex_:ex_ + 1], None, ALU.is_equal), reads=['iota', 'rank'], writes=[('Selt', sb_)])
                P.op('pe', lambda e, sb_=sb_, t=t, ex_=ex_: e.matmul(pl[0:5, 0:512], tg[:, t, ex_, :], Selt[sb_][:], start=(t == 0), stop=(t == NT - 1)),
                     reads=[('Selt', sb_)] + TG, writes=['pl'])
            P.op('act', lambda e: e.copy(row5[:], pl[0:5, 0:512]), reads=['pl'], writes=['row5'])
            for g in range(4):
                P.op('pe', lambda e, g=g: e.transpose(pl[:, g * 8:g * 8 + 5], row5[0:5, g * 128:(g + 1) * 128], idf[0:5, 0:5]), reads=['row5', 'idf'], writes=['pl'])
            P.op('dve', lambda e: e.tensor_copy(idxf[:, :, 0:5], pl[:, 0:32].rearrange('p (g c) -> p g c', c=8)[:, :, 0:5]), reads=['pl'], writes=['idxf'])
            P.op('dve', lambda e: e.scalar_tensor_tensor(idxv[:], idxf[:, :, 0], 128.0, idxf[:, :, 1], ALU.mult, ALU.add), reads=['idxf'], writes=['idxv'])
            P.op('dve', lambda e, eb=eb: e.tensor_copy(idxi[eb][:], idxv[:]), reads=['idxv'], writes=[('idxi', eb)])
            P.op('dve', lambda e, eb=eb: e.tensor_tensor(gate[eb][:], idxf[:, :, 2], idxf[:, :, 3], ALU.add), reads=['idxf'], writes=[('gate', eb)])
            P.op('dve', lambda e, eb=eb: e.tensor_tensor(gate[eb][:], gate[eb][:], idxf[:, :, 4], ALU.add), reads=['idxf', ('gate', eb)], writes=[('gate', eb)])
            for g in range(4):
                P.idma(lambda e, g=g, eb=eb: e.indirect_dma_start(out=xe[:, g, :], out_offset=None, in_=D['hb'][:, :],
                                                                   in_offset=bass.IndirectOffsetOnAxis(ap=idxi[eb][:, g:g + 1], axis=0), bounds_check=P.breg(e), oob_is_err=False),
                       reads=['d_hb', ('idxi', eb)], writes=[('xe', g)])
            for g in range(4):
                for k in range(8):
                    P.op('pe', lambda e, g=g, k=k: e.transpose(pT[:, k * 128:(k + 1) * 128], xe[:, g, k * 128:(k + 1) * 128], idb[:]), reads=[('xe', g), 'idb'], writes=['pT'])
                eng = 'act' if g % 2 == 0 else 'dve'
                if eng == 'act':
                    P.op('act', lambda e, g=g, xeT=xeT: e.copy(xeT[:, :, g * 128:(g + 1) * 128], pT[:].rearrange('p (k t) -> p k t', t=128)), reads=['pT'], writes=[('xeT', eb, g)])
                else:
                    P.op('dve', lambda e, g=g, xeT=xeT: e.tensor_copy(xeT[:, :, g * 128:(g + 1) * 128], pT[:].rearrange('p (k t) -> p k t', t=128)), reads=['pT'], writes=[('xeT', eb, g)])

        def stage2(ex_):
            nonlocal npy, nsg
            eb = ex_ % 2
            xeT = xeTs[eb]
            XET = [('xeT', eb, g) for g in range(4)]
            for fc in range(8):
                for k in range(8):
                    P.op('pe', lambda e, fc=fc, k=k, eb=eb, xeT=xeT: e.matmul(pg_[:], wg[eb][:, k, fc * 128:(fc + 1) * 128], xeT[:, k, :], start=(k == 0), stop=(k == 7)),
                         reads=XET + [('wg', eb, k)], writes=['pg'])
                for k in range(8):
                    P.op('pe', lambda e, fc=fc, k=k, eb=eb, xeT=xeT: e.matmul(pu_[:], wu[eb][:, k, fc * 128:(fc + 1) * 128], xeT[:, k, :], start=(k == 0), stop=(k == 7)),
                         reads=XET + [('wu', eb, k)], writes=['pu'])
                sb2 = nsg % 2
                nsg += 1
                P.op('act', lambda e, sb2=sb2: e.activation(sg[sb2][:], pg_[:], AF.Silu), reads=['pg'], writes=[('sg', sb2)])
                P.op('dve', lambda e, sb2=sb2, fc=fc: e.tensor_tensor(hid[:, fc, :], sg[sb2][:], pu_[:], ALU.mult), reads=[('sg', sb2), 'pu'], writes=[('hid', fc)])
            HID = [('hid', fc) for fc in range(8)]
            for g in range(4):
                yb = g % 2
                for hf in range(2):
                    pb = npy % 2
                    npy += 1
                    for fc in range(8):
                        P.op('pe', lambda e, pb=pb, fc=fc, g=g, hf=hf, eb=eb: e.matmul(py[pb][:], hid[:, fc, g * 128:(g + 1) * 128], wd[eb][:, fc, hf * 512:(hf + 1) * 512], start=(fc == 0), stop=(fc == 7)),
                             reads=HID + [('wd', eb, fc)], writes=[('py', pb)])
                    if hf == 0:
                        P.op('act', lambda e, pb=pb, yb=yb, g=g, eb=eb: e.activation(ye[yb][:, 0:512], py[pb][:], AF.Copy, scale=gate[eb][:, g:g + 1]), reads=[('py', pb), ('gate', eb)], writes=[('ye', yb, 0)])
                    else:
                        P.op('dve', lambda e, pb=pb, yb=yb, g=g, eb=eb: e.tensor_scalar(ye[yb][:, 512:1024], py[pb][:], gate[eb][:, g:g + 1], None, ALU.mult), reads=[('py', pb), ('gate', eb)], writes=[('ye', yb, 1)])
                P.idma(lambda e, g=g, eb=eb, yb=yb: e.indirect_dma_start(out=D['xw'][:, :], out_offset=bass.IndirectOffsetOnAxis(ap=idxi[eb][:, g:g + 1], axis=0), in_=ye[yb][:],
                                                                          in_offset=None, bounds_check=P.breg(e), oob_is_err=False, compute_op=ALU.add),
                       reads=[('ye', yb, 0), ('ye', yb, 1), ('idxi', eb), 'd_xw'], writes=['d_xw'])

        stage1(0)
        for ex_ in range(NE):
            if ex_ + 1 < NE:
                load_w(ex_ + 1)
                stage1(ex_ + 1)
            stage2(ex_)
        P.emit()


def phase_G(P, l, D, last):
    xdst = D['out'] if last else D['xw']
    with ExitStack() as s:
        wp = P.sb(s, 'h_wp', [128, 2, 1024], BF16)
        wgt = P.sb(s, 'h_wgt', [128, 8, 1024], BF16)
        gpl = P.sb(s, 'h_gpl', [128, 1024], F32)
        gpg = P.sb(s, 'h_gpg', [128, 1024], F32)
        idb = P.sb(s, 'h_idb', [128, 128], BF16)
        xt = [P.sb(s, 'h_xt%d' % i, [128, 1024], F32) for i in range(2)]
        pb_ = [P.sb(s, 'h_pb%d' % i, [128, 256], BF16) for i in range(2)]
        junk = P.sb(s, 'h_junk', [128, 1024], BF16)
        ss = P.sb(s, 'h_ss', [128, 4], F32)
        ssf = P.sb(s, 'h_ssf', [128, 2], F32)
        junkf = P.sb(s, 'h_junkf', [128, 1024], BF16)
        xn = P.sb(s, 'h_xn', [128, 1024], BF16)
        xTs = [P.sb(s, 'h_xT%d' % i, [128, 10, 128], BF16) for i in range(2)]
        er = P.sb(s, 'h_er', [128, 1024], F32)
        gt = P.sb(s, 'h_gt', [128, 1024], F32)
        pT = P.ps(s, 'h_pT', [128, 2048], BF16)
        pe_ = P.ps(s, 'h_pe', [128, 1024])
        pg_ = P.ps(s, 'h_pg', [128, 1024])
        for k in range(2):
            P.dma('pool', wp[:, k, :], D['w_ple'][l, k * 128:(k + 1) * 128, :], writes=[('wp', k)])
        for k in range(8):
            P.dma('pool', wgt[:, k, :], D['w_ple_gate'][l, k * 128:(k + 1) * 128, :], writes=[('wgt', k)])
        P.dma('sp', gpl[:], D['g_ple'][l].partition_broadcast(128), writes=['gpl'])
        P.dma('sp', gpg[:], D['g_ple_gate'][l].partition_broadcast(128), writes=['gpg'])
        P.dma('pool', idb[:], D['ident'], writes=['idb'])
        def front(t):
            b = t % 2
            xT = xTs[b]
            tk = slice(t * 128, (t + 1) * 128)
            P.dma('sp', xt[b][:], D['xw'][tk, :], reads=[('d_xw', t)], writes=[('xt', b)])
            P.dma('pool', pb_[b][:], D['p'][l, tk, :], writes=[('pb', b)])
            P.op('dve', lambda e: e.memset(ssf[:, 0:1], 0.0), writes=['ssf'])
            P.op('act', lambda e, b=b: e.activation(junkf[:], xt[b][:], AF.Square, accum_out=ssf[:, 0:1]), reads=[('xt', b), 'ssf'], writes=['junkf', 'ssf'])
            P.op('act', lambda e: e.activation(ssf[:, 0:1], ssf[:, 0:1], AF.Sqrt, bias=EPS, scale=1.0 / 1024), reads=['ssf'], writes=['ssf'])
            P.op('dve', lambda e: e.reciprocal(ssf[:, 0:1], ssf[:, 0:1]), reads=['ssf'], writes=['ssf'])
            P.op('dve', lambda e, b=b: e.scalar_tensor_tensor(xn[:], xt[b][:], ssf[:, 0:1], gpg[:], ALU.mult, ALU.mult), reads=[('xt', b), 'ssf', 'gpg'], writes=['xn'])
            for k in range(8):
                P.op('pe', lambda e, k=k: e.transpose(pT[:, k * 128:(k + 1) * 128], xn[:, k * 128:(k + 1) * 128], idb[:]), reads=['xn', 'idb'], writes=[('pT', 0)])
            for k in range(2):
                P.op('pe', lambda e, k=k, b=b: e.transpose(pT[:, (8 + k) * 128:(9 + k) * 128], pb_[b][:, k * 128:(k + 1) * 128], idb[:]), reads=[('pb', b), 'idb'], writes=[('pT', 1)])
            P.op('act', lambda e, xT=xT: e.copy(xT[:, 0:8, :].rearrange('p k t -> p (k t)'), pT[:, 0:1024]), reads=[('pT', 0)], writes=[('xTa', b)])
            P.op('dve', lambda e, xT=xT: e.tensor_copy(xT[:, 8:10, :].rearrange('p k t -> p (k t)'), pT[:, 1024:1280]), reads=[('pT', 1)], writes=[('xTb', b)])

        def back(t):
            b = t % 2
            xT = xTs[b]
            tk = slice(t * 128, (t + 1) * 128)
            for hf in range(2):
                for k in range(2):
                    P.op('pe', lambda e, hf=hf, k=k, xT=xT: e.matmul(pe_[:, hf * 512:(hf + 1) * 512], xT[:, 8 + k, :], wp[:, k, hf * 512:(hf + 1) * 512], start=(k == 0), stop=(k == 1)),
                         reads=[('xTb', b), ('wp', k)], writes=[('pe', hf)])
                for k in range(8):
                    P.op('pe', lambda e, hf=hf, k=k, xT=xT: e.matmul(pg_[:, hf * 512:(hf + 1) * 512], xT[:, k, :], wgt[:, k, hf * 512:(hf + 1) * 512], start=(k == 0), stop=(k == 7)),
                         reads=[('xTa', b), ('wgt', k)], writes=[('pg', hf)])
            P.op('dve', lambda e: e.memset(ss[:, 1:3], 0.0), reads=['ss'], writes=['ss'])
            for hf in range(2):
                P.op('act', lambda e, hf=hf: e.activation(junk[:, hf * 512:(hf + 1) * 512], pe_[:, hf * 512:(hf + 1) * 512], AF.Square, accum_out=ss[:, 1 + hf:2 + hf]), reads=[('pe', hf), 'ss'], writes=['junk', 'ss'])
            P.op('dve', lambda e: e.tensor_tensor(ss[:, 1:2], ss[:, 1:2], ss[:, 2:3], ALU.add), reads=['ss'], writes=['ss'])
            P.op('act', lambda e: e.activation(ss[:, 1:2], ss[:, 1:2], AF.Sqrt, bias=EPS, scale=1.0 / 1024), reads=['ss'], writes=['ss'])
            P.op('dve', lambda e: e.reciprocal(ss[:, 1:2], ss[:, 1:2]), reads=['ss'], writes=['ss'])
            for hf in range(2):
                hs = slice(hf * 512, (hf + 1) * 512)
                P.op('dve', lambda e, hs=hs: e.scalar_tensor_tensor(er[:, hs], pe_[:, hs], ss[:, 1:2], gpl[:, hs], ALU.mult, ALU.mult), reads=[('pe', hf), 'ss', 'gpl'], writes=['er'])
                P.op('act', lambda e, hs=hs: e.activation(gt[:, hs], pg_[:, hs], AF.Sigmoid), reads=[('pg', hf)], writes=['gt'])
            P.op('dve', lambda e: e.tensor_tensor(er[:], er[:], gt[:], ALU.mult), reads=['er', 'gt'], writes=['er'])
            P.op('dve', lambda e, b=b: e.tensor_tensor(xt[b][:], xt[b][:], er[:], ALU.add), reads=['er', ('xt', b)], writes=[('xt', b)])
            P.dma('sp', xdst[tk, :], xt[b][:], reads=[('xt', b)], writes=[('d_xw', t)])

        front(0)
        for t in range(NT):
            if t + 1 < NT:
                front(t + 1)
            back(t)
        P.emit()


WEIGHTS = [('g_mix', [4, 1024]), ('w_in', [4, 1024, 3224]), ('ln_v_g', [4, 4, 64]), ('ln_v_b', [4, 4, 64]), ('w_s', [4, 4, 128, 128]),
           ('b_s', [4, 4, 128]), ('q_norm_g', [4, 64]), ('k_norm_g', [4, 64]), ('conv_w', [4, 5, 1152]), ('a_log', [4, 2, 6]),
           ('dt_bias', [4, 2, 6]), ('o_norm_g', [4, 64]), ('w_out', [4, 1024, 1024]), ('g_ffn', [4, 1024]), ('w_router', [4, 1024, 16]),
           ('w_e_gate', [4, 16, 1024, 1024]), ('w_e_up', [4, 16, 1024, 1024]), ('w_e_down', [4, 16, 1024, 1024]), ('w_ple', [4, 256, 1024]),
           ('g_ple', [4, 1024]), ('g_ple_gate', [4, 1024]), ('w_ple_gate', [4, 1024, 1024])]


def make_consts():
    c = {}
    c['ident'] = np.eye(128, dtype=np.float32)
    c['ones64'] = np.ones((64, 64), np.float32)
    c['ones128'] = np.ones((128, 128), np.float32)
    half = 8
    c['invf'] = (np.float32(500000.0) ** (-np.arange(half, dtype=np.float32) * np.float32(2.0) / np.float32(16))).astype(np.float32)
    a = np.arange(128)[:, None]
    b = np.arange(128)[None, :]
    mA = (a >= b).astype(np.float32)
    mB = (a <= b).astype(np.float32)
    c['mab'] = np.concatenate([mA, mB, mA, mB], axis=1)
    sel = np.zeros((65, 64), np.float32)
    sel[64, :] = 1.0
    c['sel65'] = sel
    p = np.arange(64)[:, None]
    f = np.arange(64)[None, :]
    c['triF'] = (p <= f).astype(np.float32)
    c['triB'] = (p >= f).astype(np.float32)

    def m12(fw, bw):
        return np.ascontiguousarray(np.stack([fw] * 6 + [bw] * 6, axis=1).astype(np.float32))
    c['mW'] = m12(f > p, f < p)
    c['mWt'] = m12(p > f, p < f)
    c['mI'] = m12(f >= p, f <= p)
    c['triS'] = (a < b).astype(np.float32)
    c['iota512'] = np.ascontiguousarray(np.broadcast_to(np.arange(512, dtype=np.float32)[None, :], (128, 512)))
    tpv = np.zeros((128, NT, 2), np.float32)
    tpv[:, :, 0] = np.arange(NT)[None, :]
    tpv[:, :, 1] = np.arange(128)[:, None]
    c['tp'] = tpv
    return c


SCRATCH = [('cs', [S, 16], F32), ('vn', [S, 256], BF16), ('qkT', [6, 128, S], BF16), ('vaug', [S, 390], BF16), ('gate_s', [S, 384], F32),
           ('ab', [S, 24], F32), ('uT', [256, S], F32), ('cT', [1152, S], F32), ('yT', [1024, S], BF16), ('v_tm', [S, 384], F32),
           ('k_tm', [S, 384], F32), ('qT_g', [384, S], BF16), ('kT_g', [384, S], BF16), ('gb', [S, 24], F32), ('o_fb', [2, S, 384], F32),
           ('xw', [S, 1024], F32), ('hb', [S, 1024], BF16)]


def build(n_layers=4, phases=None, dbg=()):
    P = Prog()
    D = {}
    D['x'] = P.dram('x', [S, 1024], F32, 'ExternalInput')
    D['p'] = P.dram('p', [n_layers, S, 256], F32, 'ExternalInput')
    D['positions'] = P.dram('positions', [128, NT], I32, 'ExternalInput')
    for n, shp in WEIGHTS:
        D[n] = P.dram(n, [n_layers] + list(shp[1:]), F32, 'ExternalInput')
    for n, v in make_consts().items():
        D[n] = P.dram(n, list(v.shape), F32, 'ExternalInput')
    for n, shp, dt in SCRATCH:
        D[n] = P.dram(n, shp, dt, 'ExternalOutput' if n in dbg else 'Internal')
    D['out'] = P.dram('out', [S, 1024], F32, 'ExternalOutput')
    allp = phases is None
    if allp or 'R' in phases:
        phase_rope(P, D)
    for l in range(n_layers):
        first = (l == 0)
        last = (l == n_layers - 1)
        if allp or 'A' in phases:
            phase_A(P, l, D, first)
        if allp or 'B' in phases:
            phase_B(P, l, D)
        if allp or 'C' in phases:
            phase_C(P, l, D)
        if allp or 'D1' in phases:
            phase_D1(P, l, D)
        if allp or 'D2' in phases:
            phase_D2(P, l, D)
        if allp or 'E' in phases:
            phase_E(P, l, D, first)
        if allp or 'F' in phases:
            phase_F(P, l, D)
        if allp or 'G' in phases:
            phase_G(P, l, D, last and allp)
    return P


def kernel(**inputs):
    n = 8
    P = build(4)
    consts = make_consts()
    shared = {k: np.ascontiguousarray(np.asarray(inputs[k], dtype=np.float32)) for k, _ in WEIGHTS}
    shared.update(consts)
    x = np.asarray(inputs['x'], dtype=np.float32)
    p = np.asarray(inputs['p'], dtype=np.float32)
    pos = np.asarray(inputs['positions']).astype(np.int32)
    in_maps = []
    for c in range(n):
        m = dict(shared)
        m['x'] = np.ascontiguousarray(x[c])
        m['p'] = np.ascontiguousarray(p[:, c])
        m['positions'] = np.ascontiguousarray(pos[c].reshape(NT, 128).T)
        in_maps.append(m)
    res = run_bass_kernel_spmd(P.nc, in_maps, core_ids=list(range(n)))
    return np.stack([np.asarray(res.results[c]['out'], dtype=np.float32) for c in range(n)], axis=0)
```

```python
import numpy as np
from contextlib import ExitStack
import concourse.bass as bass
import concourse.mybir as mybir
from concourse.bass_utils import run_bass_kernel_spmd

F32 = mybir.dt.float32
BF16 = mybir.dt.bfloat16
I32 = mybir.dt.int32
ALU = mybir.AluOpType
AF = mybir.ActivationFunctionType
AX = mybir.AxisListType

ENGS = ['pe', 'dve', 'act', 'pool', 'sp']
DMA_ENGS = ['sp', 'pool', 'act']
NDS = 8
EPS = 1e-6
S = 4096
NT = 32


NOWAW = frozenset(['d_vn', 'd_qkT', 'd_vaug', 'd_gate', 'd_ab', 'd_uT', 'd_cT', 'd_vtm', 'd_ktm', 'd_qkTg', 'd_yT', 'd_ofb', 'd_hb', 'd_gb', 'd_cs'])


class Prog:
    def __init__(self):
        self.nc = bass.Bass("TRN2", target_bir_lowering=False)
        self.stack = ExitStack()
        self.sems = {}
        for e in ENGS:
            self.sems[e] = self.stack.enter_context(self.nc.semaphore('s_' + e))
        self.dcount = {}
        for e in DMA_ENGS:
            for i in range(NDS):
                k = 'd_%s_%d' % (e, i)
                self.sems[k] = self.stack.enter_context(self.nc.semaphore(k))
                self.dcount[k] = 0
        self.dnext = {e: 0 for e in DMA_ENGS}
        self.cnt = {e: 0 for e in ENGS}
        self.waited = {e: {} for e in ENGS}
        self.q = {e: [] for e in ENGS}
        self.lastw = {}
        self.readers = {}
        self.multiw = {}
        self.nops = 0
        self.xkeys = set(['pT', 'pv', 'pq', 'pk', 'pbv', 'pg', 'pf', 'ptr', 'pm', 'pss', 'ppv', 'pd', 'ptb', 'po', 'pbig', 'pl', 'pu', 'py', 'pe', 'pA', 'pB', 'pC', 'pD', 'pS'])

    def _deps(self, eng, reads, writes):
        deps = {}

        def add(m):
            if m is None:
                return
            k, v = m
            if eng == 'pe' and k == 'pe':
                return
            if deps.get(k, 0) < v:
                deps[k] = v
        for r in reads:
            add(self.lastw.get(r))
            for m in self.multiw.get(r, ()):
                add(m)
        for w in writes:
            if w in NOWAW:
                continue
            add(self.lastw.get(w))
            for m in self.readers.get(w, ()):
                add(m)
        out = []
        wd = self.waited[eng]
        for k, v in deps.items():
            if wd.get(k, 0) < v:
                wd[k] = v
                out.append((k, v))
        return out

    def _mark(self, mark, reads, writes):
        for w in writes:
            if w in NOWAW:
                self.multiw.setdefault(w, []).append(mark)
                continue
            self.lastw[w] = mark
            self.readers[w] = []
        for r in reads:
            if r in writes:
                continue
            self.readers.setdefault(r, []).append(mark)

    cut = None
    pc = 0

    def isx(self, k):
        n = k[0] if isinstance(k, tuple) else k
        return isinstance(n, str) and n in self.xkeys

    def op(self, eng, fn, reads=(), writes=()):
        self.pc += 1
        if self.cut is not None and self.pc > self.cut:
            return
        xr = [r for r in reads if self.isx(r) and r not in writes]
        if xr:
            writes = list(writes) + xr
        waits = self._deps(eng, reads, writes)
        self.cnt[eng] += 1
        mark = (eng, self.cnt[eng])
        self.q[eng].append((waits, fn, (eng, 1)))
        self._mark(mark, reads, writes)
        self.nops += 1

    def dma(self, eng, out, in_, reads=(), writes=(), **kw):
        self.pc += 1
        if self.cut is not None and self.pc > self.cut:
            return
        waits = self._deps(eng, reads, writes)
        i = self.dnext[eng]
        self.dnext[eng] = (i + 1) % NDS
        k = 'd_%s_%d' % (eng, i)
        c = self.dcount[k]
        wd = self.waited[eng]
        if c > 0 and wd.get(k, 0) < 16 * c:
            wd[k] = 16 * c
            waits.append((k, 16 * c))
        self.dcount[k] = c + 1
        mark = (k, 16 * (c + 1))
        self.q[eng].append((waits, (lambda e: e.dma_start(out=out, in_=in_, **kw)), (k, 16)))
        self._mark(mark, reads, writes)
        self.nops += 1

    _breg = None

    def breg(self, e):
        if self._breg is None:
            self._breg = e.to_reg(S - 1)
        return self._breg

    def idma(self, fn, reads=(), writes=()):
        eng = 'pool'
        self.pc += 1
        waits = self._deps(eng, reads, writes)
        i = self.dnext[eng]
        self.dnext[eng] = (i + 1) % NDS
        k = 'd_%s_%d' % (eng, i)
        c = self.dcount[k]
        wd = self.waited[eng]
        if c > 0 and wd.get(k, 0) < 16 * c:
            wd[k] = 16 * c
            waits.append((k, 16 * c))
        self.dcount[k] = c + 1
        mark = (k, 16 * (c + 1))
        self.q[eng].append((waits, fn, (k, 16)))
        self._mark(mark, reads, writes)
        self.nops += 1

    def barrier(self):
        for e in ENGS:
            waits = []
            wd = self.waited[e]
            for o in ENGS:
                if o != e and self.cnt[o] > wd.get(o, 0):
                    wd[o] = self.cnt[o]
                    waits.append((o, self.cnt[o]))
            for k, c in self.dcount.items():
                if 16 * c > wd.get(k, 0):
                    wd[k] = 16 * c
                    waits.append((k, 16 * c))
            if waits:
                self.q[e].append((waits, None, None))
        self.lastw = {}
        self.readers = {}
        self.multiw = {}

    def emit(self):
        self.barrier()
        nc = self.nc
        sems = self.sems
        q = self.q

        def replay(name, e):
            for waits, fn, inc in q[name]:
                for k, v in waits:
                    e.wait_ge(sems[k], v)
                if fn is not None:
                    ins = fn(e)
                    ins.then_inc(sems[inc[0]], inc[1])

        with nc.Block() as block:
            @block.tensor
            def _(e):
                replay('pe', e)

            @block.vector
            def _(e):
                replay('dve', e)

            @block.scalar
            def _(e):
                replay('act', e)

            @block.gpsimd
            def _(e):
                replay('pool', e)

            @block.sync
            def _(e):
                replay('sp', e)
        self.q = {e: [] for e in ENGS}

    uid = 0

    def sb(self, stack, name, shape, dt):
        self.uid += 1
        return stack.enter_context(self.nc.sbuf_tensor('%s_%d' % (name, self.uid), list(shape), dt))

    def ps(self, stack, name, shape, dt=F32, keys=()):
        for k in keys:
            self.xkeys.add(k)
        self.uid += 1
        return stack.enter_context(self.nc.psum_tensor('%s_%d' % (name, self.uid), list(shape), dt))

    def dram(self, name, shape, dt, kind="Internal"):
        return self.nc.dram_tensor(name, list(shape), dt, kind=kind).ap()


def ssl(a, n, d):
    return slice(a, a + (n - 1) * d + 1, d)


def bc(ap, shape):
    return ap.to_broadcast(list(shape))


def phase_rope(P, D):
    with ExitStack() as s:
        pi_ = P.sb(s, 'r_pi', [128, NT], I32)
        pf = P.sb(s, 'r_pf', [128, NT], F32)
        invf = P.sb(s, 'r_invf', [128, 8], F32)
        ang = P.sb(s, 'r_ang', [128, 2, NT, 8], F32)
        kk = P.sb(s, 'r_kk', [128, 2, NT, 8], F32)
        ki = P.sb(s, 'r_ki', [128, 2, NT, 8], I32)
        cs = P.sb(s, 'r_cs', [128, NT, 16], F32)
        P.dma('sp', pi_[:], D['positions'], writes=['pi'])
        P.dma('sp', invf[:], D['invf'].partition_broadcast(128), writes=['invf'])
        P.op('dve', lambda e: e.tensor_copy(pf[:], pi_[:]), reads=['pi'], writes=['pf'])
        P.op('dve', lambda e: e.tensor_tensor(ang[:, 1], bc(pf[:].unsqueeze(2), [128, NT, 8]), bc(invf[:].unsqueeze(1), [128, NT, 8]), ALU.mult),
             reads=['pf', 'invf'], writes=['ang1'])
        P.op('dve', lambda e: e.tensor_scalar(ang[:, 0], ang[:, 1], float(np.pi / 2), None, ALU.add), reads=['ang1'], writes=['ang0'])
        A = ang[:].rearrange('p a t c -> p (a t c)')
        K = kk[:].rearrange('p a t c -> p (a t c)')
        KI = ki[:].rearrange('p a t c -> p (a t c)')
        P.op('dve', lambda e: e.tensor_scalar(K, A, float(1.0 / (2 * np.pi)), None, ALU.mult), reads=['ang0', 'ang1'], writes=['kk'])
        P.op('dve', lambda e: e.tensor_copy(KI, K), reads=['kk'], writes=['ki'])
        P.op('dve', lambda e: e.tensor_copy(K, KI), reads=['ki'], writes=['kk'])
        C1 = 6.28125
        C2 = float(2 * np.pi - 6.28125)
        P.op('dve', lambda e: e.scalar_tensor_tensor(A, K, -C1, A, ALU.mult, ALU.add), reads=['kk', 'ang0', 'ang1'], writes=['ang'])
        P.op('dve', lambda e: e.scalar_tensor_tensor(A, K, -C2, A, ALU.mult, ALU.add), reads=['kk', 'ang'], writes=['ang'])
        P.op('dve', lambda e: e.tensor_scalar(A, A, 3.1415925, -3.1415925, ALU.min, ALU.max), reads=['ang'], writes=['ang'])
        P.op('act', lambda e: e.activation(cs[:, :, 0:8], ang[:, 0], AF.Sin), reads=['ang'], writes=['cs0'])
        P.op('act', lambda e: e.activation(cs[:, :, 8:16], ang[:, 1], AF.Sin), reads=['ang'], writes=['cs1'])
        P.dma('sp', D['cs'].rearrange('(t p) c -> p t c', p=128), cs[:], reads=['cs0', 'cs1'], writes=['d_cs'])
        P.emit()


def phase_A(P, l, D, first):
    xsrc = D['x'] if first else D['xw']
    with ExitStack() as s:
        wbf = P.sb(s, 'a_wbf', [128, 8, 3224], BF16)
        gmix = P.sb(s, 'a_gmix', [128, 1024], F32)
        lng = P.sb(s, 'a_lng', [128, 256], F32)
        lnb = P.sb(s, 'a_lnb', [128, 256], F32)
        qkg = P.sb(s, 'a_qkg', [128, 2, 64], F32)
        cs = P.sb(s, 'a_cs', [128, NT, 16], F32)
        idb = P.sb(s, 'a_idb', [128, 128], BF16)
        xts = [P.sb(s, 'a_xt%d' % i, [128, 1024], F32) for i in range(2)]
        junk = P.sb(s, 'a_junk', [128, 1024], BF16)
        ss = P.sb(s, 'a_ss', [128, 2], F32)
        xn = P.sb(s, 'a_xn', [128, 1024], BF16)
        xnT = [P.sb(s, 'a_xnT%d' % i, [128, 8, 512], BF16) for i in range(2)]
        ge = P.sb(s, 'a_ge', [128, 4, 64], F32)
        cen = P.sb(s, 'a_cen', [128, 4, 64], F32)
        sq = P.sb(s, 'a_sq', [128, 4, 64], F32)
        m4 = P.sb(s, 'a_m4', [128, 8], F32)
        vnb = [P.sb(s, 'a_vnb%d' % i, [128, 256], BF16) for i in range(2)]
        sqq = P.sb(s, 'a_sqq', [128, 12, 64], F32)
        ss12 = P.sb(s, 'a_ss12', [128, 12], F32)
        qk32 = P.sb(s, 'a_qk32', [128, 12, 64], F32)
        rt = P.sb(s, 'a_rt', [128, 4, 12, 8], F32)
        qkb = P.sb(s, 'a_qkb', [128, 12, 64], BF16)
        qkTs = [P.sb(s, 'a_qkTs%d' % i, [128, 6, 128], BF16) for i in range(2)]
        vaug = [P.sb(s, 'a_vaug%d' % i, [128, 6, 65], BF16) for i in range(2)]
        gs = [P.sb(s, 'a_gs%d' % i, [128, 408], F32) for i in range(2)]
        fo = [P.sb(s, 'a_fo%d' % i, [128, 512], F32) for i in range(2)]
        pT = P.ps(s, 'a_pT', [128, 1024], BF16)
        pv = P.ps(s, 'a_pv', [128, 512])
        pq = P.ps(s, 'a_pq', [128, 512])
        pk = P.ps(s, 'a_pk', [128, 512])
        pbv = P.ps(s, 'a_pbv', [128, 512])
        pg = P.ps(s, 'a_pg', [128, 512])
        pf = [P.ps(s, 'a_pf%d' % i, [128, 512]) for i in range(2)]

        for k in range(8):
            P.dma('pool', wbf[:, k, :], D['w_in'][l, k * 128:(k + 1) * 128, :], writes=[('wbf', k)])
        P.dma('sp', gmix[:], D['g_mix'][l].partition_broadcast(128), writes=['gmix'])
        P.dma('sp', lng[:], D['ln_v_g'][l].rearrange('g d -> (g d)').partition_broadcast(128), writes=['lng'])
        P.dma('sp', lnb[:], D['ln_v_b'][l].rearrange('g d -> (g d)').partition_broadcast(128), writes=['lnb'])
        P.dma('sp', qkg[:, 0, :], D['q_norm_g'][l].partition_broadcast(128), writes=['qkg0'])
        P.dma('sp', qkg[:, 1, :], D['k_norm_g'][l].partition_broadcast(128), writes=['qkg1'])
        P.dma('sp', cs[:], D['cs'].rearrange('(t p) c -> p t c', p=128), reads=['d_cs'], writes=['cs'])
        P.dma('pool', idb[:], D['ident'], writes=['idb'])
        for i in range(2):
            P.op('pool', lambda e, i=i: e.memset(vaug[i][:, :, 64:65], 1.0), writes=[('vaug', i)])
        WB = [('wbf', k) for k in range(8)]
        fcount = [0]

        def front(t):
            if True:
                g, j = t // 4, t % 4
                XT = xnT[g % 2]
                kxt = ('xnT', g % 2)
                b = t % 2
                xt = xts[b]
                P.dma('sp', xt[:], xsrc[t * 128:(t + 1) * 128, :], writes=[('xt', b)])
                P.op('dve', lambda e, b=b: e.memset(ss[:, b:b + 1], 0.0), writes=[('ss', b)])
                P.op('act', lambda e, xt=xt, b=b: e.activation(junk[:], xt[:], AF.Square, accum_out=ss[:, b:b + 1]),
                     reads=[('xt', b), ('ss', b)], writes=['junk', ('ss', b)])
                P.op('act', lambda e, b=b: e.activation(ss[:, b:b + 1], ss[:, b:b + 1], AF.Sqrt, bias=EPS, scale=1.0 / 1024),
                     reads=[('ss', b)], writes=[('ss', b)])
                P.op('dve', lambda e, b=b: e.reciprocal(ss[:, b:b + 1], ss[:, b:b + 1]), reads=[('ss', b)], writes=[('ss', b)])
                P.op('dve', lambda e, xt=xt, b=b: e.scalar_tensor_tensor(xn[:], xt[:], ss[:, b:b + 1], gmix[:], ALU.mult, ALU.mult),
                     reads=[('xt', b), ('ss', b), 'gmix'], writes=['xn'])
                for k in range(8):
                    P.op('pe', lambda e, k=k: e.transpose(pT[:, k * 128:(k + 1) * 128], xn[:, k * 128:(k + 1) * 128], idb[:]),
                         reads=['xn', 'idb'], writes=['pT'])
                P.op('act', lambda e, XT=XT, j=j: e.copy(XT[:, :, j * 128:(j + 1) * 128], pT[:].rearrange('p (k t) -> p k t', t=128)),
                     reads=['pT'], writes=[kxt + (j,)])

        def back(t):
            if True:
                g, j = t // 4, t % 4
                XT = xnT[g % 2]
                kxt = ('xnT', g % 2)
                b = t % 2
                for (pp, nm, c0, c1) in ((pv, 'pv', 256, 512), (pq, 'pq', 512, 896), (pk, 'pk', 896, 1280), (pbv, 'pbv', 1280, 1664), (pg, 'pg', 2816, 3224)):
                    for k in range(8):
                        P.op('pe', lambda e, pp=pp, k=k, c0=c0, c1=c1, XT=XT, j=j: e.matmul(pp[:, 0:c1 - c0], XT[:, k, j * 128:(j + 1) * 128], wbf[:, k, c0:c1], start=(k == 0), stop=(k == 7)),
                             reads=[kxt + (j,), ('wbf', k)], writes=[nm])
                GE = ge[:].rearrange('p a b -> p (a b)')
                P.op('act', lambda e: e.activation(GE, pv[:, 0:256], AF.Gelu_apprx_tanh), reads=['pv'], writes=['ge'])
                P.op('dve', lambda e: e.tensor_reduce(m4[:, 0:4], ge[:], AX.X, ALU.add), reads=['ge'], writes=['m4a'])
                P.op('dve', lambda e: e.tensor_scalar(m4[:, 0:4], m4[:, 0:4], 1.0 / 64, None, ALU.mult), reads=['m4a'], writes=['m4a'])
                P.op('dve', lambda e: e.tensor_tensor(cen[:], ge[:], bc(m4[:, 0:4].unsqueeze(2), [128, 4, 64]), ALU.subtract), reads=['ge', 'm4a'], writes=['cen'])
                P.op('act', lambda e: e.activation(sq[:], cen[:], AF.Square), reads=['cen'], writes=['sq'])
                P.op('dve', lambda e: e.tensor_reduce(m4[:, 4:8], sq[:], AX.X, ALU.add), reads=['sq'], writes=['m4b'])
                P.op('act', lambda e: e.activation(m4[:, 4:8], m4[:, 4:8], AF.Sqrt, bias=EPS, scale=1.0 / 64), reads=['m4b'], writes=['m4b'])
                P.op('dve', lambda e: e.reciprocal(m4[:, 4:8], m4[:, 4:8]), reads=['m4b'], writes=['m4b'])
                P.op('dve', lambda e: e.tensor_tensor(cen[:], cen[:], bc(m4[:, 4:8].unsqueeze(2), [128, 4, 64]), ALU.mult), reads=['cen', 'm4b'], writes=['cen'])
                CEN = cen[:].rearrange('p a b -> p (a b)')
                P.op('pool', lambda e: e.tensor_tensor(CEN, CEN, lng[:], ALU.mult), reads=['cen', 'lng'], writes=['cen'])
                P.op('pool', lambda e, b=b: e.tensor_tensor(vnb[b][:], CEN, lnb[:], ALU.add), reads=['cen', 'lnb'], writes=[('vnb', b)])
                P.dma('sp', D['vn'][t * 128:(t + 1) * 128, :], vnb[b][:], reads=[('vnb', b)], writes=['d_vn'])
                P.op('act', lambda e: e.activation(sqq[:, 0:6, :].rearrange('p a b -> p (a b)'), pq[:, 0:384], AF.Square), reads=['pq'], writes=['sqq0'])
                P.op('act', lambda e: e.activation(sqq[:, 6:12, :].rearrange('p a b -> p (a b)'), pk[:, 0:384], AF.Square), reads=['pk'], writes=['sqq1'])
                P.op('dve', lambda e: e.tensor_reduce(ss12[:], sqq[:], AX.X, ALU.add), reads=['sqq0', 'sqq1'], writes=['ss12'])
                P.op('act', lambda e: e.activation(ss12[:], ss12[:], AF.Sqrt, bias=EPS, scale=1.0 / 64), reads=['ss12'], writes=['ss12'])
                P.op('dve', lambda e: e.reciprocal(ss12[:], ss12[:]), reads=['ss12'], writes=['ss12'])
                P.op('dve', lambda e: e.tensor_tensor(qk32[:, 0:6, :], pq[:, 0:384].rearrange('p (a b) -> p a b', b=64), bc(ss12[:, 0:6].unsqueeze(2), [128, 6, 64]), ALU.mult),
                     reads=['pq', 'ss12'], writes=['qk32a'])
                P.op('dve', lambda e: e.tensor_tensor(qk32[:, 6:12, :], pk[:, 0:384].rearrange('p (a b) -> p a b', b=64), bc(ss12[:, 6:12].unsqueeze(2), [128, 6, 64]), ALU.mult),
                     reads=['pk', 'ss12'], writes=['qk32b'])
                P.op('pool', lambda e: e.tensor_tensor(qk32[:, 0:6, :], qk32[:, 0:6, :], bc(qkg[:, 0:1, :], [128, 6, 64]), ALU.mult), reads=['qk32a', 'qkg0'], writes=['qk32a'])
                P.op('pool', lambda e: e.tensor_tensor(qk32[:, 6:12, :], qk32[:, 6:12, :], bc(qkg[:, 1:2, :], [128, 6, 64]), ALU.mult), reads=['qk32b', 'qkg1'], writes=['qk32b'])
                cosb = bc(cs[:, t:t + 1, 0:8], [128, 12, 8])
                sinb = bc(cs[:, t:t + 1, 8:16], [128, 12, 8])
                x1 = qk32[:, :, 0:8]
                x2 = qk32[:, :, 8:16]
                P.op('pool', lambda e, cosb=cosb: e.tensor_tensor(rt[:, 0], x1, cosb, ALU.mult), reads=['qk32a', 'qk32b', 'cs'], writes=['rt0'])
                P.op('pool', lambda e, sinb=sinb: e.tensor_tensor(rt[:, 1], x2, sinb, ALU.mult), reads=['qk32a', 'qk32b', 'cs'], writes=['rt1'])
                P.op('dve', lambda e, cosb=cosb: e.tensor_tensor(rt[:, 2], x2, cosb, ALU.mult), reads=['qk32a', 'qk32b', 'cs'], writes=['rt2'])
                P.op('dve', lambda e, sinb=sinb: e.tensor_tensor(rt[:, 3], x1, sinb, ALU.mult), reads=['qk32a', 'qk32b', 'cs'], writes=['rt3'])
                P.op('act', lambda e: e.copy(qkb[:], qk32[:]), reads=['qk32a', 'qk32b'], writes=['qkb'])
                P.op('dve', lambda e: e.tensor_tensor(qkb[:, :, 0:8], rt[:, 0], rt[:, 1], ALU.subtract), reads=['rt0', 'rt1', 'qkb'], writes=['qkb'])
                P.op('dve', lambda e: e.tensor_tensor(qkb[:, :, 8:16], rt[:, 2], rt[:, 3], ALU.add), reads=['rt2', 'rt3', 'qkb'], writes=['qkb'])
                for i in range(6):
                    P.op('pe', lambda e, i=i: e.transpose(pT[:, i * 128:(i + 1) * 128], qkb[:, 2 * i:2 * i + 2, :].rearrange('p a b -> p (a b)'), idb[:]),
                         reads=['qkb', 'idb'], writes=['pT'])
                P.op('act', lambda e, b=b: e.copy(qkTs[b][:], pT[:, 0:768].rearrange('p (k t) -> p k t', t=128)), reads=['pT'], writes=[('qkTs', b)])
                P.dma('sp', D['qkT'][:, :, t * 128:(t + 1) * 128].rearrange('i p t -> p i t'), qkTs[b][:], reads=[('qkTs', b)], writes=['d_qkT'])
                P.op('act', lambda e, b=b: e.copy(vaug[b][:, :, 0:64], pbv[:, 0:384].rearrange('p (a b) -> p a b', b=64)), reads=['pbv', ('vaug', b)], writes=[('vaug', b)])
                P.dma('sp', D['vaug'][t * 128:(t + 1) * 128, :], vaug[b][:].rearrange('p a b -> p (a b)'), reads=[('vaug', b)], writes=['d_vaug'])
                P.op('act', lambda e, b=b: e.activation(gs[b][:, 0:384], pg[:, 0:384], AF.Silu), reads=['pg'], writes=[('gs', b)])
                P.op('dve', lambda e, b=b: e.tensor_copy(gs[b][:, 384:408], pg[:, 384:408]), reads=['pg', ('gs', b)], writes=[('gs', b)])
                P.dma('sp', D['gate_s'][t * 128:(t + 1) * 128, :], gs[b][:, 0:384], reads=[('gs', b)], writes=['d_gate'])
                P.dma('sp', D['ab'][t * 128:(t + 1) * 128, :], gs[b][:, 384:408], reads=[('gs', b)], writes=['d_ab'])

        def fmaj(g):
            XT = xnT[g % 2]
            kxt = ('xnT', g % 2)
            allx = [kxt + (j,) for j in range(4)]
            for ci in range(11):
                c0 = ci * 128 if ci < 2 else 1664 + (ci - 2) * 128
                fb = fcount[0] % 2
                fcount[0] += 1
                for k in range(8):
                    P.op('pe', lambda e, fb=fb, k=k, c0=c0, XT=XT: e.matmul(pf[fb][:], wbf[:, k, c0:c0 + 128], XT[:, k, :], start=(k == 0), stop=(k == 7)),
                         reads=allx + [('wbf', k)], writes=[('pf', fb)])
                if ci < 2:
                    P.op('act', lambda e, fb=fb: e.activation(fo[fb][:], pf[fb][:], AF.Gelu_apprx_tanh), reads=[('pf', fb)], writes=[('fo', fb)])
                    P.dma('sp', D['uT'][ci * 128:(ci + 1) * 128, g * 512:(g + 1) * 512], fo[fb][:], reads=[('fo', fb)], writes=['d_uT'])
                else:
                    P.op('dve', lambda e, fb=fb: e.tensor_copy(fo[fb][:], pf[fb][:]), reads=[('pf', fb)], writes=[('fo', fb)])
                    P.dma('sp', D['cT'][(ci - 2) * 128:(ci - 1) * 128, g * 512:(g + 1) * 512], fo[fb][:], reads=[('fo', fb)], writes=['d_cT'])

        front(0)
        for t in range(NT):
            if t + 1 < NT:
                front(t + 1)
            back(t)
            if t % 4 == 3:
                fmaj(t // 4)
        P.emit()


def phase_B(P, l, D):
    with ExitStack() as s:
        ws32 = P.sb(s, 'b_ws32', [128, 4, 128], F32)
        idf = P.sb(s, 'b_idf', [128, 128], F32)
        wsT = P.sb(s, 'b_wsT', [128, 4, 128], BF16)
        bias = P.sb(s, 'b_bias', [64, 4, 128], F32)
        vn = [P.sb(s, 'b_vn%d' % i, [128, 4, 256], BF16) for i in range(2)]
        ut = [P.sb(s, 'b_ut%d' % i, [64, 4, 512], F32) for i in range(2)]
        mx = P.sb(s, 'b_mx', [64, 4, 128], F32)
        yb = [P.sb(s, 'b_yb%d' % i, [64, 4, 512], BF16) for i in range(2)]
        ptr = P.ps(s, 'b_ptr', [128, 512])
        pm = [P.ps(s, 'b_pm%d' % i, [64, 512]) for i in range(4)]
        P.dma('sp', ws32[:], D['w_s'][l].rearrange('g i j -> i g j'), writes=['ws32'])
        P.dma('sp', idf[:], D['ident'], writes=['idf'])
        P.dma('sp', bias[:].rearrange('p g i -> p (g i)'), D['b_s'][l].rearrange('g i -> (g i)').partition_broadcast(64), writes=['bias'])
        for g in range(4):
            P.op('pe', lambda e, g=g: e.transpose(ptr[:, g * 128:(g + 1) * 128], ws32[:, g, :], idf[:]), reads=['ws32', 'idf'], writes=['ptr'])
        P.op('dve', lambda e: e.tensor_copy(wsT[:].rearrange('p g i -> p (g i)'), ptr[:]), reads=['ptr'], writes=['wsT'])
        for it in range(8):
            b = it % 2
            P.dma('sp', vn[b][:], D['vn'][it * 512:(it + 1) * 512, :].rearrange('(c p) n -> p c n', p=128), reads=['d_vn'], writes=[('vn', b)])
            P.dma('sp', ut[b][:], D['uT'][:, it * 512:(it + 1) * 512].rearrange('(g d) t -> d g t', d=64), reads=['d_uT'], writes=[('ut', b)])
            for g in range(4):
                for c in range(4):
                    P.op('pe', lambda e, g=g, c=c, b=b: e.matmul(pm[g][:, c * 128:(c + 1) * 128], vn[b][:, c, g * 64:(g + 1) * 64], wsT[:, g, :], start=True, stop=True),
                         reads=[('vn', b), 'wsT'], writes=[('pm', g)])
                P.op('dve', lambda e, g=g: e.tensor_tensor(mx[:], pm[g][:].rearrange('p (c i) -> p c i', i=128), bc(bias[:, g:g + 1, :], [64, 4, 128]), ALU.add),
                     reads=[('pm', g), 'bias'], writes=['mx'])
                P.op('dve', lambda e, g=g, b=b: e.tensor_tensor(yb[b][:, g, :], mx[:].rearrange('p c i -> p (c i)'), ut[b][:, g, :], ALU.mult),
                     reads=['mx', ('ut', b)], writes=[('yb', b)])
            P.dma('sp', D['yT'][0:256, it * 512:(it + 1) * 512].rearrange('(g d) t -> d g t', d=64), yb[b][:], reads=[('yb', b)], writes=['d_yT'])
        P.emit()


PATS = (1, 4, 16)
KPAD = 1024


def phase_C(P, l, D):
    with ExitStack() as s:
        vs = {}
        for d in PATS:
            nt = d * (S // d // 128 + 1)
            vs[d] = P.sb(s, 'c_vs%d' % d, [128, nt, 390], BF16)
        mab = P.sb(s, 'c_mab', [128, 512], BF16)
        sel = P.sb(s, 'c_sel', [65, 64], F32)
        qh = [P.sb(s, 'c_qh%d' % i, [64, S], BF16) for i in range(2)]
        kh = [P.sb(s, 'c_kh%d' % i, [64, S + 2 * KPAD], BF16) for i in range(2)]
        pex = [P.sb(s, 'c_pex%d' % i, [128, 512], BF16) for i in range(3)]
        acc = P.sb(s, 'c_acc', [65, S], F32)
        rd = P.sb(s, 'c_rd', [64, 512], F32)
        yb = [P.sb(s, 'c_yb%d' % i, [64, 512], BF16) for i in range(2)]
        pss = [P.ps(s, 'c_ps%d' % i, [128, 512]) for i in range(3)]
        ppv = [P.ps(s, 'c_pv%d' % i, [65, 512]) for i in range(3)]
        pd = P.ps(s, 'c_pd', [64, 512])
        P.dma('pool', mab[:], D['mab'], writes=['mab'])
        P.dma('sp', sel[:], D['sel65'], writes=['sel'])
        for i in range(2):
            P.op('pool', lambda e, i=i: e.memset(kh[i][:, 0:KPAD], 0.0), writes=[('kh', i)])
            P.op('pool', lambda e, i=i: e.memset(kh[i][:, KPAD + S:], 0.0), writes=[('kh', i)])
        for d in PATS:
            L = S // d
            nqb = L // 128
            P.op('pool', lambda e, d=d: e.memset(vs[d][:].rearrange('p a b -> p (a b)'), 0.0), writes=[('vs', d)])
            vsrc = D['vaug'].rearrange('(j r) c -> r j c', r=d)
            for r in range(d):
                tb = r * (nqb + 1)
                if nqb > 1:
                    P.dma('sp', vs[d][:, tb + 1:tb + nqb, :], vsrc[r, 64:64 + (nqb - 1) * 128, :].rearrange('(k p) c -> p k c', p=128),
                          reads=['d_vaug', ('vs', d)], writes=[('vs', d)])
                P.dma('sp', vs[d][64:128, tb, :], vsrc[r, 0:64, :], reads=['d_vaug', ('vs', d)], writes=[('vs', d)])
                P.dma('sp', vs[d][0:64, tb + nqb, :], vsrc[r, L - 64:L, :], reads=['d_vaug', ('vs', d)], writes=[('vs', d)])
        for h in range(6):
            hb = h % 2
            P.dma('sp', qh[hb][:], D['qkT'][h // 2, (h % 2) * 64:(h % 2) * 64 + 64, :], reads=['d_qkT'], writes=[('qh', hb)])
            P.dma('sp', kh[hb][:, KPAD:KPAD + S], D['qkT'][3 + h // 2, (h % 2) * 64:(h % 2) * 64 + 64, :], reads=['d_qkT'], writes=[('kh', hb)])
            its = []
            for pi, d in enumerate(PATS):
                L = S // d
                nqb = L // 128
                for r in range(d):
                    tb = r * (nqb + 1)
                    for qb0 in range(0, nqb, 2):
                        its.append((pi, d, r, tb, qb0))

            def stage1(n):
                pi, d, r, tb, qb0 = its[n]
                ib = n % 3
                combos = ((qb0, qb0), (qb0 + 1, qb0), (qb0 + 1, qb0 + 1), (qb0 + 2, qb0 + 1))
                for ci, (kt, qb) in enumerate(combos):
                    k0 = KPAD + r + d * (kt * 128 - 64)
                    q0 = r + d * (qb * 128)
                    P.op('pe', lambda e, ib=ib, ci=ci, k0=k0, q0=q0, d=d, hb=hb: e.matmul(
                        pss[ib][:, ci * 128:(ci + 1) * 128], kh[hb][:, ssl(k0, 128, d)], qh[hb][:, ssl(q0, 128, d)], start=True, stop=True),
                        reads=[('kh', hb), ('qh', hb)], writes=[('pss', ib)])
                P.op('act', lambda e, ib=ib: e.activation(pex[ib][:], pss[ib][:], AF.Exp, scale=0.125), reads=[('pss', ib)], writes=[('pex', ib)])
                P.op('dve', lambda e, ib=ib: e.tensor_tensor(pex[ib][:], pex[ib][:], mab[:], ALU.mult), reads=[('pex', ib), 'mab'], writes=[('pex', ib)])

            def stage2(n):
                pi, d, r, tb, qb0 = its[n]
                ib = n % 3
                combos = ((qb0, qb0), (qb0 + 1, qb0), (qb0 + 1, qb0 + 1), (qb0 + 2, qb0 + 1))
                for ci, (kt, qb) in enumerate(combos):
                    qi = qb - qb0
                    P.op('pe', lambda e, ib=ib, ci=ci, kt=kt, qi=qi, d=d, tb=tb, h=h: e.matmul(
                        ppv[ib][:, qi * 128:(qi + 1) * 128], vs[d][:, tb + kt, h * 65:(h + 1) * 65], pex[ib][:, ci * 128:(ci + 1) * 128],
                        start=(ci % 2 == 0), stop=(ci % 2 == 1)), reads=[('vs', d), ('pex', ib)], writes=[('ppv', ib)])
                a0 = r + d * (qb0 * 128)
                av = acc[:, ssl(a0, 256, d)]
                if pi == 0:
                    P.op('dve', lambda e, av=av, ib=ib: e.tensor_copy(av, ppv[ib][:, 0:256]), reads=[('ppv', ib)], writes=['acc'])
                else:
                    P.op('dve', lambda e, av=av, ib=ib: e.tensor_tensor(av, av, ppv[ib][:, 0:256], ALU.add), reads=[('ppv', ib), 'acc'], writes=['acc'])

            stage1(0)
            for n in range(len(its)):
                if n + 1 < len(its):
                    stage1(n + 1)
                stage2(n)
            for c4 in range(8):
                yb_ = yb[c4 % 2]
                P.op('pe', lambda e, c4=c4: e.matmul(pd[:], sel[:], acc[:, c4 * 512:(c4 + 1) * 512], start=True, stop=True), reads=['sel', 'acc'], writes=['pd'])
                P.op('dve', lambda e: e.reciprocal(rd[:], pd[:]), reads=['pd'], writes=['rd'])
                P.op('dve', lambda e, c4=c4, yb_=yb_: e.tensor_tensor(yb_[:], acc[0:64, c4 * 512:(c4 + 1) * 512], rd[:], ALU.mult), reads=['acc', 'rd'], writes=[('yb', c4 % 2)])
                P.dma('sp', D['yT'][256 + h * 64:256 + (h + 1) * 64, c4 * 512:(c4 + 1) * 512], yb_[:], reads=[('yb', c4 % 2)], writes=['d_yT'])
        P.emit()


def phase_D1(P, l, D):
    with ExitStack() as s:
        cw = P.sb(s, 'd_cw', [128, 5, 9], F32)
        idf = P.sb(s, 'd_idf', [128, 128], F32)
        raw = [P.sb(s, 'd_raw%d' % i, [128, S + 4], F32) for i in range(2)]
        cv = P.sb(s, 'd_cv', [128, S], F32)
        tm = [P.sb(s, 'd_tm%d' % i, [128, 4, 128], F32) for i in range(2)]
        sq = P.sb(s, 'd_sq', [128, 8, 64], F32)
        r8 = P.sb(s, 'd_r8', [128, 8], F32)
        fT = [P.sb(s, 'd_fT%d' % i, [128, 512], BF16) for i in range(2)]
        abt = P.sb(s, 'd_abt', [128, NT, 24], F32)
        gbt = P.sb(s, 'd_gbt', [128, NT, 24], F32)
        dtb = P.sb(s, 'd_dtb', [128, 12], F32)
        nA = P.sb(s, 'd_nA', [128, 12], F32)
        ptr = [P.ps(s, 'd_ptr%d' % i, [128, 512]) for i in range(2)]
        ptb = [P.ps(s, 'd_ptb%d' % i, [128, 512]) for i in range(2)]
        for k in range(5):
            P.dma('sp', cw[:, k, :], D['conv_w'][l, k].rearrange('(c p) -> p c', p=128), writes=['cw'], allow_slow_non_contiguous=True)
        P.dma('sp', idf[:], D['ident'], writes=['idf'])
        for i in range(2):
            P.op('pool', lambda e, i=i: e.memset(raw[i][:, 0:2], 0.0), writes=[('raw', i)])
            P.op('pool', lambda e, i=i: e.memset(raw[i][:, S + 2:S + 4], 0.0), writes=[('raw', i)])
        P.dma('sp', abt[:], D['ab'].rearrange('(t p) c -> p t c', p=128), reads=['d_ab'], writes=['abt'])
        P.dma('sp', dtb[:], D['dt_bias'][l].rearrange('a h -> (a h)').partition_broadcast(128), writes=['dtb'])
        P.dma('sp', nA[:], D['a_log'][l].rearrange('a h -> (a h)').partition_broadcast(128), writes=['nA'])
        P.op('act', lambda e: e.activation(nA[:], nA[:], AF.Exp), reads=['nA'], writes=['nA'])
        P.op('dve', lambda e: e.tensor_scalar(nA[:], nA[:], -1.0, None, ALU.mult), reads=['nA'], writes=['nA'])
        P.op('dve', lambda e: e.tensor_tensor(gbt[:, :, 0:12], abt[:, :, 0:12], bc(dtb[:].unsqueeze(1), [128, NT, 12]), ALU.add), reads=['abt', 'dtb'], writes=['gbt0'])
        P.op('act', lambda e: e.activation(gbt[:, :, 0:12], gbt[:, :, 0:12], AF.Exp), reads=['gbt0'], writes=['gbt0'])
        P.op('act', lambda e: e.activation(gbt[:, :, 0:12], gbt[:, :, 0:12], AF.Ln, bias=1.0), reads=['gbt0'], writes=['gbt0'])
        P.op('dve', lambda e: e.tensor_tensor(gbt[:, :, 0:12], gbt[:, :, 0:12], bc(nA[:].unsqueeze(1), [128, NT, 12]), ALU.mult), reads=['gbt0', 'nA'], writes=['gbt0'])
        P.op('act', lambda e: e.activation(gbt[:, :, 12:24], abt[:, :, 12:24], AF.Sigmoid), reads=['abt'], writes=['gbt1'])
        P.dma('sp', D['gb'].rearrange('(t p) c -> p t c', p=128), gbt[:], reads=['gbt0', 'gbt1'], writes=['d_gb'])
        n4 = 0
        import os
        CUT = int(os.environ.get('D1CUT', '99'))
        for c in range(int(os.environ.get('D1C0', '0')), int(os.environ.get('D1C1', '9'))):
            rb = c % 2
            R = raw[rb]
            P.dma('sp', R[:, 2:S + 2], D['cT'][c * 128:(c + 1) * 128, :], reads=['d_cT'], writes=[('raw', rb)])
            P.op('dve', lambda e, R=R, c=c: e.tensor_scalar(cv[:], R[:, 0:S], cw[:, 0, c:c + 1], None, ALU.mult), reads=[('raw', rb), 'cw'], writes=['cv'])
            for k in range(1, 5):
                eng = 'dve'
                P.op(eng, lambda e, R=R, c=c, k=k: e.scalar_tensor_tensor(cv[:], R[:, k:k + S], cw[:, k, c:c + 1], cv[:], ALU.mult, ALU.add),
                     reads=[('raw', rb), 'cw', 'cv'], writes=['cv'])
            P.op('act', lambda e: e.activation(cv[:], cv[:], AF.Silu), reads=['cv'], writes=['cv'])
            def stA(t4, c=c):
                pb = (c * 8 + t4) % 2
                for j in range(4):
                    t = t4 * 4 + j
                    P.op('pe', lambda e, pb=pb, j=j, t=t: e.transpose(ptr[pb][:, j * 128:(j + 1) * 128], cv[:, t * 128:(t + 1) * 128], idf[:]),
                         reads=['cv', 'idf'], writes=[('ptr', pb)])
                TM = tm[pb]
                PV = ptr[pb][:].rearrange('p (j h d) -> p (j h) d', h=2, d=64)
                if c >= 6:
                    P.op('act', lambda e, pb=pb, TM=TM: e.copy(TM[:].rearrange('p j c -> p (j c)'), ptr[pb][:]), reads=[('ptr', pb)], writes=[('tm', pb)])
                    P.dma('sp', D['v_tm'][t4 * 512:(t4 + 1) * 512, (c - 6) * 128:(c - 5) * 128].rearrange('(j p) c -> p j c', p=128), TM[:], reads=[('tm', pb)], writes=['d_vtm'])
                else:
                    P.op('act', lambda e, pb=pb: e.activation(sq[:].rearrange('p a b -> p (a b)'), ptr[pb][:], AF.Square), reads=[('ptr', pb)], writes=['sq'])
                    P.op('dve', lambda e: e.tensor_reduce(r8[:], sq[:], AX.X, ALU.add), reads=['sq'], writes=['r8'])
                    P.op('act', lambda e: e.activation(r8[:], r8[:], AF.Sqrt, bias=EPS, scale=1.0), reads=['r8'], writes=['r8'])
                    P.op('dve', lambda e: e.reciprocal(r8[:], r8[:]), reads=['r8'], writes=['r8'])
                    if c < 3:
                        P.op('dve', lambda e: e.tensor_scalar(r8[:], r8[:], 0.125, None, ALU.mult), reads=['r8'], writes=['r8'])
                    P.op('dve', lambda e, TM=TM, PV=PV: e.tensor_tensor(TM[:].rearrange('p j (h d) -> p (j h) d', d=64), PV, bc(r8[:].unsqueeze(2), [128, 8, 64]), ALU.mult),
                         reads=[('ptr', pb), 'r8'], writes=[('tm', pb)])
                    if c >= 3:
                        P.dma('sp', D['k_tm'][t4 * 512:(t4 + 1) * 512, (c - 3) * 128:(c - 2) * 128].rearrange('(j p) c -> p j c', p=128), TM[:], reads=[('tm', pb)], writes=['d_ktm'])

            def stB(t4, c=c):
                pb = (c * 8 + t4) % 2
                TM = tm[pb]
                if c < 6:
                    for j in range(4):
                        P.op('pe', lambda e, pb=pb, j=j, TM=TM: e.transpose(ptb[pb][:, j * 128:(j + 1) * 128], TM[:, j, :], idf[:]), reads=[('tm', pb), 'idf'], writes=[('ptb', pb)])
                    P.op('act', lambda e, pb=pb: e.copy(fT[pb][:], ptb[pb][:]), reads=[('ptb', pb)], writes=[('fT', pb)])
                    dst = D['qT_g'] if c < 3 else D['kT_g']
                    cc = c if c < 3 else c - 3
                    P.dma('sp', dst[cc * 128:(cc + 1) * 128, t4 * 512:(t4 + 1) * 512], fT[pb][:], reads=[('fT', pb)], writes=['d_qkTg'])

            stA(0)
            for t4 in range(8):
                if t4 + 1 < 8:
                    stA(t4 + 1)
                stB(t4)
        P.emit()


def phase_D2(P, l, D):
    import os
    C = 64
    NCH = S // C
    NST = int(os.environ.get('D2N', str(NCH)))
    MD = BF16 if os.environ.get('D2BF', '1') == '1' else F32
    with ExitStack() as s:
        def T12(name, dt=F32):
            return P.sb(s, 'e_' + name, [64, 12, 64], dt)
        ones = P.sb(s, 'e_ones', [64, 64], F32)
        idf = P.sb(s, 'e_idf', [64, 64], F32)
        idm = P.sb(s, 'e_idm', [64, 64], MD)
        idbc = T12('idbc')
        triF = P.sb(s, 'e_triF', [64, 64], F32)
        triB = P.sb(s, 'e_triB', [64, 64], F32)
        mW, mWt, mI = T12('mW'), T12('mWt'), T12('mI')
        St, St2, Sm = T12('S'), T12('S2'), T12('Sm', MD)
        ktm = [T12('ktm%d' % i) for i in range(2)]
        vtm = [T12('vtm%d' % i) for i in range(2)]
        kT = [T12('kT%d' % i, MD) for i in range(2)]
        qT = [T12('qT%d' % i, MD) for i in range(2)]
        gbv = [P.sb(s, 'e_gb%d' % i, [64, 24], F32) for i in range(2)]
        gc = P.sb(s, 'e_gc', [64, 12], F32)
        egc = [P.sb(s, 'e_egc%d' % i, [64, 12], F32) for i in range(2)]
        egl = [P.sb(s, 'e_egl%d' % i, [64, 12], F32) for i in range(2)]
        egd = P.sb(s, 'e_egd', [64, 12], F32)
        Dg = P.sb(s, 'e_Dg', [64, 24, 64], F32)
        diff, Ea, Eb = T12('diff'), T12('Ea'), T12('Eb')
        W, Wt = T12('W', MD), T12('Wt', MD)
        A1, A1t, A2, A2t = T12('A1', MD), T12('A1t', MD), T12('A2', MD), T12('A2t', MD)
        nxTI = T12('nxTI', MD)
        Yt = [T12('Yt0', MD), T12('Yt1', MD)]
        Yf = [T12('Yf0', MD), T12('Yf1', MD)]
        QKm = [T12('QKm0', MD), T12('QKm1', MD)]
        kd = [T12('kd0', MD), T12('kd1', MD)]
        Rr, Rm, vnew, o1 = T12('R'), T12('Rm', MD), T12('vnew', MD), T12('o1')
        ob = [T12('ob0'), T12('ob1')]
        pA = P.ps(s, 'e_pA', [64, 1024])
        pB = P.ps(s, 'e_pB', [64, 1024])
        pC = P.ps(s, 'e_pC', [64, 1024])
        pS = P.ps(s, 'e_pS', [64, 1024])

        def pv(p, h):
            return p[:, h * 512:h * 512 + 384].rearrange('p (j t) -> p j t', t=64)

        def sv(t, h):
            return t[:, h * 6:(h + 1) * 6, :]

        def pcol(p, j):
            c0 = (j // 6) * 512 + (j % 6) * 64
            return p[:, c0:c0 + 64]

        def mm12(pt, pn, lfn, rfn, rfun):
            for j in range(12):
                o_, l_, r_ = pcol(pt, j), lfn(j), rfn(j)
                P.op('pe', lambda e, o_=o_, l_=l_, r_=r_: e.matmul(o_, l_, r_, start=True, stop=True), reads=rfun(j // 6), writes=[(pn, j // 6)])

        def bcol(ap12, h):
            return bc(ap12[:, h * 6:(h + 1) * 6].unsqueeze(2), [64, 6, 64])

        P.dma('sp', ones[:], D['ones64'], writes=['ones'])
        P.dma('sp', idf[:], D['ident'][0:64, 0:64], writes=['idf'])
        P.dma('sp', triF[:], D['triF'], writes=['triF'])
        P.dma('sp', triB[:], D['triB'], writes=['triB'])
        P.dma('sp', mW[:], D['mW'], writes=['mW'])
        P.dma('sp', mWt[:], D['mWt'], writes=['mWt'])
        P.dma('sp', mI[:], D['mI'], writes=['mI'])
        P.op('dve', lambda e: e.tensor_copy(idbc[:], bc(idf[:].unsqueeze(1), [64, 12, 64])), reads=['idf'], writes=['idbc'])
        P.op('dve', lambda e: e.tensor_copy(idm[:], idf[:]), reads=['idf'], writes=['idm'])
        P.op('dve', lambda e: e.memset(St[:].rearrange('p a b -> p (a b)'), 0.0), writes=[('S', 0), ('S', 1)])
        P.op('pool', lambda e: e.memset(Sm[:].rearrange('p a b -> p (a b)'), 0.0), writes=[('Sm', 0), ('Sm', 1)])

        def prep(i):
            b = i % 2
            cf = i
            cb = NCH - 1 - i
            K_, V_, KT_, QT_, GB_ = ktm[b], vtm[b], kT[b], qT[b], gbv[b]
            EGC, EGL, QKM, KD = egc[b], egl[b], QKm[b], kd[b]
            for h, cc in ((0, cf), (1, cb)):
                sl = slice(h * 6, h * 6 + 6)
                tk = slice(cc * C, (cc + 1) * C)
                P.dma('sp', K_[:, sl, :], D['k_tm'][tk, :].rearrange('t (h d) -> t h d', d=64), reads=['d_ktm'], writes=[('ktm', b, h)])
                P.dma('sp', V_[:, sl, :], D['v_tm'][tk, :].rearrange('t (h d) -> t h d', d=64), reads=['d_vtm'], writes=[('vtm', b, h)])
                P.dma('sp', KT_[:, sl, :], D['kT_g'][:, tk].rearrange('(h d) t -> d h t', d=64), reads=['d_qkTg'], writes=[('kT', b, h)])
                P.dma('sp', QT_[:, sl, :], D['qT_g'][:, tk].rearrange('(h d) t -> d h t', d=64), reads=['d_qkTg'], writes=[('qT', b, h)])
                P.dma('sp', GB_[:, h * 6:h * 6 + 6], D['gb'][tk, h * 6:h * 6 + 6], reads=['d_gb'], writes=[('gb', b, h)])
                P.dma('sp', GB_[:, 12 + h * 6:12 + h * 6 + 6], D['gb'][tk, 12 + h * 6:12 + h * 6 + 6], reads=['d_gb'], writes=[('gbb', b, h)])
            beta = GB_[:, 12:24]
            DgF = Dg[:].rearrange('p a b -> p (a b)')
            for h in range(2):
                tri = triF if h == 0 else triB
                trin = 'triF' if h == 0 else 'triB'
                rG, rBt = ('gb', b, h), ('gbb', b, h)
                P.op('pe', lambda e, h=h, tri=tri, GB_=GB_: e.matmul(pA[:, h * 512:h * 512 + 6], tri[:], GB_[:, h * 6:h * 6 + 6], start=True, stop=True), reads=[trin, rG], writes=[('pA', h)])
                P.op('dve', lambda e, h=h: e.tensor_copy(gc[:, h * 6:h * 6 + 6], pA[:, h * 512:h * 512 + 6]), reads=[('pA', h)], writes=[('gc', h)])
                P.op('act', lambda e, h=h, EGC=EGC: e.activation(EGC[:, h * 6:h * 6 + 6], pA[:, h * 512:h * 512 + 6], AF.Exp), reads=[('pA', h)], writes=[('egc', b, h)])
                P.op('dve', lambda e, h=h: e.tensor_tensor(Dg[:, h * 6:h * 6 + 6, :], sv(idbc, 0), bcol(gc, h), ALU.mult), reads=['idbc', ('gc', h)], writes=[('Dg', h)])
                P.op('pool', lambda e, h=h, beta=beta: e.tensor_tensor(Dg[:, 12 + h * 6:12 + h * 6 + 6, :], sv(idbc, 0), bcol(beta, h), ALU.mult), reads=['idbc', rBt], writes=[('Dgb', h)])
                P.op('pe', lambda e, h=h: e.matmul(pB[:, h * 512:h * 512 + 384], ones[:], DgF[:, h * 384:(h + 1) * 384], start=True, stop=True), reads=['ones', ('Dg', h)], writes=[('pB', h)])
                P.op('pe', lambda e, h=h: e.matmul(pC[:, h * 512:h * 512 + 384], ones[:], DgF[:, 768 + h * 384:768 + (h + 1) * 384], start=True, stop=True), reads=['ones', ('Dgb', h)], writes=[('pC', h)])
                P.op('dve', lambda e, h=h: e.tensor_tensor(sv(diff, h), pv(pB, h), bcol(gc, h), ALU.subtract), reads=[('pB', h), ('gc', h)], writes=[('diff', h)])
                lc = h * 512 + (63 if h == 0 else 0)
                lastv = pB[:, lc:lc + 64 * 5 + 1:64]
                P.op('act', lambda e, h=h, lastv=lastv, EGL=EGL: e.activation(EGL[:, h * 6:h * 6 + 6], lastv, AF.Exp), reads=[('pB', h)], writes=[('egl', b, h)])
                P.op('dve', lambda e, h=h, lastv=lastv: e.tensor_tensor(egd[:, h * 6:h * 6 + 6], lastv, gc[:, h * 6:h * 6 + 6], ALU.subtract), reads=[('pB', h), ('gc', h)], writes=[('egd', h)])
                P.op('act', lambda e, h=h: e.activation(egd[:, h * 6:h * 6 + 6], egd[:, h * 6:h * 6 + 6], AF.Exp), reads=[('egd', h)], writes=[('egd', h)])
                P.op('pool', lambda e, h=h, K_=K_, KD=KD: e.tensor_tensor(sv(KD, h), sv(K_, h), bcol(egd, h), ALU.mult), reads=[('ktm', b, h), ('egd', h)], writes=[('kd', b, h)])
                P.op('act', lambda e, h=h: e.activation(sv(Ea, h), sv(diff, h), AF.Exp), reads=[('diff', h)], writes=[('Ea', h)])
                P.op('act', lambda e, h=h: e.activation(sv(Eb, h), sv(diff, h), AF.Exp, scale=-1.0), reads=[('diff', h)], writes=[('Eb', h)])
            yield
            mm12(pA, 'pA', lambda j: KT_[:, j, :], lambda j: KT_[:, j, :], lambda h: [('kT', b, h)])
            for h in range(2):
                rBt = ('gbb', b, h)
                P.op('dve', lambda e, h=h: e.scalar_tensor_tensor(sv(Eb, h), sv(Eb, h), 1.0, sv(mWt, h), ALU.min, ALU.mult), reads=[('Eb', h), 'mWt'], writes=[('Eb', h)])
                P.op('dve', lambda e, h=h: e.tensor_tensor(sv(Eb, h), sv(Eb, h), pv(pC, h), ALU.mult), reads=[('Eb', h), ('pC', h)], writes=[('Eb', h)])
                P.op('dve', lambda e, h=h: e.tensor_tensor(sv(Wt, h), sv(Eb, h), pv(pA, h), ALU.mult), reads=[('Eb', h), ('pA', h)], writes=[('Wt', h)])
            yield
            mm12(pC, 'pC', lambda j: KT_[:, j, :], lambda j: QT_[:, j, :], lambda h: [('kT', b, h), ('qT', b, h)])
            for h in range(2):
                rBt = ('gbb', b, h)
                P.op('dve', lambda e, h=h: e.scalar_tensor_tensor(sv(diff, h), sv(Ea, h), 1.0, sv(mI, h), ALU.min, ALU.mult), reads=[('Ea', h), 'mI'], writes=[('diff', h)])
                P.op('dve', lambda e, h=h, QKM=QKM: e.tensor_tensor(sv(QKM, h), sv(diff, h), pv(pC, h), ALU.mult), reads=[('diff', h), ('pC', h)], writes=[('QKm', b, h)])
                P.op('dve', lambda e, h=h: e.scalar_tensor_tensor(sv(Ea, h), sv(Ea, h), 1.0, sv(mW, h), ALU.min, ALU.mult), reads=[('Ea', h), 'mW', ('diff', h)], writes=[('Ea', h)])
                P.op('pool', lambda e, h=h, beta=beta: e.tensor_tensor(sv(Ea, h), sv(Ea, h), bcol(beta, h), ALU.mult), reads=[('Ea', h), rBt], writes=[('Ea', h)])
                P.op('dve', lambda e, h=h: e.tensor_tensor(sv(W, h), sv(Ea, h), pv(pA, h), ALU.mult), reads=[('Ea', h), ('pA', h)], writes=[('W', h)])
                P.op('pool', lambda e, h=h: e.tensor_tensor(sv(Yt[0], h), sv(idbc, h), sv(W, h), ALU.subtract), reads=['idbc', ('W', h)], writes=[('Yt0', h)])
            yield
            cur, curT, cn, cnT = W, Wt, 'W', 'Wt'
            yi = 0
            bufs = [(A1, A1t, 'A1', 'A1t'), (A2, A2t, 'A2', 'A2t')]
            for lev in range(5):
                nx, nxT, nn, nnT = bufs[lev % 2]
                mm12(pB, 'pB', lambda j, cur=cur: cur[:, j, :], lambda j, curT=curT: curT[:, j, :], lambda h, cn=cn, cnT=cnT: [(cn, h), (cnT, h)])
                for h in range(2):
                    if lev < 4:
                        P.op('act', lambda e, h=h, nxT=nxT: e.copy(sv(nxT, h), pv(pB, h)), reads=[('pB', h)], writes=[(nnT, h)])
                    P.op('dve', lambda e, h=h: e.tensor_tensor(sv(nxTI, h), pv(pB, h), sv(idbc, h), ALU.add), reads=[('pB', h), 'idbc'], writes=[('nxTI', h)])
                if lev < 4:
                    mm12(pC, 'pC', lambda j, curT=curT: curT[:, j, :], lambda j, cur=cur: cur[:, j, :], lambda h, cn=cn, cnT=cnT: [(cn, h), (cnT, h)])
                    for h in range(2):
                        P.op('dve', lambda e, h=h, nx=nx: e.tensor_copy(sv(nx, h), pv(pC, h)), reads=[('pC', h)], writes=[(nn, h)])
                Yc = Yt[yi]
                yc_ = 'Yt%d' % yi
                if lev < 4:
                    Yn, yn_ = Yt[1 - yi], ('Yt%d' % (1 - yi),)
                else:
                    Yn, yn_ = Yf[b], ('Yf', b)
                for j in range(12):
                    h = j // 6
                    P.op('pe', lambda e, j=j, Yc=Yc: e.matmul(pcol(pA, j), nxTI[:, j, :], Yc[:, j, :], start=True, stop=True), reads=[('nxTI', h), (yc_, h)], writes=[('pA', h)])
                for h in range(2):
                    P.op('dve', lambda e, h=h, Yn=Yn: e.tensor_copy(sv(Yn, h), pv(pA, h)), reads=[('pA', h)], writes=[yn_ + (h,)])
                yi = 1 - yi
                cur, curT, cn, cnT = nx, nxT, nn, nnT
                yield

        def scan(i):
            b = i % 2
            cf = i
            cb = NCH - 1 - i
            V_, KT_, QT_, GB_ = vtm[b], kT[b], qT[b], gbv[b]
            EGC, EGL, QKM, KD, YF = egc[b], egl[b], QKm[b], kd[b], Yf[b]
            beta = GB_[:, 12:24]
            mm12(pS, 'pS', lambda j: KT_[:, j, :], lambda j: Sm[:, j, :], lambda h: [('kT', b, h), ('Sm', h)])
            for h in range(2):
                P.op('dve', lambda e, h=h, EGC=EGC: e.tensor_tensor(sv(Rr, h), pv(pS, h), bcol(EGC, h), ALU.mult), reads=[('pS', h), ('egc', b, h)], writes=[('R', h)])
                P.op('dve', lambda e, h=h, V_=V_: e.tensor_tensor(sv(Rm, h), sv(V_, h), sv(Rr, h), ALU.subtract), reads=[('R', h), ('vtm', b, h)], writes=[('Rm', h)])
            yield
            mm12(pS, 'pS', lambda j: QT_[:, j, :], lambda j: Sm[:, j, :], lambda h: [('qT', b, h), ('Sm', h)])
            for h in range(2):
                P.op('act', lambda e, h=h: e.copy(sv(o1, h), pv(pS, h)), reads=[('pS', h)], writes=[('o1', h)])
                P.op('pool', lambda e, h=h, EGC=EGC: e.tensor_tensor(sv(o1, h), sv(o1, h), bcol(EGC, h), ALU.mult), reads=[('o1', h), ('egc', b, h)], writes=[('o1', h)])
            yield
            yield
            mm12(pS, 'pS', lambda j: YF[:, j, :], lambda j: Rm[:, j, :], lambda h: [('Yf', b, h), ('Rm', h)])
            for h in range(2):
                P.op('dve', lambda e, h=h, beta=beta: e.tensor_tensor(sv(vnew, h), pv(pS, h), bcol(beta, h), ALU.mult), reads=[('pS', h), ('gbb', b, h)], writes=[('vnew', h)])
            yield
            mm12(pS, 'pS', lambda j: KD[:, j, :], lambda j: vnew[:, j, :], lambda h: [('kd', b, h), ('vnew', h)])
            OB = ob[b]
            for h in range(2):
                P.op('pool', lambda e, h=h, EGL=EGL: e.tensor_tensor(sv(St2, h), sv(St, h), bcol(EGL, h), ALU.mult), reads=[('S', h), ('egl', b, h)], writes=[('S2', h)])
                P.op('dve', lambda e, h=h: e.tensor_tensor(sv(St, h), sv(St2, h), pv(pS, h), ALU.add), reads=[('S2', h), ('pS', h)], writes=[('S', h)])
                P.op('act', lambda e, h=h: e.copy(sv(Sm, h), sv(St, h)), reads=[('S', h)], writes=[('Sm', h)])
            yield
            mm12(pS, 'pS', lambda j: QKM[:, j, :], lambda j: vnew[:, j, :], lambda h: [('QKm', b, h), ('vnew', h)])
            for h in range(2):
                cc = cf if h == 0 else cb
                P.op('dve', lambda e, h=h, OB=OB: e.tensor_tensor(sv(OB, h), sv(o1, h), pv(pS, h), ALU.add), reads=[('o1', h), ('pS', h)], writes=[('ob', b, h)])
                P.dma('sp', D['o_fb'][h, cc * C:(cc + 1) * C, :].rearrange('t (h d) -> t h d', d=64), sv(OB, h), reads=[('ob', b, h)], writes=['d_ofb'])

        def run(gens):
            gens = [g for g in gens if g is not None]
            while gens:
                for g in list(gens):
                    try:
                        next(g)
                    except StopIteration:
                        gens.remove(g)

        run([prep(0)])
        for i in range(NST):
            run([prep(i + 1) if i + 1 < NST else None, scan(i)])
        P.emit()


def phase_E(P, l, D, first):
    xsrc = D['x'] if first else D['xw']
    with ExitStack() as s:
        wo = P.sb(s, 'f_wo', [128, 8, 1024], BF16)
        ong = P.sb(s, 'f_ong', [128, 64], F32)
        idb = P.sb(s, 'f_idb', [128, 128], BF16)
        of_ = [P.sb(s, 'f_of%d' % i, [128, 2, 384], F32) for i in range(2)]
        gt = [P.sb(s, 'f_gt%d' % i, [128, 384], F32) for i in range(2)]
        o = P.sb(s, 'f_o', [128, 6, 64], F32)
        sq = P.sb(s, 'f_sq', [128, 6, 64], F32)
        r6 = P.sb(s, 'f_r6', [128, 6], F32)
        ycb = P.sb(s, 'f_ycb', [128, 384], BF16)
        yT = [P.sb(s, 'f_yT%d' % i, [128, 8, 128], BF16) for i in range(2)]
        xt = [P.sb(s, 'f_xt%d' % i, [128, 1024], F32) for i in range(2)]
        pT = P.ps(s, 'f_pT', [128, 512], BF16)
        po = [P.ps(s, 'f_po%d' % i, [128, 512]) for i in range(2)]
        for k in range(8):
            P.dma('pool', wo[:, k, :], D['w_out'][l, k * 128:(k + 1) * 128, :], writes=[('wo', k)])
        P.dma('sp', ong[:], D['o_norm_g'][l].partition_broadcast(128), writes=['ong'])
        P.dma('pool', idb[:], D['ident'], writes=['idb'])
        def front(t):
            b = t % 2
            tk = slice(t * 128, (t + 1) * 128)
            P.dma('sp', of_[b][:], D['o_fb'][:, tk, :].rearrange('a t c -> t a c'), reads=['d_ofb'], writes=[('of', b)])
            P.dma('sp', gt[b][:], D['gate_s'][tk, :], reads=['d_gate'], writes=[('gt', b)])
            P.dma('sp', xt[b][:], xsrc[tk, :], writes=[('xt', b)])
            P.dma('sp', yT[b][:, 0:5, :], D['yT'][0:640, tk].rearrange('(k p) t -> p k t', p=128), reads=['d_yT'], writes=[('yT', b, 0)])
            OF = o[:].rearrange('p a b -> p (a b)')
            P.op('dve', lambda e, b=b: e.tensor_tensor(OF, of_[b][:, 0, :], of_[b][:, 1, :], ALU.add), reads=[('of', b)], writes=['o'])
            P.op('act', lambda e: e.activation(sq[:], o[:], AF.Square), reads=['o'], writes=['sq'])
            P.op('dve', lambda e: e.tensor_reduce(r6[:], sq[:], AX.X, ALU.add), reads=['sq'], writes=['r6'])
            P.op('act', lambda e: e.activation(r6[:], r6[:], AF.Sqrt, bias=EPS, scale=1.0 / 64), reads=['r6'], writes=['r6'])
            P.op('dve', lambda e: e.reciprocal(r6[:], r6[:]), reads=['r6'], writes=['r6'])
            P.op('dve', lambda e: e.tensor_tensor(o[:], o[:], bc(r6[:].unsqueeze(2), [128, 6, 64]), ALU.mult), reads=['o', 'r6'], writes=['o'])
            P.op('pool', lambda e: e.tensor_tensor(o[:], o[:], bc(ong[:].unsqueeze(1), [128, 6, 64]), ALU.mult), reads=['o', 'ong'], writes=['o'])
            P.op('pool', lambda e, b=b: e.tensor_tensor(ycb[:], OF, gt[b][:], ALU.mult), reads=['o', ('gt', b)], writes=['ycb'])
            for k in range(3):
                P.op('pe', lambda e, k=k: e.transpose(pT[:, k * 128:(k + 1) * 128], ycb[:, k * 128:(k + 1) * 128], idb[:]), reads=['ycb', 'idb'], writes=['pT'])
            P.op('act', lambda e, b=b: e.copy(yT[b][:, 5:8, :], pT[:, 0:384].rearrange('p (k t) -> p k t', t=128)), reads=['pT'], writes=[('yT', b, 1)])
        def back(t):
            b = t % 2
            tk = slice(t * 128, (t + 1) * 128)
            for hf in range(2):
                for k in range(8):
                    P.op('pe', lambda e, hf=hf, k=k, b=b: e.matmul(po[hf][:], yT[b][:, k, :], wo[:, k, hf * 512:(hf + 1) * 512], start=(k == 0), stop=(k == 7)),
                         reads=[('yT', b, 0), ('yT', b, 1), ('wo', k)], writes=[('po', hf)])
                P.op('dve', lambda e, hf=hf, b=b: e.tensor_tensor(xt[b][:, hf * 512:(hf + 1) * 512], xt[b][:, hf * 512:(hf + 1) * 512], po[hf][:], ALU.add),
                     reads=[('po', hf), ('xt', b)], writes=[('xt', b)])
            P.dma('sp', D['xw'][tk, :], xt[b][:], reads=[('xt', b)], writes=[('d_xw', t)])

        front(0)
        for t in range(NT):
            if t + 1 < NT:
                front(t + 1)
            back(t)
        P.emit()


def phase_F(P, l, D):
    import os
    NE = int(os.environ.get('FNE', '16'))
    with ExitStack() as s:
        gff = P.sb(s, 'g_gff', [128, 1024], F32)
        idf = P.sb(s, 'g_idf', [128, 128], F32)
        idb = P.sb(s, 'g_idb', [128, 128], BF16)
        wr = P.sb(s, 'g_wr', [128, 8, 16], F32)
        aff = P.sb(s, 'g_aff', [128, NT, 16], F32)
        sel = P.sb(s, 'g_sel', [128, NT, 16], F32)
        rank = P.sb(s, 'g_rank', [128, NT, 16], F32)
        cA = P.sb(s, 'g_cA', [128, NT, 16], F32)
        cB = P.sb(s, 'g_cB', [128, NT, 16], F32)
        selb = P.sb(s, 'g_selb', [128, NT * 16], BF16)
        triS = P.sb(s, 'g_triS', [128, 128], BF16)
        onesb = P.sb(s, 'g_onesb', [128, 128], BF16)
        tg = P.sb(s, 'g_tg', [128, NT, 16, 5], BF16)
        tp = P.sb(s, 'g_tp', [128, NT, 2], F32)
        iota = P.sb(s, 'g_iota', [128, 512], F32)
        Selt = [P.sb(s, 'g_Selt%d' % i, [128, 512], BF16) for i in range(2)]
        idxf = P.sb(s, 'g_idxf', [128, 4, 8], F32)
        row5 = P.sb(s, 'g_row5', [5, 512], F32)
        idxv = P.sb(s, 'g_idxv', [128, 4], F32)
        idxi = [P.sb(s, 'g_idxi%d' % i, [128, 4], I32) for i in range(2)]
        gate = [P.sb(s, 'g_gate%d' % i, [128, 4], F32) for i in range(2)]
        affT2 = P.sb(s, 'g_affT2', [16, S], F32)
        bj = P.sb(s, 'g_bj', [16, S], BF16)
        bs = P.sb(s, 'g_bs', [16, 8], F32)
        ones16 = P.sb(s, 'g_ones16', [16, 128], F32)
        dthr = P.sb(s, 'g_dthr', [16, 16], F32)
        thrb = P.sb(s, 'g_thrb', [128, 16], F32)
        xt = P.sb(s, 'g_xt', [128, 1024], F32)
        junk = P.sb(s, 'g_junk', [128, 1024], BF16)
        ss = P.sb(s, 'g_ss', [128, 4], F32)
        h32 = P.sb(s, 'g_h32', [128, 1024], F32)
        hb16 = P.sb(s, 'g_hb16', [128, 1024], BF16)
        hT32 = P.sb(s, 'g_hT32', [128, 8, 128], F32)
        sm = P.sb(s, 'g_sm', [128, 4], F32)
        ex = P.sb(s, 'g_ex', [128, 16], F32)
        wg = [P.sb(s, 'g_wg%d' % i, [128, 8, 1024], BF16) for i in range(2)]
        wu = [P.sb(s, 'g_wu%d' % i, [128, 8, 1024], BF16) for i in range(2)]
        wd = [P.sb(s, 'g_wd%d' % i, [128, 8, 1024], BF16) for i in range(2)]
        xe = P.sb(s, 'g_xe', [128, 4, 1024], BF16)
        xeTs = [P.sb(s, 'g_xeT%d' % i, [128, 8, 512], BF16) for i in range(2)]
        hid = P.sb(s, 'g_hid', [128, 8, 512], BF16)
        sg = [P.sb(s, 'g_sg%d' % i, [128, 512], BF16) for i in range(2)]
        ye = [P.sb(s, 'g_ye%d' % i, [128, 1024], F32) for i in range(2)]
        pbig = P.ps(s, 'g_pbig', [128, 1024])
        pl = P.ps(s, 'g_pl', [128, 512])
        pT = P.ps(s, 'g_pT', [128, 1024], BF16)
        pg_ = P.ps(s, 'g_pg', [128, 512])
        pu_ = P.ps(s, 'g_pu', [128, 512])
        py = [P.ps(s, 'g_py%d' % i, [128, 512]) for i in range(2)]

        def load_w(ex_):
            eb = ex_ % 2
            for k in range(8):
                P.dma('pool', wg[eb][:, k, :], D['w_e_gate'][l, ex_, k * 128:(k + 1) * 128, :], writes=[('wg', eb, k)])
                P.dma('pool', wu[eb][:, k, :], D['w_e_up'][l, ex_, k * 128:(k + 1) * 128, :], writes=[('wu', eb, k)])
            for k in range(8):
                P.dma('pool', wd[eb][:, k, :], D['w_e_down'][l, ex_, k * 128:(k + 1) * 128, :], writes=[('wd', eb, k)])

        P.dma('sp', gff[:], D['g_ffn'][l].partition_broadcast(128), writes=['gff'])
        P.dma('sp', idf[:], D['ident'], writes=['idf'])
        P.dma('pool', idb[:], D['ident'], writes=['idb'])
        P.dma('pool', triS[:], D['triS'], writes=['triS'])
        P.dma('pool', onesb[:], D['ones128'], writes=['onesb'])
        P.dma('sp', wr[:], D['w_router'][l].rearrange('(k p) e -> p k e', p=128), writes=['wr'])
        P.dma('sp', ones16[:], D['ones128'][0:16, :], writes=['ones16'])
        P.dma('sp', tp[:], D['tp'], writes=['tp'])
        P.dma('sp', iota[:], D['iota512'], writes=['iota'])
        load_w(0)
        for t in range(NT):
            tk = slice(t * 128, (t + 1) * 128)
            P.dma('sp', xt[:], D['xw'][tk, :], reads=['d_xw'], writes=['xt'])
            P.op('dve', lambda e: e.memset(ss[:, 0:1], 0.0), writes=['ss'])
            P.op('act', lambda e: e.activation(junk[:], xt[:], AF.Square, accum_out=ss[:, 0:1]), reads=['xt', 'ss'], writes=['junk', 'ss'])
            P.op('act', lambda e: e.activation(ss[:, 0:1], ss[:, 0:1], AF.Sqrt, bias=EPS, scale=1.0 / 1024), reads=['ss'], writes=['ss'])
            P.op('dve', lambda e: e.reciprocal(ss[:, 0:1], ss[:, 0:1]), reads=['ss'], writes=['ss'])
            P.op('dve', lambda e: e.scalar_tensor_tensor(h32[:], xt[:], ss[:, 0:1], gff[:], ALU.mult, ALU.mult), reads=['xt', 'ss', 'gff'], writes=['h32'])
            P.op('act', lambda e: e.copy(hb16[:], h32[:]), reads=['h32'], writes=['hb16'])
            P.dma('sp', D['hb'][tk, :], hb16[:], reads=['hb16'], writes=['d_hb'])
            for k in range(8):
                P.op('pe', lambda e, k=k: e.transpose(pbig[:, k * 128:(k + 1) * 128], h32[:, k * 128:(k + 1) * 128], idf[:]), reads=['h32', 'idf'], writes=[('pbig', k // 4)])
            P.op('act', lambda e: e.copy(hT32[:, 0:4, :], pbig[:, 0:512].rearrange('p (k t) -> p k t', t=128)), reads=[('pbig', 0)], writes=['hT32a'])
            P.op('dve', lambda e: e.tensor_copy(hT32[:, 4:8, :], pbig[:, 512:1024].rearrange('p (k t) -> p k t', t=128)), reads=[('pbig', 1)], writes=['hT32b'])
            for k in range(8):
                P.op('pe', lambda e, k=k: e.matmul(pl[:, 0:16], hT32[:, k, :], wr[:, k, :], start=(k == 0), stop=(k == 7)), reads=['hT32a', 'hT32b', 'wr'], writes=['pl'])
            P.op('dve', lambda e: e.tensor_reduce(sm[:, 0:1], pl[:, 0:16], AX.X, ALU.max), reads=['pl'], writes=['sm'])
            P.op('dve', lambda e: e.tensor_scalar(sm[:, 1:2], sm[:, 0:1], -1.0, None, ALU.mult), reads=['sm'], writes=['sm'])
            P.op('dve', lambda e: e.memset(sm[:, 2:3], 0.0), reads=['sm'], writes=['sm'])
            P.op('act', lambda e: e.activation(ex[:], pl[:, 0:16], AF.Exp, bias=sm[:, 1:2], accum_out=sm[:, 2:3]), reads=['pl', 'sm'], writes=['ex', 'sm'])
            P.op('dve', lambda e: e.reciprocal(sm[:, 3:4], sm[:, 2:3]), reads=['sm'], writes=['sm'])
            P.op('dve', lambda e, t=t: e.tensor_scalar(aff[:, t, :], ex[:], sm[:, 3:4], None, ALU.mult), reads=['ex', 'sm'], writes=[('aff', t)])
            P.op('pe', lambda e, t=t: e.transpose(pl[0:16, 128:256], aff[:, t, :], idf[:]), reads=[('aff', t), 'idf'], writes=['pl'])
            P.op('act', lambda e, t=t: e.mul(affT2[:, t * 128:(t + 1) * 128], pl[0:16, 128:256], 2.0), reads=['pl'], writes=['affT2'])
        lo, hi, half, mid2, cnt, gef, tt = (bs[:, i:i + 1] for i in range(7))
        P.op('dve', lambda e: e.memset(bs[:], 0.0), writes=['bs'])
        P.op('dve', lambda e: e.memset(hi, 1.0), reads=['bs'], writes=['bs'])
        for itn in range(30):
            P.op('dve', lambda e: e.tensor_tensor(mid2, lo, hi, ALU.add), reads=['bs'], writes=['bs'])
            P.op('dve', lambda e: e.tensor_scalar(half, mid2, 0.5, None, ALU.mult), reads=['bs'], writes=['bs'])
            P.op('dve', lambda e: e.memset(cnt, 0.0), reads=['bs'], writes=['bs'])
            P.op('dve', lambda e: e.tensor_scalar(bj[:], affT2[:], mid2, 0.0, ALU.is_ge, ALU.add, accum_out=cnt), reads=['affT2', 'bs'], writes=['bj', 'bs'])
            P.op('dve', lambda e: e.tensor_scalar(gef, cnt, 511.5, None, ALU.is_ge), reads=['bs'], writes=['bs'])
            P.op('dve', lambda e: e.tensor_tensor(tt, half, lo, ALU.subtract), reads=['bs'], writes=['bs'])
            P.op('dve', lambda e: e.tensor_tensor(tt, tt, gef, ALU.mult), reads=['bs'], writes=['bs'])
            P.op('dve', lambda e: e.tensor_tensor(lo, lo, tt, ALU.add), reads=['bs'], writes=['bs'])
            P.op('dve', lambda e: e.tensor_tensor(tt, hi, half, ALU.subtract), reads=['bs'], writes=['bs'])
            P.op('dve', lambda e: e.tensor_tensor(tt, tt, gef, ALU.mult), reads=['bs'], writes=['bs'])
            P.op('dve', lambda e: e.tensor_tensor(hi, half, tt, ALU.add), reads=['bs'], writes=['bs'])
        P.op('dve', lambda e: e.tensor_scalar(dthr[:], idf[0:16, 0:16], lo, None, ALU.mult), reads=['idf', 'bs'], writes=['dthr'])
        P.op('pe', lambda e: e.matmul(pl[:, 256:272], ones16[:], dthr[:], start=True, stop=True), reads=['ones16', 'dthr'], writes=['pl'])
        P.op('dve', lambda e: e.tensor_copy(thrb[:], pl[:, 256:272]), reads=['pl'], writes=['thrb'])
        AFF = [('aff', t) for t in range(NT)]
        P.op('dve', lambda e: e.tensor_tensor(sel[:], aff[:], bc(thrb[:].unsqueeze(1), [128, NT, 16]), ALU.is_ge), reads=AFF + ['thrb'], writes=['sel'])
        P.op('dve', lambda e: e.tensor_copy(selb[:], sel[:].rearrange('p t e -> p (t e)')), reads=['sel'], writes=['selb'])
        P.op('pe', lambda e: e.matmul(pg_[:], triS[:], selb[:], start=True, stop=True), reads=['triS', 'selb'], writes=['pg'])
        P.op('pe', lambda e: e.matmul(pu_[:], onesb[:], selb[:], start=True, stop=True), reads=['onesb', 'selb'], writes=['pu'])
        P.op('dve', lambda e: e.tensor_copy(cA[:].rearrange('p t e -> p (t e)'), pu_[:]), reads=['pu'], writes=['cA'])
        src, dst, sn, dn = cA, cB, 'cA', 'cB'
        for sft in (1, 2, 4, 8, 16):
            P.op('pool', lambda e, src=src, dst=dst, sft=sft: e.tensor_copy(dst[:, 0:sft, :], src[:, 0:sft, :]), reads=[sn], writes=[dn])
            P.op('dve', lambda e, src=src, dst=dst, sft=sft: e.tensor_tensor(dst[:, sft:NT, :], src[:, sft:NT, :], src[:, 0:NT - sft, :], ALU.add), reads=[sn], writes=[dn])
            src, dst, sn, dn = dst, src, dn, sn
        P.op('dve', lambda e, src=src: e.tensor_tensor(rank[:].rearrange('p t e -> p (t e)'), src[:].rearrange('p t e -> p (t e)'), pu_[:], ALU.subtract), reads=[sn, 'pu'], writes=['rank'])
        P.op('dve', lambda e: e.tensor_tensor(rank[:].rearrange('p t e -> p (t e)'), rank[:].rearrange('p t e -> p (t e)'), pg_[:], ALU.add), reads=['rank', 'pg'], writes=['rank'])
        P.op('dve', lambda e: e.scalar_tensor_tensor(rank[:], rank[:], 1.0, sel[:], ALU.add, ALU.mult), reads=['rank', 'sel'], writes=['rank'])
        P.op('dve', lambda e: e.tensor_scalar(rank[:], rank[:], -1.0, None, ALU.add), reads=['rank'], writes=['rank'])
        P.op('dve', lambda e: e.tensor_copy(tg[:, :, :, 0:2], bc(tp[:].unsqueeze(2), [128, NT, 16, 2])), reads=['tp'], writes=['tg0'])
        P.op('dve', lambda e: e.tensor_copy(tg[:, :, :, 2], aff[:]), reads=AFF, writes=['tg1'])
        P.op('dve', lambda e: e.tensor_tensor(cA[:], aff[:], tg[:, :, :, 2], ALU.subtract), reads=AFF + ['tg1', 'cA', 'cB'], writes=['cA'])
        P.op('dve', lambda e: e.tensor_copy(tg[:, :, :, 3], cA[:]), reads=['cA'], writes=['tg2'])
        P.op('dve', lambda e: e.tensor_tensor(cB[:], cA[:], tg[:, :, :, 3], ALU.subtract), reads=['cA', 'tg2', 'cB'], writes=['cB'])
        P.op('dve', lambda e: e.tensor_copy(tg[:, :, :, 4], cB[:]), reads=['cB'], writes=['tg3'])
        TG = ['tg0', 'tg1', 'tg2', 'tg3']
        nsel = 0
        npy = 0
        nsg = 0
        def stage1(ex_):
            nonlocal nsel
            eb = ex_ % 2
            xeT = xeTs[eb]
            for t in range(NT):
                sb_ = nsel % 2
                nsel += 1
                P.op('dve', lambda e, sb_=sb_, t=t, ex_=ex_: e.tensor_scalar(Selt[sb_][:], iota[:], rank[:, t, ex_:ex_ + 1], None, ALU.is_equal), reads=['iota', 'rank'], writes=[('Selt', sb_)])
                P.op('pe', lambda e, sb_=sb_, t=t, ex_=ex_: e.matmul(pl[0:5, 0:512], tg[:, t, ex_, :], Selt[sb_][:], start=(t == 0), stop=(t == NT - 1)),
                     reads=[('Selt', sb_)] + TG, writes=['pl'])
            P.op('act', lambda e: e.copy(row5[:], pl[0:5, 0:512]), reads=['pl'], writes=['row5'])
            for g in range(4):
                P.op('pe', lambda e, g=g: e.transpose(pl[:, g * 8:g * 8 + 5], row5[0:5, g * 128:(g + 1) * 128], idf[0:5, 0:5]), reads=['row5', 'idf'], writes=['pl'])
            P.op('dve', lambda e: e.tensor_copy(idxf[:, :, 0:5], pl[:, 0:32].rearrange('p (g c) -> p g c', c=8)[:, :, 0:5]), reads=['pl'], writes=['idxf'])
            P.op('dve', lambda e: e.scalar_tensor_tensor(idxv[:], idxf[:, :, 0], 128.0, idxf[:, :, 1], ALU.mult, ALU.add), reads=['idxf'], writes=['idxv'])
            P.op('dve', lambda e, eb=eb: e.tensor_copy(idxi[eb][:], idxv[:]), reads=['idxv'], writes=[('idxi', eb)])
            P.op('dve', lambda e, eb=eb: e.tensor_tensor(gate[eb][:], idxf[:, :, 2], idxf[:, :, 3], ALU.add), reads=['idxf'], writes=[('gate', eb)])
            P.op('dve', lambda e, eb=eb: e.tensor_tensor(gate[eb][:], gate[eb][:], idxf[:, :, 4], ALU.add), reads=['idxf', ('gate', eb)], writes=[('gate', eb)])
            for g in range(4):
                P.idma(lambda e, g=g, eb=eb: e.indirect_dma_start(out=xe[:, g, :], out_offset=None, in_=D['hb'][:, :],
                                                                   in_offset=bass.IndirectOffsetOnAxis(ap=idxi[eb][:, g:g + 1], axis=0), bounds_check=P.breg(e), oob_is_err=False),
                       reads=['d_hb', ('idxi', eb)], writes=[('xe', g)])
            for g in range(4):
                for k in range(8):
                    P.op('pe', lambda e, g=g, k=k: e.transpose(pT[:, k * 128:(k + 1) * 128], xe[:, g, k * 128:(k + 1) * 128], idb[:]), reads=[('xe', g), 'idb'], writes=['pT'])
                eng = 'act' if g % 2 == 0 else 'dve'
                if eng == 'act':
                    P.op('act', lambda e, g=g, xeT=xeT: e.copy(xeT[:, :, g * 128:(g + 1) * 128], pT[:].rearrange('p (k t) -> p k t', t=128)), reads=['pT'], writes=[('xeT', eb, g)])
                else:
                    P.op('dve', lambda e, g=g, xeT=xeT: e.tensor_copy(xeT[:, :, g * 128:(g + 1) * 128], pT[:].rearrange('p (k t) -> p k t', t=128)), reads=['pT'], writes=[('xeT', eb, g)])

        def stage2(ex_):
            nonlocal npy, nsg
            eb = ex_ % 2
            xeT = xeTs[eb]
            XET = [('xeT', eb, g) for g in range(4)]
            for fc in range(8):
                for k in range(8):
                    P.op('pe', lambda e, fc=fc, k=k, eb=eb, xeT=xeT: e.matmul(pg_[:], wg[eb][:, k, fc * 128:(fc + 1) * 128], xeT[:, k, :], start=(k == 0), stop=(k == 7)),
                         reads=XET + [('wg', eb, k)], writes=['pg'])
                for k in range(8):
                    P.op('pe', lambda e, fc=fc, k=k, eb=eb, xeT=xeT: e.matmul(pu_[:], wu[eb][:, k, fc * 128:(fc + 1) * 128], xeT[:, k, :], start=(k == 0), stop=(k == 7)),
                         reads=XET + [('wu', eb, k)], writes=['pu'])
                sb2 = nsg % 2
                nsg += 1
                P.op('act', lambda e, sb2=sb2: e.activation(sg[sb2][:], pg_[:], AF.Silu), reads=['pg'], writes=[('sg', sb2)])
                P.op('dve', lambda e, sb2=sb2, fc=fc: e.tensor_tensor(hid[:, fc, :], sg[sb2][:], pu_[:], ALU.mult), reads=[('sg', sb2), 'pu'], writes=[('hid', fc)])
            HID = [('hid', fc) for fc in range(8)]
            for g in range(4):
                yb = g % 2
                for hf in range(2):
                    pb = npy % 2
                    npy += 1
                    for fc in range(8):
                        P.op('pe', lambda e, pb=pb, fc=fc, g=g, hf=hf, eb=eb: e.matmul(py[pb][:], hid[:, fc, g * 128:(g + 1) * 128], wd[eb][:, fc, hf * 512:(hf + 1) * 512], start=(fc == 0), stop=(fc == 7)),
                             reads=HID + [('wd', eb, fc)], writes=[('py', pb)])
                    if hf == 0:
                        P.op('act', lambda e, pb=pb, yb=yb, g=g, eb=eb: e.activation(ye[yb][:, 0:512], py[pb][:], AF.Copy, scale=gate[eb][:, g:g + 1]), reads=[('py', pb), ('gate', eb)], writes=[('ye', yb, 0)])
                    else:
                        P.op('dve', lambda e, pb=pb, yb=yb, g=g, eb=eb: e.tensor_scalar(ye[yb][:, 512:1024], py[pb][:], gate[eb][:, g:g + 1], None, ALU.mult), reads=[('py', pb), ('gate', eb)], writes=[('ye', yb, 1)])
                P.idma(lambda e, g=g, eb=eb, yb=yb: e.indirect_dma_start(out=D['xw'][:, :], out_offset=bass.IndirectOffsetOnAxis(ap=idxi[eb][:, g:g + 1], axis=0), in_=ye[yb][:],
                                                                          in_offset=None, bounds_check=P.breg(e), oob_is_err=False, compute_op=ALU.add),
                       reads=[('ye', yb, 0), ('ye', yb, 1), ('idxi', eb), 'd_xw'], writes=['d_xw'])

        stage1(0)
        for ex_ in range(NE):
            if ex_ + 1 < NE:
                load_w(ex_ + 1)
                stage1(ex_ + 1)
            stage2(ex_)
        P.emit()


def phase_G(P, l, D, last):
    xdst = D['out'] if last else D['xw']
    with ExitStack() as s:
        wp = P.sb(s, 'h_wp', [128, 2, 1024], BF16)
        wgt = P.sb(s, 'h_wgt', [128, 8, 1024], BF16)
        gpl = P.sb(s, 'h_gpl', [128, 1024], F32)
        gpg = P.sb(s, 'h_gpg', [128, 1024], F32)
        idb = P.sb(s, 'h_idb', [128, 128], BF16)
        xt = [P.sb(s, 'h_xt%d' % i, [128, 1024], F32) for i in range(2)]
        pb_ = [P.sb(s, 'h_pb%d' % i, [128, 256], BF16) for i in range(2)]
        junk = P.sb(s, 'h_junk', [128, 1024], BF16)
        ss = P.sb(s, 'h_ss', [128, 4], F32)
        ssf = P.sb(s, 'h_ssf', [128, 2], F32)
        junkf = P.sb(s, 'h_junkf', [128, 1024], BF16)
        xn = P.sb(s, 'h_xn', [128, 1024], BF16)
        xTs = [P.sb(s, 'h_xT%d' % i, [128, 10, 128], BF16) for i in range(2)]
        er = P.sb(s, 'h_er', [128, 1024], F32)
        gt = P.sb(s, 'h_gt', [128, 1024], F32)
        pT = P.ps(s, 'h_pT', [128, 2048], BF16)
        pe_ = P.ps(s, 'h_pe', [128, 1024])
        pg_ = P.ps(s, 'h_pg', [128, 1024])
        for k in range(2):
            P.dma('pool', wp[:, k, :], D['w_ple'][l, k * 128:(k + 1) * 128, :], writes=[('wp', k)])
        for k in range(8):
            P.dma('pool', wgt[:, k, :], D['w_ple_gate'][l, k * 128:(k + 1) * 128, :], writes=[('wgt', k)])
        P.dma('sp', gpl[:], D['g_ple'][l].partition_broadcast(128), writes=['gpl'])
        P.dma('sp', gpg[:], D['g_ple_gate'][l].partition_broadcast(128), writes=['gpg'])
        P.dma('pool', idb[:], D['ident'], writes=['idb'])
        def front(t):
            b = t % 2
            xT = xTs[b]
            tk = slice(t * 128, (t + 1) * 128)
            P.dma('sp', xt[b][:], D['xw'][tk, :], reads=[('d_xw', t)], writes=[('xt', b)])
            P.dma('pool', pb_[b][:], D['p'][l, tk, :], writes=[('pb', b)])
            P.op('dve', lambda e: e.memset(ssf[:, 0:1], 0.0), writes=['ssf'])
            P.op('act', lambda e, b=b: e.activation(junkf[:], xt[b][:], AF.Square, accum_out=ssf[:, 0:1]), reads=[('xt', b), 'ssf'], writes=['junkf', 'ssf'])
            P.op('act', lambda e: e.activation(ssf[:, 0:1], ssf[:, 0:1], AF.Sqrt, bias=EPS, scale=1.0 / 1024), reads=['ssf'], writes=['ssf'])
            P.op('dve', lambda e: e.reciprocal(ssf[:, 0:1], ssf[:, 0:1]), reads=['ssf'], writes=['ssf'])
            P.op('dve', lambda e, b=b: e.scalar_tensor_tensor(xn[:], xt[b][:], ssf[:, 0:1], gpg[:], ALU.mult, ALU.mult), reads=[('xt', b), 'ssf', 'gpg'], writes=['xn'])
            for k in range(8):
                P.op('pe', lambda e, k=k: e.transpose(pT[:, k * 128:(k + 1) * 128], xn[:, k * 128:(k + 1) * 128], idb[:]), reads=['xn', 'idb'], writes=[('pT', 0)])
            for k in range(2):
                P.op('pe', lambda e, k=k, b=b: e.transpose(pT[:, (8 + k) * 128:(9 + k) * 128], pb_[b][:, k * 128:(k + 1) * 128], idb[:]), reads=[('pb', b), 'idb'], writes=[('pT', 1)])
            P.op('act', lambda e, xT=xT: e.copy(xT[:, 0:8, :].rearrange('p k t -> p (k t)'), pT[:, 0:1024]), reads=[('pT', 0)], writes=[('xTa', b)])
            P.op('dve', lambda e, xT=xT: e.tensor_copy(xT[:, 8:10, :].rearrange('p k t -> p (k t)'), pT[:, 1024:1280]), reads=[('pT', 1)], writes=[('xTb', b)])

        def back(t):
            b = t % 2
            xT = xTs[b]
            tk = slice(t * 128, (t + 1) * 128)
            for hf in range(2):
                for k in range(2):
                    P.op('pe', lambda e, hf=hf, k=k, xT=xT: e.matmul(pe_[:, hf * 512:(hf + 1) * 512], xT[:, 8 + k, :], wp[:, k, hf * 512:(hf + 1) * 512], start=(k == 0), stop=(k == 1)),
                         reads=[('xTb', b), ('wp', k)], writes=[('pe', hf)])
                for k in range(8):
                    P.op('pe', lambda e, hf=hf, k=k, xT=xT: e.matmul(pg_[:, hf * 512:(hf + 1) * 512], xT[:, k, :], wgt[:, k, hf * 512:(hf + 1) * 512], start=(k == 0), stop=(k == 7)),
                         reads=[('xTa', b), ('wgt', k)], writes=[('pg', hf)])
            P.op('dve', lambda e: e.memset(ss[:, 1:3], 0.0), reads=['ss'], writes=['ss'])
            for hf in range(2):
                P.op('act', lambda e, hf=hf: e.activation(junk[:, hf * 512:(hf + 1) * 512], pe_[:, hf * 512:(hf + 1) * 512], AF.Square, accum_out=ss[:, 1 + hf:2 + hf]), reads=[('pe', hf), 'ss'], writes=['junk', 'ss'])
            P.op('dve', lambda e: e.tensor_tensor(ss[:, 1:2], ss[:, 1:2], ss[:, 2:3], ALU.add), reads=['ss'], writes=['ss'])
            P.op('act', lambda e: e.activation(ss[:, 1:2], ss[:, 1:2], AF.Sqrt, bias=EPS, scale=1.0 / 1024), reads=['ss'], writes=['ss'])
            P.op('dve', lambda e: e.reciprocal(ss[:, 1:2], ss[:, 1:2]), reads=['ss'], writes=['ss'])
            for hf in range(2):
                hs = slice(hf * 512, (hf + 1) * 512)
                P.op('dve', lambda e, hs=hs: e.scalar_tensor_tensor(er[:, hs], pe_[:, hs], ss[:, 1:2], gpl[:, hs], ALU.mult, ALU.mult), reads=[('pe', hf), 'ss', 'gpl'], writes=['er'])
                P.op('act', lambda e, hs=hs: e.activation(gt[:, hs], pg_[:, hs], AF.Sigmoid), reads=[('pg', hf)], writes=['gt'])
            P.op('dve', lambda e: e.tensor_tensor(er[:], er[:], gt[:], ALU.mult), reads=['er', 'gt'], writes=['er'])
            P.op('dve', lambda e, b=b: e.tensor_tensor(xt[b][:], xt[b][:], er[:], ALU.add), reads=['er', ('xt', b)], writes=[('xt', b)])
            P.dma('sp', xdst[tk, :], xt[b][:], reads=[('xt', b)], writes=[('d_xw', t)])

        front(0)
        for t in range(NT):
            if t + 1 < NT:
                front(t + 1)
            back(t)
        P.emit()


WEIGHTS = [('g_mix', [4, 1024]), ('w_in', [4, 1024, 3224]), ('ln_v_g', [4, 4, 64]), ('ln_v_b', [4, 4, 64]), ('w_s', [4, 4, 128, 128]),
           ('b_s', [4, 4, 128]), ('q_norm_g', [4, 64]), ('k_norm_g', [4, 64]), ('conv_w', [4, 5, 1152]), ('a_log', [4, 2, 6]),
           ('dt_bias', [4, 2, 6]), ('o_norm_g', [4, 64]), ('w_out', [4, 1024, 1024]), ('g_ffn', [4, 1024]), ('w_router', [4, 1024, 16]),
           ('w_e_gate', [4, 16, 1024, 1024]), ('w_e_up', [4, 16, 1024, 1024]), ('w_e_down', [4, 16, 1024, 1024]), ('w_ple', [4, 256, 1024]),
           ('g_ple', [4, 1024]), ('g_ple_gate', [4, 1024]), ('w_ple_gate', [4, 1024, 1024])]


def make_consts():
    c = {}
    c['ident'] = np.eye(128, dtype=np.float32)
    c['ones64'] = np.ones((64, 64), np.float32)
    c['ones128'] = np.ones((128, 128), np.float32)
    half = 8
    c['invf'] = (np.float32(500000.0) ** (-np.arange(half, dtype=np.float32) * np.float32(2.0) / np.float32(16))).astype(np.float32)
    a = np.arange(128)[:, None]
    b = np.arange(128)[None, :]
    mA = (a >= b).astype(np.float32)
    mB = (a <= b).astype(np.float32)
    c['mab'] = np.concatenate([mA, mB, mA, mB], axis=1)
    sel = np.zeros((65, 64), np.float32)
    sel[64, :] = 1.0
    c['sel65'] = sel
    p = np.arange(64)[:, None]
    f = np.arange(64)[None, :]
    c['triF'] = (p <= f).astype(np.float32)
    c['triB'] = (p >= f).astype(np.float32)

    def m12(fw, bw):
        return np.ascontiguousarray(np.stack([fw] * 6 + [bw] * 6, axis=1).astype(np.float32))
    c['mW'] = m12(f > p, f < p)
    c['mWt'] = m12(p > f, p < f)
    c['mI'] = m12(f >= p, f <= p)
    c['triS'] = (a < b).astype(np.float32)
    c['iota512'] = np.ascontiguousarray(np.broadcast_to(np.arange(512, dtype=np.float32)[None, :], (128, 512)))
    tpv = np.zeros((128, NT, 2), np.float32)
    tpv[:, :, 0] = np.arange(NT)[None, :]
    tpv[:, :, 1] = np.arange(128)[:, None]
    c['tp'] = tpv
    return c


SCRATCH = [('cs', [S, 16], F32), ('vn', [S, 256], BF16), ('qkT', [6, 128, S], BF16), ('vaug', [S, 390], BF16), ('gate_s', [S, 384], F32),
           ('ab', [S, 24], F32), ('uT', [256, S], F32), ('cT', [1152, S], F32), ('yT', [1024, S], BF16), ('v_tm', [S, 384], F32),
           ('k_tm', [S, 384], F32), ('qT_g', [384, S], BF16), ('kT_g', [384, S], BF16), ('gb', [S, 24], F32), ('o_fb', [2, S, 384], F32),
           ('xw', [S, 1024], F32), ('hb', [S, 1024], BF16)]


def build(n_layers=4, phases=None, dbg=()):
    P = Prog()
    D = {}
    D['x'] = P.dram('x', [S, 1024], F32, 'ExternalInput')
    D['p'] = P.dram('p', [n_layers, S, 256], F32, 'ExternalInput')
    D['positions'] = P.dram('positions', [128, NT], I32, 'ExternalInput')
    for n, shp in WEIGHTS:
        D[n] = P.dram(n, [n_layers] + list(shp[1:]), F32, 'ExternalInput')
    for n, v in make_consts().items():
        D[n] = P.dram(n, list(v.shape), F32, 'ExternalInput')
    for n, shp, dt in SCRATCH:
        D[n] = P.dram(n, shp, dt, 'ExternalOutput' if n in dbg else 'Internal')
    D['out'] = P.dram('out', [S, 1024], F32, 'ExternalOutput')
    allp = phases is None
    if allp or 'R' in phases:
        phase_rope(P, D)
    for l in range(n_layers):
        first = (l == 0)
        last = (l == n_layers - 1)
        if allp or 'A' in phases:
            phase_A(P, l, D, first)
        if allp or 'B' in phases:
            phase_B(P, l, D)
        if allp or 'C' in phases:
            phase_C(P, l, D)
        if allp or 'D1' in phases:
            phase_D1(P, l, D)
        if allp or 'D2' in phases:
            phase_D2(P, l, D)
        if allp or 'E' in phases:
            phase_E(P, l, D, first)
        if allp or 'F' in phases:
            phase_F(P, l, D)
        if allp or 'G' in phases:
            phase_G(P, l, D, last and allp)
    return P


def kernel(**inputs):
    n = 8
    P = build(4)
    consts = make_consts()
    shared = {k: np.ascontiguousarray(np.asarray(inputs[k], dtype=np.float32)) for k, _ in WEIGHTS}
    shared.update(consts)
    x = np.asarray(inputs['x'], dtype=np.float32)
    p = np.asarray(inputs['p'], dtype=np.float32)
    pos = np.asarray(inputs['positions']).astype(np.int32)
    in_maps = []
    for c in range(n):
        m = dict(shared)
        m['x'] = np.ascontiguousarray(x[c])
        m['p'] = np.ascontiguousarray(p[:, c])
        m['positions'] = np.ascontiguousarray(pos[c].reshape(NT, 128).T)
        in_maps.append(m)
    res = run_bass_kernel_spmd(P.nc, in_maps, core_ids=list(range(n)))
    return np.stack([np.asarray(res.results[c]['out'], dtype=np.float32) for c in range(n)], axis=0)
```

```python
import numpy as np
from contextlib import ExitStack
import concourse.bass as bass
import concourse.mybir as mybir
from concourse.bass_utils import run_bass_kernel_spmd

F32 = mybir.dt.float32
BF16 = mybir.dt.bfloat16
I32 = mybir.dt.int32
ALU = mybir.AluOpType
AF = mybir.ActivationFunctionType
AX = mybir.AxisListType

ENGS = ['pe', 'dve', 'act', 'pool', 'sp']
DMA_ENGS = ['sp', 'pool', 'act']
NDS = 8
EPS = 1e-6
S = 4096
NT = 32


NOWAW = frozenset(['d_vn', 'd_qkT', 'd_vaug', 'd_gate', 'd_ab', 'd_uT', 'd_cT', 'd_vtm', 'd_ktm', 'd_qkTg', 'd_yT', 'd_ofb', 'd_hb', 'd_gb', 'd_cs'])


class Prog:
    def __init__(self):
        self.nc = bass.Bass("TRN2", target_bir_lowering=False)
        self.stack = ExitStack()
        self.sems = {}
        for e in ENGS:
            self.sems[e] = self.stack.enter_context(self.nc.semaphore('s_' + e))
        self.dcount = {}
        for e in DMA_ENGS:
            for i in range(NDS):
                k = 'd_%s_%d' % (e, i)
                self.sems[k] = self.stack.enter_context(self.nc.semaphore(k))
                self.dcount[k] = 0
        self.dnext = {e: 0 for e in DMA_ENGS}
        self.cnt = {e: 0 for e in ENGS}
        self.waited = {e: {} for e in ENGS}
        self.q = {e: [] for e in ENGS}
        self.lastw = {}
        self.readers = {}
        self.multiw = {}
        self.nops = 0
        self.xkeys = set(['pT', 'pv', 'pq', 'pk', 'pbv', 'pg', 'pf', 'ptr', 'pm', 'pss', 'ppv', 'pd', 'ptb', 'po', 'pbig', 'pl', 'pu', 'py', 'pe', 'pA', 'pB', 'pC', 'pD', 'pS'])

    def _deps(self, eng, reads, writes):
        deps = {}

        def add(m):
            if m is None:
                return
            k, v = m
            if eng == 'pe' and k == 'pe':
                return
            if deps.get(k, 0) < v:
                deps[k] = v
        for r in reads:
            add(self.lastw.get(r))
            for m in self.multiw.get(r, ()):
                add(m)
        for w in writes:
            if w in NOWAW:
                continue
            add(self.lastw.get(w))
            for m in self.readers.get(w, ()):
                add(m)
        out = []
        wd = self.waited[eng]
        for k, v in deps.items():
            if wd.get(k, 0) < v:
                wd[k] = v
                out.append((k, v))
        return out

    def _mark(self, mark, reads, writes):
        for w in writes:
            if w in NOWAW:
                self.multiw.setdefault(w, []).append(mark)
                continue
            self.lastw[w] = mark
            self.readers[w] = []
        for r in reads:
            if r in writes:
                continue
            self.readers.setdefault(r, []).append(mark)

    cut = None
    pc = 0

    def isx(self, k):
        n = k[0] if isinstance(k, tuple) else k
        return isinstance(n, str) and n in self.xkeys

    def op(self, eng, fn, reads=(), writes=()):
        self.pc += 1
        if self.cut is not None and self.pc > self.cut:
            return
        xr = [r for r in reads if self.isx(r) and r not in writes]
        if xr:
            writes = list(writes) + xr
        waits = self._deps(eng, reads, writes)
        self.cnt[eng] += 1
        mark = (eng, self.cnt[eng])
        self.q[eng].append((waits, fn, (eng, 1)))
        self._mark(mark, reads, writes)
        self.nops += 1

    def dma(self, eng, out, in_, reads=(), writes=(), **kw):
        self.pc += 1
        if self.cut is not None and self.pc > self.cut:
            return
        waits = self._deps(eng, reads, writes)
        i = self.dnext[eng]
        self.dnext[eng] = (i + 1) % NDS
        k = 'd_%s_%d' % (eng, i)
        c = self.dcount[k]
        wd = self.waited[eng]
        if c > 0 and wd.get(k, 0) < 16 * c:
            wd[k] = 16 * c
            waits.append((k, 16 * c))
        self.dcount[k] = c + 1
        mark = (k, 16 * (c + 1))
        self.q[eng].append((waits, (lambda e: e.dma_start(out=out, in_=in_, **kw)), (k, 16)))
        self._mark(mark, reads, writes)
        self.nops += 1

    _breg = None

    def breg(self, e):
        if self._breg is None:
            self._breg = e.to_reg(S - 1)
        return self._breg

    def idma(self, fn, reads=(), writes=()):
        eng = 'pool'
        self.pc += 1
        waits = self._deps(eng, reads, writes)
        i = self.dnext[eng]
        self.dnext[eng] = (i + 1) % NDS
        k = 'd_%s_%d' % (eng, i)
        c = self.dcount[k]
        wd = self.waited[eng]
        if c > 0 and wd.get(k, 0) < 16 * c:
            wd[k] = 16 * c
            waits.append((k, 16 * c))
        self.dcount[k] = c + 1
        mark = (k, 16 * (c + 1))
        self.q[eng].append((waits, fn, (k, 16)))
        self._mark(mark, reads, writes)
        self.nops += 1

    def barrier(self):
        for e in ENGS:
            waits = []
            wd = self.waited[e]
            for o in ENGS:
                if o != e and self.cnt[o] > wd.get(o, 0):
                    wd[o] = self.cnt[o]
                    waits.append((o, self.cnt[o]))
            for k, c in self.dcount.items():
                if 16 * c > wd.get(k, 0):
                    wd[k] = 16 * c
                    waits.append((k, 16 * c))
            if waits:
                self.q[e].append((waits, None, None))
        self.lastw = {}
        self.readers = {}
        self.multiw = {}

    def emit(self):
        self.barrier()
        nc = self.nc
        sems = self.sems
        q = self.q

        def replay(name, e):
            for waits, fn, inc in q[name]:
                for k, v in waits:
                    e.wait_ge(sems[k], v)
                if fn is not None:
                    ins = fn(e)
                    ins.then_inc(sems[inc[0]], inc[1])

        with nc.Block() as block:
            @block.tensor
            def _(e):
                replay('pe', e)

            @block.vector
            def _(e):
                replay('dve', e)

            @block.scalar
            def _(e):
                replay('act', e)

            @block.gpsimd
            def _(e):
                replay('pool', e)

            @block.sync
            def _(e):
                replay('sp', e)
        self.q = {e: [] for e in ENGS}

    uid = 0

    def sb(self, stack, name, shape, dt):
        self.uid += 1
        return stack.enter_context(self.nc.sbuf_tensor('%s_%d' % (name, self.uid), list(shape), dt))

    def ps(self, stack, name, shape, dt=F32, keys=()):
        for k in keys:
            self.xkeys.add(k)
        self.uid += 1
        return stack.enter_context(self.nc.psum_tensor('%s_%d' % (name, self.uid), list(shape), dt))

    def dram(self, name, shape, dt, kind="Internal"):
        return self.nc.dram_tensor(name, list(shape), dt, kind=kind).ap()


def ssl(a, n, d):
    return slice(a, a + (n - 1) * d + 1, d)


def bc(ap, shape):
    return ap.to_broadcast(list(shape))


def phase_rope(P, D):
    with ExitStack() as s:
        pi_ = P.sb(s, 'r_pi', [128, NT], I32)
        pf = P.sb(s, 'r_pf', [128, NT], F32)
        invf = P.sb(s, 'r_invf', [128, 8], F32)
        ang = P.sb(s, 'r_ang', [128, 2, NT, 8], F32)
        kk = P.sb(s, 'r_kk', [128, 2, NT, 8], F32)
        ki = P.sb(s, 'r_ki', [128, 2, NT, 8], I32)
        cs = P.sb(s, 'r_cs', [128, NT, 16], F32)
        P.dma('sp', pi_[:], D['positions'], writes=['pi'])
        P.dma('sp', invf[:], D['invf'].partition_broadcast(128), writes=['invf'])
        P.op('dve', lambda e: e.tensor_copy(pf[:], pi_[:]), reads=['pi'], writes=['pf'])
        P.op('dve', lambda e: e.tensor_tensor(ang[:, 1], bc(pf[:].unsqueeze(2), [128, NT, 8]), bc(invf[:].unsqueeze(1), [128, NT, 8]), ALU.mult),
             reads=['pf', 'invf'], writes=['ang1'])
        P.op('dve', lambda e: e.tensor_scalar(ang[:, 0], ang[:, 1], float(np.pi / 2), None, ALU.add), reads=['ang1'], writes=['ang0'])
        A = ang[:].rearrange('p a t c -> p (a t c)')
        K = kk[:].rearrange('p a t c -> p (a t c)')
        KI = ki[:].rearrange('p a t c -> p (a t c)')
        P.op('dve', lambda e: e.tensor_scalar(K, A, float(1.0 / (2 * np.pi)), None, ALU.mult), reads=['ang0', 'ang1'], writes=['kk'])
        P.op('dve', lambda e: e.tensor_copy(KI, K), reads=['kk'], writes=['ki'])
        P.op('dve', lambda e: e.tensor_copy(K, KI), reads=['ki'], writes=['kk'])
        C1 = 6.28125
        C2 = float(2 * np.pi - 6.28125)
        P.op('dve', lambda e: e.scalar_tensor_tensor(A, K, -C1, A, ALU.mult, ALU.add), reads=['kk', 'ang0', 'ang1'], writes=['ang'])
        P.op('dve', lambda e: e.scalar_tensor_tensor(A, K, -C2, A, ALU.mult, ALU.add), reads=['kk', 'ang'], writes=['ang'])
        P.op('dve', lambda e: e.tensor_scalar(A, A, 3.1415925, -3.1415925, ALU.min, ALU.max), reads=['ang'], writes=['ang'])
        P.op('act', lambda e: e.activation(cs[:, :, 0:8], ang[:, 0], AF.Sin), reads=['ang'], writes=['cs0'])
        P.op('act', lambda e: e.activation(cs[:, :, 8:16], ang[:, 1], AF.Sin), reads=['ang'], writes=['cs1'])
        P.dma('sp', D['cs'].rearrange('(t p) c -> p t c', p=128), cs[:], reads=['cs0', 'cs1'], writes=['d_cs'])
        P.emit()


def phase_A(P, l, D, first):
    xsrc = D['x'] if first else D['xw']
    with ExitStack() as s:
        wbf = P.sb(s, 'a_wbf', [128, 8, 3224], BF16)
        gmix = P.sb(s, 'a_gmix', [128, 1024], F32)
        lng = P.sb(s, 'a_lng', [128, 256], F32)
        lnb = P.sb(s, 'a_lnb', [128, 256], F32)
        qkg = P.sb(s, 'a_qkg', [128, 2, 64], F32)
        cs = P.sb(s, 'a_cs', [128, NT, 16], F32)
        idb = P.sb(s, 'a_idb', [128, 128], BF16)
        xts = [P.sb(s, 'a_xt%d' % i, [128, 1024], F32) for i in range(2)]
        junk = P.sb(s, 'a_junk', [128, 1024], BF16)
        ss = P.sb(s, 'a_ss', [128, 2], F32)
        xn = P.sb(s, 'a_xn', [128, 1024], BF16)
        xnT = [P.sb(s, 'a_xnT%d' % i, [128, 8, 512], BF16) for i in range(2)]
        ge = P.sb(s, 'a_ge', [128, 4, 64], F32)
        cen = P.sb(s, 'a_cen', [128, 4, 64], F32)
        sq = P.sb(s, 'a_sq', [128, 4, 64], F32)
        m4 = P.sb(s, 'a_m4', [128, 8], F32)
        vnb = [P.sb(s, 'a_vnb%d' % i, [128, 256], BF16) for i in range(2)]
        sqq = P.sb(s, 'a_sqq', [128, 12, 64], F32)
        ss12 = P.sb(s, 'a_ss12', [128, 12], F32)
        qk32 = P.sb(s, 'a_qk32', [128, 12, 64], F32)
        rt = P.sb(s, 'a_rt', [128, 4, 12, 8], F32)
        qkb = P.sb(s, 'a_qkb', [128, 12, 64], BF16)
        qkTs = [P.sb(s, 'a_qkTs%d' % i, [128, 6, 128], BF16) for i in range(2)]
        vaug = [P.sb(s, 'a_vaug%d' % i, [128, 6, 65], BF16) for i in range(2)]
        gs = [P.sb(s, 'a_gs%d' % i, [128, 408], F32) for i in range(2)]
        fo = [P.sb(s, 'a_fo%d' % i, [128, 512], F32) for i in range(2)]
        pT = P.ps(s, 'a_pT', [128, 1024], BF16)
        pv = P.ps(s, 'a_pv', [128, 512])
        pq = P.ps(s, 'a_pq', [128, 512])
        pk = P.ps(s, 'a_pk', [128, 512])
        pbv = P.ps(s, 'a_pbv', [128, 512])
        pg = P.ps(s, 'a_pg', [128, 512])
        pf = [P.ps(s, 'a_pf%d' % i, [128, 512]) for i in range(2)]

        for k in range(8):
            P.dma('pool', wbf[:, k, :], D['w_in'][l, k * 128:(k + 1) * 128, :], writes=[('wbf', k)])
        P.dma('sp', gmix[:], D['g_mix'][l].partition_broadcast(128), writes=['gmix'])
        P.dma('sp', lng[:], D['ln_v_g'][l].rearrange('g d -> (g d)').partition_broadcast(128), writes=['lng'])
        P.dma('sp', lnb[:], D['ln_v_b'][l].rearrange('g d -> (g d)').partition_broadcast(128), writes=['lnb'])
        P.dma('sp', qkg[:, 0, :], D['q_norm_g'][l].partition_broadcast(128), writes=['qkg0'])
        P.dma('sp', qkg[:, 1, :], D['k_norm_g'][l].partition_broadcast(128), writes=['qkg1'])
        P.dma('sp', cs[:], D['cs'].rearrange('(t p) c -> p t c', p=128), reads=['d_cs'], writes=['cs'])
        P.dma('pool', idb[:], D['ident'], writes=['idb'])
        for i in range(2):
            P.op('pool', lambda e, i=i: e.memset(vaug[i][:, :, 64:65], 1.0), writes=[('vaug', i)])
        WB = [('wbf', k) for k in range(8)]
        fcount = [0]

        def front(t):
            if True:
                g, j = t // 4, t % 4
                XT = xnT[g % 2]
                kxt = ('xnT', g % 2)
                b = t % 2
                xt = xts[b]
                P.dma('sp', xt[:], xsrc[t * 128:(t + 1) * 128, :], writes=[('xt', b)])
                P.op('dve', lambda e, b=b: e.memset(ss[:, b:b + 1], 0.0), writes=[('ss', b)])
                P.op('act', lambda e, xt=xt, b=b: e.activation(junk[:], xt[:], AF.Square, accum_out=ss[:, b:b + 1]),
                     reads=[('xt', b), ('ss', b)], writes=['junk', ('ss', b)])
                P.op('act', lambda e, b=b: e.activation(ss[:, b:b + 1], ss[:, b:b + 1], AF.Sqrt, bias=EPS, scale=1.0 / 1024),
                     reads=[('ss', b)], writes=[('ss', b)])
                P.op('dve', lambda e, b=b: e.reciprocal(ss[:, b:b + 1], ss[:, b:b + 1]), reads=[('ss', b)], writes=[('ss', b)])
                P.op('dve', lambda e, xt=xt, b=b: e.scalar_tensor_tensor(xn[:], xt[:], ss[:, b:b + 1], gmix[:], ALU.mult, ALU.mult),
                     reads=[('xt', b), ('ss', b), 'gmix'], writes=['xn'])
                for k in range(8):
                    P.op('pe', lambda e, k=k: e.transpose(pT[:, k * 128:(k + 1) * 128], xn[:, k * 128:(k + 1) * 128], idb[:]),
                         reads=['xn', 'idb'], writes=['pT'])
                P.op('act', lambda e, XT=XT, j=j: e.copy(XT[:, :, j * 128:(j + 1) * 128], pT[:].rearrange('p (k t) -> p k t', t=128)),
                     reads=['pT'], writes=[kxt + (j,)])

        def back(t):
            if True:
                g, j = t // 4, t % 4
                XT = xnT[g % 2]
                kxt = ('xnT', g % 2)
                b = t % 2
                for (pp, nm, c0, c1) in ((pv, 'pv', 256, 512), (pq, 'pq', 512, 896), (pk, 'pk', 896, 1280), (pbv, 'pbv', 1280, 1664), (pg, 'pg', 2816, 3224)):
                    for k in range(8):
                        P.op('pe', lambda e, pp=pp, k=k, c0=c0, c1=c1, XT=XT, j=j: e.matmul(pp[:, 0:c1 - c0], XT[:, k, j * 128:(j + 1) * 128], wbf[:, k, c0:c1], start=(k == 0), stop=(k == 7)),
                             reads=[kxt + (j,), ('wbf', k)], writes=[nm])
                GE = ge[:].rearrange('p a b -> p (a b)')
                P.op('act', lambda e: e.activation(GE, pv[:, 0:256], AF.Gelu_apprx_tanh), reads=['pv'], writes=['ge'])
                P.op('dve', lambda e: e.tensor_reduce(m4[:, 0:4], ge[:], AX.X, ALU.add), reads=['ge'], writes=['m4a'])
                P.op('dve', lambda e: e.tensor_scalar(m4[:, 0:4], m4[:, 0:4], 1.0 / 64, None, ALU.mult), reads=['m4a'], writes=['m4a'])
                P.op('dve', lambda e: e.tensor_tensor(cen[:], ge[:], bc(m4[:, 0:4].unsqueeze(2), [128, 4, 64]), ALU.subtract), reads=['ge', 'm4a'], writes=['cen'])
                P.op('act', lambda e: e.activation(sq[:], cen[:], AF.Square), reads=['cen'], writes=['sq'])
                P.op('dve', lambda e: e.tensor_reduce(m4[:, 4:8], sq[:], AX.X, ALU.add), reads=['sq'], writes=['m4b'])
                P.op('act', lambda e: e.activation(m4[:, 4:8], m4[:, 4:8], AF.Sqrt, bias=EPS, scale=1.0 / 64), reads=['m4b'], writes=['m4b'])
                P.op('dve', lambda e: e.reciprocal(m4[:, 4:8], m4[:, 4:8]), reads=['m4b'], writes=['m4b'])
                P.op('dve', lambda e: e.tensor_tensor(cen[:], cen[:], bc(m4[:, 4:8].unsqueeze(2), [128, 4, 64]), ALU.mult), reads=['cen', 'm4b'], writes=['cen'])
                CEN = cen[:].rearrange('p a b -> p (a b)')
                P.op('pool', lambda e: e.tensor_tensor(CEN, CEN, lng[:], ALU.mult), reads=['cen', 'lng'], writes=['cen'])
                P.op('pool', lambda e, b=b: e.tensor_tensor(vnb[b][:], CEN, lnb[:], ALU.add), reads=['cen', 'lnb'], writes=[('vnb', b)])
                P.dma('sp', D['vn'][t * 128:(t + 1) * 128, :], vnb[b][:], reads=[('vnb', b)], writes=['d_vn'])
                P.op('act', lambda e: e.activation(sqq[:, 0:6, :].rearrange('p a b -> p (a b)'), pq[:, 0:384], AF.Square), reads=['pq'], writes=['sqq0'])
                P.op('act', lambda e: e.activation(sqq[:, 6:12, :].rearrange('p a b -> p (a b)'), pk[:, 0:384], AF.Square), reads=['pk'], writes=['sqq1'])
                P.op('dve', lambda e: e.tensor_reduce(ss12[:], sqq[:], AX.X, ALU.add), reads=['sqq0', 'sqq1'], writes=['ss12'])
                P.op('act', lambda e: e.activation(ss12[:], ss12[:], AF.Sqrt, bias=EPS, scale=1.0 / 64), reads=['ss12'], writes=['ss12'])
                P.op('dve', lambda e: e.reciprocal(ss12[:], ss12[:]), reads=['ss12'], writes=['ss12'])
                P.op('dve', lambda e: e.tensor_tensor(qk32[:, 0:6, :], pq[:, 0:384].rearrange('p (a b) -> p a b', b=64), bc(ss12[:, 0:6].unsqueeze(2), [128, 6, 64]), ALU.mult),
                     reads=['pq', 'ss12'], writes=['qk32a'])
                P.op('dve', lambda e: e.tensor_tensor(qk32[:, 6:12, :], pk[:, 0:384].rearrange('p (a b) -> p a b', b=64), bc(ss12[:, 6:12].unsqueeze(2), [128, 6, 64]), ALU.mult),
                     reads=['pk', 'ss12'], writes=['qk32b'])
                P.op('pool', lambda e: e.tensor_tensor(qk32[:, 0:6, :], qk32[:, 0:6, :], bc(qkg[:, 0:1, :], [128, 6, 64]), ALU.mult), reads=['qk32a', 'qkg0'], writes=['qk32a'])
                P.op('pool', lambda e: e.tensor_tensor(qk32[:, 6:12, :], qk32[:, 6:12, :], bc(qkg[:, 1:2, :], [128, 6, 64]), ALU.mult), reads=['qk32b', 'qkg1'], writes=['qk32b'])
                cosb = bc(cs[:, t:t + 1, 0:8], [128, 12, 8])
                sinb = bc(cs[:, t:t + 1, 8:16], [128, 12, 8])
                x1 = qk32[:, :, 0:8]
                x2 = qk32[:, :, 8:16]
                P.op('pool', lambda e, cosb=cosb: e.tensor_tensor(rt[:, 0], x1, cosb, ALU.mult), reads=['qk32a', 'qk32b', 'cs'], writes=['rt0'])
                P.op('pool', lambda e, sinb=sinb: e.tensor_tensor(rt[:, 1], x2, sinb, ALU.mult), reads=['qk32a', 'qk32b', 'cs'], writes=['rt1'])
                P.op('dve', lambda e, cosb=cosb: e.tensor_tensor(rt[:, 2], x2, cosb, ALU.mult), reads=['qk32a', 'qk32b', 'cs'], writes=['rt2'])
                P.op('dve', lambda e, sinb=sinb: e.tensor_tensor(rt[:, 3], x1, sinb, ALU.mult), reads=['qk32a', 'qk32b', 'cs'], writes=['rt3'])
                P.op('act', lambda e: e.copy(qkb[:], qk32[:]), reads=['qk32a', 'qk32b'], writes=['qkb'])
                P.op('dve', lambda e: e.tensor_tensor(qkb[:, :, 0:8], rt[:, 0], rt[:, 1], ALU.subtract), reads=['rt0', 'rt1', 'qkb'], writes=['qkb'])
                P.op('dve', lambda e: e.tensor_tensor(qkb[:, :, 8:16], rt[:, 2], rt[:, 3], ALU.add), reads=['rt2', 'rt3', 'qkb'], writes=['qkb'])
                for i in range(6):
                    P.op('pe', lambda e, i=i: e.transpose(pT[:, i * 128:(i + 1) * 128], qkb[:, 2 * i:2 * i + 2, :].rearrange('p a b -> p (a b)'), idb[:]),
                         reads=['qkb', 'idb'], writes=['pT'])
                P.op('act', lambda e, b=b: e.copy(qkTs[b][:], pT[:, 0:768].rearrange('p (k t) -> p k t', t=128)), reads=['pT'], writes=[('qkTs', b)])
                P.dma('sp', D['qkT'][:, :, t * 128:(t + 1) * 128].rearrange('i p t -> p i t'), qkTs[b][:], reads=[('qkTs', b)], writes=['d_qkT'])
                P.op('act', lambda e, b=b: e.copy(vaug[b][:, :, 0:64], pbv[:, 0:384].rearrange('p (a b) -> p a b', b=64)), reads=['pbv', ('vaug', b)], writes=[('vaug', b)])
                P.dma('sp', D['vaug'][t * 128:(t + 1) * 128, :], vaug[b][:].rearrange('p a b -> p (a b)'), reads=[('vaug', b)], writes=['d_vaug'])
                P.op('act', lambda e, b=b: e.activation(gs[b][:, 0:384], pg[:, 0:384], AF.Silu), reads=['pg'], writes=[('gs', b)])
                P.op('dve', lambda e, b=b: e.tensor_copy(gs[b][:, 384:408], pg[:, 384:408]), reads=['pg', ('gs', b)], writes=[('gs', b)])
                P.dma('sp', D['gate_s'][t * 128:(t + 1) * 128, :], gs[b][:, 0:384], reads=[('gs', b)], writes=['d_gate'])
                P.dma('sp', D['ab'][t * 128:(t + 1) * 128, :], gs[b][:, 384:408], reads=[('gs', b)], writes=['d_ab'])

        def fmaj(g):
            XT = xnT[g % 2]
            kxt = ('xnT', g % 2)
            allx = [kxt + (j,) for j in range(4)]
            for ci in range(11):
                c0 = ci * 128 if ci < 2 else 1664 + (ci - 2) * 128
                fb = fcount[0] % 2
                fcount[0] += 1
                for k in range(8):
                    P.op('pe', lambda e, fb=fb, k=k, c0=c0, XT=XT: e.matmul(pf[fb][:], wbf[:, k, c0:c0 + 128], XT[:, k, :], start=(k == 0), stop=(k == 7)),
                         reads=allx + [('wbf', k)], writes=[('pf', fb)])
                if ci < 2:
                    P.op('act', lambda e, fb=fb: e.activation(fo[fb][:], pf[fb][:], AF.Gelu_apprx_tanh), reads=[('pf', fb)], writes=[('fo', fb)])
                    P.dma('sp', D['uT'][ci * 128:(ci + 1) * 128, g * 512:(g + 1) * 512], fo[fb][:], reads=[('fo', fb)], writes=['d_uT'])
                else:
                    P.op('dve', lambda e, fb=fb: e.tensor_copy(fo[fb][:], pf[fb][:]), reads=[('pf', fb)], writes=[('fo', fb)])
                    P.dma('sp', D['cT'][(ci - 2) * 128:(ci - 1) * 128, g * 512:(g + 1) * 512], fo[fb][:], reads=[('fo', fb)], writes=['d_cT'])

        front(0)
        for t in range(NT):
            if t + 1 < NT:
                front(t + 1)
            back(t)
            if t % 4 == 3:
                fmaj(t // 4)
        P.emit()


def phase_B(P, l, D):
    with ExitStack() as s:
        ws32 = P.sb(s, 'b_ws32', [128, 4, 128], F32)
        idf = P.sb(s, 'b_idf', [128, 128], F32)
        wsT = P.sb(s, 'b_wsT', [128, 4, 128], BF16)
        bias = P.sb(s, 'b_bias', [64, 4, 128], F32)
        vn = [P.sb(s, 'b_vn%d' % i, [128, 4, 256], BF16) for i in range(2)]
        ut = [P.sb(s, 'b_ut%d' % i, [64, 4, 512], F32) for i in range(2)]
        mx = P.sb(s, 'b_mx', [64, 4, 128], F32)
        yb = [P.sb(s, 'b_yb%d' % i, [64, 4, 512], BF16) for i in range(2)]
        ptr = P.ps(s, 'b_ptr', [128, 512])
        pm = [P.ps(s, 'b_pm%d' % i, [64, 512]) for i in range(4)]
        P.dma('sp', ws32[:], D['w_s'][l].rearrange('g i j -> i g j'), writes=['ws32'])
        P.dma('sp', idf[:], D['ident'], writes=['idf'])
        P.dma('sp', bias[:].rearrange('p g i -> p (g i)'), D['b_s'][l].rearrange('g i -> (g i)').partition_broadcast(64), writes=['bias'])
        for g in range(4):
            P.op('pe', lambda e, g=g: e.transpose(ptr[:, g * 128:(g + 1) * 128], ws32[:, g, :], idf[:]), reads=['ws32', 'idf'], writes=['ptr'])
        P.op('dve', lambda e: e.tensor_copy(wsT[:].rearrange('p g i -> p (g i)'), ptr[:]), reads=['ptr'], writes=['wsT'])
        for it in range(8):
            b = it % 2
            P.dma('sp', vn[b][:], D['vn'][it * 512:(it + 1) * 512, :].rearrange('(c p) n -> p c n', p=128), reads=['d_vn'], writes=[('vn', b)])
            P.dma('sp', ut[b][:], D['uT'][:, it * 512:(it + 1) * 512].rearrange('(g d) t -> d g t', d=64), reads=['d_uT'], writes=[('ut', b)])
            for g in range(4):
                for c in range(4):
                    P.op('pe', lambda e, g=g, c=c, b=b: e.matmul(pm[g][:, c * 128:(c + 1) * 128], vn[b][:, c, g * 64:(g + 1) * 64], wsT[:, g, :], start=True, stop=True),
                         reads=[('vn', b), 'wsT'], writes=[('pm', g)])
                P.op('dve', lambda e, g=g: e.tensor_tensor(mx[:], pm[g][:].rearrange('p (c i) -> p c i', i=128), bc(bias[:, g:g + 1, :], [64, 4, 128]), ALU.add),
                     reads=[('pm', g), 'bias'], writes=['mx'])
                P.op('dve', lambda e, g=g, b=b: e.tensor_tensor(yb[b][:, g, :], mx[:].rearrange('p c i -> p (c i)'), ut[b][:, g, :], ALU.mult),
                     reads=['mx', ('ut', b)], writes=[('yb', b)])
            P.dma('sp', D['yT'][0:256, it * 512:(it + 1) * 512].rearrange('(g d) t -> d g t', d=64), yb[b][:], reads=[('yb', b)], writes=['d_yT'])
        P.emit()


PATS = (1, 4, 16)
KPAD = 1024


def phase_C(P, l, D):
    with ExitStack() as s:
        vs = {}
        for d in PATS:
            nt = d * (S // d // 128 + 1)
            vs[d] = P.sb(s, 'c_vs%d' % d, [128, nt, 390], BF16)
        mab = P.sb(s, 'c_mab', [128, 512], BF16)
        sel = P.sb(s, 'c_sel', [65, 64], F32)
        qh = [P.sb(s, 'c_qh%d' % i, [64, S], BF16) for i in range(2)]
        kh = [P.sb(s, 'c_kh%d' % i, [64, S + 2 * KPAD], BF16) for i in range(2)]
        pex = [P.sb(s, 'c_pex%d' % i, [128, 512], BF16) for i in range(3)]
        acc = P.sb(s, 'c_acc', [65, S], F32)
        rd = P.sb(s, 'c_rd', [64, 512], F32)
        yb = [P.sb(s, 'c_yb%d' % i, [64, 512], BF16) for i in range(2)]
        pss = [P.ps(s, 'c_ps%d' % i, [128, 512]) for i in range(3)]
        ppv = [P.ps(s, 'c_pv%d' % i, [65, 512]) for i in range(3)]
        pd = P.ps(s, 'c_pd', [64, 512])
        P.dma('pool', mab[:], D['mab'], writes=['mab'])
        P.dma('sp', sel[:], D['sel65'], writes=['sel'])
        for i in range(2):
            P.op('pool', lambda e, i=i: e.memset(kh[i][:, 0:KPAD], 0.0), writes=[('kh', i)])
            P.op('pool', lambda e, i=i: e.memset(kh[i][:, KPAD + S:], 0.0), writes=[('kh', i)])
        for d in PATS:
            L = S // d
            nqb = L // 128
            P.op('pool', lambda e, d=d: e.memset(vs[d][:].rearrange('p a b -> p (a b)'), 0.0), writes=[('vs', d)])
            vsrc = D['vaug'].rearrange('(j r) c -> r j c', r=d)
            for r in range(d):
                tb = r * (nqb + 1)
                if nqb > 1:
                    P.dma('sp', vs[d][:, tb + 1:tb + nqb, :], vsrc[r, 64:64 + (nqb - 1) * 128, :].rearrange('(k p) c -> p k c', p=128),
                          reads=['d_vaug', ('vs', d)], writes=[('vs', d)])
                P.dma('sp', vs[d][64:128, tb, :], vsrc[r, 0:64, :], reads=['d_vaug', ('vs', d)], writes=[('vs', d)])
                P.dma('sp', vs[d][0:64, tb + nqb, :], vsrc[r, L - 64:L, :], reads=['d_vaug', ('vs', d)], writes=[('vs', d)])
        for h in range(6):
            hb = h % 2
            P.dma('sp', qh[hb][:], D['qkT'][h // 2, (h % 2) * 64:(h % 2) * 64 + 64, :], reads=['d_qkT'], writes=[('qh', hb)])
            P.dma('sp', kh[hb][:, KPAD:KPAD + S], D['qkT'][3 + h // 2, (h % 2) * 64:(h % 2) * 64 + 64, :], reads=['d_qkT'], writes=[('kh', hb)])
            its = []
            for pi, d in enumerate(PATS):
                L = S // d
                nqb = L // 128
                for r in range(d):
                    tb = r * (nqb + 1)
                    for qb0 in range(0, nqb, 2):
                        its.append((pi, d, r, tb, qb0))

            def stage1(n):
                pi, d, r, tb, qb0 = its[n]
                ib = n % 3
                combos = ((qb0, qb0), (qb0 + 1, qb0), (qb0 + 1, qb0 + 1), (qb0 + 2, qb0 + 1))
                for ci, (kt, qb) in enumerate(combos):
                    k0 = KPAD + r + d * (kt * 128 - 64)
                    q0 = r + d * (qb * 128)
                    P.op('pe', lambda e, ib=ib, ci=ci, k0=k0, q0=q0, d=d, hb=hb: e.matmul(
                        pss[ib][:, ci * 128:(ci + 1) * 128], kh[hb][:, ssl(k0, 128, d)], qh[hb][:, ssl(q0, 128, d)], start=True, stop=True),
                        reads=[('kh', hb), ('qh', hb)], writes=[('pss', ib)])
                P.op('act', lambda e, ib=ib: e.activation(pex[ib][:], pss[ib][:], AF.Exp, scale=0.125), reads=[('pss', ib)], writes=[('pex', ib)])
                P.op('dve', lambda e, ib=ib: e.tensor_tensor(pex[ib][:], pex[ib][:], mab[:], ALU.mult), reads=[('pex', ib), 'mab'], writes=[('pex', ib)])

            def stage2(n):
                pi, d, r, tb, qb0 = its[n]
                ib = n % 3
                combos = ((qb0, qb0), (qb0 + 1, qb0), (qb0 + 1, qb0 + 1), (qb0 + 2, qb0 + 1))
                for ci, (kt, qb) in enumerate(combos):
                    qi = qb - qb0
                    P.op('pe', lambda e, ib=ib, ci=ci, kt=kt, qi=qi, d=d, tb=tb, h=h: e.matmul(
                        ppv[ib][:, qi * 128:(qi + 1) * 128], vs[d][:, tb + kt, h * 65:(h + 1) * 65], pex[ib][:, ci * 128:(ci + 1) * 128],
                        start=(ci % 2 == 0), stop=(ci % 2 == 1)), reads=[('vs', d), ('pex', ib)], writes=[('ppv', ib)])
                a0 = r + d * (qb0 * 128)
                av = acc[:, ssl(a0, 256, d)]
                if pi == 0:
                    P.op('dve', lambda e, av=av, ib=ib: e.tensor_copy(av, ppv[ib][:, 0:256]), reads=[('ppv', ib)], writes=['acc'])
                else:
                    P.op('dve', lambda e, av=av, ib=ib: e.tensor_tensor(av, av, ppv[ib][:, 0:256], ALU.add), reads=[('ppv', ib), 'acc'], writes=['acc'])

            stage1(0)
            for n in range(len(its)):
                if n + 1 < len(its):
                    stage1(n + 1)
                stage2(n)
            for c4 in range(8):
                yb_ = yb[c4 % 2]
                P.op('pe', lambda e, c4=c4: e.matmul(pd[:], sel[:], acc[:, c4 * 512:(c4 + 1) * 512], start=True, stop=True), reads=['sel', 'acc'], writes=['pd'])
                P.op('dve', lambda e: e.reciprocal(rd[:], pd[:]), reads=['pd'], writes=['rd'])
                P.op('dve', lambda e, c4=c4, yb_=yb_: e.tensor_tensor(yb_[:], acc[0:64, c4 * 512:(c4 + 1) * 512], rd[:], ALU.mult), reads=['acc', 'rd'], writes=[('yb', c4 % 2)])
                P.dma('sp', D['yT'][256 + h * 64:256 + (h + 1) * 64, c4 * 512:(c4 + 1) * 512], yb_[:], reads=[('yb', c4 % 2)], writes=['d_yT'])
        P.emit()


def phase_D1(P, l, D):
    with ExitStack() as s:
        cw = P.sb(s, 'd_cw', [128, 5, 9], F32)
        idf = P.sb(s, 'd_idf', [128, 128], F32)
        raw = [P.sb(s, 'd_raw%d' % i, [128, S + 4], F32) for i in range(2)]
        cv = P.sb(s, 'd_cv', [128, S], F32)
        tm = [P.sb(s, 'd_tm%d' % i, [128, 4, 128], F32) for i in range(2)]
        sq = P.sb(s, 'd_sq', [128, 8, 64], F32)
        r8 = P.sb(s, 'd_r8', [128, 8], F32)
        fT = [P.sb(s, 'd_fT%d' % i, [128, 512], BF16) for i in range(2)]
        abt = P.sb(s, 'd_abt', [128, NT, 24], F32)
        gbt = P.sb(s, 'd_gbt', [128, NT, 24], F32)
        dtb = P.sb(s, 'd_dtb', [128, 12], F32)
        nA = P.sb(s, 'd_nA', [128, 12], F32)
        ptr = [P.ps(s, 'd_ptr%d' % i, [128, 512]) for i in range(2)]
        ptb = [P.ps(s, 'd_ptb%d' % i, [128, 512]) for i in range(2)]
        for k in range(5):
            P.dma('sp', cw[:, k, :], D['conv_w'][l, k].rearrange('(c p) -> p c', p=128), writes=['cw'], allow_slow_non_contiguous=True)
        P.dma('sp', idf[:], D['ident'], writes=['idf'])
        for i in range(2):
            P.op('pool', lambda e, i=i: e.memset(raw[i][:, 0:2], 0.0), writes=[('raw', i)])
            P.op('pool', lambda e, i=i: e.memset(raw[i][:, S + 2:S + 4], 0.0), writes=[('raw', i)])
        P.dma('sp', abt[:], D['ab'].rearrange('(t p) c -> p t c', p=128), reads=['d_ab'], writes=['abt'])
        P.dma('sp', dtb[:], D['dt_bias'][l].rearrange('a h -> (a h)').partition_broadcast(128), writes=['dtb'])
        P.dma('sp', nA[:], D['a_log'][l].rearrange('a h -> (a h)').partition_broadcast(128), writes=['nA'])
        P.op('act', lambda e: e.activation(nA[:], nA[:], AF.Exp), reads=['nA'], writes=['nA'])
        P.op('dve', lambda e: e.tensor_scalar(nA[:], nA[:], -1.0, None, ALU.mult), reads=['nA'], writes=['nA'])
        P.op('dve', lambda e: e.tensor_tensor(gbt[:, :, 0:12], abt[:, :, 0:12], bc(dtb[:].unsqueeze(1), [128, NT, 12]), ALU.add), reads=['abt', 'dtb'], writes=['gbt0'])
        P.op('act', lambda e: e.activation(gbt[:, :, 0:12], gbt[:, :, 0:12], AF.Exp), reads=['gbt0'], writes=['gbt0'])
        P.op('act', lambda e: e.activation(gbt[:, :, 0:12], gbt[:, :, 0:12], AF.Ln, bias=1.0), reads=['gbt0'], writes=['gbt0'])
        P.op('dve', lambda e: e.tensor_tensor(gbt[:, :, 0:12], gbt[:, :, 0:12], bc(nA[:].unsqueeze(1), [128, NT, 12]), ALU.mult), reads=['gbt0', 'nA'], writes=['gbt0'])
        P.op('act', lambda e: e.activation(gbt[:, :, 12:24], abt[:, :, 12:24], AF.Sigmoid), reads=['abt'], writes=['gbt1'])
        P.dma('sp', D['gb'].rearrange('(t p) c -> p t c', p=128), gbt[:], reads=['gbt0', 'gbt1'], writes=['d_gb'])
        n4 = 0
        import os
        CUT = int(os.environ.get('D1CUT', '99'))
        for c in range(int(os.environ.get('D1C0', '0')), int(os.environ.get('D1C1', '9'))):
            rb = c % 2
            R = raw[rb]
            P.dma('sp', R[:, 2:S + 2], D['cT'][c * 128:(c + 1) * 128, :], reads=['d_cT'], writes=[('raw', rb)])
            P.op('dve', lambda e, R=R, c=c: e.tensor_scalar(cv[:], R[:, 0:S], cw[:, 0, c:c + 1], None, ALU.mult), reads=[('raw', rb), 'cw'], writes=['cv'])
            for k in range(1, 5):
                eng = 'dve'
                P.op(eng, lambda e, R=R, c=c, k=k: e.scalar_tensor_tensor(cv[:], R[:, k:k + S], cw[:, k, c:c + 1], cv[:], ALU.mult, ALU.add),
                     reads=[('raw', rb), 'cw', 'cv'], writes=['cv'])
            P.op('act', lambda e: e.activation(cv[:], cv[:], AF.Silu), reads=['cv'], writes=['cv'])
            def stA(t4, c=c):
                pb = (c * 8 + t4) % 2
                for j in range(4):
                    t = t4 * 4 + j
                    P.op('pe', lambda e, pb=pb, j=j, t=t: e.transpose(ptr[pb][:, j * 128:(j + 1) * 128], cv[:, t * 128:(t + 1) * 128], idf[:]),
                         reads=['cv', 'idf'], writes=[('ptr', pb)])
                TM = tm[pb]
                PV = ptr[pb][:].rearrange('p (j h d) -> p (j h) d', h=2, d=64)
                if c >= 6:
                    P.op('act', lambda e, pb=pb, TM=TM: e.copy(TM[:].rearrange('p j c -> p (j c)'), ptr[pb][:]), reads=[('ptr', pb)], writes=[('tm', pb)])
                    P.dma('sp', D['v_tm'][t4 * 512:(t4 + 1) * 512, (c - 6) * 128:(c - 5) * 128].rearrange('(j p) c -> p j c', p=128), TM[:], reads=[('tm', pb)], writes=['d_vtm'])
                else:
                    P.op('act', lambda e, pb=pb: e.activation(sq[:].rearrange('p a b -> p (a b)'), ptr[pb][:], AF.Square), reads=[('ptr', pb)], writes=['sq'])
                    P.op('dve', lambda e: e.tensor_reduce(r8[:], sq[:], AX.X, ALU.add), reads=['sq'], writes=['r8'])
                    P.op('act', lambda e: e.activation(r8[:], r8[:], AF.Sqrt, bias=EPS, scale=1.0), reads=['r8'], writes=['r8'])
                    P.op('dve', lambda e: e.reciprocal(r8[:], r8[:]), reads=['r8'], writes=['r8'])
                    if c < 3:
                        P.op('dve', lambda e: e.tensor_scalar(r8[:], r8[:], 0.125, None, ALU.mult), reads=['r8'], writes=['r8'])
                    P.op('dve', lambda e, TM=TM, PV=PV: e.tensor_tensor(TM[:].rearrange('p j (h d) -> p (j h) d', d=64), PV, bc(r8[:].unsqueeze(2), [128, 8, 64]), ALU.mult),
                         reads=[('ptr', pb), 'r8'], writes=[('tm', pb)])
                    if c >= 3:
                        P.dma('sp', D['k_tm'][t4 * 512:(t4 + 1) * 512, (c - 3) * 128:(c - 2) * 128].rearrange('(j p) c -> p j c', p=128), TM[:], reads=[('tm', pb)], writes=['d_ktm'])

            def stB(t4, c=c):
                pb = (c * 8 + t4) % 2
                TM = tm[pb]
                if c < 6:
                    for j in range(4):
                        P.op('pe', lambda e, pb=pb, j=j, TM=TM: e.transpose(ptb[pb][:, j * 128:(j + 1) * 128], TM[:, j, :], idf[:]), reads=[('tm', pb), 'idf'], writes=[('ptb', pb)])
                    P.op('act', lambda e, pb=pb: e.copy(fT[pb][:], ptb[pb][:]), reads=[('ptb', pb)], writes=[('fT', pb)])
                    dst = D['qT_g'] if c < 3 else D['kT_g']
                    cc = c if c < 3 else c - 3
                    P.dma('sp', dst[cc * 128:(cc + 1) * 128, t4 * 512:(t4 + 1) * 512], fT[pb][:], reads=[('fT', pb)], writes=['d_qkTg'])

            stA(0)
            for t4 in range(8):
                if t4 + 1 < 8:
                    stA(t4 + 1)
                stB(t4)
        P.emit()


def phase_D2(P, l, D):
    import os
    C = 64
    NCH = S // C
    NST = int(os.environ.get('D2N', str(NCH)))
    MD = BF16 if os.environ.get('D2BF', '1') == '1' else F32
    with ExitStack() as s:
        def T12(name, dt=F32):
            return P.sb(s, 'e_' + name, [64, 12, 64], dt)
        ones = P.sb(s, 'e_ones', [64, 64], F32)
        idf = P.sb(s, 'e_idf', [64, 64], F32)
        idm = P.sb(s, 'e_idm', [64, 64], MD)
        idbc = T12('idbc')
        triF = P.sb(s, 'e_triF', [64, 64], F32)
        triB = P.sb(s, 'e_triB', [64, 64], F32)
        mW, mWt, mI = T12('mW'), T12('mWt'), T12('mI')
        St, St2, Sm = T12('S'), T12('S2'), T12('Sm', MD)
        ktm = [T12('ktm%d' % i) for i in range(2)]
        vtm = [T12('vtm%d' % i) for i in range(2)]
        kT = [T12('kT%d' % i, MD) for i in range(2)]
        qT = [T12('qT%d' % i, MD) for i in range(2)]
        gbv = [P.sb(s, 'e_gb%d' % i, [64, 24], F32) for i in range(2)]
        gc = P.sb(s, 'e_gc', [64, 12], F32)
        egc = [P.sb(s, 'e_egc%d' % i, [64, 12], F32) for i in range(2)]
        egl = [P.sb(s, 'e_egl%d' % i, [64, 12], F32) for i in range(2)]
        egd = P.sb(s, 'e_egd', [64, 12], F32)
        Dg = P.sb(s, 'e_Dg', [64, 24, 64], F32)
        diff, Ea, Eb = T12('diff'), T12('Ea'), T12('Eb')
        W, Wt = T12('W', MD), T12('Wt', MD)
        A1, A1t, A2, A2t = T12('A1', MD), T12('A1t', MD), T12('A2', MD), T12('A2t', MD)
        nxTI = T12('nxTI', MD)
        Yt = [T12('Yt0', MD), T12('Yt1', MD)]
        Yf = [T12('Yf0', MD), T12('Yf1', MD)]
        QKm = [T12('QKm0', MD), T12('QKm1', MD)]
        kd = [T12('kd0', MD), T12('kd1', MD)]
        Rr, Rm, vnew, o1 = T12('R'), T12('Rm', MD), T12('vnew', MD), T12('o1')
        ob = [T12('ob0'), T12('ob1')]
        pA = P.ps(s, 'e_pA', [64, 1024])
        pB = P.ps(s, 'e_pB', [64, 1024])
        pC = P.ps(s, 'e_pC', [64, 1024])
        pS = P.ps(s, 'e_pS', [64, 1024])

        def pv(p, h):
            return p[:, h * 512:h * 512 + 384].rearrange('p (j t) -> p j t', t=64)

        def sv(t, h):
            return t[:, h * 6:(h + 1) * 6, :]

        def pcol(p, j):
            c0 = (j // 6) * 512 + (j % 6) * 64
            return p[:, c0:c0 + 64]

        def mm12(pt, pn, lfn, rfn, rfun):
            for j in range(12):
                o_, l_, r_ = pcol(pt, j), lfn(j), rfn(j)
                P.op('pe', lambda e, o_=o_, l_=l_, r_=r_: e.matmul(o_, l_, r_, start=True, stop=True), reads=rfun(j // 6), writes=[(pn, j // 6)])

        def bcol(ap12, h):
            return bc(ap12[:, h * 6:(h + 1) * 6].unsqueeze(2), [64, 6, 64])

        P.dma('sp', ones[:], D['ones64'], writes=['ones'])
        P.dma('sp', idf[:], D['ident'][0:64, 0:64], writes=['idf'])
        P.dma('sp', triF[:], D['triF'], writes=['triF'])
        P.dma('sp', triB[:], D['triB'], writes=['triB'])
        P.dma('sp', mW[:], D['mW'], writes=['mW'])
        P.dma('sp', mWt[:], D['mWt'], writes=['mWt'])
        P.dma('sp', mI[:], D['mI'], writes=['mI'])
        P.op('dve', lambda e: e.tensor_copy(idbc[:], bc(idf[:].unsqueeze(1), [64, 12, 64])), reads=['idf'], writes=['idbc'])
        P.op('dve', lambda e: e.tensor_copy(idm[:], idf[:]), reads=['idf'], writes=['idm'])
        P.op('dve', lambda e: e.memset(St[:].rearrange('p a b -> p (a b)'), 0.0), writes=[('S', 0), ('S', 1)])
        P.op('pool', lambda e: e.memset(Sm[:].rearrange('p a b -> p (a b)'), 0.0), writes=[('Sm', 0), ('Sm', 1)])

        def prep(i):
            b = i % 2
            cf = i
            cb = NCH - 1 - i
            K_, V_, KT_, QT_, GB_ = ktm[b], vtm[b], kT[b], qT[b], gbv[b]
            EGC, EGL, QKM, KD = egc[b], egl[b], QKm[b], kd[b]
            for h, cc in ((0, cf), (1, cb)):
                sl = slice(h * 6, h * 6 + 6)
                tk = slice(cc * C, (cc + 1) * C)
                P.dma('sp', K_[:, sl, :], D['k_tm'][tk, :].rearrange('t (h d) -> t h d', d=64), reads=['d_ktm'], writes=[('ktm', b, h)])
                P.dma('sp', V_[:, sl, :], D['v_tm'][tk, :].rearrange('t (h d) -> t h d', d=64), reads=['d_vtm'], writes=[('vtm', b, h)])
                P.dma('sp', KT_[:, sl, :], D['kT_g'][:, tk].rearrange('(h d) t -> d h t', d=64), reads=['d_qkTg'], writes=[('kT', b, h)])
                P.dma('sp', QT_[:, sl, :], D['qT_g'][:, tk].rearrange('(h d) t -> d h t', d=64), reads=['d_qkTg'], writes=[('qT', b, h)])
                P.dma('sp', GB_[:, h * 6:h * 6 + 6], D['gb'][tk, h * 6:h * 6 + 6], reads=['d_gb'], writes=[('gb', b, h)])
                P.dma('sp', GB_[:, 12 + h * 6:12 + h * 6 + 6], D['gb'][tk, 12 + h * 6:12 + h * 6 + 6], reads=['d_gb'], writes=[('gbb', b, h)])
            beta = GB_[:, 12:24]
            DgF = Dg[:].rearrange('p a b -> p (a b)')
            for h in range(2):
                tri = triF if h == 0 else triB
                trin = 'triF' if h == 0 else 'triB'
                rG, rBt = ('gb', b, h), ('gbb', b, h)
                P.op('pe', lambda e, h=h, tri=tri, GB_=GB_: e.matmul(pA[:, h * 512:h * 512 + 6], tri[:], GB_[:, h * 6:h * 6 + 6], start=True, stop=True), reads=[trin, rG], writes=[('pA', h)])
                P.op('dve', lambda e, h=h: e.tensor_copy(gc[:, h * 6:h * 6 + 6], pA[:, h * 512:h * 512 + 6]), reads=[('pA', h)], writes=[('gc', h)])
                P.op('act', lambda e, h=h, EGC=EGC: e.activation(EGC[:, h * 6:h * 6 + 6], pA[:, h * 512:h * 512 + 6], AF.Exp), reads=[('pA', h)], writes=[('egc', b, h)])
                P.op('dve', lambda e, h=h: e.tensor_tensor(Dg[:, h * 6:h * 6 + 6, :], sv(idbc, 0), bcol(gc, h), ALU.mult), reads=['idbc', ('gc', h)], writes=[('Dg', h)])
                P.op('pool', lambda e, h=h, beta=beta: e.tensor_tensor(Dg[:, 12 + h * 6:12 + h * 6 + 6, :], sv(idbc, 0), bcol(beta, h), ALU.mult), reads=['idbc', rBt], writes=[('Dgb', h)])
                P.op('pe', lambda e, h=h: e.matmul(pB[:, h * 512:h * 512 + 384], ones[:], DgF[:, h * 384:(h + 1) * 384], start=True, stop=True), reads=['ones', ('Dg', h)], writes=[('pB', h)])
                P.op('pe', lambda e, h=h: e.matmul(pC[:, h * 512:h * 512 + 384], ones[:], DgF[:, 768 + h * 384:768 + (h + 1) * 384], start=True, stop=True), reads=['ones', ('Dgb', h)], writes=[('pC', h)])
                P.op('dve', lambda e, h=h: e.tensor_tensor(sv(diff, h), pv(pB, h), bcol(gc, h), ALU.subtract), reads=[('pB', h), ('gc', h)], writes=[('diff', h)])
                lc = h * 512 + (63 if h == 0 else 0)
                lastv = pB[:, lc:lc + 64 * 5 + 1:64]
                P.op('act', lambda e, h=h, lastv=lastv, EGL=EGL: e.activation(EGL[:, h * 6:h * 6 + 6], lastv, AF.Exp), reads=[('pB', h)], writes=[('egl', b, h)])
                P.op('dve', lambda e, h=h, lastv=lastv: e.tensor_tensor(egd[:, h * 6:h * 6 + 6], lastv, gc[:, h * 6:h * 6 + 6], ALU.subtract), reads=[('pB', h), ('gc', h)], writes=[('egd', h)])
                P.op('act', lambda e, h=h: e.activation(egd[:, h * 6:h * 6 + 6], egd[:, h * 6:h * 6 + 6], AF.Exp), reads=[('egd', h)], writes=[('egd', h)])
                P.op('pool', lambda e, h=h, K_=K_, KD=KD: e.tensor_tensor(sv(KD, h), sv(K_, h), bcol(egd, h), ALU.mult), reads=[('ktm', b, h), ('egd', h)], writes=[('kd', b, h)])
                P.op('act', lambda e, h=h: e.activation(sv(Ea, h), sv(diff, h), AF.Exp), reads=[('diff', h)], writes=[('Ea', h)])
                P.op('act', lambda e, h=h: e.activation(sv(Eb, h), sv(diff, h), AF.Exp, scale=-1.0), reads=[('diff', h)], writes=[('Eb', h)])
            yield
            mm12(pA, 'pA', lambda j: KT_[:, j, :], lambda j: KT_[:, j, :], lambda h: [('kT', b, h)])
            for h in range(2):
                rBt = ('gbb', b, h)
                P.op('dve', lambda e, h=h: e.scalar_tensor_tensor(sv(Eb, h), sv(Eb, h), 1.0, sv(mWt, h), ALU.min, ALU.mult), reads=[('Eb', h), 'mWt'], writes=[('Eb', h)])
                P.op('dve', lambda e, h=h: e.tensor_tensor(sv(Eb, h), sv(Eb, h), pv(pC, h), ALU.mult), reads=[('Eb', h), ('pC', h)], writes=[('Eb', h)])
                P.op('dve', lambda e, h=h: e.tensor_tensor(sv(Wt, h), sv(Eb, h), pv(pA, h), ALU.mult), reads=[('Eb', h), ('pA', h)], writes=[('Wt', h)])
            yield
            mm12(pC, 'pC', lambda j: KT_[:, j, :], lambda j: QT_[:, j, :], lambda h: [('kT', b, h), ('qT', b, h)])
            for h in range(2):
                rBt = ('gbb', b, h)
                P.op('dve', lambda e, h=h: e.scalar_tensor_tensor(sv(diff, h), sv(Ea, h), 1.0, sv(mI, h), ALU.min, ALU.mult), reads=[('Ea', h), 'mI'], writes=[('diff', h)])
                P.op('dve', lambda e, h=h, QKM=QKM: e.tensor_tensor(sv(QKM, h), sv(diff, h), pv(pC, h), ALU.mult), reads=[('diff', h), ('pC', h)], writes=[('QKm', b, h)])
                P.op('dve', lambda e, h=h: e.scalar_tensor_tensor(sv(Ea, h), sv(Ea, h), 1.0, sv(mW, h), ALU.min, ALU.mult), reads=[('Ea', h), 'mW', ('diff', h)], writes=[('Ea', h)])
                P.op('pool', lambda e, h=h, beta=beta: e.tensor_tensor(sv(Ea, h), sv(Ea, h), bcol(beta, h), ALU.mult), reads=[('Ea', h), rBt], writes=[('Ea', h)])
                P.op('dve', lambda e, h=h: e.tensor_tensor(sv(W, h), sv(Ea, h), pv(pA, h), ALU.mult), reads=[('Ea', h), ('pA', h)], writes=[('W', h)])
                P.op('pool', lambda e, h=h: e.tensor_tensor(sv(Yt[0], h), sv(idbc, h), sv(W, h), ALU.subtract), reads=['idbc', ('W', h)], writes=[('Yt0', h)])
            yield
            cur, curT, cn, cnT = W, Wt, 'W', 'Wt'
            yi = 0
            bufs = [(A1, A1t, 'A1', 'A1t'), (A2, A2t, 'A2', 'A2t')]
            for lev in range(5):
                nx, nxT, nn, nnT = bufs[lev % 2]
                mm12(pB, 'pB', lambda j, cur=cur: cur[:, j, :], lambda j, curT=curT: curT[:, j, :], lambda h, cn=cn, cnT=cnT: [(cn, h), (cnT, h)])
                for h in range(2):
                    if lev < 4:
                        P.op('act', lambda e, h=h, nxT=nxT: e.copy(sv(nxT, h), pv(pB, h)), reads=[('pB', h)], writes=[(nnT, h)])
                    P.op('dve', lambda e, h=h: e.tensor_tensor(sv(nxTI, h), pv(pB, h), sv(idbc, h), ALU.add), reads=[('pB', h), 'idbc'], writes=[('nxTI', h)])
                if lev < 4:
                    mm12(pC, 'pC', lambda j, curT=curT: curT[:, j, :], lambda j, cur=cur: cur[:, j, :], lambda h, cn=cn, cnT=cnT: [(cn, h), (cnT, h)])
                    for h in range(2):
                        P.op('dve', lambda e, h=h, nx=nx: e.tensor_copy(sv(nx, h), pv(pC, h)), reads=[('pC', h)], writes=[(nn, h)])
                Yc = Yt[yi]
                yc_ = 'Yt%d' % yi
                if lev < 4:
                    Yn, yn_ = Yt[1 - yi], ('Yt%d' % (1 - yi),)
                else:
                    Yn, yn_ = Yf[b], ('Yf', b)
                for j in range(12):
                    h = j // 6
                    P.op('pe', lambda e, j=j, Yc=Yc: e.matmul(pcol(pA, j), nxTI[:, j, :], Yc[:, j, :], start=True, stop=True), reads=[('nxTI', h), (yc_, h)], writes=[('pA', h)])
                for h in range(2):
                    P.op('dve', lambda e, h=h, Yn=Yn: e.tensor_copy(sv(Yn, h), pv(pA, h)), reads=[('pA', h)], writes=[yn_ + (h,)])
                yi = 1 - yi
                cur, curT, cn, cnT = nx, nxT, nn, nnT
                yield

        def scan(i):
            b = i % 2
            cf = i
            cb = NCH - 1 - i
            V_, KT_, QT_, GB_ = vtm[b], kT[b], qT[b], gbv[b]
            EGC, EGL, QKM, KD, YF = egc[b], egl[b], QKm[b], kd[b], Yf[b]
            beta = GB_[:, 12:24]
            mm12(pS, 'pS', lambda j: KT_[:, j, :], lambda j: Sm[:, j, :], lambda h: [('kT', b, h), ('Sm', h)])
            for h in range(2):
                P.op('dve', lambda e, h=h, EGC=EGC: e.tensor_tensor(sv(Rr, h), pv(pS, h), bcol(EGC, h), ALU.mult), reads=[('pS', h), ('egc', b, h)], writes=[('R', h)])
                P.op('dve', lambda e, h=h, V_=V_: e.tensor_tensor(sv(Rm, h), sv(V_, h), sv(Rr, h), ALU.subtract), reads=[('R', h), ('vtm', b, h)], writes=[('Rm', h)])
            yield
            mm12(pS, 'pS', lambda j: QT_[:, j, :], lambda j: Sm[:, j, :], lambda h: [('qT', b, h), ('Sm', h)])
            for h in range(2):
                P.op('act', lambda e, h=h: e.copy(sv(o1, h), pv(pS, h)), reads=[('pS', h)], writes=[('o1', h)])
                P.op('pool', lambda e, h=h, EGC=EGC: e.tensor_tensor(sv(o1, h), sv(o1, h), bcol(EGC, h), ALU.mult), reads=[('o1', h), ('egc', b, h)], writes=[('o1', h)])
            yield
            yield
            mm12(pS, 'pS', lambda j: YF[:, j, :], lambda j: Rm[:, j, :], lambda h: [('Yf', b, h), ('Rm', h)])
            for h in range(2):
                P.op('dve', lambda e, h=h, beta=beta: e.tensor_tensor(sv(vnew, h), pv(pS, h), bcol(beta, h), ALU.mult), reads=[('pS', h), ('gbb', b, h)], writes=[('vnew', h)])
            yield
            mm12(pS, 'pS', lambda j: KD[:, j, :], lambda j: vnew[:, j, :], lambda h: [('kd', b, h), ('vnew', h)])
            OB = ob[b]
            for h in range(2):
                P.op('pool', lambda e, h=h, EGL=EGL: e.tensor_tensor(sv(St2, h), sv(St, h), bcol(EGL, h), ALU.mult), reads=[('S', h), ('egl', b, h)], writes=[('S2', h)])
                P.op('dve', lambda e, h=h: e.tensor_tensor(sv(St, h), sv(St2, h), pv(pS, h), ALU.add), reads=[('S2', h), ('pS', h)], writes=[('S', h)])
                P.op('act', lambda e, h=h: e.copy(sv(Sm, h), sv(St, h)), reads=[('S', h)], writes=[('Sm', h)])
            yield
            mm12(pS, 'pS', lambda j: QKM[:, j, :], lambda j: vnew[:, j, :], lambda h: [('QKm', b, h), ('vnew', h)])
            for h in range(2):
                cc = cf if h == 0 else cb
                P.op('dve', lambda e, h=h, OB=OB: e.tensor_tensor(sv(OB, h), sv(o1, h), pv(pS, h), ALU.add), reads=[('o1', h), ('pS', h)], writes=[('ob', b, h)])
                P.dma('sp', D['o_fb'][h, cc * C:(cc + 1) * C, :].rearrange('t (h d) -> t h d', d=64), sv(OB, h), reads=[('ob', b, h)], writes=['d_ofb'])

        def run(gens):
            gens = [g for g in gens if g is not None]
            while gens:
                for g in list(gens):
                    try:
                        next(g)
                    except StopIteration:
                        gens.remove(g)

        run([prep(0)])
        for i in range(NST):
            run([prep(i + 1) if i + 1 < NST else None, scan(i)])
        P.emit()


def phase_E(P, l, D, first):
    xsrc = D['x'] if first else D['xw']
    with ExitStack() as s:
        wo = P.sb(s, 'f_wo', [128, 8, 1024], BF16)
        ong = P.sb(s, 'f_ong', [128, 64], F32)
        idb = P.sb(s, 'f_idb', [128, 128], BF16)
        of_ = [P.sb(s, 'f_of%d' % i, [128, 2, 384], F32) for i in range(2)]
        gt = [P.sb(s, 'f_gt%d' % i, [128, 384], F32) for i in range(2)]
        o = P.sb(s, 'f_o', [128, 6, 64], F32)
        sq = P.sb(s, 'f_sq', [128, 6, 64], F32)
        r6 = P.sb(s, 'f_r6', [128, 6], F32)
        ycb = P.sb(s, 'f_ycb', [128, 384], BF16)
        yT = [P.sb(s, 'f_yT%d' % i, [128, 8, 128], BF16) for i in range(2)]
        xt = [P.sb(s, 'f_xt%d' % i, [128, 1024], F32) for i in range(2)]
        pT = P.ps(s, 'f_pT', [128, 512], BF16)
        po = [P.ps(s, 'f_po%d' % i, [128, 512]) for i in range(2)]
        for k in range(8):
            P.dma('pool', wo[:, k, :], D['w_out'][l, k * 128:(k + 1) * 128, :], writes=[('wo', k)])
        P.dma('sp', ong[:], D['o_norm_g'][l].partition_broadcast(128), writes=['ong'])
        P.dma('pool', idb[:], D['ident'], writes=['idb'])
        def front(t):
            b = t % 2
            tk = slice(t * 128, (t + 1) * 128)
            P.dma('sp', of_[b][:], D['o_fb'][:, tk, :].rearrange('a t c -> t a c'), reads=['d_ofb'], writes=[('of', b)])
            P.dma('sp', gt[b][:], D['gate_s'][tk, :], reads=['d_gate'], writes=[('gt', b)])
            P.dma('sp', xt[b][:], xsrc[tk, :], writes=[('xt', b)])
            P.dma('sp', yT[b][:, 0:5, :], D['yT'][0:640, tk].rearrange('(k p) t -> p k t', p=128), reads=['d_yT'], writes=[('yT', b, 0)])
            OF = o[:].rearrange('p a b -> p (a b)')
            P.op('dve', lambda e, b=b: e.tensor_tensor(OF, of_[b][:, 0, :], of_[b][:, 1, :], ALU.add), reads=[('of', b)], writes=['o'])
            P.op('act', lambda e: e.activation(sq[:], o[:], AF.Square), reads=['o'], writes=['sq'])
            P.op('dve', lambda e: e.tensor_reduce(r6[:], sq[:], AX.X, ALU.add), reads=['sq'], writes=['r6'])
            P.op('act', lambda e: e.activation(r6[:], r6[:], AF.Sqrt, bias=EPS, scale=1.0 / 64), reads=['r6'], writes=['r6'])
            P.op('dve', lambda e: e.reciprocal(r6[:], r6[:]), reads=['r6'], writes=['r6'])
            P.op('dve', lambda e: e.tensor_tensor(o[:], o[:], bc(r6[:].unsqueeze(2), [128, 6, 64]), ALU.mult), reads=['o', 'r6'], writes=['o'])
            P.op('pool', lambda e: e.tensor_tensor(o[:], o[:], bc(ong[:].unsqueeze(1), [128, 6, 64]), ALU.mult), reads=['o', 'ong'], writes=['o'])
            P.op('pool', lambda e, b=b: e.tensor_tensor(ycb[:], OF, gt[b][:], ALU.mult), reads=['o', ('gt', b)], writes=['ycb'])
            for k in range(3):
                P.op('pe', lambda e, k=k: e.transpose(pT[:, k * 128:(k + 1) * 128], ycb[:, k * 128:(k + 1) * 128], idb[:]), reads=['ycb', 'idb'], writes=['pT'])
            P.op('act', lambda e, b=b: e.copy(yT[b][:, 5:8, :], pT[:, 0:384].rearrange('p (k t) -> p k t', t=128)), reads=['pT'], writes=[('yT', b, 1)])
        def back(t):
            b = t % 2
            tk = slice(t * 128, (t + 1) * 128)
            for hf in range(2):
                for k in range(8):
                    P.op('pe', lambda e, hf=hf, k=k, b=b: e.matmul(po[hf][:], yT[b][:, k, :], wo[:, k, hf * 512:(hf + 1) * 512], start=(k == 0), stop=(k == 7)),
                         reads=[('yT', b, 0), ('yT', b, 1), ('wo', k)], writes=[('po', hf)])
                P.op('dve', lambda e, hf=hf, b=b: e.tensor_tensor(xt[b][:, hf * 512:(hf + 1) * 512], xt[b][:, hf * 512:(hf + 1) * 512], po[hf][:], ALU.add),
                     reads=[('po', hf), ('xt', b)], writes=[('xt', b)])
            P.dma('sp', D['xw'][tk, :], xt[b][:], reads=[('xt', b)], writes=[('d_xw', t)])

        front(0)
        for t in range(NT):
            if t + 1 < NT:
                front(t + 1)
            back(t)
        P.emit()


def phase_F(P, l, D):
    import os
    NE = int(os.environ.get('FNE', '16'))
    with ExitStack() as s:
        gff = P.sb(s, 'g_gff', [128, 1024], F32)
        idf = P.sb(s, 'g_idf', [128, 128], F32)
        idb = P.sb(s, 'g_idb', [128, 128], BF16)
        wr = P.sb(s, 'g_wr', [128, 8, 16], F32)
        aff = P.sb(s, 'g_aff', [128, NT, 16], F32)
        sel = P.sb(s, 'g_sel', [128, NT, 16], F32)
        rank = P.sb(s, 'g_rank', [128, NT, 16], F32)
        cA = P.sb(s, 'g_cA', [128, NT, 16], F32)
        cB = P.sb(s, 'g_cB', [128, NT, 16], F32)
        selb = P.sb(s, 'g_selb', [128, NT * 16], BF16)
        triS = P.sb(s, 'g_triS', [128, 128], BF16)
        onesb = P.sb(s, 'g_onesb', [128, 128], BF16)
        tg = P.sb(s, 'g_tg', [128, NT, 16, 5], BF16)
        tp = P.sb(s, 'g_tp', [128, NT, 2], F32)
        iota = P.sb(s, 'g_iota', [128, 512], F32)
        Selt = [P.sb(s, 'g_Selt%d' % i, [128, 512], BF16) for i in range(2)]
        idxf = P.sb(s, 'g_idxf', [128, 4, 8], F32)
        row5 = P.sb(s, 'g_row5', [5, 512], F32)
        idxv = P.sb(s, 'g_idxv', [128, 4], F32)
        idxi = [P.sb(s, 'g_idxi%d' % i, [128, 4], I32) for i in range(2)]
        gate = [P.sb(s, 'g_gate%d' % i, [128, 4], F32) for i in range(2)]
        affT2 = P.sb(s, 'g_affT2', [16, S], F32)
        bs = P.sb(s, 'g_bs', [16, 8], F32)
        ones16 = P.sb(s, 'g_ones16', [16, 128], F32)
        dthr = P.sb(s, 'g_dthr', [16, 16], F32)
        thrb = P.sb(s, 'g_thrb', [128, 16], F32)
        xt = P.sb(s, 'g_xt', [128, 1024], F32)
        junk = P.sb(s, 'g_junk', [128, 1024], BF16)
        ss = P.sb(s, 'g_ss', [128, 4], F32)
        h32s = [P.sb(s, 'g_h32_%d' % i, [128, 1024], F32) for i in range(2)]
        hb16 = P.sb(s, 'g_hb16', [128, 1024], BF16)
        hT32 = P.sb(s, 'g_hT32', [128, 8, 128], F32)
        sm = P.sb(s, 'g_sm', [128, 4], F32)
        ex = P.sb(s, 'g_ex', [128, 16], F32)
        wg = [P.sb(s, 'g_wg%d' % i, [128, 8, 1024], BF16) for i in range(2)]
        wu = [P.sb(s, 'g_wu%d' % i, [128, 8, 1024], BF16) for i in range(2)]
        wd = [P.sb(s, 'g_wd%d' % i, [128, 8, 1024], BF16) for i in range(2)]
        xe = P.sb(s, 'g_xe', [128, 4, 1024], BF16)
        bjv = xe[0:16, :, :].rearrange('p g c -> p (g c)')
        xeTs = [P.sb(s, 'g_xeT%d' % i, [128, 8, 512], BF16) for i in range(2)]
        hid = P.sb(s, 'g_hid', [128, 8, 512], BF16)
        sg = [P.sb(s, 'g_sg%d' % i, [128, 512], BF16) for i in range(2)]
        ye = [P.sb(s, 'g_ye%d' % i, [128, 1024], F32) for i in range(2)]
        pbig = P.ps(s, 'g_pbig', [128, 1024])
        pl = P.ps(s, 'g_pl', [128, 512])
        pT = P.ps(s, 'g_pT', [128, 1024], BF16)
        pg_ = P.ps(s, 'g_pg', [128, 512])
        pu_ = P.ps(s, 'g_pu', [128, 512])
        py = [P.ps(s, 'g_py%d' % i, [128, 512]) for i in range(2)]

        def load_w(ex_):
            eb = ex_ % 2
            for k in range(8):
                P.dma('pool', wg[eb][:, k, :], D['w_e_gate'][l, ex_, k * 128:(k + 1) * 128, :], writes=[('wg', eb, k)])
                P.dma('pool', wu[eb][:, k, :], D['w_e_up'][l, ex_, k * 128:(k + 1) * 128, :], writes=[('wu', eb, k)])
            for k in range(8):
                P.dma('pool', wd[eb][:, k, :], D['w_e_down'][l, ex_, k * 128:(k + 1) * 128, :], writes=[('wd', eb, k)])

        P.dma('sp', gff[:], D['g_ffn'][l].partition_broadcast(128), writes=['gff'])
        P.dma('sp', idf[:], D['ident'], writes=['idf'])
        P.dma('pool', idb[:], D['ident'], writes=['idb'])
        P.dma('pool', triS[:], D['triS'], writes=['triS'])
        P.dma('pool', onesb[:], D['ones128'], writes=['onesb'])
        P.dma('sp', wr[:], D['w_router'][l].rearrange('(k p) e -> p k e', p=128), writes=['wr'])
        P.dma('sp', ones16[:], D['ones128'][0:16, :], writes=['ones16'])
        P.dma('sp', tp[:], D['tp'], writes=['tp'])
        P.dma('sp', iota[:], D['iota512'], writes=['iota'])
        load_w(0)
        def frontF(t):
            tk = slice(t * 128, (t + 1) * 128)
            h32 = h32s[t % 2]
            kh = ('h32', t % 2)
            P.dma('sp', xt[:], D['xw'][tk, :], reads=['d_xw'], writes=['xt'])
            P.op('dve', lambda e: e.memset(ss[:, 0:1], 0.0), writes=['ss'])
            P.op('act', lambda e: e.activation(junk[:], xt[:], AF.Square, accum_out=ss[:, 0:1]), reads=['xt', 'ss'], writes=['junk', 'ss'])
            P.op('act', lambda e: e.activation(ss[:, 0:1], ss[:, 0:1], AF.Sqrt, bias=EPS, scale=1.0 / 1024), reads=['ss'], writes=['ss'])
            P.op('dve', lambda e: e.reciprocal(ss[:, 0:1], ss[:, 0:1]), reads=['ss'], writes=['ss'])
            P.op('dve', lambda e, h32=h32: e.scalar_tensor_tensor(h32[:], xt[:], ss[:, 0:1], gff[:], ALU.mult, ALU.mult), reads=['xt', 'ss', 'gff'], writes=[kh])
            P.op('act', lambda e, h32=h32: e.copy(hb16[:], h32[:]), reads=[kh], writes=['hb16'])
            P.dma('sp', D['hb'][tk, :], hb16[:], reads=['hb16'], writes=['d_hb'])

        def backF(t):
            h32 = h32s[t % 2]
            kh = ('h32', t % 2)
            for k in range(8):
                P.op('pe', lambda e, k=k, h32=h32: e.transpose(pbig[:, k * 128:(k + 1) * 128], h32[:, k * 128:(k + 1) * 128], idf[:]), reads=[kh, 'idf'], writes=[('pbig', k // 4)])
            P.op('act', lambda e: e.copy(hT32[:, 0:4, :], pbig[:, 0:512].rearrange('p (k t) -> p k t', t=128)), reads=[('pbig', 0)], writes=['hT32a'])
            P.op('dve', lambda e: e.tensor_copy(hT32[:, 4:8, :], pbig[:, 512:1024].rearrange('p (k t) -> p k t', t=128)), reads=[('pbig', 1)], writes=['hT32b'])
            for k in range(8):
                P.op('pe', lambda e, k=k: e.matmul(pl[:, 0:16], hT32[:, k, :], wr[:, k, :], start=(k == 0), stop=(k == 7)), reads=['hT32a', 'hT32b', 'wr'], writes=['pl'])
            P.op('dve', lambda e: e.tensor_reduce(sm[:, 0:1], pl[:, 0:16], AX.X, ALU.max), reads=['pl'], writes=['sm'])
            P.op('dve', lambda e: e.tensor_scalar(sm[:, 1:2], sm[:, 0:1], -1.0, None, ALU.mult), reads=['sm'], writes=['sm'])
            P.op('dve', lambda e: e.memset(sm[:, 2:3], 0.0), reads=['sm'], writes=['sm'])
            P.op('act', lambda e: e.activation(ex[:], pl[:, 0:16], AF.Exp, bias=sm[:, 1:2], accum_out=sm[:, 2:3]), reads=['pl', 'sm'], writes=['ex', 'sm'])
            P.op('dve', lambda e: e.reciprocal(sm[:, 3:4], sm[:, 2:3]), reads=['sm'], writes=['sm'])
            P.op('dve', lambda e, t=t: e.tensor_scalar(aff[:, t, :], ex[:], sm[:, 3:4], None, ALU.mult), reads=['ex', 'sm'], writes=[('aff', t)])
            P.op('pe', lambda e, t=t: e.transpose(pl[0:16, 128:256], aff[:, t, :], idf[:]), reads=[('aff', t), 'idf'], writes=['pl'])
            P.op('act', lambda e, t=t: e.mul(affT2[:, t * 128:(t + 1) * 128], pl[0:16, 128:256], 2.0), reads=['pl'], writes=['affT2'])

        frontF(0)
        for t in range(NT):
            if t + 1 < NT:
                frontF(t + 1)
            backF(t)
        lo, hi, half, mid2, cnt, gef, tt = (bs[:, i:i + 1] for i in range(7))
        P.op('dve', lambda e: e.memset(bs[:], 0.0), writes=['bs'])
        P.op('dve', lambda e: e.memset(hi, 1.0), reads=['bs'], writes=['bs'])
        for itn in range(27):
            P.op('dve', lambda e: e.tensor_tensor(mid2, lo, hi, ALU.add), reads=['bs'], writes=['bs'])
            P.op('dve', lambda e: e.tensor_scalar(half, mid2, 0.5, None, ALU.mult), reads=['bs'], writes=['bs'])
            P.op('dve', lambda e: e.memset(cnt, 0.0), reads=['bs'], writes=['bs'])
            P.op('dve', lambda e: e.tensor_scalar(bjv, affT2[:], mid2, 0.0, ALU.is_ge, ALU.add, accum_out=cnt), reads=['affT2', 'bs'], writes=['bj', 'bs'])
            P.op('dve', lambda e: e.tensor_scalar(gef, cnt, 511.5, None, ALU.is_ge), reads=['bs'], writes=['bs'])
            P.op('dve', lambda e: e.tensor_tensor(tt, half, lo, ALU.subtract), reads=['bs'], writes=['bs'])
            P.op('dve', lambda e: e.tensor_tensor(tt, tt, gef, ALU.mult), reads=['bs'], writes=['bs'])
            P.op('dve', lambda e: e.tensor_tensor(lo, lo, tt, ALU.add), reads=['bs'], writes=['bs'])
            P.op('dve', lambda e: e.tensor_tensor(tt, hi, half, ALU.subtract), reads=['bs'], writes=['bs'])
            P.op('dve', lambda e: e.tensor_tensor(tt, tt, gef, ALU.mult), reads=['bs'], writes=['bs'])
            P.op('dve', lambda e: e.tensor_tensor(hi, half, tt, ALU.add), reads=['bs'], writes=['bs'])
        P.op('dve', lambda e: e.tensor_scalar(dthr[:], idf[0:16, 0:16], lo, None, ALU.mult), reads=['idf', 'bs'], writes=['dthr'])
        P.op('pe', lambda e: e.matmul(pl[:, 256:272], ones16[:], dthr[:], start=True, stop=True), reads=['ones16', 'dthr'], writes=['pl'])
        P.op('dve', lambda e: e.tensor_copy(thrb[:], pl[:, 256:272]), reads=['pl'], writes=['thrb'])
        AFF = [('aff', t) for t in range(NT)]
        P.op('dve', lambda e: e.tensor_tensor(sel[:], aff[:], bc(thrb[:].unsqueeze(1), [128, NT, 16]), ALU.is_ge), reads=AFF + ['thrb'], writes=['sel'])
        P.op('dve', lambda e: e.tensor_copy(selb[:], sel[:].rearrange('p t e -> p (t e)')), reads=['sel'], writes=['selb'])
        P.op('pe', lambda e: e.matmul(pg_[:], triS[:], selb[:], start=True, stop=True), reads=['triS', 'selb'], writes=['pg'])
        P.op('pe', lambda e: e.matmul(pu_[:], onesb[:], selb[:], start=True, stop=True), reads=['onesb', 'selb'], writes=['pu'])
        P.op('dve', lambda e: e.tensor_copy(cA[:].rearrange('p t e -> p (t e)'), pu_[:]), reads=['pu'], writes=['cA'])
        src, dst, sn, dn = cA, cB, 'cA', 'cB'
        for sft in (1, 2, 4, 8, 16):
            P.op('pool', lambda e, src=src, dst=dst, sft=sft: e.tensor_copy(dst[:, 0:sft, :], src[:, 0:sft, :]), reads=[sn], writes=[dn])
            P.op('dve', lambda e, src=src, dst=dst, sft=sft: e.tensor_tensor(dst[:, sft:NT, :], src[:, sft:NT, :], src[:, 0:NT - sft, :], ALU.add), reads=[sn], writes=[dn])
            src, dst, sn, dn = dst, src, dn, sn
        P.op('dve', lambda e, src=src: e.tensor_tensor(rank[:].rearrange('p t e -> p (t e)'), src[:].rearrange('p t e -> p (t e)'), pu_[:], ALU.subtract), reads=[sn, 'pu'], writes=['rank'])
        P.op('dve', lambda e: e.tensor_tensor(rank[:].rearrange('p t e -> p (t e)'), rank[:].rearrange('p t e -> p (t e)'), pg_[:], ALU.add), reads=['rank', 'pg'], writes=['rank'])
        P.op('dve', lambda e: e.scalar_tensor_tensor(rank[:], rank[:], 1.0, sel[:], ALU.add, ALU.mult), reads=['rank', 'sel'], writes=['rank'])
        P.op('dve', lambda e: e.tensor_scalar(rank[:], rank[:], -1.0, None, ALU.add), reads=['rank'], writes=['rank'])
        P.op('dve', lambda e: e.tensor_copy(tg[:, :, :, 0:2], bc(tp[:].unsqueeze(2), [128, NT, 16, 2])), reads=['tp'], writes=['tg0'])
        P.op('dve', lambda e: e.tensor_copy(tg[:, :, :, 2], aff[:]), reads=AFF, writes=['tg1'])
        P.op('dve', lambda e: e.tensor_tensor(cA[:], aff[:], tg[:, :, :, 2], ALU.subtract), reads=AFF + ['tg1', 'cA', 'cB'], writes=['cA'])
        P.op('dve', lambda e: e.tensor_copy(tg[:, :, :, 3], cA[:]), reads=['cA'], writes=['tg2'])
        P.op('dve', lambda e: e.tensor_tensor(cB[:], cA[:], tg[:, :, :, 3], ALU.subtract), reads=['cA', 'tg2', 'cB'], writes=['cB'])
        P.op('dve', lambda e: e.tensor_copy(tg[:, :, :, 4], cB[:]), reads=['cB'], writes=['tg3'])
        TG = ['tg0', 'tg1', 'tg2', 'tg3']
        nsel = 0
        npy = 0
        nsg = 0
        def stage1(ex_):
            nonlocal nsel
            eb = ex_ % 2
            xeT = xeTs[eb]
            for t in range(NT):
                sb_ = nsel % 2
                nsel += 1
                P.op('dve', lambda e, sb_=sb_, t=t, ex_=ex_: e.tensor_scalar(Selt[sb_][:], iota[:], rank[:, t, ex_:ex_ + 1], None, ALU.is_equal), reads=['iota', 'rank'], writes=[('Selt', sb_)])
                P.op('pe', lambda e, sb_=sb_, t=t, ex_=ex_: e.matmul(pl[0:5, 0:512], tg[:, t, ex_, :], Selt[sb_][:], start=(t == 0), stop=(t == NT - 1)),
                     reads=[('Selt', sb_)] + TG, writes=['pl'])
            P.op('act', lambda e: e.copy(row5[:], pl[0:5, 0:512]), reads=['pl'], writes=['row5'])
            for g in range(4):
                P.op('pe', lambda e, g=g: e.transpose(pl[:, g * 8:g * 8 + 5], row5[0:5, g * 128:(g + 1) * 128], idf[0:5, 0:5]), reads=['row5', 'idf'], writes=['pl'])
            P.op('dve', lambda e: e.tensor_copy(idxf[:, :, 0:5], pl[:, 0:32].rearrange('p (g c) -> p g c', c=8)[:, :, 0:5]), reads=['pl'], writes=['idxf'])
            P.op('dve', lambda e: e.scalar_tensor_tensor(idxv[:], idxf[:, :, 0], 128.0, idxf[:, :, 1], ALU.mult, ALU.add), reads=['idxf'], writes=['idxv'])
            P.op('dve', lambda e, eb=eb: e.tensor_copy(idxi[eb][:], idxv[:]), reads=['idxv'], writes=[('idxi', eb)])
            P.op('dve', lambda e, eb=eb: e.tensor_tensor(gate[eb][:], idxf[:, :, 2], idxf[:, :, 3], ALU.add), reads=['idxf'], writes=[('gate', eb)])
            P.op('dve', lambda e, eb=eb: e.tensor_tensor(gate[eb][:], gate[eb][:], idxf[:, :, 4], ALU.add), reads=['idxf', ('gate', eb)], writes=[('gate', eb)])
            for g in range(4):
                P.idma(lambda e, g=g, eb=eb: e.indirect_dma_start(out=xe[:, g, :], out_offset=None, in_=D['hb'][:, :],
                                                                   in_offset=bass.IndirectOffsetOnAxis(ap=idxi[eb][:, g:g + 1], axis=0), bounds_check=P.breg(e), oob_is_err=False),
                       reads=['d_hb', ('idxi', eb)], writes=[('xe', g)])
            for g in range(4):
                for k in range(8):
                    P.op('pe', lambda e, g=g, k=k: e.transpose(pT[:, k * 128:(k + 1) * 128], xe[:, g, k * 128:(k + 1) * 128], idb[:]), reads=[('xe', g), 'idb'], writes=['pT'])
                eng = 'act' if g % 2 == 0 else 'dve'
                if eng == 'act':
                    P.op('act', lambda e, g=g, xeT=xeT: e.copy(xeT[:, :, g * 128:(g + 1) * 128], pT[:].rearrange('p (k t) -> p k t', t=128)), reads=['pT'], writes=[('xeT', eb, g)])
                else:
                    P.op('dve', lambda e, g=g, xeT=xeT: e.tensor_copy(xeT[:, :, g * 128:(g + 1) * 128], pT[:].rearrange('p (k t) -> p k t', t=128)), reads=['pT'], writes=[('xeT', eb, g)])

        def stage2(ex_):
            nonlocal npy, nsg
            eb = ex_ % 2
            xeT = xeTs[eb]
            XET = [('xeT', eb, g) for g in range(4)]
            for fc in range(8):
                for k in range(8):
                    P.op('pe', lambda e, fc=fc, k=k, eb=eb, xeT=xeT: e.matmul(pg_[:], wg[eb][:, k, fc * 128:(fc + 1) * 128], xeT[:, k, :], start=(k == 0), stop=(k == 7)),
                         reads=XET + [('wg', eb, k)], writes=['pg'])
                for k in range(8):
                    P.op('pe', lambda e, fc=fc, k=k, eb=eb, xeT=xeT: e.matmul(pu_[:], wu[eb][:, k, fc * 128:(fc + 1) * 128], xeT[:, k, :], start=(k == 0), stop=(k == 7)),
                         reads=XET + [('wu', eb, k)], writes=['pu'])
                sb2 = nsg % 2
                nsg += 1
                P.op('act', lambda e, sb2=sb2: e.activation(sg[sb2][:], pg_[:], AF.Silu), reads=['pg'], writes=[('sg', sb2)])
                P.op('dve', lambda e, sb2=sb2, fc=fc: e.tensor_tensor(hid[:, fc, :], sg[sb2][:], pu_[:], ALU.mult), reads=[('sg', sb2), 'pu'], writes=[('hid', fc)])
            HID = [('hid', fc) for fc in range(8)]
            for g in range(4):
                yb = g % 2
                for hf in range(2):
                    pb = npy % 2
                    npy += 1
                    for fc in range(8):
                        P.op('pe', lambda e, pb=pb, fc=fc, g=g, hf=hf, eb=eb: e.matmul(py[pb][:], hid[:, fc, g * 128:(g + 1) * 128], wd[eb][:, fc, hf * 512:(hf + 1) * 512], start=(fc == 0), stop=(fc == 7)),
                             reads=HID + [('wd', eb, fc)], writes=[('py', pb)])
                    if hf == 0:
                        P.op('act', lambda e, pb=pb, yb=yb, g=g, eb=eb: e.activation(ye[yb][:, 0:512], py[pb][:], AF.Copy, scale=gate[eb][:, g:g + 1]), reads=[('py', pb), ('gate', eb)], writes=[('ye', yb, 0)])
                    else:
                        P.op('dve', lambda e, pb=pb, yb=yb, g=g, eb=eb: e.tensor_scalar(ye[yb][:, 512:1024], py[pb][:], gate[eb][:, g:g + 1], None, ALU.mult), reads=[('py', pb), ('gate', eb)], writes=[('ye', yb, 1)])
                P.idma(lambda e, g=g, eb=eb, yb=yb: e.indirect_dma_start(out=D['xw'][:, :], out_offset=bass.IndirectOffsetOnAxis(ap=idxi[eb][:, g:g + 1], axis=0), in_=ye[yb][:],
                                                                          in_offset=None, bounds_check=P.breg(e), oob_is_err=False, compute_op=ALU.add),
                       reads=[('ye', yb, 0), ('ye', yb, 1), ('idxi', eb), 'd_xw'], writes=['d_xw'])

        stage1(0)
        for ex_ in range(NE):
            if ex_ + 1 < NE:
                load_w(ex_ + 1)
                stage1(ex_ + 1)
            stage2(ex_)
        P.emit()


def phase_G(P, l, D, last):
    xdst = D['out'] if last else D['xw']
    with ExitStack() as s:
        wp = P.sb(s, 'h_wp', [128, 2, 1024], BF16)
        wgt = P.sb(s, 'h_wgt', [128, 8, 1024], BF16)
        gpl = P.sb(s, 'h_gpl', [128, 1024], F32)
        gpg = P.sb(s, 'h_gpg', [128, 1024], F32)
        idb = P.sb(s, 'h_idb', [128, 128], BF16)
        xt = [P.sb(s, 'h_xt%d' % i, [128, 1024], F32) for i in range(2)]
        pb_ = [P.sb(s, 'h_pb%d' % i, [128, 256], BF16) for i in range(2)]
        junk = P.sb(s, 'h_junk', [128, 1024], BF16)
        ss = P.sb(s, 'h_ss', [128, 4], F32)
        ssf = P.sb(s, 'h_ssf', [128, 2], F32)
        junkf = P.sb(s, 'h_junkf', [128, 1024], BF16)
        xn = P.sb(s, 'h_xn', [128, 1024], BF16)
        xTs = [P.sb(s, 'h_xT%d' % i, [128, 10, 128], BF16) for i in range(2)]
        er = P.sb(s, 'h_er', [128, 1024], F32)
        gt = P.sb(s, 'h_gt', [128, 1024], F32)
        pT = P.ps(s, 'h_pT', [128, 2048], BF16)
        pe_ = P.ps(s, 'h_pe', [128, 1024])
        pg_ = P.ps(s, 'h_pg', [128, 1024])
        for k in range(2):
            P.dma('pool', wp[:, k, :], D['w_ple'][l, k * 128:(k + 1) * 128, :], writes=[('wp', k)])
        for k in range(8):
            P.dma('pool', wgt[:, k, :], D['w_ple_gate'][l, k * 128:(k + 1) * 128, :], writes=[('wgt', k)])
        P.dma('sp', gpl[:], D['g_ple'][l].partition_broadcast(128), writes=['gpl'])
        P.dma('sp', gpg[:], D['g_ple_gate'][l].partition_broadcast(128), writes=['gpg'])
        P.dma('pool', idb[:], D['ident'], writes=['idb'])
        def front(t):
            b = t % 2
            xT = xTs[b]
            tk = slice(t * 128, (t + 1) * 128)
            P.dma('sp', xt[b][:], D['xw'][tk, :], reads=[('d_xw', t)], writes=[('xt', b)])
            P.dma('pool', pb_[b][:], D['p'][l, tk, :], writes=[('pb', b)])
            P.op('dve', lambda e: e.memset(ssf[:, 0:1], 0.0), writes=['ssf'])
            P.op('act', lambda e, b=b: e.activation(junkf[:], xt[b][:], AF.Square, accum_out=ssf[:, 0:1]), reads=[('xt', b), 'ssf'], writes=['junkf', 'ssf'])
            P.op('act', lambda e: e.activation(ssf[:, 0:1], ssf[:, 0:1], AF.Sqrt, bias=EPS, scale=1.0 / 1024), reads=['ssf'], writes=['ssf'])
            P.op('dve', lambda e: e.reciprocal(ssf[:, 0:1], ssf[:, 0:1]), reads=['ssf'], writes=['ssf'])
            P.op('dve', lambda e, b=b: e.scalar_tensor_tensor(xn[:], xt[b][:], ssf[:, 0:1], gpg[:], ALU.mult, ALU.mult), reads=[('xt', b), 'ssf', 'gpg'], writes=['xn'])
            for k in range(8):
                P.op('pe', lambda e, k=k: e.transpose(pT[:, k * 128:(k + 1) * 128], xn[:, k * 128:(k + 1) * 128], idb[:]), reads=['xn', 'idb'], writes=[('pT', 0)])
            for k in range(2):
                P.op('pe', lambda e, k=k, b=b: e.transpose(pT[:, (8 + k) * 128:(9 + k) * 128], pb_[b][:, k * 128:(k + 1) * 128], idb[:]), reads=[('pb', b), 'idb'], writes=[('pT', 1)])
            P.op('act', lambda e, xT=xT: e.copy(xT[:, 0:8, :].rearrange('p k t -> p (k t)'), pT[:, 0:1024]), reads=[('pT', 0)], writes=[('xTa', b)])
            P.op('dve', lambda e, xT=xT: e.tensor_copy(xT[:, 8:10, :].rearrange('p k t -> p (k t)'), pT[:, 1024:1280]), reads=[('pT', 1)], writes=[('xTb', b)])

        def back(t):
            b = t % 2
            xT = xTs[b]
            tk = slice(t * 128, (t + 1) * 128)
            for hf in range(2):
                for k in range(2):
                    P.op('pe', lambda e, hf=hf, k=k, xT=xT: e.matmul(pe_[:, hf * 512:(hf + 1) * 512], xT[:, 8 + k, :], wp[:, k, hf * 512:(hf + 1) * 512], start=(k == 0), stop=(k == 1)),
                         reads=[('xTb', b), ('wp', k)], writes=[('pe', hf)])
                for k in range(8):
                    P.op('pe', lambda e, hf=hf, k=k, xT=xT: e.matmul(pg_[:, hf * 512:(hf + 1) * 512], xT[:, k, :], wgt[:, k, hf * 512:(hf + 1) * 512], start=(k == 0), stop=(k == 7)),
                         reads=[('xTa', b), ('wgt', k)], writes=[('pg', hf)])
            P.op('dve', lambda e: e.memset(ss[:, 1:3], 0.0), reads=['ss'], writes=['ss'])
            for hf in range(2):
                P.op('act', lambda e, hf=hf: e.activation(junk[:, hf * 512:(hf + 1) * 512], pe_[:, hf * 512:(hf + 1) * 512], AF.Square, accum_out=ss[:, 1 + hf:2 + hf]), reads=[('pe', hf), 'ss'], writes=['junk', 'ss'])
            P.op('dve', lambda e: e.tensor_tensor(ss[:, 1:2], ss[:, 1:2], ss[:, 2:3], ALU.add), reads=['ss'], writes=['ss'])
            P.op('act', lambda e: e.activation(ss[:, 1:2], ss[:, 1:2], AF.Sqrt, bias=EPS, scale=1.0 / 1024), reads=['ss'], writes=['ss'])
            P.op('dve', lambda e: e.reciprocal(ss[:, 1:2], ss[:, 1:2]), reads=['ss'], writes=['ss'])
            for hf in range(2):
                hs = slice(hf * 512, (hf + 1) * 512)
                P.op('dve', lambda e, hs=hs: e.scalar_tensor_tensor(er[:, hs], pe_[:, hs], ss[:, 1:2], gpl[:, hs], ALU.mult, ALU.mult), reads=[('pe', hf), 'ss', 'gpl'], writes=['er'])
                P.op('act', lambda e, hs=hs: e.activation(gt[:, hs], pg_[:, hs], AF.Sigmoid), reads=[('pg', hf)], writes=['gt'])
            P.op('dve', lambda e: e.tensor_tensor(er[:], er[:], gt[:], ALU.mult), reads=['er', 'gt'], writes=['er'])
            P.op('dve', lambda e, b=b: e.tensor_tensor(xt[b][:], xt[b][:], er[:], ALU.add), reads=['er', ('xt', b)], writes=[('xt', b)])
            P.dma('sp', xdst[tk, :], xt[b][:], reads=[('xt', b)], writes=[('d_xw', t)])

        front(0)
        for t in range(NT):
            if t + 1 < NT:
                front(t + 1)
            back(t)
        P.emit()


WEIGHTS = [('g_mix', [4, 1024]), ('w_in', [4, 1024, 3224]), ('ln_v_g', [4, 4, 64]), ('ln_v_b', [4, 4, 64]), ('w_s', [4, 4, 128, 128]),
           ('b_s', [4, 4, 128]), ('q_norm_g', [4, 64]), ('k_norm_g', [4, 64]), ('conv_w', [4, 5, 1152]), ('a_log', [4, 2, 6]),
           ('dt_bias', [4, 2, 6]), ('o_norm_g', [4, 64]), ('w_out', [4, 1024, 1024]), ('g_ffn', [4, 1024]), ('w_router', [4, 1024, 16]),
           ('w_e_gate', [4, 16, 1024, 1024]), ('w_e_up', [4, 16, 1024, 1024]), ('w_e_down', [4, 16, 1024, 1024]), ('w_ple', [4, 256, 1024]),
           ('g_ple', [4, 1024]), ('g_ple_gate', [4, 1024]), ('w_ple_gate', [4, 1024, 1024])]


def make_consts():
    c = {}
    c['ident'] = np.eye(128, dtype=np.float32)
    c['ones64'] = np.ones((64, 64), np.float32)
    c['ones128'] = np.ones((128, 128), np.float32)
    half = 8
    c['invf'] = (np.float32(500000.0) ** (-np.arange(half, dtype=np.float32) * np.float32(2.0) / np.float32(16))).astype(np.float32)
    a = np.arange(128)[:, None]
    b = np.arange(128)[None, :]
    mA = (a >= b).astype(np.float32)
    mB = (a <= b).astype(np.float32)
    c['mab'] = np.concatenate([mA, mB, mA, mB], axis=1)
    sel = np.zeros((65, 64), np.float32)
    sel[64, :] = 1.0
    c['sel65'] = sel
    p = np.arange(64)[:, None]
    f = np.arange(64)[None, :]
    c['triF'] = (p <= f).astype(np.float32)
    c['triB'] = (p >= f).astype(np.float32)

    def m12(fw, bw):
        return np.ascontiguousarray(np.stack([fw] * 6 + [bw] * 6, axis=1).astype(np.float32))
    c['mW'] = m12(f > p, f < p)
    c['mWt'] = m12(p > f, p < f)
    c['mI'] = m12(f >= p, f <= p)
    c['triS'] = (a < b).astype(np.float32)
    c['iota512'] = np.ascontiguousarray(np.broadcast_to(np.arange(512, dtype=np.float32)[None, :], (128, 512)))
    tpv = np.zeros((128, NT, 2), np.float32)
    tpv[:, :, 0] = np.arange(NT)[None, :]
    tpv[:, :, 1] = np.arange(128)[:, None]
    c['tp'] = tpv
    return c


SCRATCH = [('cs', [S, 16], F32), ('vn', [S, 256], BF16), ('qkT', [6, 128, S], BF16), ('vaug', [S, 390], BF16), ('gate_s', [S, 384], F32),
           ('ab', [S, 24], F32), ('uT', [256, S], F32), ('cT', [1152, S], F32), ('yT', [1024, S], BF16), ('v_tm', [S, 384], F32),
           ('k_tm', [S, 384], F32), ('qT_g', [384, S], BF16), ('kT_g', [384, S], BF16), ('gb', [S, 24], F32), ('o_fb', [2, S, 384], F32),
           ('xw', [S, 1024], F32), ('hb', [S, 1024], BF16)]


def build(n_layers=4, phases=None, dbg=()):
    P = Prog()
    D = {}
    D['x'] = P.dram('x', [S, 1024], F32, 'ExternalInput')
    D['p'] = P.dram('p', [n_layers, S, 256], F32, 'ExternalInput')
    D['positions'] = P.dram('positions', [128, NT], I32, 'ExternalInput')
    for n, shp in WEIGHTS:
        D[n] = P.dram(n, [n_layers] + list(shp[1:]), F32, 'ExternalInput')
    for n, v in make_consts().items():
        D[n] = P.dram(n, list(v.shape), F32, 'ExternalInput')
    for n, shp, dt in SCRATCH:
        D[n] = P.dram(n, shp, dt, 'ExternalOutput' if n in dbg else 'Internal')
    D['out'] = P.dram('out', [S, 1024], F32, 'ExternalOutput')
    allp = phases is None
    if allp or 'R' in phases:
        phase_rope(P, D)
    for l in range(n_layers):
        first = (l == 0)
        last = (l == n_layers - 1)
        if allp or 'A' in phases:
            phase_A(P, l, D, first)
        if allp or 'B' in phases:
            phase_B(P, l, D)
        if allp or 'C' in phases:
            phase_C(P, l, D)
        if allp or 'D1' in phases:
            phase_D1(P, l, D)
        if allp or 'D2' in phases:
            phase_D2(P, l, D)
        if allp or 'E' in phases:
            phase_E(P, l, D, first)
        if allp or 'F' in phases:
            phase_F(P, l, D)
        if allp or 'G' in phases:
            phase_G(P, l, D, last and allp)
    return P


def kernel(**inputs):
    n = 8
    P = build(4)
    consts = make_consts()
    shared = {k: np.ascontiguousarray(np.asarray(inputs[k], dtype=np.float32)) for k, _ in WEIGHTS}
    shared.update(consts)
    x = np.asarray(inputs['x'], dtype=np.float32)
    p = np.asarray(inputs['p'], dtype=np.float32)
    pos = np.asarray(inputs['positions']).astype(np.int32)
    in_maps = []
    for c in range(n):
        m = dict(shared)
        m['x'] = np.ascontiguousarray(x[c])
        m['p'] = np.ascontiguousarray(p[:, c])
        m['positions'] = np.ascontiguousarray(pos[c].reshape(NT, 128).T)
        in_maps.append(m)
    res = run_bass_kernel_spmd(P.nc, in_maps, core_ids=list(range(n)))
    return np.stack([np.asarray(res.results[c]['out'], dtype=np.float32) for c in range(n)], axis=0)
```

```python
import numpy as np
from contextlib import ExitStack
import concourse.bass as bass
import concourse.mybir as mybir
from concourse.bass_utils import run_bass_kernel_spmd

F32 = mybir.dt.float32
BF16 = mybir.dt.bfloat16
I32 = mybir.dt.int32
ALU = mybir.AluOpType
AF = mybir.ActivationFunctionType
AX = mybir.AxisListType

ENGS = ['pe', 'dve', 'act', 'pool', 'sp']
DMA_ENGS = ['sp', 'pool', 'act']
NDS = 8
EPS = 1e-6
S = 4096
NT = 32


NOWAW = frozenset(['d_vn', 'd_qkT', 'd_vaug', 'd_gate', 'd_ab', 'd_uT', 'd_cT', 'd_vtm', 'd_ktm', 'd_qkTg', 'd_yT', 'd_ofb', 'd_hb', 'd_gb', 'd_cs'])


class Prog:
    def __init__(self):
        self.nc = bass.Bass("TRN2", target_bir_lowering=False)
        self.stack = ExitStack()
        self.sems = {}
        for e in ENGS:
            self.sems[e] = self.stack.enter_context(self.nc.semaphore('s_' + e))
        self.dcount = {}
        for e in DMA_ENGS:
            for i in range(NDS):
                k = 'd_%s_%d' % (e, i)
                self.sems[k] = self.stack.enter_context(self.nc.semaphore(k))
                self.dcount[k] = 0
        self.dnext = {e: 0 for e in DMA_ENGS}
        self.cnt = {e: 0 for e in ENGS}
        self.waited = {e: {} for e in ENGS}
        self.q = {e: [] for e in ENGS}
        self.lastw = {}
        self.readers = {}
        self.multiw = {}
        self.nops = 0
        self.xkeys = set(['pT', 'pv', 'pq', 'pk', 'pbv', 'pg', 'pf', 'ptr', 'pm', 'pss', 'ppv', 'pd', 'ptb', 'po', 'pbig', 'pl', 'pu', 'py', 'pe', 'pA', 'pB', 'pC', 'pD', 'pS'])

    def _deps(self, eng, reads, writes):
        deps = {}

        def add(m):
            if m is None:
                return
            k, v = m
            if eng == 'pe' and k == 'pe':
                return
            if deps.get(k, 0) < v:
                deps[k] = v
        for r in reads:
            add(self.lastw.get(r))
            for m in self.multiw.get(r, ()):
                add(m)
        for w in writes:
            if w in NOWAW:
                continue
            add(self.lastw.get(w))
            for m in self.readers.get(w, ()):
                add(m)
        out = []
        wd = self.waited[eng]
        for k, v in deps.items():
            if wd.get(k, 0) < v:
                wd[k] = v
                out.append((k, v))
        return out

    def _mark(self, mark, reads, writes):
        for w in writes:
            if w in NOWAW:
                self.multiw.setdefault(w, []).append(mark)
                continue
            self.lastw[w] = mark
            self.readers[w] = []
        for r in reads:
            if r in writes:
                continue
            self.readers.setdefault(r, []).append(mark)

    cut = None
    pc = 0

    def isx(self, k):
        n = k[0] if isinstance(k, tuple) else k
        return isinstance(n, str) and n in self.xkeys

    def op(self, eng, fn, reads=(), writes=()):
        self.pc += 1
        if self.cut is not None and self.pc > self.cut:
            return
        xr = [r for r in reads if self.isx(r) and r not in writes]
        if xr:
            writes = list(writes) + xr
        waits = self._deps(eng, reads, writes)
        self.cnt[eng] += 1
        mark = (eng, self.cnt[eng])
        self.q[eng].append((waits, fn, (eng, 1)))
        self._mark(mark, reads, writes)
        self.nops += 1

    def dma(self, eng, out, in_, reads=(), writes=(), **kw):
        self.pc += 1
        if self.cut is not None and self.pc > self.cut:
            return
        waits = self._deps(eng, reads, writes)
        i = self.dnext[eng]
        self.dnext[eng] = (i + 1) % NDS
        k = 'd_%s_%d' % (eng, i)
        c = self.dcount[k]
        wd = self.waited[eng]
        if c > 0 and wd.get(k, 0) < 16 * c:
            wd[k] = 16 * c
            waits.append((k, 16 * c))
        self.dcount[k] = c + 1
        mark = (k, 16 * (c + 1))
        self.q[eng].append((waits, (lambda e: e.dma_start(out=out, in_=in_, **kw)), (k, 16)))
        self._mark(mark, reads, writes)
        self.nops += 1

    _breg = None

    def breg(self, e):
        if self._breg is None:
            self._breg = e.to_reg(S - 1)
        return self._breg

    def idma(self, fn, reads=(), writes=()):
        eng = 'pool'
        self.pc += 1
        waits = self._deps(eng, reads, writes)
        i = self.dnext[eng]
        self.dnext[eng] = (i + 1) % NDS
        k = 'd_%s_%d' % (eng, i)
        c = self.dcount[k]
        wd = self.waited[eng]
        if c > 0 and wd.get(k, 0) < 16 * c:
            wd[k] = 16 * c
            waits.append((k, 16 * c))
        self.dcount[k] = c + 1
        mark = (k, 16 * (c + 1))
        self.q[eng].append((waits, fn, (k, 16)))
        self._mark(mark, reads, writes)
        self.nops += 1

    def barrier(self):
        for e in ENGS:
            waits = []
            wd = self.waited[e]
            for o in ENGS:
                if o != e and self.cnt[o] > wd.get(o, 0):
                    wd[o] = self.cnt[o]
                    waits.append((o, self.cnt[o]))
            for k, c in self.dcount.items():
                if 16 * c > wd.get(k, 0):
                    wd[k] = 16 * c
                    waits.append((k, 16 * c))
            if waits:
                self.q[e].append((waits, None, None))
        self.lastw = {}
        self.readers = {}
        self.multiw = {}

    def emit(self):
        self.barrier()
        nc = self.nc
        sems = self.sems
        q = self.q

        def replay(name, e):
            for waits, fn, inc in q[name]:
                for k, v in waits:
                    e.wait_ge(sems[k], v)
                if fn is not None:
                    ins = fn(e)
                    ins.then_inc(sems[inc[0]], inc[1])

        with nc.Block() as block:
            @block.tensor
            def _(e):
                replay('pe', e)

            @block.vector
            def _(e):
                replay('dve', e)

            @block.scalar
            def _(e):
                replay('act', e)

            @block.gpsimd
            def _(e):
                replay('pool', e)

            @block.sync
            def _(e):
                replay('sp', e)
        self.q = {e: [] for e in ENGS}

    uid = 0

    def sb(self, stack, name, shape, dt):
        self.uid += 1
        return stack.enter_context(self.nc.sbuf_tensor('%s_%d' % (name, self.uid), list(shape), dt))

    def ps(self, stack, name, shape, dt=F32, keys=()):
        for k in keys:
            self.xkeys.add(k)
        self.uid += 1
        return stack.enter_context(self.nc.psum_tensor('%s_%d' % (name, self.uid), list(shape), dt))

    def dram(self, name, shape, dt, kind="Internal"):
        return self.nc.dram_tensor(name, list(shape), dt, kind=kind).ap()


def ssl(a, n, d):
    return slice(a, a + (n - 1) * d + 1, d)


def bc(ap, shape):
    return ap.to_broadcast(list(shape))


def phase_rope(P, D):
    with ExitStack() as s:
        pi_ = P.sb(s, 'r_pi', [128, NT], I32)
        pf = P.sb(s, 'r_pf', [128, NT], F32)
        invf = P.sb(s, 'r_invf', [128, 8], F32)
        ang = P.sb(s, 'r_ang', [128, 2, NT, 8], F32)
        kk = P.sb(s, 'r_kk', [128, 2, NT, 8], F32)
        ki = P.sb(s, 'r_ki', [128, 2, NT, 8], I32)
        cs = P.sb(s, 'r_cs', [128, NT, 16], F32)
        P.dma('sp', pi_[:], D['positions'], writes=['pi'])
        P.dma('sp', invf[:], D['invf'].partition_broadcast(128), writes=['invf'])
        P.op('dve', lambda e: e.tensor_copy(pf[:], pi_[:]), reads=['pi'], writes=['pf'])
        P.op('dve', lambda e: e.tensor_tensor(ang[:, 1], bc(pf[:].unsqueeze(2), [128, NT, 8]), bc(invf[:].unsqueeze(1), [128, NT, 8]), ALU.mult),
             reads=['pf', 'invf'], writes=['ang1'])
        P.op('dve', lambda e: e.tensor_scalar(ang[:, 0], ang[:, 1], float(np.pi / 2), None, ALU.add), reads=['ang1'], writes=['ang0'])
        A = ang[:].rearrange('p a t c -> p (a t c)')
        K = kk[:].rearrange('p a t c -> p (a t c)')
        KI = ki[:].rearrange('p a t c -> p (a t c)')
        P.op('dve', lambda e: e.tensor_scalar(K, A, float(1.0 / (2 * np.pi)), None, ALU.mult), reads=['ang0', 'ang1'], writes=['kk'])
        P.op('dve', lambda e: e.tensor_copy(KI, K), reads=['kk'], writes=['ki'])
        P.op('dve', lambda e: e.tensor_copy(K, KI), reads=['ki'], writes=['kk'])
        C1 = 6.28125
        C2 = float(2 * np.pi - 6.28125)
        P.op('dve', lambda e: e.scalar_tensor_tensor(A, K, -C1, A, ALU.mult, ALU.add), reads=['kk', 'ang0', 'ang1'], writes=['ang'])
        P.op('dve', lambda e: e.scalar_tensor_tensor(A, K, -C2, A, ALU.mult, ALU.add), reads=['kk', 'ang'], writes=['ang'])
        P.op('dve', lambda e: e.tensor_scalar(A, A, 3.1415925, -3.1415925, ALU.min, ALU.max), reads=['ang'], writes=['ang'])
        P.op('act', lambda e: e.activation(cs[:, :, 0:8], ang[:, 0], AF.Sin), reads=['ang'], writes=['cs0'])
        P.op('act', lambda e: e.activation(cs[:, :, 8:16], ang[:, 1], AF.Sin), reads=['ang'], writes=['cs1'])
        P.dma('sp', D['cs'].rearrange('(t p) c -> p t c', p=128), cs[:], reads=['cs0', 'cs1'], writes=['d_cs'])
        P.emit()


def phase_A(P, l, D, first):
    xsrc = D['x'] if first else D['xw']
    with ExitStack() as s:
        wbf = P.sb(s, 'a_wbf', [128, 8, 3224], BF16)
        gmix = P.sb(s, 'a_gmix', [128, 1024], F32)
        lng = P.sb(s, 'a_lng', [128, 256], F32)
        lnb = P.sb(s, 'a_lnb', [128, 256], F32)
        qkg = P.sb(s, 'a_qkg', [128, 2, 64], F32)
        cs = P.sb(s, 'a_cs', [128, NT, 16], F32)
        idb = P.sb(s, 'a_idb', [128, 128], BF16)
        xts = [P.sb(s, 'a_xt%d' % i, [128, 1024], F32) for i in range(2)]
        junk = P.sb(s, 'a_junk', [128, 1024], BF16)
        ss = P.sb(s, 'a_ss', [128, 2], F32)
        xn = P.sb(s, 'a_xn', [128, 1024], BF16)
        xnT = [P.sb(s, 'a_xnT%d' % i, [128, 8, 512], BF16) for i in range(2)]
        ge = P.sb(s, 'a_ge', [128, 4, 64], F32)
        cen = P.sb(s, 'a_cen', [128, 4, 64], F32)
        sq = P.sb(s, 'a_sq', [128, 4, 64], F32)
        m4 = P.sb(s, 'a_m4', [128, 8], F32)
        vnb = [P.sb(s, 'a_vnb%d' % i, [128, 256], BF16) for i in range(2)]
        sqq = P.sb(s, 'a_sqq', [128, 12, 64], F32)
        ss12 = P.sb(s, 'a_ss12', [128, 12], F32)
        qk32 = P.sb(s, 'a_qk32', [128, 12, 64], F32)
        rt = P.sb(s, 'a_rt', [128, 4, 12, 8], F32)
        qkb = P.sb(s, 'a_qkb', [128, 12, 64], BF16)
        qkTs = [P.sb(s, 'a_qkTs%d' % i, [128, 6, 128], BF16) for i in range(2)]
        vaug = [P.sb(s, 'a_vaug%d' % i, [128, 6, 65], BF16) for i in range(2)]
        gs = [P.sb(s, 'a_gs%d' % i, [128, 408], F32) for i in range(2)]
        fo = [P.sb(s, 'a_fo%d' % i, [128, 512], F32) for i in range(2)]
        pT = P.ps(s, 'a_pT', [128, 1024], BF16)
        pv = P.ps(s, 'a_pv', [128, 512])
        pq = P.ps(s, 'a_pq', [128, 512])
        pk = P.ps(s, 'a_pk', [128, 512])
        pbv = P.ps(s, 'a_pbv', [128, 512])
        pg = P.ps(s, 'a_pg', [128, 512])
        pf = [P.ps(s, 'a_pf%d' % i, [128, 512]) for i in range(2)]

        for k in range(8):
            P.dma('pool', wbf[:, k, :], D['w_in'][l, k * 128:(k + 1) * 128, :], writes=[('wbf', k)])
        P.dma('sp', gmix[:], D['g_mix'][l].partition_broadcast(128), writes=['gmix'])
        P.dma('sp', lng[:], D['ln_v_g'][l].rearrange('g d -> (g d)').partition_broadcast(128), writes=['lng'])
        P.dma('sp', lnb[:], D['ln_v_b'][l].rearrange('g d -> (g d)').partition_broadcast(128), writes=['lnb'])
        P.dma('sp', qkg[:, 0, :], D['q_norm_g'][l].partition_broadcast(128), writes=['qkg0'])
        P.dma('sp', qkg[:, 1, :], D['k_norm_g'][l].partition_broadcast(128), writes=['qkg1'])
        P.dma('sp', cs[:], D['cs'].rearrange('(t p) c -> p t c', p=128), reads=['d_cs'], writes=['cs'])
        P.dma('pool', idb[:], D['ident'], writes=['idb'])
        for i in range(2):
            P.op('pool', lambda e, i=i: e.memset(vaug[i][:, :, 64:65], 1.0), writes=[('vaug', i)])
        WB = [('wbf', k) for k in range(8)]
        fcount = [0]

        def front(t):
            if True:
                g, j = t // 4, t % 4
                XT = xnT[g % 2]
                kxt = ('xnT', g % 2)
                b = t % 2
                xt = xts[b]
                P.dma('sp', xt[:], xsrc[t * 128:(t + 1) * 128, :], writes=[('xt', b)])
                P.op('dve', lambda e, b=b: e.memset(ss[:, b:b + 1], 0.0), writes=[('ss', b)])
                P.op('act', lambda e, xt=xt, b=b: e.activation(junk[:], xt[:], AF.Square, accum_out=ss[:, b:b + 1]),
                     reads=[('xt', b), ('ss', b)], writes=['junk', ('ss', b)])
                P.op('act', lambda e, b=b: e.activation(ss[:, b:b + 1], ss[:, b:b + 1], AF.Sqrt, bias=EPS, scale=1.0 / 1024),
                     reads=[('ss', b)], writes=[('ss', b)])
                P.op('dve', lambda e, b=b: e.reciprocal(ss[:, b:b + 1], ss[:, b:b + 1]), reads=[('ss', b)], writes=[('ss', b)])
                P.op('dve', lambda e, xt=xt, b=b: e.scalar_tensor_tensor(xn[:], xt[:], ss[:, b:b + 1], gmix[:], ALU.mult, ALU.mult),
                     reads=[('xt', b), ('ss', b), 'gmix'], writes=['xn'])
                for k in range(8):
                    P.op('pe', lambda e, k=k: e.transpose(pT[:, k * 128:(k + 1) * 128], xn[:, k * 128:(k + 1) * 128], idb[:]),
                         reads=['xn', 'idb'], writes=['pT'])
                P.op('act', lambda e, XT=XT, j=j: e.copy(XT[:, :, j * 128:(j + 1) * 128], pT[:].rearrange('p (k t) -> p k t', t=128)),
                     reads=['pT'], writes=[kxt + (j,)])

        def back(t):
            if True:
                g, j = t // 4, t % 4
                XT = xnT[g % 2]
                kxt = ('xnT', g % 2)
                b = t % 2
                for (pp, nm, c0, c1) in ((pv, 'pv', 256, 512), (pq, 'pq', 512, 896), (pk, 'pk', 896, 1280), (pbv, 'pbv', 1280, 1664), (pg, 'pg', 2816, 3224)):
                    for k in range(8):
                        P.op('pe', lambda e, pp=pp, k=k, c0=c0, c1=c1, XT=XT, j=j: e.matmul(pp[:, 0:c1 - c0], XT[:, k, j * 128:(j + 1) * 128], wbf[:, k, c0:c1], start=(k == 0), stop=(k == 7)),
                             reads=[kxt + (j,), ('wbf', k)], writes=[nm])
                GE = ge[:].rearrange('p a b -> p (a b)')
                P.op('act', lambda e: e.activation(GE, pv[:, 0:256], AF.Gelu_apprx_tanh), reads=['pv'], writes=['ge'])
                P.op('dve', lambda e: e.tensor_reduce(m4[:, 0:4], ge[:], AX.X, ALU.add), reads=['ge'], writes=['m4a'])
                P.op('dve', lambda e: e.tensor_scalar(m4[:, 0:4], m4[:, 0:4], 1.0 / 64, None, ALU.mult), reads=['m4a'], writes=['m4a'])
                P.op('dve', lambda e: e.tensor_tensor(cen[:], ge[:], bc(m4[:, 0:4].unsqueeze(2), [128, 4, 64]), ALU.subtract), reads=['ge', 'm4a'], writes=['cen'])
                P.op('act', lambda e: e.activation(sq[:], cen[:], AF.Square), reads=['cen'], writes=['sq'])
                P.op('dve', lambda e: e.tensor_reduce(m4[:, 4:8], sq[:], AX.X, ALU.add), reads=['sq'], writes=['m4b'])
                P.op('act', lambda e: e.activation(m4[:, 4:8], m4[:, 4:8], AF.Sqrt, bias=EPS, scale=1.0 / 64), reads=['m4b'], writes=['m4b'])
                P.op('dve', lambda e: e.reciprocal(m4[:, 4:8], m4[:, 4:8]), reads=['m4b'], writes=['m4b'])
                P.op('dve', lambda e: e.tensor_tensor(cen[:], cen[:], bc(m4[:, 4:8].unsqueeze(2), [128, 4, 64]), ALU.mult), reads=['cen', 'm4b'], writes=['cen'])
                CEN = cen[:].rearrange('p a b -> p (a b)')
                P.op('pool', lambda e: e.tensor_tensor(CEN, CEN, lng[:], ALU.mult), reads=['cen', 'lng'], writes=['cen'])
                P.op('pool', lambda e, b=b: e.tensor_tensor(vnb[b][:], CEN, lnb[:], ALU.add), reads=['cen', 'lnb'], writes=[('vnb', b)])
                P.dma('sp', D['vn'][t * 128:(t + 1) * 128, :], vnb[b][:], reads=[('vnb', b)], writes=['d_vn'])
                P.op('act', lambda e: e.activation(sqq[:, 0:6, :].rearrange('p a b -> p (a b)'), pq[:, 0:384], AF.Square), reads=['pq'], writes=['sqq0'])
                P.op('act', lambda e: e.activation(sqq[:, 6:12, :].rearrange('p a b -> p (a b)'), pk[:, 0:384], AF.Square), reads=['pk'], writes=['sqq1'])
                P.op('dve', lambda e: e.tensor_reduce(ss12[:], sqq[:], AX.X, ALU.add), reads=['sqq0', 'sqq1'], writes=['ss12'])
                P.op('act', lambda e: e.activation(ss12[:], ss12[:], AF.Sqrt, bias=EPS, scale=1.0 / 64), reads=['ss12'], writes=['ss12'])
                P.op('dve', lambda e: e.reciprocal(ss12[:], ss12[:]), reads=['ss12'], writes=['ss12'])
                P.op('dve', lambda e: e.tensor_tensor(qk32[:, 0:6, :], pq[:, 0:384].rearrange('p (a b) -> p a b', b=64), bc(ss12[:, 0:6].unsqueeze(2), [128, 6, 64]), ALU.mult),
                     reads=['pq', 'ss12'], writes=['qk32a'])
                P.op('dve', lambda e: e.tensor_tensor(qk32[:, 6:12, :], pk[:, 0:384].rearrange('p (a b) -> p a b', b=64), bc(ss12[:, 6:12].unsqueeze(2), [128, 6, 64]), ALU.mult),
                     reads=['pk', 'ss12'], writes=['qk32b'])
                P.op('pool', lambda e: e.tensor_tensor(qk32[:, 0:6, :], qk32[:, 0:6, :], bc(qkg[:, 0:1, :], [128, 6, 64]), ALU.mult), reads=['qk32a', 'qkg0'], writes=['qk32a'])
                P.op('pool', lambda e: e.tensor_tensor(qk32[:, 6:12, :], qk32[:, 6:12, :], bc(qkg[:, 1:2, :], [128, 6, 64]), ALU.mult), reads=['qk32b', 'qkg1'], writes=['qk32b'])
                cosb = bc(cs[:, t:t + 1, 0:8], [128, 12, 8])
                sinb = bc(cs[:, t:t + 1, 8:16], [128, 12, 8])
                x1 = qk32[:, :, 0:8]
                x2 = qk32[:, :, 8:16]
                P.op('pool', lambda e, cosb=cosb: e.tensor_tensor(rt[:, 0], x1, cosb, ALU.mult), reads=['qk32a', 'qk32b', 'cs'], writes=['rt0'])
                P.op('pool', lambda e, sinb=sinb: e.tensor_tensor(rt[:, 1], x2, sinb, ALU.mult), reads=['qk32a', 'qk32b', 'cs'], writes=['rt1'])
                P.op('dve', lambda e, cosb=cosb: e.tensor_tensor(rt[:, 2], x2, cosb, ALU.mult), reads=['qk32a', 'qk32b', 'cs'], writes=['rt2'])
                P.op('dve', lambda e, sinb=sinb: e.tensor_tensor(rt[:, 3], x1, sinb, ALU.mult), reads=['qk32a', 'qk32b', 'cs'], writes=['rt3'])
                P.op('act', lambda e: e.copy(qkb[:], qk32[:]), reads=['qk32a', 'qk32b'], writes=['qkb'])
                P.op('dve', lambda e: e.tensor_tensor(qkb[:, :, 0:8], rt[:, 0], rt[:, 1], ALU.subtract), reads=['rt0', 'rt1', 'qkb'], writes=['qkb'])
                P.op('dve', lambda e: e.tensor_tensor(qkb[:, :, 8:16], rt[:, 2], rt[:, 3], ALU.add), reads=['rt2', 'rt3', 'qkb'], writes=['qkb'])
                for i in range(6):
                    P.op('pe', lambda e, i=i: e.transpose(pT[:, i * 128:(i + 1) * 128], qkb[:, 2 * i:2 * i + 2, :].rearrange('p a b -> p (a b)'), idb[:]),
                         reads=['qkb', 'idb'], writes=['pT'])
                P.op('act', lambda e, b=b: e.copy(qkTs[b][:], pT[:, 0:768].rearrange('p (k t) -> p k t', t=128)), reads=['pT'], writes=[('qkTs', b)])
                P.dma('sp', D['qkT'][:, :, t * 128:(t + 1) * 128].rearrange('i p t -> p i t'), qkTs[b][:], reads=[('qkTs', b)], writes=['d_qkT'])
                P.op('act', lambda e, b=b: e.copy(vaug[b][:, :, 0:64], pbv[:, 0:384].rearrange('p (a b) -> p a b', b=64)), reads=['pbv', ('vaug', b)], writes=[('vaug', b)])
                P.dma('sp', D['vaug'][t * 128:(t + 1) * 128, :], vaug[b][:].rearrange('p a b -> p (a b)'), reads=[('vaug', b)], writes=['d_vaug'])
                P.op('act', lambda e, b=b: e.activation(gs[b][:, 0:384], pg[:, 0:384], AF.Silu), reads=['pg'], writes=[('gs', b)])
                P.op('dve', lambda e, b=b: e.tensor_copy(gs[b][:, 384:408], pg[:, 384:408]), reads=['pg', ('gs', b)], writes=[('gs', b)])
                P.dma('sp', D['gate_s'][t * 128:(t + 1) * 128, :], gs[b][:, 0:384], reads=[('gs', b)], writes=['d_gate'])
                P.dma('sp', D['ab'][t * 128:(t + 1) * 128, :], gs[b][:, 384:408], reads=[('gs', b)], writes=['d_ab'])

        def fmaj(g):
            XT = xnT[g % 2]
            kxt = ('xnT', g % 2)
            allx = [kxt + (j,) for j in range(4)]
            for ci in range(11):
                c0 = ci * 128 if ci < 2 else 1664 + (ci - 2) * 128
                fb = fcount[0] % 2
                fcount[0] += 1
                for k in range(8):
                    P.op('pe', lambda e, fb=fb, k=k, c0=c0, XT=XT: e.matmul(pf[fb][:], wbf[:, k, c0:c0 + 128], XT[:, k, :], start=(k == 0), stop=(k == 7)),
                         reads=allx + [('wbf', k)], writes=[('pf', fb)])
                if ci < 2:
                    P.op('act', lambda e, fb=fb: e.activation(fo[fb][:], pf[fb][:], AF.Gelu_apprx_tanh), reads=[('pf', fb)], writes=[('fo', fb)])
                    P.dma('sp', D['uT'][ci * 128:(ci + 1) * 128, g * 512:(g + 1) * 512], fo[fb][:], reads=[('fo', fb)], writes=['d_uT'])
                else:
                    P.op('dve', lambda e, fb=fb: e.tensor_copy(fo[fb][:], pf[fb][:]), reads=[('pf', fb)], writes=[('fo', fb)])
                    P.dma('sp', D['cT'][(ci - 2) * 128:(ci - 1) * 128, g * 512:(g + 1) * 512], fo[fb][:], reads=[('fo', fb)], writes=['d_cT'])

        front(0)
        for t in range(NT):
            if t + 1 < NT:
                front(t + 1)
            back(t)
            if t % 4 == 3:
                fmaj(t // 4)
        P.emit()


def phase_B(P, l, D):
    with ExitStack() as s:
        ws32 = P.sb(s, 'b_ws32', [128, 4, 128], F32)
        idf = P.sb(s, 'b_idf', [128, 128], F32)
        wsT = P.sb(s, 'b_wsT', [128, 4, 128], BF16)
        bias = P.sb(s, 'b_bias', [64, 4, 128], F32)
        vn = [P.sb(s, 'b_vn%d' % i, [128, 4, 256], BF16) for i in range(2)]
        ut = [P.sb(s, 'b_ut%d' % i, [64, 4, 512], F32) for i in range(2)]
        mx = P.sb(s, 'b_mx', [64, 4, 128], F32)
        yb = [P.sb(s, 'b_yb%d' % i, [64, 4, 512], BF16) for i in range(2)]
        ptr = P.ps(s, 'b_ptr', [128, 512])
        pm = [P.ps(s, 'b_pm%d' % i, [64, 512]) for i in range(4)]
        P.dma('sp', ws32[:], D['w_s'][l].rearrange('g i j -> i g j'), writes=['ws32'])
        P.dma('sp', idf[:], D['ident'], writes=['idf'])
        P.dma('sp', bias[:].rearrange('p g i -> p (g i)'), D['b_s'][l].rearrange('g i -> (g i)').partition_broadcast(64), writes=['bias'])
        for g in range(4):
            P.op('pe', lambda e, g=g: e.transpose(ptr[:, g * 128:(g + 1) * 128], ws32[:, g, :], idf[:]), reads=['ws32', 'idf'], writes=['ptr'])
        P.op('dve', lambda e: e.tensor_copy(wsT[:].rearrange('p g i -> p (g i)'), ptr[:]), reads=['ptr'], writes=['wsT'])
        for it in range(8):
            b = it % 2
            P.dma('sp', vn[b][:], D['vn'][it * 512:(it + 1) * 512, :].rearrange('(c p) n -> p c n', p=128), reads=['d_vn'], writes=[('vn', b)])
            P.dma('sp', ut[b][:], D['uT'][:, it * 512:(it + 1) * 512].rearrange('(g d) t -> d g t', d=64), reads=['d_uT'], writes=[('ut', b)])
            for g in range(4):
                for c in range(4):
                    P.op('pe', lambda e, g=g, c=c, b=b: e.matmul(pm[g][:, c * 128:(c + 1) * 128], vn[b][:, c, g * 64:(g + 1) * 64], wsT[:, g, :], start=True, stop=True),
                         reads=[('vn', b), 'wsT'], writes=[('pm', g)])
                P.op('dve', lambda e, g=g: e.tensor_tensor(mx[:], pm[g][:].rearrange('p (c i) -> p c i', i=128), bc(bias[:, g:g + 1, :], [64, 4, 128]), ALU.add),
                     reads=[('pm', g), 'bias'], writes=['mx'])
                P.op('dve', lambda e, g=g, b=b: e.tensor_tensor(yb[b][:, g, :], mx[:].rearrange('p c i -> p (c i)'), ut[b][:, g, :], ALU.mult),
                     reads=['mx', ('ut', b)], writes=[('yb', b)])
            P.dma('sp', D['yT'][0:256, it * 512:(it + 1) * 512].rearrange('(g d) t -> d g t', d=64), yb[b][:], reads=[('yb', b)], writes=['d_yT'])
        P.emit()


PATS = (1, 4, 16)
KPAD = 1024


def phase_C(P, l, D):
    with ExitStack() as s:
        vs = {}
        for d in PATS:
            nt = d * (S // d // 128 + 1)
            vs[d] = P.sb(s, 'c_vs%d' % d, [128, nt, 390], BF16)
        mab = P.sb(s, 'c_mab', [128, 512], BF16)
        sel = P.sb(s, 'c_sel', [65, 64], F32)
        qh = [P.sb(s, 'c_qh%d' % i, [64, S], BF16) for i in range(2)]
        kh = [P.sb(s, 'c_kh%d' % i, [64, S + 2 * KPAD], BF16) for i in range(2)]
        pex = [P.sb(s, 'c_pex%d' % i, [128, 512], BF16) for i in range(3)]
        acc = P.sb(s, 'c_acc', [65, S], F32)
        rd = P.sb(s, 'c_rd', [64, 512], F32)
        yb = [P.sb(s, 'c_yb%d' % i, [64, 512], BF16) for i in range(2)]
        pss = [P.ps(s, 'c_ps%d' % i, [128, 512]) for i in range(3)]
        ppv = [P.ps(s, 'c_pv%d' % i, [65, 512]) for i in range(3)]
        pd = P.ps(s, 'c_pd', [64, 512])
        P.dma('pool', mab[:], D['mab'], writes=['mab'])
        P.dma('sp', sel[:], D['sel65'], writes=['sel'])
        for i in range(2):
            P.op('pool', lambda e, i=i: e.memset(kh[i][:, 0:KPAD], 0.0), writes=[('kh', i)])
            P.op('pool', lambda e, i=i: e.memset(kh[i][:, KPAD + S:], 0.0), writes=[('kh', i)])
        for d in PATS:
            L = S // d
            nqb = L // 128
            P.op('pool', lambda e, d=d: e.memset(vs[d][:].rearrange('p a b -> p (a b)'), 0.0), writes=[('vs', d)])
            vsrc = D['vaug'].rearrange('(j r) c -> r j c', r=d)
            for r in range(d):
                tb = r * (nqb + 1)
                if nqb > 1:
                    P.dma('sp', vs[d][:, tb + 1:tb + nqb, :], vsrc[r, 64:64 + (nqb - 1) * 128, :].rearrange('(k p) c -> p k c', p=128),
                          reads=['d_vaug', ('vs', d)], writes=[('vs', d)])
                P.dma('sp', vs[d][64:128, tb, :], vsrc[r, 0:64, :], reads=['d_vaug', ('vs', d)], writes=[('vs', d)])
                P.dma('sp', vs[d][0:64, tb + nqb, :], vsrc[r, L - 64:L, :], reads=['d_vaug', ('vs', d)], writes=[('vs', d)])
        for h in range(6):
            hb = h % 2
            P.dma('sp', qh[hb][:], D['qkT'][h // 2, (h % 2) * 64:(h % 2) * 64 + 64, :], reads=['d_qkT'], writes=[('qh', hb)])
            P.dma('sp', kh[hb][:, KPAD:KPAD + S], D['qkT'][3 + h // 2, (h % 2) * 64:(h % 2) * 64 + 64, :], reads=['d_qkT'], writes=[('kh', hb)])
            its = []
            for pi, d in enumerate(PATS):
                L = S // d
                nqb = L // 128
                for r in range(d):
                    tb = r * (nqb + 1)
                    for qb0 in range(0, nqb, 2):
                        its.append((pi, d, r, tb, qb0))

            def stage1(n):
                pi, d, r, tb, qb0 = its[n]
                ib = n % 3
                combos = ((qb0, qb0), (qb0 + 1, qb0), (qb0 + 1, qb0 + 1), (qb0 + 2, qb0 + 1))
                for ci, (kt, qb) in enumerate(combos):
                    k0 = KPAD + r + d * (kt * 128 - 64)
                    q0 = r + d * (qb * 128)
                    P.op('pe', lambda e, ib=ib, ci=ci, k0=k0, q0=q0, d=d, hb=hb: e.matmul(
                        pss[ib][:, ci * 128:(ci + 1) * 128], kh[hb][:, ssl(k0, 128, d)], qh[hb][:, ssl(q0, 128, d)], start=True, stop=True),
                        reads=[('kh', hb), ('qh', hb)], writes=[('pss', ib)])
                P.op('act', lambda e, ib=ib: e.activation(pex[ib][:], pss[ib][:], AF.Exp, scale=0.125), reads=[('pss', ib)], writes=[('pex', ib)])
                P.op('dve', lambda e, ib=ib: e.tensor_tensor(pex[ib][:], pex[ib][:], mab[:], ALU.mult), reads=[('pex', ib), 'mab'], writes=[('pex', ib)])

            def stage2(n):
                pi, d, r, tb, qb0 = its[n]
                ib = n % 3
                combos = ((qb0, qb0), (qb0 + 1, qb0), (qb0 + 1, qb0 + 1), (qb0 + 2, qb0 + 1))
                for ci, (kt, qb) in enumerate(combos):
                    qi = qb - qb0
                    P.op('pe', lambda e, ib=ib, ci=ci, kt=kt, qi=qi, d=d, tb=tb, h=h: e.matmul(
                        ppv[ib][:, qi * 128:(qi + 1) * 128], vs[d][:, tb + kt, h * 65:(h + 1) * 65], pex[ib][:, ci * 128:(ci + 1) * 128],
                        start=(ci % 2 == 0), stop=(ci % 2 == 1)), reads=[('vs', d), ('pex', ib)], writes=[('ppv', ib)])
                a0 = r + d * (qb0 * 128)
                av = acc[:, ssl(a0, 256, d)]
                if pi == 0:
                    P.op('dve', lambda e, av=av, ib=ib: e.tensor_copy(av, ppv[ib][:, 0:256]), reads=[('ppv', ib)], writes=['acc'])
                else:
                    P.op('dve', lambda e, av=av, ib=ib: e.tensor_tensor(av, av, ppv[ib][:, 0:256], ALU.add), reads=[('ppv', ib), 'acc'], writes=['acc'])

            stage1(0)
            for n in range(len(its)):
                if n + 1 < len(its):
                    stage1(n + 1)
                stage2(n)
            for c4 in range(8):
                yb_ = yb[c4 % 2]
                P.op('pe', lambda e, c4=c4: e.matmul(pd[:], sel[:], acc[:, c4 * 512:(c4 + 1) * 512], start=True, stop=True), reads=['sel', 'acc'], writes=['pd'])
                P.op('dve', lambda e: e.reciprocal(rd[:], pd[:]), reads=['pd'], writes=['rd'])
                P.op('dve', lambda e, c4=c4, yb_=yb_: e.tensor_tensor(yb_[:], acc[0:64, c4 * 512:(c4 + 1) * 512], rd[:], ALU.mult), reads=['acc', 'rd'], writes=[('yb', c4 % 2)])
                P.dma('sp', D['yT'][256 + h * 64:256 + (h + 1) * 64, c4 * 512:(c4 + 1) * 512], yb_[:], reads=[('yb', c4 % 2)], writes=['d_yT'])
        P.emit()


def phase_D1(P, l, D):
    with ExitStack() as s:
        cw = P.sb(s, 'd_cw', [128, 5, 9], F32)
        idf = P.sb(s, 'd_idf', [128, 128], F32)
        raw = [P.sb(s, 'd_raw%d' % i, [128, S + 4], F32) for i in range(2)]
        cv = P.sb(s, 'd_cv', [128, S], F32)
        tm = [P.sb(s, 'd_tm%d' % i, [128, 4, 128], F32) for i in range(2)]
        sq = P.sb(s, 'd_sq', [128, 8, 64], F32)
        r8 = P.sb(s, 'd_r8', [128, 8], F32)
        fT = [P.sb(s, 'd_fT%d' % i, [128, 512], BF16) for i in range(2)]
        abt = P.sb(s, 'd_abt', [128, NT, 24], F32)
        gbt = P.sb(s, 'd_gbt', [128, NT, 24], F32)
        dtb = P.sb(s, 'd_dtb', [128, 12], F32)
        nA = P.sb(s, 'd_nA', [128, 12], F32)
        ptr = [P.ps(s, 'd_ptr%d' % i, [128, 512]) for i in range(2)]
        ptb = [P.ps(s, 'd_ptb%d' % i, [128, 512]) for i in range(2)]
        for k in range(5):
            P.dma('sp', cw[:, k, :], D['conv_w'][l, k].rearrange('(c p) -> p c', p=128), writes=['cw'], allow_slow_non_contiguous=True)
        P.dma('sp', idf[:], D['ident'], writes=['idf'])
        for i in range(2):
            P.op('pool', lambda e, i=i: e.memset(raw[i][:, 0:2], 0.0), writes=[('raw', i)])
            P.op('pool', lambda e, i=i: e.memset(raw[i][:, S + 2:S + 4], 0.0), writes=[('raw', i)])
        P.dma('sp', abt[:], D['ab'].rearrange('(t p) c -> p t c', p=128), reads=['d_ab'], writes=['abt'])
        P.dma('sp', dtb[:], D['dt_bias'][l].rearrange('a h -> (a h)').partition_broadcast(128), writes=['dtb'])
        P.dma('sp', nA[:], D['a_log'][l].rearrange('a h -> (a h)').partition_broadcast(128), writes=['nA'])
        P.op('act', lambda e: e.activation(nA[:], nA[:], AF.Exp), reads=['nA'], writes=['nA'])
        P.op('dve', lambda e: e.tensor_scalar(nA[:], nA[:], -1.0, None, ALU.mult), reads=['nA'], writes=['nA'])
        P.op('dve', lambda e: e.tensor_tensor(gbt[:, :, 0:12], abt[:, :, 0:12], bc(dtb[:].unsqueeze(1), [128, NT, 12]), ALU.add), reads=['abt', 'dtb'], writes=['gbt0'])
        P.op('act', lambda e: e.activation(gbt[:, :, 0:12], gbt[:, :, 0:12], AF.Exp), reads=['gbt0'], writes=['gbt0'])
        P.op('act', lambda e: e.activation(gbt[:, :, 0:12], gbt[:, :, 0:12], AF.Ln, bias=1.0), reads=['gbt0'], writes=['gbt0'])
        P.op('dve', lambda e: e.tensor_tensor(gbt[:, :, 0:12], gbt[:, :, 0:12], bc(nA[:].unsqueeze(1), [128, NT, 12]), ALU.mult), reads=['gbt0', 'nA'], writes=['gbt0'])
        P.op('act', lambda e: e.activation(gbt[:, :, 12:24], abt[:, :, 12:24], AF.Sigmoid), reads=['abt'], writes=['gbt1'])
        P.dma('sp', D['gb'].rearrange('(t p) c -> p t c', p=128), gbt[:], reads=['gbt0', 'gbt1'], writes=['d_gb'])
        n4 = 0
        import os
        CUT = int(os.environ.get('D1CUT', '99'))
        for c in range(int(os.environ.get('D1C0', '0')), int(os.environ.get('D1C1', '9'))):
            rb = c % 2
            R = raw[rb]
            P.dma('sp', R[:, 2:S + 2], D['cT'][c * 128:(c + 1) * 128, :], reads=['d_cT'], writes=[('raw', rb)])
            P.op('dve', lambda e, R=R, c=c: e.tensor_scalar(cv[:], R[:, 0:S], cw[:, 0, c:c + 1], None, ALU.mult), reads=[('raw', rb), 'cw'], writes=['cv'])
            for k in range(1, 5):
                eng = 'dve'
                P.op(eng, lambda e, R=R, c=c, k=k: e.scalar_tensor_tensor(cv[:], R[:, k:k + S], cw[:, k, c:c + 1], cv[:], ALU.mult, ALU.add),
                     reads=[('raw', rb), 'cw', 'cv'], writes=['cv'])
            P.op('act', lambda e: e.activation(cv[:], cv[:], AF.Silu), reads=['cv'], writes=['cv'])
            def stA(t4, c=c):
                pb = (c * 8 + t4) % 2
                for j in range(4):
                    t = t4 * 4 + j
                    P.op('pe', lambda e, pb=pb, j=j, t=t: e.transpose(ptr[pb][:, j * 128:(j + 1) * 128], cv[:, t * 128:(t + 1) * 128], idf[:]),
                         reads=['cv', 'idf'], writes=[('ptr', pb)])
                TM = tm[pb]
                PV = ptr[pb][:].rearrange('p (j h d) -> p (j h) d', h=2, d=64)
                if c >= 6:
                    P.op('act', lambda e, pb=pb, TM=TM: e.copy(TM[:].rearrange('p j c -> p (j c)'), ptr[pb][:]), reads=[('ptr', pb)], writes=[('tm', pb)])
                    P.dma('sp', D['v_tm'][t4 * 512:(t4 + 1) * 512, (c - 6) * 128:(c - 5) * 128].rearrange('(j p) c -> p j c', p=128), TM[:], reads=[('tm', pb)], writes=['d_vtm'])
                else:
                    P.op('act', lambda e, pb=pb: e.activation(sq[:].rearrange('p a b -> p (a b)'), ptr[pb][:], AF.Square), reads=[('ptr', pb)], writes=['sq'])
                    P.op('dve', lambda e: e.tensor_reduce(r8[:], sq[:], AX.X, ALU.add), reads=['sq'], writes=['r8'])
                    P.op('act', lambda e: e.activation(r8[:], r8[:], AF.Sqrt, bias=EPS, scale=1.0), reads=['r8'], writes=['r8'])
                    P.op('dve', lambda e: e.reciprocal(r8[:], r8[:]), reads=['r8'], writes=['r8'])
                    if c < 3:
                        P.op('dve', lambda e: e.tensor_scalar(r8[:], r8[:], 0.125, None, ALU.mult), reads=['r8'], writes=['r8'])
                    P.op('dve', lambda e, TM=TM, PV=PV: e.tensor_tensor(TM[:].rearrange('p j (h d) -> p (j h) d', d=64), PV, bc(r8[:].unsqueeze(2), [128, 8, 64]), ALU.mult),
                         reads=[('ptr', pb), 'r8'], writes=[('tm', pb)])
                    if c >= 3:
                        P.dma('sp', D['k_tm'][t4 * 512:(t4 + 1) * 512, (c - 3) * 128:(c - 2) * 128].rearrange('(j p) c -> p j c', p=128), TM[:], reads=[('tm', pb)], writes=['d_ktm'])

            def stB(t4, c=c):
                pb = (c * 8 + t4) % 2
                TM = tm[pb]
                if c < 6:
                    for j in range(4):
                        P.op('pe', lambda e, pb=pb, j=j, TM=TM: e.transpose(ptb[pb][:, j * 128:(j + 1) * 128], TM[:, j, :], idf[:]), reads=[('tm', pb), 'idf'], writes=[('ptb', pb)])
                    P.op('act', lambda e, pb=pb: e.copy(fT[pb][:], ptb[pb][:]), reads=[('ptb', pb)], writes=[('fT', pb)])
                    dst = D['qT_g'] if c < 3 else D['kT_g']
                    cc = c if c < 3 else c - 3
                    P.dma('sp', dst[cc * 128:(cc + 1) * 128, t4 * 512:(t4 + 1) * 512], fT[pb][:], reads=[('fT', pb)], writes=['d_qkTg'])

            stA(0)
            for t4 in range(8):
                if t4 + 1 < 8:
                    stA(t4 + 1)
                stB(t4)
        P.emit()


def phase_D2(P, l, D):
    import os
    C = 64
    NCH = S // C
    NST = int(os.environ.get('D2N', str(NCH)))
    MD = BF16 if os.environ.get('D2BF', '1') == '1' else F32
    with ExitStack() as s:
        def T12(name, dt=F32):
            return P.sb(s, 'e_' + name, [64, 12, 64], dt)
        ones = P.sb(s, 'e_ones', [64, 64], F32)
        idf = P.sb(s, 'e_idf', [64, 64], F32)
        idm = P.sb(s, 'e_idm', [64, 64], MD)
        idbc = T12('idbc')
        triF = P.sb(s, 'e_triF', [64, 64], F32)
        triB = P.sb(s, 'e_triB', [64, 64], F32)
        mW, mWt, mI = T12('mW'), T12('mWt'), T12('mI')
        St, St2, Sm = T12('S'), T12('S2'), T12('Sm', MD)
        ktm = [T12('ktm%d' % i) for i in range(2)]
        vtm = [T12('vtm%d' % i) for i in range(2)]
        kT = [T12('kT%d' % i, MD) for i in range(2)]
        qT = [T12('qT%d' % i, MD) for i in range(2)]
        gbv = [P.sb(s, 'e_gb%d' % i, [64, 24], F32) for i in range(2)]
        gc = P.sb(s, 'e_gc', [64, 12], F32)
        egc = [P.sb(s, 'e_egc%d' % i, [64, 12], F32) for i in range(2)]
        egl = [P.sb(s, 'e_egl%d' % i, [64, 12], F32) for i in range(2)]
        egd = P.sb(s, 'e_egd', [64, 12], F32)
        Dg = P.sb(s, 'e_Dg', [64, 24, 64], F32)
        diff, Ea, Eb = T12('diff'), T12('Ea'), T12('Eb')
        W, Wt = T12('W', MD), T12('Wt', MD)
        A1, A1t, A2, A2t = T12('A1', MD), T12('A1t', MD), T12('A2', MD), T12('A2t', MD)
        nxTI = T12('nxTI', MD)
        Yt = [T12('Yt0', MD), T12('Yt1', MD)]
        Yf = [T12('Yf0', MD), T12('Yf1', MD)]
        QKm = [T12('QKm0', MD), T12('QKm1', MD)]
        kd = [T12('kd0', MD), T12('kd1', MD)]
        Rr, Rm, vnew, o1 = T12('R'), T12('Rm', MD), T12('vnew', MD), T12('o1')
        ob = [T12('ob0'), T12('ob1')]
        pA = P.ps(s, 'e_pA', [64, 1024])
        pB = P.ps(s, 'e_pB', [64, 1024])
        pC = P.ps(s, 'e_pC', [64, 1024])
        pS = P.ps(s, 'e_pS', [64, 1024])

        def pv(p, h):
            return p[:, h * 512:h * 512 + 384].rearrange('p (j t) -> p j t', t=64)

        def sv(t, h):
            return t[:, h * 6:(h + 1) * 6, :]

        def pcol(p, j):
            c0 = (j // 6) * 512 + (j % 6) * 64
            return p[:, c0:c0 + 64]

        def mm12(pt, pn, lfn, rfn, rfun):
            for j in range(12):
                o_, l_, r_ = pcol(pt, j), lfn(j), rfn(j)
                P.op('pe', lambda e, o_=o_, l_=l_, r_=r_: e.matmul(o_, l_, r_, start=True, stop=True), reads=rfun(j // 6), writes=[(pn, j // 6)])

        def bcol(ap12, h):
            return bc(ap12[:, h * 6:(h + 1) * 6].unsqueeze(2), [64, 6, 64])

        P.dma('sp', ones[:], D['ones64'], writes=['ones'])
        P.dma('sp', idf[:], D['ident'][0:64, 0:64], writes=['idf'])
        P.dma('sp', triF[:], D['triF'], writes=['triF'])
        P.dma('sp', triB[:], D['triB'], writes=['triB'])
        P.dma('sp', mW[:], D['mW'], writes=['mW'])
        P.dma('sp', mWt[:], D['mWt'], writes=['mWt'])
        P.dma('sp', mI[:], D['mI'], writes=['mI'])
        P.op('dve', lambda e: e.tensor_copy(idbc[:], bc(idf[:].unsqueeze(1), [64, 12, 64])), reads=['idf'], writes=['idbc'])
        P.op('dve', lambda e: e.tensor_copy(idm[:], idf[:]), reads=['idf'], writes=['idm'])
        P.op('dve', lambda e: e.memset(St[:].rearrange('p a b -> p (a b)'), 0.0), writes=[('S', 0), ('S', 1)])
        P.op('pool', lambda e: e.memset(Sm[:].rearrange('p a b -> p (a b)'), 0.0), writes=[('Sm', 0), ('Sm', 1)])

        def prep(i):
            b = i % 2
            cf = i
            cb = NCH - 1 - i
            K_, V_, KT_, QT_, GB_ = ktm[b], vtm[b], kT[b], qT[b], gbv[b]
            EGC, EGL, QKM, KD = egc[b], egl[b], QKm[b], kd[b]
            for h, cc in ((0, cf), (1, cb)):
                sl = slice(h * 6, h * 6 + 6)
                tk = slice(cc * C, (cc + 1) * C)
                P.dma('sp', K_[:, sl, :], D['k_tm'][tk, :].rearrange('t (h d) -> t h d', d=64), reads=['d_ktm'], writes=[('ktm', b, h)])
                P.dma('sp', V_[:, sl, :], D['v_tm'][tk, :].rearrange('t (h d) -> t h d', d=64), reads=['d_vtm'], writes=[('vtm', b, h)])
                P.dma('sp', KT_[:, sl, :], D['kT_g'][:, tk].rearrange('(h d) t -> d h t', d=64), reads=['d_qkTg'], writes=[('kT', b, h)])
                P.dma('sp', QT_[:, sl, :], D['qT_g'][:, tk].rearrange('(h d) t -> d h t', d=64), reads=['d_qkTg'], writes=[('qT', b, h)])
                P.dma('sp', GB_[:, h * 6:h * 6 + 6], D['gb'][tk, h * 6:h * 6 + 6], reads=['d_gb'], writes=[('gb', b, h)])
                P.dma('sp', GB_[:, 12 + h * 6:12 + h * 6 + 6], D['gb'][tk, 12 + h * 6:12 + h * 6 + 6], reads=['d_gb'], writes=[('gbb', b, h)])
            beta = GB_[:, 12:24]
            DgF = Dg[:].rearrange('p a b -> p (a b)')
            for h in range(2):
                tri = triF if h == 0 else triB
                trin = 'triF' if h == 0 else 'triB'
                rG, rBt = ('gb', b, h), ('gbb', b, h)
                P.op('pe', lambda e, h=h, tri=tri, GB_=GB_: e.matmul(pA[:, h * 512:h * 512 + 6], tri[:], GB_[:, h * 6:h * 6 + 6], start=True, stop=True), reads=[trin, rG], writes=[('pA', h)])
                P.op('dve', lambda e, h=h: e.tensor_copy(gc[:, h * 6:h * 6 + 6], pA[:, h * 512:h * 512 + 6]), reads=[('pA', h)], writes=[('gc', h)])
                P.op('act', lambda e, h=h, EGC=EGC: e.activation(EGC[:, h * 6:h * 6 + 6], pA[:, h * 512:h * 512 + 6], AF.Exp), reads=[('pA', h)], writes=[('egc', b, h)])
                P.op('dve', lambda e, h=h: e.tensor_tensor(Dg[:, h * 6:h * 6 + 6, :], sv(idbc, 0), bcol(gc, h), ALU.mult), reads=['idbc', ('gc', h)], writes=[('Dg', h)])
                P.op('pool', lambda e, h=h, beta=beta: e.tensor_tensor(Dg[:, 12 + h * 6:12 + h * 6 + 6, :], sv(idbc, 0), bcol(beta, h), ALU.mult), reads=['idbc', rBt], writes=[('Dgb', h)])
                P.op('pe', lambda e, h=h: e.matmul(pB[:, h * 512:h * 512 + 384], ones[:], DgF[:, h * 384:(h + 1) * 384], start=True, stop=True), reads=['ones', ('Dg', h)], writes=[('pB', h)])
                P.op('pe', lambda e, h=h: e.matmul(pC[:, h * 512:h * 512 + 384], ones[:], DgF[:, 768 + h * 384:768 + (h + 1) * 384], start=True, stop=True), reads=['ones', ('Dgb', h)], writes=[('pC', h)])
                P.op('dve', lambda e, h=h: e.tensor_tensor(sv(diff, h), pv(pB, h), bcol(gc, h), ALU.subtract), reads=[('pB', h), ('gc', h)], writes=[('diff', h)])
                lc = h * 512 + (63 if h == 0 else 0)
                lastv = pB[:, lc:lc + 64 * 5 + 1:64]
                P.op('act', lambda e, h=h, lastv=lastv, EGL=EGL: e.activation(EGL[:, h * 6:h * 6 + 6], lastv, AF.Exp), reads=[('pB', h)], writes=[('egl', b, h)])
                P.op('dve', lambda e, h=h, lastv=lastv: e.tensor_tensor(egd[:, h * 6:h * 6 + 6], lastv, gc[:, h * 6:h * 6 + 6], ALU.subtract), reads=[('pB', h), ('gc', h)], writes=[('egd', h)])
                P.op('act', lambda e, h=h: e.activation(egd[:, h * 6:h * 6 + 6], egd[:, h * 6:h * 6 + 6], AF.Exp), reads=[('egd', h)], writes=[('egd', h)])
                P.op('pool', lambda e, h=h, K_=K_, KD=KD: e.tensor_tensor(sv(KD, h), sv(K_, h), bcol(egd, h), ALU.mult), reads=[('ktm', b, h), ('egd', h)], writes=[('kd', b, h)])
                P.op('act', lambda e, h=h: e.activation(sv(Ea, h), sv(diff, h), AF.Exp), reads=[('diff', h)], writes=[('Ea', h)])
                P.op('act', lambda e, h=h: e.activation(sv(Eb, h), sv(diff, h), AF.Exp, scale=-1.0), reads=[('diff', h)], writes=[('Eb', h)])
            yield
            mm12(pA, 'pA', lambda j: KT_[:, j, :], lambda j: KT_[:, j, :], lambda h: [('kT', b, h)])
            for h in range(2):
                rBt = ('gbb', b, h)
                P.op('dve', lambda e, h=h: e.scalar_tensor_tensor(sv(Eb, h), sv(Eb, h), 1.0, sv(mWt, h), ALU.min, ALU.mult), reads=[('Eb', h), 'mWt'], writes=[('Eb', h)])
                P.op('dve', lambda e, h=h: e.tensor_tensor(sv(Eb, h), sv(Eb, h), pv(pC, h), ALU.mult), reads=[('Eb', h), ('pC', h)], writes=[('Eb', h)])
                P.op('dve', lambda e, h=h: e.tensor_tensor(sv(Wt, h), sv(Eb, h), pv(pA, h), ALU.mult), reads=[('Eb', h), ('pA', h)], writes=[('Wt', h)])
            yield
            mm12(pC, 'pC', lambda j: KT_[:, j, :], lambda j: QT_[:, j, :], lambda h: [('kT', b, h), ('qT', b, h)])
            for h in range(2):
                rBt = ('gbb', b, h)
                P.op('dve', lambda e, h=h: e.scalar_tensor_tensor(sv(diff, h), sv(Ea, h), 1.0, sv(mI, h), ALU.min, ALU.mult), reads=[('Ea', h), 'mI'], writes=[('diff', h)])
                P.op('dve', lambda e, h=h, QKM=QKM: e.tensor_tensor(sv(QKM, h), sv(diff, h), pv(pC, h), ALU.mult), reads=[('diff', h), ('pC', h)], writes=[('QKm', b, h)])
                P.op('dve', lambda e, h=h: e.scalar_tensor_tensor(sv(Ea, h), sv(Ea, h), 1.0, sv(mW, h), ALU.min, ALU.mult), reads=[('Ea', h), 'mW', ('diff', h)], writes=[('Ea', h)])
                P.op('pool', lambda e, h=h, beta=beta: e.tensor_tensor(sv(Ea, h), sv(Ea, h), bcol(beta, h), ALU.mult), reads=[('Ea', h), rBt], writes=[('Ea', h)])
                P.op('dve', lambda e, h=h: e.tensor_tensor(sv(W, h), sv(Ea, h), pv(pA, h), ALU.mult), reads=[('Ea', h), ('pA', h)], writes=[('W', h)])
                P.op('pool', lambda e, h=h: e.tensor_tensor(sv(Yt[0], h), sv(idbc, h), sv(W, h), ALU.subtract), reads=['idbc', ('W', h)], writes=[('Yt0', h)])
            yield
            cur, curT, cn, cnT = W, Wt, 'W', 'Wt'
            yi = 0
            bufs = [(A1, A1t, 'A1', 'A1t'), (A2, A2t, 'A2', 'A2t')]
            for lev in range(5):
                nx, nxT, nn, nnT = bufs[lev % 2]
                mm12(pB, 'pB', lambda j, cur=cur: cur[:, j, :], lambda j, curT=curT: curT[:, j, :], lambda h, cn=cn, cnT=cnT: [(cn, h), (cnT, h)])
                for h in range(2):
                    if lev < 4:
                        P.op('act', lambda e, h=h, nxT=nxT: e.copy(sv(nxT, h), pv(pB, h)), reads=[('pB', h)], writes=[(nnT, h)])
                    P.op('dve', lambda e, h=h: e.tensor_tensor(sv(nxTI, h), pv(pB, h), sv(idbc, h), ALU.add), reads=[('pB', h), 'idbc'], writes=[('nxTI', h)])
                if lev < 4:
                    mm12(pC, 'pC', lambda j, curT=curT: curT[:, j, :], lambda j, cur=cur: cur[:, j, :], lambda h, cn=cn, cnT=cnT: [(cn, h), (cnT, h)])
                    for h in range(2):
                        P.op('dve', lambda e, h=h, nx=nx: e.tensor_copy(sv(nx, h), pv(pC, h)), reads=[('pC', h)], writes=[(nn, h)])
                Yc = Yt[yi]
                yc_ = 'Yt%d' % yi
                if lev < 4:
                    Yn, yn_ = Yt[1 - yi], ('Yt%d' % (1 - yi),)
                else:
                    Yn, yn_ = Yf[b], ('Yf', b)
                for j in range(12):
                    h = j // 6
                    P.op('pe', lambda e, j=j, Yc=Yc: e.matmul(pcol(pA, j), nxTI[:, j, :], Yc[:, j, :], start=True, stop=True), reads=[('nxTI', h), (yc_, h)], writes=[('pA', h)])
                for h in range(2):
                    P.op('dve', lambda e, h=h, Yn=Yn: e.tensor_copy(sv(Yn, h), pv(pA, h)), reads=[('pA', h)], writes=[yn_ + (h,)])
                yi = 1 - yi
                cur, curT, cn, cnT = nx, nxT, nn, nnT
                yield

        def scan(i):
            b = i % 2
            cf = i
            cb = NCH - 1 - i
            V_, KT_, QT_, GB_ = vtm[b], kT[b], qT[b], gbv[b]
            EGC, EGL, QKM, KD, YF = egc[b], egl[b], QKm[b], kd[b], Yf[b]
            beta = GB_[:, 12:24]
            mm12(pS, 'pS', lambda j: KT_[:, j, :], lambda j: Sm[:, j, :], lambda h: [('kT', b, h), ('Sm', h)])
            for h in range(2):
                P.op('dve', lambda e, h=h, EGC=EGC: e.tensor_tensor(sv(Rr, h), pv(pS, h), bcol(EGC, h), ALU.mult), reads=[('pS', h), ('egc', b, h)], writes=[('R', h)])
                P.op('dve', lambda e, h=h, V_=V_: e.tensor_tensor(sv(Rm, h), sv(V_, h), sv(Rr, h), ALU.subtract), reads=[('R', h), ('vtm', b, h)], writes=[('Rm', h)])
            yield
            mm12(pS, 'pS', lambda j: QT_[:, j, :], lambda j: Sm[:, j, :], lambda h: [('qT', b, h), ('Sm', h)])
            for h in range(2):
                P.op('act', lambda e, h=h: e.copy(sv(o1, h), pv(pS, h)), reads=[('pS', h)], writes=[('o1', h)])
                P.op('pool', lambda e, h=h, EGC=EGC: e.tensor_tensor(sv(o1, h), sv(o1, h), bcol(EGC, h), ALU.mult), reads=[('o1', h), ('egc', b, h)], writes=[('o1', h)])
            yield
            yield
            mm12(pS, 'pS', lambda j: YF[:, j, :], lambda j: Rm[:, j, :], lambda h: [('Yf', b, h), ('Rm', h)])
            for h in range(2):
                P.op('dve', lambda e, h=h, beta=beta: e.tensor_tensor(sv(vnew, h), pv(pS, h), bcol(beta, h), ALU.mult), reads=[('pS', h), ('gbb', b, h)], writes=[('vnew', h)])
            yield
            mm12(pS, 'pS', lambda j: KD[:, j, :], lambda j: vnew[:, j, :], lambda h: [('kd', b, h), ('vnew', h)])
            OB = ob[b]
            for h in range(2):
                P.op('pool', lambda e, h=h, EGL=EGL: e.tensor_tensor(sv(St2, h), sv(St, h), bcol(EGL, h), ALU.mult), reads=[('S', h), ('egl', b, h)], writes=[('S2', h)])
                P.op('dve', lambda e, h=h: e.tensor_tensor(sv(St, h), sv(St2, h), pv(pS, h), ALU.add), reads=[('S2', h), ('pS', h)], writes=[('S', h)])
                P.op('act', lambda e, h=h: e.copy(sv(Sm, h), sv(St, h)), reads=[('S', h)], writes=[('Sm', h)])
            yield
            mm12(pS, 'pS', lambda j: QKM[:, j, :], lambda j: vnew[:, j, :], lambda h: [('QKm', b, h), ('vnew', h)])
            for h in range(2):
                cc = cf if h == 0 else cb
                P.op('dve', lambda e, h=h, OB=OB: e.tensor_tensor(sv(OB, h), sv(o1, h), pv(pS, h), ALU.add), reads=[('o1', h), ('pS', h)], writes=[('ob', b, h)])
                P.dma('sp', D['o_fb'][h, cc * C:(cc + 1) * C, :].rearrange('t (h d) -> t h d', d=64), sv(OB, h), reads=[('ob', b, h)], writes=['d_ofb'])

        def run(gens):
            gens = [g for g in gens if g is not None]
            while gens:
                for g in list(gens):
                    try:
                        next(g)
                    except StopIteration:
                        gens.remove(g)

        run([prep(0)])
        for i in range(NST):
            run([prep(i + 1) if i + 1 < NST else None, scan(i)])
        P.emit()


def phase_E(P, l, D, first):
    xsrc = D['x'] if first else D['xw']
    with ExitStack() as s:
        wo = P.sb(s, 'f_wo', [128, 8, 1024], BF16)
        ong = P.sb(s, 'f_ong', [128, 64], F32)
        idb = P.sb(s, 'f_idb', [128, 128], BF16)
        of_ = [P.sb(s, 'f_of%d' % i, [128, 2, 384], F32) for i in range(3)]
        gt = [P.sb(s, 'f_gt%d' % i, [128, 384], F32) for i in range(3)]
        o = P.sb(s, 'f_o', [128, 6, 64], F32)
        sq = P.sb(s, 'f_sq', [128, 6, 64], F32)
        r6 = P.sb(s, 'f_r6', [128, 6], F32)
        ycb = P.sb(s, 'f_ycb', [128, 384], BF16)
        yT = [P.sb(s, 'f_yT%d' % i, [128, 8, 128], BF16) for i in range(3)]
        xt = [P.sb(s, 'f_xt%d' % i, [128, 1024], F32) for i in range(3)]
        pT = P.ps(s, 'f_pT', [128, 512], BF16)
        po = [P.ps(s, 'f_po%d' % i, [128, 512]) for i in range(2)]
        for k in range(8):
            P.dma('pool', wo[:, k, :], D['w_out'][l, k * 128:(k + 1) * 128, :], writes=[('wo', k)])
        P.dma('sp', ong[:], D['o_norm_g'][l].partition_broadcast(128), writes=['ong'])
        P.dma('pool', idb[:], D['ident'], writes=['idb'])
        def front(t):
            b = t % 3
            tk = slice(t * 128, (t + 1) * 128)
            P.dma('sp', of_[b][:], D['o_fb'][:, tk, :].rearrange('a t c -> t a c'), reads=['d_ofb'], writes=[('of', b)])
            P.dma('sp', gt[b][:], D['gate_s'][tk, :], reads=['d_gate'], writes=[('gt', b)])
            P.dma('sp', xt[b][:], xsrc[tk, :], writes=[('xt', b)])
            P.dma('sp', yT[b][:, 0:5, :], D['yT'][0:640, tk].rearrange('(k p) t -> p k t', p=128), reads=['d_yT'], writes=[('yT', b, 0)])
            OF = o[:].rearrange('p a b -> p (a b)')
            P.op('dve', lambda e, b=b: e.tensor_tensor(OF, of_[b][:, 0, :], of_[b][:, 1, :], ALU.add), reads=[('of', b)], writes=['o'])
            P.op('act', lambda e: e.activation(sq[:], o[:], AF.Square), reads=['o'], writes=['sq'])
            P.op('dve', lambda e: e.tensor_reduce(r6[:], sq[:], AX.X, ALU.add), reads=['sq'], writes=['r6'])
            P.op('act', lambda e: e.activation(r6[:], r6[:], AF.Sqrt, bias=EPS, scale=1.0 / 64), reads=['r6'], writes=['r6'])
            P.op('dve', lambda e: e.reciprocal(r6[:], r6[:]), reads=['r6'], writes=['r6'])
            P.op('dve', lambda e: e.tensor_tensor(o[:], o[:], bc(r6[:].unsqueeze(2), [128, 6, 64]), ALU.mult), reads=['o', 'r6'], writes=['o'])
            P.op('pool', lambda e: e.tensor_tensor(o[:], o[:], bc(ong[:].unsqueeze(1), [128, 6, 64]), ALU.mult), reads=['o', 'ong'], writes=['o'])
            P.op('pool', lambda e, b=b: e.tensor_tensor(ycb[:], OF, gt[b][:], ALU.mult), reads=['o', ('gt', b)], writes=['ycb'])
            for k in range(3):
                P.op('pe', lambda e, k=k: e.transpose(pT[:, k * 128:(k + 1) * 128], ycb[:, k * 128:(k + 1) * 128], idb[:]), reads=['ycb', 'idb'], writes=['pT'])
            P.op('act', lambda e, b=b: e.copy(yT[b][:, 5:8, :], pT[:, 0:384].rearrange('p (k t) -> p k t', t=128)), reads=['pT'], writes=[('yT', b, 1)])
        def back(t):
            b = t % 3
            tk = slice(t * 128, (t + 1) * 128)
            for hf in range(2):
                for k in range(8):
                    P.op('pe', lambda e, hf=hf, k=k, b=b: e.matmul(po[hf][:], yT[b][:, k, :], wo[:, k, hf * 512:(hf + 1) * 512], start=(k == 0), stop=(k == 7)),
                         reads=[('yT', b, 0), ('yT', b, 1), ('wo', k)], writes=[('po', hf)])
                P.op('dve', lambda e, hf=hf, b=b: e.tensor_tensor(xt[b][:, hf * 512:(hf + 1) * 512], xt[b][:, hf * 512:(hf + 1) * 512], po[hf][:], ALU.add),
                     reads=[('po', hf), ('xt', b)], writes=[('xt', b)])
            P.dma('sp', D['xw'][tk, :], xt[b][:], reads=[('xt', b)], writes=[('d_xw', t)])

        front(0)
        front(1)
        for t in range(NT):
            if t + 2 < NT:
                front(t + 2)
            back(t)
        P.emit()


def phase_F(P, l, D):
    import os
    NE = int(os.environ.get('FNE', '16'))
    with ExitStack() as s:
        gff = P.sb(s, 'g_gff', [128, 1024], F32)
        idf = P.sb(s, 'g_idf', [128, 128], F32)
        idb = P.sb(s, 'g_idb', [128, 128], BF16)
        wr = P.sb(s, 'g_wr', [128, 8, 16], F32)
        aff = P.sb(s, 'g_aff', [128, NT, 16], F32)
        sel = P.sb(s, 'g_sel', [128, NT, 16], F32)
        rank = P.sb(s, 'g_rank', [128, NT, 16], F32)
        cA = P.sb(s, 'g_cA', [128, NT, 16], F32)
        cB = P.sb(s, 'g_cB', [128, NT, 16], F32)
        selb = P.sb(s, 'g_selb', [128, NT * 16], BF16)
        triS = P.sb(s, 'g_triS', [128, 128], BF16)
        onesb = P.sb(s, 'g_onesb', [128, 128], BF16)
        tg = P.sb(s, 'g_tg', [128, NT, 16, 5], BF16)
        tp = P.sb(s, 'g_tp', [128, NT, 2], F32)
        iota = P.sb(s, 'g_iota', [128, 512], F32)
        Selt = [P.sb(s, 'g_Selt%d' % i, [128, 512], BF16) for i in range(2)]
        idxf = P.sb(s, 'g_idxf', [128, 4, 8], F32)
        row5 = P.sb(s, 'g_row5', [5, 512], F32)
        idxv = P.sb(s, 'g_idxv', [128, 4], F32)
        idxi = [P.sb(s, 'g_idxi%d' % i, [128, 4], I32) for i in range(2)]
        gate = [P.sb(s, 'g_gate%d' % i, [128, 4], F32) for i in range(2)]
        affT2 = P.sb(s, 'g_affT2', [16, S], F32)
        bs = P.sb(s, 'g_bs', [16, 8], F32)
        ones16 = P.sb(s, 'g_ones16', [16, 128], F32)
        dthr = P.sb(s, 'g_dthr', [16, 16], F32)
        thrb = P.sb(s, 'g_thrb', [128, 16], F32)
        xt = P.sb(s, 'g_xt', [128, 1024], F32)
        junk = P.sb(s, 'g_junk', [128, 1024], BF16)
        ss = P.sb(s, 'g_ss', [128, 4], F32)
        h32s = [P.sb(s, 'g_h32_%d' % i, [128, 1024], F32) for i in range(2)]
        hb16 = P.sb(s, 'g_hb16', [128, 1024], BF16)
        hT32 = P.sb(s, 'g_hT32', [128, 8, 128], F32)
        sm = P.sb(s, 'g_sm', [128, 4], F32)
        ex = P.sb(s, 'g_ex', [128, 16], F32)
        wg = [P.sb(s, 'g_wg%d' % i, [128, 8, 1024], BF16) for i in range(2)]
        wu = [P.sb(s, 'g_wu%d' % i, [128, 8, 1024], BF16) for i in range(2)]
        wd = [P.sb(s, 'g_wd%d' % i, [128, 8, 1024], BF16) for i in range(2)]
        xe = P.sb(s, 'g_xe', [128, 4, 1024], BF16)
        bjv = xe[0:16, :, :].rearrange('p g c -> p (g c)')
        xeTs = [P.sb(s, 'g_xeT%d' % i, [128, 8, 512], BF16) for i in range(2)]
        hid = P.sb(s, 'g_hid', [128, 8, 512], BF16)
        sg = [P.sb(s, 'g_sg%d' % i, [128, 512], BF16) for i in range(2)]
        ye = [P.sb(s, 'g_ye%d' % i, [128, 1024], F32) for i in range(2)]
        pbig = P.ps(s, 'g_pbig', [128, 1024])
        pl = P.ps(s, 'g_pl', [128, 512])
        pT = P.ps(s, 'g_pT', [128, 1024], BF16)
        pg_ = P.ps(s, 'g_pg', [128, 512])
        pu_ = P.ps(s, 'g_pu', [128, 512])
        py = [P.ps(s, 'g_py%d' % i, [128, 512]) for i in range(2)]

        def load_w(ex_):
            eb = ex_ % 2
            for k in range(8):
                P.dma('pool', wg[eb][:, k, :], D['w_e_gate'][l, ex_, k * 128:(k + 1) * 128, :], writes=[('wg', eb, k)])
                P.dma('pool', wu[eb][:, k, :], D['w_e_up'][l, ex_, k * 128:(k + 1) * 128, :], writes=[('wu', eb, k)])
            for k in range(8):
                P.dma('pool', wd[eb][:, k, :], D['w_e_down'][l, ex_, k * 128:(k + 1) * 128, :], writes=[('wd', eb, k)])

        P.dma('sp', gff[:], D['g_ffn'][l].partition_broadcast(128), writes=['gff'])
        P.dma('sp', idf[:], D['ident'], writes=['idf'])
        P.dma('pool', idb[:], D['ident'], writes=['idb'])
        P.dma('pool', triS[:], D['triS'], writes=['triS'])
        P.dma('pool', onesb[:], D['ones128'], writes=['onesb'])
        P.dma('sp', wr[:], D['w_router'][l].rearrange('(k p) e -> p k e', p=128), writes=['wr'])
        P.dma('sp', ones16[:], D['ones128'][0:16, :], writes=['ones16'])
        P.dma('sp', tp[:], D['tp'], writes=['tp'])
        P.dma('sp', iota[:], D['iota512'], writes=['iota'])
        load_w(0)
        def frontF(t):
            tk = slice(t * 128, (t + 1) * 128)
            h32 = h32s[t % 2]
            kh = ('h32', t % 2)
            P.dma('sp', xt[:], D['xw'][tk, :], reads=['d_xw'], writes=['xt'])
            P.op('dve', lambda e: e.memset(ss[:, 0:1], 0.0), writes=['ss'])
            P.op('act', lambda e: e.activation(junk[:], xt[:], AF.Square, accum_out=ss[:, 0:1]), reads=['xt', 'ss'], writes=['junk', 'ss'])
            P.op('act', lambda e: e.activation(ss[:, 0:1], ss[:, 0:1], AF.Sqrt, bias=EPS, scale=1.0 / 1024), reads=['ss'], writes=['ss'])
            P.op('dve', lambda e: e.reciprocal(ss[:, 0:1], ss[:, 0:1]), reads=['ss'], writes=['ss'])
            P.op('dve', lambda e, h32=h32: e.scalar_tensor_tensor(h32[:], xt[:], ss[:, 0:1], gff[:], ALU.mult, ALU.mult), reads=['xt', 'ss', 'gff'], writes=[kh])
            P.op('act', lambda e, h32=h32: e.copy(hb16[:], h32[:]), reads=[kh], writes=['hb16'])
            P.dma('sp', D['hb'][tk, :], hb16[:], reads=['hb16'], writes=['d_hb'])

        def backF(t):
            h32 = h32s[t % 2]
            kh = ('h32', t % 2)
            for k in range(8):
                P.op('pe', lambda e, k=k, h32=h32: e.transpose(pbig[:, k * 128:(k + 1) * 128], h32[:, k * 128:(k + 1) * 128], idf[:]), reads=[kh, 'idf'], writes=[('pbig', k // 4)])
            P.op('act', lambda e: e.copy(hT32[:, 0:4, :], pbig[:, 0:512].rearrange('p (k t) -> p k t', t=128)), reads=[('pbig', 0)], writes=['hT32a'])
            P.op('dve', lambda e: e.tensor_copy(hT32[:, 4:8, :], pbig[:, 512:1024].rearrange('p (k t) -> p k t', t=128)), reads=[('pbig', 1)], writes=['hT32b'])
            for k in range(8):
                P.op('pe', lambda e, k=k: e.matmul(pl[:, 0:16], hT32[:, k, :], wr[:, k, :], start=(k == 0), stop=(k == 7)), reads=['hT32a', 'hT32b', 'wr'], writes=['pl'])
            P.op('dve', lambda e: e.tensor_reduce(sm[:, 0:1], pl[:, 0:16], AX.X, ALU.max), reads=['pl'], writes=['sm'])
            P.op('dve', lambda e: e.tensor_scalar(sm[:, 1:2], sm[:, 0:1], -1.0, None, ALU.mult), reads=['sm'], writes=['sm'])
            P.op('dve', lambda e: e.memset(sm[:, 2:3], 0.0), reads=['sm'], writes=['sm'])
            P.op('act', lambda e: e.activation(ex[:], pl[:, 0:16], AF.Exp, bias=sm[:, 1:2], accum_out=sm[:, 2:3]), reads=['pl', 'sm'], writes=['ex', 'sm'])
            P.op('dve', lambda e: e.reciprocal(sm[:, 3:4], sm[:, 2:3]), reads=['sm'], writes=['sm'])
            P.op('dve', lambda e, t=t: e.tensor_scalar(aff[:, t, :], ex[:], sm[:, 3:4], None, ALU.mult), reads=['ex', 'sm'], writes=[('aff', t)])
            P.op('pe', lambda e, t=t: e.transpose(pl[0:16, 128:256], aff[:, t, :], idf[:]), reads=[('aff', t), 'idf'], writes=['pl'])
            P.op('act', lambda e, t=t: e.mul(affT2[:, t * 128:(t + 1) * 128], pl[0:16, 128:256], 2.0), reads=['pl'], writes=['affT2'])

        frontF(0)
        for t in range(NT):
            if t + 1 < NT:
                frontF(t + 1)
            backF(t)
        lo, hi, half, mid2, cnt, gef, tt = (bs[:, i:i + 1] for i in range(7))
        P.op('dve', lambda e: e.memset(bs[:], 0.0), writes=['bs'])
        P.op('dve', lambda e: e.memset(hi, 1.0), reads=['bs'], writes=['bs'])
        for itn in range(27):
            P.op('dve', lambda e: e.tensor_tensor(mid2, lo, hi, ALU.add), reads=['bs'], writes=['bs'])
            P.op('dve', lambda e: e.tensor_scalar(half, mid2, 0.5, None, ALU.mult), reads=['bs'], writes=['bs'])
            P.op('dve', lambda e: e.memset(cnt, 0.0), reads=['bs'], writes=['bs'])
            P.op('dve', lambda e: e.tensor_scalar(bjv, affT2[:], mid2, 0.0, ALU.is_ge, ALU.add, accum_out=cnt), reads=['affT2', 'bs'], writes=['bj', 'bs'])
            P.op('dve', lambda e: e.tensor_scalar(gef, cnt, 511.5, None, ALU.is_ge), reads=['bs'], writes=['bs'])
            P.op('dve', lambda e: e.tensor_tensor(tt, half, lo, ALU.subtract), reads=['bs'], writes=['bs'])
            P.op('dve', lambda e: e.tensor_tensor(tt, tt, gef, ALU.mult), reads=['bs'], writes=['bs'])
            P.op('dve', lambda e: e.tensor_tensor(lo, lo, tt, ALU.add), reads=['bs'], writes=['bs'])
            P.op('dve', lambda e: e.tensor_tensor(tt, hi, half, ALU.subtract), reads=['bs'], writes=['bs'])
            P.op('dve', lambda e: e.tensor_tensor(tt, tt, gef, ALU.mult), reads=['bs'], writes=['bs'])
            P.op('dve', lambda e: e.tensor_tensor(hi, half, tt, ALU.add), reads=['bs'], writes=['bs'])
        P.op('dve', lambda e: e.tensor_scalar(dthr[:], idf[0:16, 0:16], lo, None, ALU.mult), reads=['idf', 'bs'], writes=['dthr'])
        P.op('pe', lambda e: e.matmul(pl[:, 256:272], ones16[:], dthr[:], start=True, stop=True), reads=['ones16', 'dthr'], writes=['pl'])
        P.op('dve', lambda e: e.tensor_copy(thrb[:], pl[:, 256:272]), reads=['pl'], writes=['thrb'])
        AFF = [('aff', t) for t in range(NT)]
        P.op('dve', lambda e: e.tensor_tensor(sel[:], aff[:], bc(thrb[:].unsqueeze(1), [128, NT, 16]), ALU.is_ge), reads=AFF + ['thrb'], writes=['sel'])
        P.op('dve', lambda e: e.tensor_copy(selb[:], sel[:].rearrange('p t e -> p (t e)')), reads=['sel'], writes=['selb'])
        P.op('pe', lambda e: e.matmul(pg_[:], triS[:], selb[:], start=True, stop=True), reads=['triS', 'selb'], writes=['pg'])
        P.op('pe', lambda e: e.matmul(pu_[:], onesb[:], selb[:], start=True, stop=True), reads=['onesb', 'selb'], writes=['pu'])
        P.op('dve', lambda e: e.tensor_copy(cA[:].rearrange('p t e -> p (t e)'), pu_[:]), reads=['pu'], writes=['cA'])
        src, dst, sn, dn = cA, cB, 'cA', 'cB'
        for sft in (1, 2, 4, 8, 16):
            P.op('pool', lambda e, src=src, dst=dst, sft=sft: e.tensor_copy(dst[:, 0:sft, :], src[:, 0:sft, :]), reads=[sn], writes=[dn])
            P.op('dve', lambda e, src=src, dst=dst, sft=sft: e.tensor_tensor(dst[:, sft:NT, :], src[:, sft:NT, :], src[:, 0:NT - sft, :], ALU.add), reads=[sn], writes=[dn])
            src, dst, sn, dn = dst, src, dn, sn
        P.op('dve', lambda e, src=src: e.tensor_tensor(rank[:].rearrange('p t e -> p (t e)'), src[:].rearrange('p t e -> p (t e)'), pu_[:], ALU.subtract), reads=[sn, 'pu'], writes=['rank'])
        P.op('dve', lambda e: e.tensor_tensor(rank[:].rearrange('p t e -> p (t e)'), rank[:].rearrange('p t e -> p (t e)'), pg_[:], ALU.add), reads=['rank', 'pg'], writes=['rank'])
        P.op('dve', lambda e: e.scalar_tensor_tensor(rank[:], rank[:], 1.0, sel[:], ALU.add, ALU.mult), reads=['rank', 'sel'], writes=['rank'])
        P.op('dve', lambda e: e.tensor_scalar(rank[:], rank[:], -1.0, None, ALU.add), reads=['rank'], writes=['rank'])
        P.op('dve', lambda e: e.tensor_copy(tg[:, :, :, 0:2], bc(tp[:].unsqueeze(2), [128, NT, 16, 2])), reads=['tp'], writes=['tg0'])
        P.op('dve', lambda e: e.tensor_copy(tg[:, :, :, 2], aff[:]), reads=AFF, writes=['tg1'])
        P.op('dve', lambda e: e.tensor_tensor(cA[:], aff[:], tg[:, :, :, 2], ALU.subtract), reads=AFF + ['tg1', 'cA', 'cB'], writes=['cA'])
        P.op('dve', lambda e: e.tensor_copy(tg[:, :, :, 3], cA[:]), reads=['cA'], writes=['tg2'])
        P.op('dve', lambda e: e.tensor_tensor(cB[:], cA[:], tg[:, :, :, 3], ALU.subtract), reads=['cA', 'tg2', 'cB'], writes=['cB'])
        P.op('dve', lambda e: e.tensor_copy(tg[:, :, :, 4], cB[:]), reads=['cB'], writes=['tg3'])
        TG = ['tg0', 'tg1', 'tg2', 'tg3']
        nsel = 0
        npy = 0
        nsg = 0
        def stage1(ex_):
            nonlocal nsel
            eb = ex_ % 2
            xeT = xeTs[eb]
            for t in range(NT):
                sb_ = nsel % 2
                nsel += 1
                P.op('dve', lambda e, sb_=sb_, t=t, ex_=ex_: e.tensor_scalar(Selt[sb_][:], iota[:], rank[:, t, ex_:ex_ + 1], None, ALU.is_equal), reads=['iota', 'rank'], writes=[('Selt', sb_)])
                P.op('pe', lambda e, sb_=sb_, t=t, ex_=ex_: e.matmul(pl[0:5, 0:512], tg[:, t, ex_, :], Selt[sb_][:], start=(t == 0), stop=(t == NT - 1)),
                     reads=[('Selt', sb_)] + TG, writes=['pl'])
            P.op('act', lambda e: e.copy(row5[:], pl[0:5, 0:512]), reads=['pl'], writes=['row5'])
            for g in range(4):
                P.op('pe', lambda e, g=g: e.transpose(pl[:, g * 8:g * 8 + 5], row5[0:5, g * 128:(g + 1) * 128], idf[0:5, 0:5]), reads=['row5', 'idf'], writes=['pl'])
            P.op('dve', lambda e: e.tensor_copy(idxf[:, :, 0:5], pl[:, 0:32].rearrange('p (g c) -> p g c', c=8)[:, :, 0:5]), reads=['pl'], writes=['idxf'])
            P.op('dve', lambda e: e.scalar_tensor_tensor(idxv[:], idxf[:, :, 0], 128.0, idxf[:, :, 1], ALU.mult, ALU.add), reads=['idxf'], writes=['idxv'])
            P.op('dve', lambda e, eb=eb: e.tensor_copy(idxi[eb][:], idxv[:]), reads=['idxv'], writes=[('idxi', eb)])
            P.op('dve', lambda e, eb=eb: e.tensor_tensor(gate[eb][:], idxf[:, :, 2], idxf[:, :, 3], ALU.add), reads=['idxf'], writes=[('gate', eb)])
            P.op('dve', lambda e, eb=eb: e.tensor_tensor(gate[eb][:], gate[eb][:], idxf[:, :, 4], ALU.add), reads=['idxf', ('gate', eb)], writes=[('gate', eb)])
            for g in range(4):
                P.idma(lambda e, g=g, eb=eb: e.indirect_dma_start(out=xe[:, g, :], out_offset=None, in_=D['hb'][:, :],
                                                                   in_offset=bass.IndirectOffsetOnAxis(ap=idxi[eb][:, g:g + 1], axis=0), bounds_check=P.breg(e), oob_is_err=False),
                       reads=['d_hb', ('idxi', eb)], writes=[('xe', g)])
            for g in range(4):
                for k in range(8):
                    P.op('pe', lambda e, g=g, k=k: e.transpose(pT[:, k * 128:(k + 1) * 128], xe[:, g, k * 128:(k + 1) * 128], idb[:]), reads=[('xe', g), 'idb'], writes=['pT'])
                eng = 'act' if g % 2 == 0 else 'dve'
                if eng == 'act':
                    P.op('act', lambda e, g=g, xeT=xeT: e.copy(xeT[:, :, g * 128:(g + 1) * 128], pT[:].rearrange('p (k t) -> p k t', t=128)), reads=['pT'], writes=[('xeT', eb, g)])
                else:
                    P.op('dve', lambda e, g=g, xeT=xeT: e.tensor_copy(xeT[:, :, g * 128:(g + 1) * 128], pT[:].rearrange('p (k t) -> p k t', t=128)), reads=['pT'], writes=[('xeT', eb, g)])

        def stage2(ex_):
            nonlocal npy, nsg
            eb = ex_ % 2
            xeT = xeTs[eb]
            XET = [('xeT', eb, g) for g in range(4)]
            for fc in range(8):
                for k in range(8):
                    P.op('pe', lambda e, fc=fc, k=k, eb=eb, xeT=xeT: e.matmul(pg_[:], wg[eb][:, k, fc * 128:(fc + 1) * 128], xeT[:, k, :], start=(k == 0), stop=(k == 7)),
                         reads=XET + [('wg', eb, k)], writes=['pg'])
                for k in range(8):
                    P.op('pe', lambda e, fc=fc, k=k, eb=eb, xeT=xeT: e.matmul(pu_[:], wu[eb][:, k, fc * 128:(fc + 1) * 128], xeT[:, k, :], start=(k == 0), stop=(k == 7)),
                         reads=XET + [('wu', eb, k)], writes=['pu'])
                sb2 = nsg % 2
                nsg += 1
                P.op('act', lambda e, sb2=sb2: e.activation(sg[sb2][:], pg_[:], AF.Silu), reads=['pg'], writes=[('sg', sb2)])
                P.op('dve', lambda e, sb2=sb2, fc=fc: e.tensor_tensor(hid[:, fc, :], sg[sb2][:], pu_[:], ALU.mult), reads=[('sg', sb2), 'pu'], writes=[('hid', fc)])
            HID = [('hid', fc) for fc in range(8)]
            for g in range(4):
                yb = g % 2
                for hf in range(2):
                    pb = npy % 2
                    npy += 1
                    for fc in range(8):
                        P.op('pe', lambda e, pb=pb, fc=fc, g=g, hf=hf, eb=eb: e.matmul(py[pb][:], hid[:, fc, g * 128:(g + 1) * 128], wd[eb][:, fc, hf * 512:(hf + 1) * 512], start=(fc == 0), stop=(fc == 7)),
                             reads=HID + [('wd', eb, fc)], writes=[('py', pb)])
                    if hf == 0:
                        P.op('act', lambda e, pb=pb, yb=yb, g=g, eb=eb: e.activation(ye[yb][:, 0:512], py[pb][:], AF.Copy, scale=gate[eb][:, g:g + 1]), reads=[('py', pb), ('gate', eb)], writes=[('ye', yb, 0)])
                    else:
                        P.op('dve', lambda e, pb=pb, yb=yb, g=g, eb=eb: e.tensor_scalar(ye[yb][:, 512:1024], py[pb][:], gate[eb][:, g:g + 1], None, ALU.mult), reads=[('py', pb), ('gate', eb)], writes=[('ye', yb, 1)])
                P.idma(lambda e, g=g, eb=eb, yb=yb: e.indirect_dma_start(out=D['xw'][:, :], out_offset=bass.IndirectOffsetOnAxis(ap=idxi[eb][:, g:g + 1], axis=0), in_=ye[yb][:],
                                                                          in_offset=None, bounds_check=P.breg(e), oob_is_err=False, compute_op=ALU.add),
                       reads=[('ye', yb, 0), ('ye', yb, 1), ('idxi', eb), 'd_xw'], writes=['d_xw'])

        stage1(0)
        for ex_ in range(NE):
            if ex_ + 1 < NE:
                load_w(ex_ + 1)
                stage1(ex_ + 1)
            stage2(ex_)
        P.emit()


def phase_G(P, l, D, last):
    xdst = D['out'] if last else D['xw']
    with ExitStack() as s:
        wp = P.sb(s, 'h_wp', [128, 2, 1024], BF16)
        wgt = P.sb(s, 'h_wgt', [128, 8, 1024], BF16)
        gpl = P.sb(s, 'h_gpl', [128, 1024], F32)
        gpg = P.sb(s, 'h_gpg', [128, 1024], F32)
        idb = P.sb(s, 'h_idb', [128, 128], BF16)
        xt = [P.sb(s, 'h_xt%d' % i, [128, 1024], F32) for i in range(3)]
        pb_ = [P.sb(s, 'h_pb%d' % i, [128, 256], BF16) for i in range(3)]
        junk = P.sb(s, 'h_junk', [128, 1024], BF16)
        ss = P.sb(s, 'h_ss', [128, 4], F32)
        ssf = P.sb(s, 'h_ssf', [128, 2], F32)
        junkf = P.sb(s, 'h_junkf', [128, 1024], BF16)
        xn = P.sb(s, 'h_xn', [128, 1024], BF16)
        xTs = [P.sb(s, 'h_xT%d' % i, [128, 10, 128], BF16) for i in range(3)]
        er = P.sb(s, 'h_er', [128, 1024], F32)
        gt = P.sb(s, 'h_gt', [128, 1024], F32)
        pT = P.ps(s, 'h_pT', [128, 2048], BF16)
        pe_ = P.ps(s, 'h_pe', [128, 1024])
        pg_ = P.ps(s, 'h_pg', [128, 1024])
        for k in range(2):
            P.dma('pool', wp[:, k, :], D['w_ple'][l, k * 128:(k + 1) * 128, :], writes=[('wp', k)])
        for k in range(8):
            P.dma('pool', wgt[:, k, :], D['w_ple_gate'][l, k * 128:(k + 1) * 128, :], writes=[('wgt', k)])
        P.dma('sp', gpl[:], D['g_ple'][l].partition_broadcast(128), writes=['gpl'])
        P.dma('sp', gpg[:], D['g_ple_gate'][l].partition_broadcast(128), writes=['gpg'])
        P.dma('pool', idb[:], D['ident'], writes=['idb'])
        def front(t):
            b = t % 3
            xT = xTs[b]
            tk = slice(t * 128, (t + 1) * 128)
            P.dma('sp', xt[b][:], D['xw'][tk, :], reads=[('d_xw', t)], writes=[('xt', b)])
            P.dma('pool', pb_[b][:], D['p'][l, tk, :], writes=[('pb', b)])
            P.op('dve', lambda e: e.memset(ssf[:, 0:1], 0.0), writes=['ssf'])
            P.op('act', lambda e, b=b: e.activation(junkf[:], xt[b][:], AF.Square, accum_out=ssf[:, 0:1]), reads=[('xt', b), 'ssf'], writes=['junkf', 'ssf'])
            P.op('act', lambda e: e.activation(ssf[:, 0:1], ssf[:, 0:1], AF.Sqrt, bias=EPS, scale=1.0 / 1024), reads=['ssf'], writes=['ssf'])
            P.op('dve', lambda e: e.reciprocal(ssf[:, 0:1], ssf[:, 0:1]), reads=['ssf'], writes=['ssf'])
            P.op('dve', lambda e, b=b: e.scalar_tensor_tensor(xn[:], xt[b][:], ssf[:, 0:1], gpg[:], ALU.mult, ALU.mult), reads=[('xt', b), 'ssf', 'gpg'], writes=['xn'])
            for k in range(8):
                P.op('pe', lambda e, k=k: e.transpose(pT[:, k * 128:(k + 1) * 128], xn[:, k * 128:(k + 1) * 128], idb[:]), reads=['xn', 'idb'], writes=[('pT', 0)])
            for k in range(2):
                P.op('pe', lambda e, k=k, b=b: e.transpose(pT[:, (8 + k) * 128:(9 + k) * 128], pb_[b][:, k * 128:(k + 1) * 128], idb[:]), reads=[('pb', b), 'idb'], writes=[('pT', 1)])
            P.op('act', lambda e, xT=xT: e.copy(xT[:, 0:8, :].rearrange('p k t -> p (k t)'), pT[:, 0:1024]), reads=[('pT', 0)], writes=[('xTa', b)])
            P.op('dve', lambda e, xT=xT: e.tensor_copy(xT[:, 8:10, :].rearrange('p k t -> p (k t)'), pT[:, 1024:1280]), reads=[('pT', 1)], writes=[('xTb', b)])

        def back(t):
            b = t % 3
            xT = xTs[b]
            tk = slice(t * 128, (t + 1) * 128)
            for hf in range(2):
                for k in range(2):
                    P.op('pe', lambda e, hf=hf, k=k, xT=xT: e.matmul(pe_[:, hf * 512:(hf + 1) * 512], xT[:, 8 + k, :], wp[:, k, hf * 512:(hf + 1) * 512], start=(k == 0), stop=(k == 1)),
                         reads=[('xTb', b), ('wp', k)], writes=[('pe', hf)])
                for k in range(8):
                    P.op('pe', lambda e, hf=hf, k=k, xT=xT: e.matmul(pg_[:, hf * 512:(hf + 1) * 512], xT[:, k, :], wgt[:, k, hf * 512:(hf + 1) * 512], start=(k == 0), stop=(k == 7)),
                         reads=[('xTa', b), ('wgt', k)], writes=[('pg', hf)])
            P.op('dve', lambda e: e.memset(ss[:, 1:3], 0.0), reads=['ss'], writes=['ss'])
            for hf in range(2):
                P.op('act', lambda e, hf=hf: e.activation(junk[:, hf * 512:(hf + 1) * 512], pe_[:, hf * 512:(hf + 1) * 512], AF.Square, accum_out=ss[:, 1 + hf:2 + hf]), reads=[('pe', hf), 'ss'], writes=['junk', 'ss'])
            P.op('dve', lambda e: e.tensor_tensor(ss[:, 1:2], ss[:, 1:2], ss[:, 2:3], ALU.add), reads=['ss'], writes=['ss'])
            P.op('act', lambda e: e.activation(ss[:, 1:2], ss[:, 1:2], AF.Sqrt, bias=EPS, scale=1.0 / 1024), reads=['ss'], writes=['ss'])
            P.op('dve', lambda e: e.reciprocal(ss[:, 1:2], ss[:, 1:2]), reads=['ss'], writes=['ss'])
            for hf in range(2):
                hs = slice(hf * 512, (hf + 1) * 512)
                P.op('dve', lambda e, hs=hs: e.scalar_tensor_tensor(er[:, hs], pe_[:, hs], ss[:, 1:2], gpl[:, hs], ALU.mult, ALU.mult), reads=[('pe', hf), 'ss', 'gpl'], writes=['er'])
                P.op('act', lambda e, hs=hs: e.activation(gt[:, hs], pg_[:, hs], AF.Sigmoid), reads=[('pg', hf)], writes=['gt'])
            P.op('dve', lambda e: e.tensor_tensor(er[:], er[:], gt[:], ALU.mult), reads=['er', 'gt'], writes=['er'])
            P.op('dve', lambda e, b=b: e.tensor_tensor(xt[b][:], xt[b][:], er[:], ALU.add), reads=['er', ('xt', b)], writes=[('xt', b)])
            P.dma('sp', xdst[tk, :], xt[b][:], reads=[('xt', b)], writes=[('d_xw', t)])

        front(0)
        front(1)
        for t in range(NT):
            if t + 2 < NT:
                front(t + 2)
            back(t)
        P.emit()


WEIGHTS = [('g_mix', [4, 1024]), ('w_in', [4, 1024, 3224]), ('ln_v_g', [4, 4, 64]), ('ln_v_b', [4, 4, 64]), ('w_s', [4, 4, 128, 128]),
           ('b_s', [4, 4, 128]), ('q_norm_g', [4, 64]), ('k_norm_g', [4, 64]), ('conv_w', [4, 5, 1152]), ('a_log', [4, 2, 6]),
           ('dt_bias', [4, 2, 6]), ('o_norm_g', [4, 64]), ('w_out', [4, 1024, 1024]), ('g_ffn', [4, 1024]), ('w_router', [4, 1024, 16]),
           ('w_e_gate', [4, 16, 1024, 1024]), ('w_e_up', [4, 16, 1024, 1024]), ('w_e_down', [4, 16, 1024, 1024]), ('w_ple', [4, 256, 1024]),
           ('g_ple', [4, 1024]), ('g_ple_gate', [4, 1024]), ('w_ple_gate', [4, 1024, 1024])]


def make_consts():
    c = {}
    c['ident'] = np.eye(128, dtype=np.float32)
    c['ones64'] = np.ones((64, 64), np.float32)
    c['ones128'] = np.ones((128, 128), np.float32)
    half = 8
    c['invf'] = (np.float32(500000.0) ** (-np.arange(half, dtype=np.float32) * np.float32(2.0) / np.float32(16))).astype(np.float32)
    a = np.arange(128)[:, None]
    b = np.arange(128)[None, :]
    mA = (a >= b).astype(np.float32)
    mB = (a <= b).astype(np.float32)
    c['mab'] = np.concatenate([mA, mB, mA, mB], axis=1)
    sel = np.zeros((65, 64), np.float32)
    sel[64, :] = 1.0
    c['sel65'] = sel
    p = np.arange(64)[:, None]
    f = np.arange(64)[None, :]
    c['triF'] = (p <= f).astype(np.float32)
    c['triB'] = (p >= f).astype(np.float32)

    def m12(fw, bw):
        return np.ascontiguousarray(np.stack([fw] * 6 + [bw] * 6, axis=1).astype(np.float32))
    c['mW'] = m12(f > p, f < p)
    c['mWt'] = m12(p > f, p < f)
    c['mI'] = m12(f >= p, f <= p)
    c['triS'] = (a < b).astype(np.float32)
    c['iota512'] = np.ascontiguousarray(np.broadcast_to(np.arange(512, dtype=np.float32)[None, :], (128, 512)))
    tpv = np.zeros((128, NT, 2), np.float32)
    tpv[:, :, 0] = np.arange(NT)[None, :]
    tpv[:, :, 1] = np.arange(128)[:, None]
    c['tp'] = tpv
    return c


SCRATCH = [('cs', [S, 16], F32), ('vn', [S, 256], BF16), ('qkT', [6, 128, S], BF16), ('vaug', [S, 390], BF16), ('gate_s', [S, 384], F32),
           ('ab', [S, 24], F32), ('uT', [256, S], F32), ('cT', [1152, S], F32), ('yT', [1024, S], BF16), ('v_tm', [S, 384], F32),
           ('k_tm', [S, 384], F32), ('qT_g', [384, S], BF16), ('kT_g', [384, S], BF16), ('gb', [S, 24], F32), ('o_fb', [2, S, 384], F32),
           ('xw', [S, 1024], F32), ('hb', [S, 1024], BF16)]


def build(n_layers=4, phases=None, dbg=()):
    P = Prog()
    D = {}
    D['x'] = P.dram('x', [S, 1024], F32, 'ExternalInput')
    D['p'] = P.dram('p', [n_layers, S, 256], F32, 'ExternalInput')
    D['positions'] = P.dram('positions', [128, NT], I32, 'ExternalInput')
    for n, shp in WEIGHTS:
        D[n] = P.dram(n, [n_layers] + list(shp[1:]), F32, 'ExternalInput')
    for n, v in make_consts().items():
        D[n] = P.dram(n, list(v.shape), F32, 'ExternalInput')
    for n, shp, dt in SCRATCH:
        D[n] = P.dram(n, shp, dt, 'ExternalOutput' if n in dbg else 'Internal')
    D['out'] = P.dram('out', [S, 1024], F32, 'ExternalOutput')
    allp = phases is None
    if allp or 'R' in phases:
        phase_rope(P, D)
    for l in range(n_layers):
        first = (l == 0)
        last = (l == n_layers - 1)
        if allp or 'A' in phases:
            phase_A(P, l, D, first)
        if allp or 'B' in phases:
            phase_B(P, l, D)
        if allp or 'C' in phases:
            phase_C(P, l, D)
        if allp or 'D1' in phases:
            phase_D1(P, l, D)
        if allp or 'D2' in phases:
            phase_D2(P, l, D)
        if allp or 'E' in phases:
            phase_E(P, l, D, first)
        if allp or 'F' in phases:
            phase_F(P, l, D)
        if allp or 'G' in phases:
            phase_G(P, l, D, last and allp)
    return P


def kernel(**inputs):
    n = 8
    P = build(4)
    consts = make_consts()
    shared = {k: np.ascontiguousarray(np.asarray(inputs[k], dtype=np.float32)) for k, _ in WEIGHTS}
    shared.update(consts)
    x = np.asarray(inputs['x'], dtype=np.float32)
    p = np.asarray(inputs['p'], dtype=np.float32)
    pos = np.asarray(inputs['positions']).astype(np.int32)
    in_maps = []
    for c in range(n):
        m = dict(shared)
        m['x'] = np.ascontiguousarray(x[c])
        m['p'] = np.ascontiguousarray(p[:, c])
        m['positions'] = np.ascontiguousarray(pos[c].reshape(NT, 128).T)
        in_maps.append(m)
    res = run_bass_kernel_spmd(P.nc, in_maps, core_ids=list(range(n)))
    return np.stack([np.asarray(res.results[c]['out'], dtype=np.float32) for c in range(n)], axis=0)
```

```python
import numpy as np
from contextlib import ExitStack
import concourse.bass as bass
import concourse.mybir as mybir
from concourse.bass_utils import run_bass_kernel_spmd

F32 = mybir.dt.float32
BF16 = mybir.dt.bfloat16
I32 = mybir.dt.int32
ALU = mybir.AluOpType
AF = mybir.ActivationFunctionType
AX = mybir.AxisListType

ENGS = ['pe', 'dve', 'act', 'pool', 'sp']
DMA_ENGS = ['sp', 'pool', 'act']
NDS = 8
EPS = 1e-6
S = 4096
NT = 32


NOWAW = frozenset(['d_vn', 'd_qkT', 'd_vaug', 'd_gate', 'd_ab', 'd_uT', 'd_cT', 'd_vtm', 'd_ktm', 'd_qkTg', 'd_yT', 'd_ofb', 'd_hb', 'd_gb', 'd_cs'])


class Prog:
    def __init__(self):
        self.nc = bass.Bass("TRN2", target_bir_lowering=False)
        self.stack = ExitStack()
        self.sems = {}
        for e in ENGS:
            self.sems[e] = self.stack.enter_context(self.nc.semaphore('s_' + e))
        self.dcount = {}
        for e in DMA_ENGS:
            for i in range(NDS):
                k = 'd_%s_%d' % (e, i)
                self.sems[k] = self.stack.enter_context(self.nc.semaphore(k))
                self.dcount[k] = 0
        self.dnext = {e: 0 for e in DMA_ENGS}
        self.cnt = {e: 0 for e in ENGS}
        self.waited = {e: {} for e in ENGS}
        self.q = {e: [] for e in ENGS}
        self.lastw = {}
        self.readers = {}
        self.multiw = {}
        self.nops = 0
        self.xkeys = set(['pT', 'pv', 'pq', 'pk', 'pbv', 'pg', 'pf', 'ptr', 'pm', 'pss', 'ppv', 'pd', 'ptb', 'po', 'pbig', 'pl', 'pu', 'py', 'pe', 'pA', 'pB', 'pC', 'pD', 'pS'])

    def _deps(self, eng, reads, writes):
        deps = {}

        def add(m):
            if m is None:
                return
            k, v = m
            if eng == 'pe' and k == 'pe':
                return
            if deps.get(k, 0) < v:
                deps[k] = v
        for r in reads:
            add(self.lastw.get(r))
            for m in self.multiw.get(r, ()):
                add(m)
        for w in writes:
            if w in NOWAW:
                continue
            add(self.lastw.get(w))
            for m in self.readers.get(w, ()):
                add(m)
        out = []
        wd = self.waited[eng]
        for k, v in deps.items():
            if wd.get(k, 0) < v:
                wd[k] = v
                out.append((k, v))
        return out

    def _mark(self, mark, reads, writes):
        for w in writes:
            if w in NOWAW:
                self.multiw.setdefault(w, []).append(mark)
                continue
            self.lastw[w] = mark
            self.readers[w] = []
        for r in reads:
            if r in writes:
                continue
            self.readers.setdefault(r, []).append(mark)

    cut = None
    pc = 0

    def isx(self, k):
        n = k[0] if isinstance(k, tuple) else k
        return isinstance(n, str) and n in self.xkeys

    def op(self, eng, fn, reads=(), writes=()):
        self.pc += 1
        if self.cut is not None and self.pc > self.cut:
            return
        xr = [r for r in reads if self.isx(r) and r not in writes]
        if xr:
            writes = list(writes) + xr
        waits = self._deps(eng, reads, writes)
        self.cnt[eng] += 1
        mark = (eng, self.cnt[eng])
        self.q[eng].append((waits, fn, (eng, 1)))
        self._mark(mark, reads, writes)
        self.nops += 1

    def dma(self, eng, out, in_, reads=(), writes=(), **kw):
        self.pc += 1
        if self.cut is not None and self.pc > self.cut:
            return
        waits = self._deps(eng, reads, writes)
        i = self.dnext[eng]
        self.dnext[eng] = (i + 1) % NDS
        k = 'd_%s_%d' % (eng, i)
        c = self.dcount[k]
        wd = self.waited[eng]
        if c > 0 and wd.get(k, 0) < 16 * c:
            wd[k] = 16 * c
            waits.append((k, 16 * c))
        self.dcount[k] = c + 1
        mark = (k, 16 * (c + 1))
        self.q[eng].append((waits, (lambda e: e.dma_start(out=out, in_=in_, **kw)), (k, 16)))
        self._mark(mark, reads, writes)
        self.nops += 1

    _breg = None

    def breg(self, e):
        if self._breg is None:
            self._breg = e.to_reg(S - 1)
        return self._breg

    def idma(self, fn, reads=(), writes=()):
        eng = 'pool'
        self.pc += 1
        waits = self._deps(eng, reads, writes)
        i = self.dnext[eng]
        self.dnext[eng] = (i + 1) % NDS
        k = 'd_%s_%d' % (eng, i)
        c = self.dcount[k]
        wd = self.waited[eng]
        if c > 0 and wd.get(k, 0) < 16 * c:
            wd[k] = 16 * c
            waits.append((k, 16 * c))
        self.dcount[k] = c + 1
        mark = (k, 16 * (c + 1))
        self.q[eng].append((waits, fn, (k, 16)))
        self._mark(mark, reads, writes)
        self.nops += 1

    def barrier(self):
        for e in ENGS:
            waits = []
            wd = self.waited[e]
            for o in ENGS:
                if o != e and self.cnt[o] > wd.get(o, 0):
                    wd[o] = self.cnt[o]
                    waits.append((o, self.cnt[o]))
            for k, c in self.dcount.items():
                if 16 * c > wd.get(k, 0):
                    wd[k] = 16 * c
                    waits.append((k, 16 * c))
            if waits:
                self.q[e].append((waits, None, None))
        self.lastw = {}
        self.readers = {}
        self.multiw = {}

    def emit(self):
        self.barrier()
        nc = self.nc
        sems = self.sems
        q = self.q

        def replay(name, e):
            for waits, fn, inc in q[name]:
                for k, v in waits:
                    e.wait_ge(sems[k], v)
                if fn is not None:
                    ins = fn(e)
                    ins.then_inc(sems[inc[0]], inc[1])

        with nc.Block() as block:
            @block.tensor
            def _(e):
                replay('pe', e)

            @block.vector
            def _(e):
                replay('dve', e)

            @block.scalar
            def _(e):
                replay('act', e)

            @block.gpsimd
            def _(e):
                replay('pool', e)

            @block.sync
            def _(e):
                replay('sp', e)
        self.q = {e: [] for e in ENGS}

    uid = 0

    def sb(self, stack, name, shape, dt):
        self.uid += 1
        return stack.enter_context(self.nc.sbuf_tensor('%s_%d' % (name, self.uid), list(shape), dt))

    def ps(self, stack, name, shape, dt=F32, keys=()):
        for k in keys:
            self.xkeys.add(k)
        self.uid += 1
        return stack.enter_context(self.nc.psum_tensor('%s_%d' % (name, self.uid), list(shape), dt))

    def dram(self, name, shape, dt, kind="Internal"):
        return self.nc.dram_tensor(name, list(shape), dt, kind=kind).ap()


def ssl(a, n, d):
    return slice(a, a + (n - 1) * d + 1, d)


def bc(ap, shape):
    return ap.to_broadcast(list(shape))


def phase_rope(P, D):
    with ExitStack() as s:
        pi_ = P.sb(s, 'r_pi', [128, NT], I32)
        pf = P.sb(s, 'r_pf', [128, NT], F32)
        invf = P.sb(s, 'r_invf', [128, 8], F32)
        ang = P.sb(s, 'r_ang', [128, 2, NT, 8], F32)
        kk = P.sb(s, 'r_kk', [128, 2, NT, 8], F32)
        ki = P.sb(s, 'r_ki', [128, 2, NT, 8], I32)
        cs = P.sb(s, 'r_cs', [128, NT, 16], F32)
        P.dma('sp', pi_[:], D['positions'], writes=['pi'])
        P.dma('sp', invf[:], D['invf'].partition_broadcast(128), writes=['invf'])
        P.op('dve', lambda e: e.tensor_copy(pf[:], pi_[:]), reads=['pi'], writes=['pf'])
        P.op('dve', lambda e: e.tensor_tensor(ang[:, 1], bc(pf[:].unsqueeze(2), [128, NT, 8]), bc(invf[:].unsqueeze(1), [128, NT, 8]), ALU.mult),
             reads=['pf', 'invf'], writes=['ang1'])
        P.op('dve', lambda e: e.tensor_scalar(ang[:, 0], ang[:, 1], float(np.pi / 2), None, ALU.add), reads=['ang1'], writes=['ang0'])
        A = ang[:].rearrange('p a t c -> p (a t c)')
        K = kk[:].rearrange('p a t c -> p (a t c)')
        KI = ki[:].rearrange('p a t c -> p (a t c)')
        P.op('dve', lambda e: e.tensor_scalar(K, A, float(1.0 / (2 * np.pi)), None, ALU.mult), reads=['ang0', 'ang1'], writes=['kk'])
        P.op('dve', lambda e: e.tensor_copy(KI, K), reads=['kk'], writes=['ki'])
        P.op('dve', lambda e: e.tensor_copy(K, KI), reads=['ki'], writes=['kk'])
        C1 = 6.28125
        C2 = float(2 * np.pi - 6.28125)
        P.op('dve', lambda e: e.scalar_tensor_tensor(A, K, -C1, A, ALU.mult, ALU.add), reads=['kk', 'ang0', 'ang1'], writes=['ang'])
        P.op('dve', lambda e: e.scalar_tensor_tensor(A, K, -C2, A, ALU.mult, ALU.add), reads=['kk', 'ang'], writes=['ang'])
        P.op('dve', lambda e: e.tensor_scalar(A, A, 3.1415925, -3.1415925, ALU.min, ALU.max), reads=['ang'], writes=['ang'])
        P.op('act', lambda e: e.activation(cs[:, :, 0:8], ang[:, 0], AF.Sin), reads=['ang'], writes=['cs0'])
        P.op('act', lambda e: e.activation(cs[:, :, 8:16], ang[:, 1], AF.Sin), reads=['ang'], writes=['cs1'])
        P.dma('sp', D['cs'].rearrange('(t p) c -> p t c', p=128), cs[:], reads=['cs0', 'cs1'], writes=['d_cs'])
        P.emit()


def phase_A(P, l, D, first):
    xsrc = D['x'] if first else D['xw']
    with ExitStack() as s:
        wbf = P.sb(s, 'a_wbf', [128, 8, 3224], BF16)
        gmix = P.sb(s, 'a_gmix', [128, 1024], F32)
        lng = P.sb(s, 'a_lng', [128, 256], F32)
        lnb = P.sb(s, 'a_lnb', [128, 256], F32)
        qkg = P.sb(s, 'a_qkg', [128, 2, 64], F32)
        cs = P.sb(s, 'a_cs', [128, NT, 16], F32)
        idb = P.sb(s, 'a_idb', [128, 128], BF16)
        xts = [P.sb(s, 'a_xt%d' % i, [128, 1024], F32) for i in range(2)]
        junk = P.sb(s, 'a_junk', [128, 1024], BF16)
        ss = P.sb(s, 'a_ss', [128, 2], F32)
        xn = P.sb(s, 'a_xn', [128, 1024], BF16)
        xnT = [P.sb(s, 'a_xnT%d' % i, [128, 8, 512], BF16) for i in range(2)]
        ge = P.sb(s, 'a_ge', [128, 4, 64], F32)
        cen = P.sb(s, 'a_cen', [128, 4, 64], F32)
        sq = P.sb(s, 'a_sq', [128, 4, 64], F32)
        m4 = P.sb(s, 'a_m4', [128, 8], F32)
        vnb = [P.sb(s, 'a_vnb%d' % i, [128, 256], BF16) for i in range(2)]
        sqq = P.sb(s, 'a_sqq', [128, 12, 64], F32)
        ss12 = P.sb(s, 'a_ss12', [128, 12], F32)
        qk32 = P.sb(s, 'a_qk32', [128, 12, 64], F32)
        rt = P.sb(s, 'a_rt', [128, 4, 12, 8], F32)
        qkb = P.sb(s, 'a_qkb', [128, 12, 64], BF16)
        qkTs = [P.sb(s, 'a_qkTs%d' % i, [128, 6, 128], BF16) for i in range(2)]
        vaug = [P.sb(s, 'a_vaug%d' % i, [128, 6, 65], BF16) for i in range(2)]
        gs = [P.sb(s, 'a_gs%d' % i, [128, 408], F32) for i in range(2)]
        fo = [P.sb(s, 'a_fo%d' % i, [128, 512], F32) for i in range(2)]
        pT = P.ps(s, 'a_pT', [128, 1024], BF16)
        pv = P.ps(s, 'a_pv', [128, 512])
        pq = P.ps(s, 'a_pq', [128, 512])
        pk = P.ps(s, 'a_pk', [128, 512])
        pbv = P.ps(s, 'a_pbv', [128, 512])
        pg = P.ps(s, 'a_pg', [128, 512])
        pf = [P.ps(s, 'a_pf%d' % i, [128, 512]) for i in range(2)]

        for k in range(8):
            P.dma('pool', wbf[:, k, :], D['w_in'][l, k * 128:(k + 1) * 128, :], writes=[('wbf', k)])
        P.dma('sp', gmix[:], D['g_mix'][l].partition_broadcast(128), writes=['gmix'])
        P.dma('sp', lng[:], D['ln_v_g'][l].rearrange('g d -> (g d)').partition_broadcast(128), writes=['lng'])
        P.dma('sp', lnb[:], D['ln_v_b'][l].rearrange('g d -> (g d)').partition_broadcast(128), writes=['lnb'])
        P.dma('sp', qkg[:, 0, :], D['q_norm_g'][l].partition_broadcast(128), writes=['qkg0'])
        P.dma('sp', qkg[:, 1, :], D['k_norm_g'][l].partition_broadcast(128), writes=['qkg1'])
        P.dma('sp', cs[:], D['cs'].rearrange('(t p) c -> p t c', p=128), reads=['d_cs'], writes=['cs'])
        P.dma('pool', idb[:], D['ident'], writes=['idb'])
        for i in range(2):
            P.op('pool', lambda e, i=i: e.memset(vaug[i][:, :, 64:65], 1.0), writes=[('vaug', i)])
        WB = [('wbf', k) for k in range(8)]
        fcount = [0]

        def front(t):
            if True:
                g, j = t // 4, t % 4
                XT = xnT[g % 2]
                kxt = ('xnT', g % 2)
                b = t % 2
                xt = xts[b]
                P.dma('sp', xt[:], xsrc[t * 128:(t + 1) * 128, :], writes=[('xt', b)])
                P.op('dve', lambda e, b=b: e.memset(ss[:, b:b + 1], 0.0), writes=[('ss', b)])
                P.op('act', lambda e, xt=xt, b=b: e.activation(junk[:], xt[:], AF.Square, accum_out=ss[:, b:b + 1]),
                     reads=[('xt', b), ('ss', b)], writes=['junk', ('ss', b)])
                P.op('act', lambda e, b=b: e.activation(ss[:, b:b + 1], ss[:, b:b + 1], AF.Sqrt, bias=EPS, scale=1.0 / 1024),
                     reads=[('ss', b)], writes=[('ss', b)])
                P.op('dve', lambda e, b=b: e.reciprocal(ss[:, b:b + 1], ss[:, b:b + 1]), reads=[('ss', b)], writes=[('ss', b)])
                P.op('dve', lambda e, xt=xt, b=b: e.scalar_tensor_tensor(xn[:], xt[:], ss[:, b:b + 1], gmix[:], ALU.mult, ALU.mult),
                     reads=[('xt', b), ('ss', b), 'gmix'], writes=['xn'])
                for k in range(8):
                    P.op('pe', lambda e, k=k: e.transpose(pT[:, k * 128:(k + 1) * 128], xn[:, k * 128:(k + 1) * 128], idb[:]),
                         reads=['xn', 'idb'], writes=['pT'])
                P.op('act', lambda e, XT=XT, j=j: e.copy(XT[:, :, j * 128:(j + 1) * 128], pT[:].rearrange('p (k t) -> p k t', t=128)),
                     reads=['pT'], writes=[kxt + (j,)])

        def back(t):
            if True:
                g, j = t // 4, t % 4
                XT = xnT[g % 2]
                kxt = ('xnT', g % 2)
                b = t % 2
                for (pp, nm, c0, c1) in ((pv, 'pv', 256, 512), (pq, 'pq', 512, 896), (pk, 'pk', 896, 1280), (pbv, 'pbv', 1280, 1664), (pg, 'pg', 2816, 3224)):
                    for k in range(8):
                        P.op('pe', lambda e, pp=pp, k=k, c0=c0, c1=c1, XT=XT, j=j: e.matmul(pp[:, 0:c1 - c0], XT[:, k, j * 128:(j + 1) * 128], wbf[:, k, c0:c1], start=(k == 0), stop=(k == 7)),
                             reads=[kxt + (j,), ('wbf', k)], writes=[nm])
                GE = ge[:].rearrange('p a b -> p (a b)')
                P.op('act', lambda e: e.activation(GE, pv[:, 0:256], AF.Gelu_apprx_tanh), reads=['pv'], writes=['ge'])
                P.op('dve', lambda e: e.tensor_reduce(m4[:, 0:4], ge[:], AX.X, ALU.add), reads=['ge'], writes=['m4a'])
                P.op('dve', lambda e: e.tensor_scalar(m4[:, 0:4], m4[:, 0:4], 1.0 / 64, None, ALU.mult), reads=['m4a'], writes=['m4a'])
                P.op('dve', lambda e: e.tensor_tensor(cen[:], ge[:], bc(m4[:, 0:4].unsqueeze(2), [128, 4, 64]), ALU.subtract), reads=['ge', 'm4a'], writes=['cen'])
                P.op('act', lambda e: e.activation(sq[:], cen[:], AF.Square), reads=['cen'], writes=['sq'])
                P.op('dve', lambda e: e.tensor_reduce(m4[:, 4:8], sq[:], AX.X, ALU.add), reads=['sq'], writes=['m4b'])
                P.op('act', lambda e: e.activation(m4[:, 4:8], m4[:, 4:8], AF.Sqrt, bias=EPS, scale=1.0 / 64), reads=['m4b'], writes=['m4b'])
                P.op('dve', lambda e: e.reciprocal(m4[:, 4:8], m4[:, 4:8]), reads=['m4b'], writes=['m4b'])
                P.op('dve', lambda e: e.tensor_tensor(cen[:], cen[:], bc(m4[:, 4:8].unsqueeze(2), [128, 4, 64]), ALU.mult), reads=['cen', 'm4b'], writes=['cen'])
                CEN = cen[:].rearrange('p a b -> p (a b)')
                P.op('pool', lambda e: e.tensor_tensor(CEN, CEN, lng[:], ALU.mult), reads=['cen', 'lng'], writes=['cen'])
                P.op('pool', lambda e, b=b: e.tensor_tensor(vnb[b][:], CEN, lnb[:], ALU.add), reads=['cen', 'lnb'], writes=[('vnb', b)])
                P.dma('sp', D['vn'][t * 128:(t + 1) * 128, :], vnb[b][:], reads=[('vnb', b)], writes=['d_vn'])
                P.op('act', lambda e: e.activation(sqq[:, 0:6, :].rearrange('p a b -> p (a b)'), pq[:, 0:384], AF.Square), reads=['pq'], writes=['sqq0'])
                P.op('act', lambda e: e.activation(sqq[:, 6:12, :].rearrange('p a b -> p (a b)'), pk[:, 0:384], AF.Square), reads=['pk'], writes=['sqq1'])
                P.op('dve', lambda e: e.tensor_reduce(ss12[:], sqq[:], AX.X, ALU.add), reads=['sqq0', 'sqq1'], writes=['ss12'])
                P.op('act', lambda e: e.activation(ss12[:], ss12[:], AF.Sqrt, bias=EPS, scale=1.0 / 64), reads=['ss12'], writes=['ss12'])
                P.op('dve', lambda e: e.reciprocal(ss12[:], ss12[:]), reads=['ss12'], writes=['ss12'])
                P.op('dve', lambda e: e.tensor_tensor(qk32[:, 0:6, :], pq[:, 0:384].rearrange('p (a b) -> p a b', b=64), bc(ss12[:, 0:6].unsqueeze(2), [128, 6, 64]), ALU.mult),
                     reads=['pq', 'ss12'], writes=['qk32a'])
                P.op('dve', lambda e: e.tensor_tensor(qk32[:, 6:12, :], pk[:, 0:384].rearrange('p (a b) -> p a b', b=64), bc(ss12[:, 6:12].unsqueeze(2), [128, 6, 64]), ALU.mult),
                     reads=['pk', 'ss12'], writes=['qk32b'])
                P.op('pool', lambda e: e.tensor_tensor(qk32[:, 0:6, :], qk32[:, 0:6, :], bc(qkg[:, 0:1, :], [128, 6, 64]), ALU.mult), reads=['qk32a', 'qkg0'], writes=['qk32a'])
                P.op('pool', lambda e: e.tensor_tensor(qk32[:, 6:12, :], qk32[:, 6:12, :], bc(qkg[:, 1:2, :], [128, 6, 64]), ALU.mult), reads=['qk32b', 'qkg1'], writes=['qk32b'])
                cosb = bc(cs[:, t:t + 1, 0:8], [128, 12, 8])
                sinb = bc(cs[:, t:t + 1, 8:16], [128, 12, 8])
                x1 = qk32[:, :, 0:8]
                x2 = qk32[:, :, 8:16]
                P.op('pool', lambda e, cosb=cosb: e.tensor_tensor(rt[:, 0], x1, cosb, ALU.mult), reads=['qk32a', 'qk32b', 'cs'], writes=['rt0'])
                P.op('pool', lambda e, sinb=sinb: e.tensor_tensor(rt[:, 1], x2, sinb, ALU.mult), reads=['qk32a', 'qk32b', 'cs'], writes=['rt1'])
                P.op('dve', lambda e, cosb=cosb: e.tensor_tensor(rt[:, 2], x2, cosb, ALU.mult), reads=['qk32a', 'qk32b', 'cs'], writes=['rt2'])
                P.op('dve', lambda e, sinb=sinb: e.tensor_tensor(rt[:, 3], x1, sinb, ALU.mult), reads=['qk32a', 'qk32b', 'cs'], writes=['rt3'])
                P.op('act', lambda e: e.copy(qkb[:], qk32[:]), reads=['qk32a', 'qk32b'], writes=['qkb'])
                P.op('dve', lambda e: e.tensor_tensor(qkb[:, :, 0:8], rt[:, 0], rt[:, 1], ALU.subtract), reads=['rt0', 'rt1', 'qkb'], writes=['qkb'])
                P.op('dve', lambda e: e.tensor_tensor(qkb[:, :, 8:16], rt[:, 2], rt[:, 3], ALU.add), reads=['rt2', 'rt3', 'qkb'], writes=['qkb'])
                for i in range(6):
                    P.op('pe', lambda e, i=i: e.transpose(pT[:, i * 128:(i + 1) * 128], qkb[:, 2 * i:2 * i + 2, :].rearrange('p a b -> p (a b)'), idb[:]),
                         reads=['qkb', 'idb'], writes=['pT'])
                P.op('act', lambda e, b=b: e.copy(qkTs[b][:], pT[:, 0:768].rearrange('p (k t) -> p k t', t=128)), reads=['pT'], writes=[('qkTs', b)])
                P.dma('sp', D['qkT'][:, :, t * 128:(t + 1) * 128].rearrange('i p t -> p i t'), qkTs[b][:], reads=[('qkTs', b)], writes=['d_qkT'])
                P.op('act', lambda e, b=b: e.copy(vaug[b][:, :, 0:64], pbv[:, 0:384].rearrange('p (a b) -> p a b', b=64)), reads=['pbv', ('vaug', b)], writes=[('vaug', b)])
                P.dma('sp', D['vaug'][t * 128:(t + 1) * 128, :], vaug[b][:].rearrange('p a b -> p (a b)'), reads=[('vaug', b)], writes=['d_vaug'])
                P.op('act', lambda e, b=b: e.activation(gs[b][:, 0:384], pg[:, 0:384], AF.Silu), reads=['pg'], writes=[('gs', b)])
                P.op('dve', lambda e, b=b: e.tensor_copy(gs[b][:, 384:408], pg[:, 384:408]), reads=['pg', ('gs', b)], writes=[('gs', b)])
                P.dma('sp', D['gate_s'][t * 128:(t + 1) * 128, :], gs[b][:, 0:384], reads=[('gs', b)], writes=['d_gate'])
                P.dma('sp', D['ab'][t * 128:(t + 1) * 128, :], gs[b][:, 384:408], reads=[('gs', b)], writes=['d_ab'])

        def fmaj(g):
            XT = xnT[g % 2]
            kxt = ('xnT', g % 2)
            allx = [kxt + (j,) for j in range(4)]
            for ci in range(11):
                c0 = ci * 128 if ci < 2 else 1664 + (ci - 2) * 128
                fb = fcount[0] % 2
                fcount[0] += 1
                for k in range(8):
                    P.op('pe', lambda e, fb=fb, k=k, c0=c0, XT=XT: e.matmul(pf[fb][:], wbf[:, k, c0:c0 + 128], XT[:, k, :], start=(k == 0), stop=(k == 7)),
                         reads=allx + [('wbf', k)], writes=[('pf', fb)])
                if ci < 2:
                    P.op('act', lambda e, fb=fb: e.activation(fo[fb][:], pf[fb][:], AF.Gelu_apprx_tanh), reads=[('pf', fb)], writes=[('fo', fb)])
                    P.dma('sp', D['uT'][ci * 128:(ci + 1) * 128, g * 512:(g + 1) * 512], fo[fb][:], reads=[('fo', fb)], writes=['d_uT'])
                else:
                    P.op('dve', lambda e, fb=fb: e.tensor_copy(fo[fb][:], pf[fb][:]), reads=[('pf', fb)], writes=[('fo', fb)])
                    P.dma('sp', D['cT'][(ci - 2) * 128:(ci - 1) * 128, g * 512:(g + 1) * 512], fo[fb][:], reads=[('fo', fb)], writes=['d_cT'])

        front(0)
        for t in range(NT):
            if t + 1 < NT:
                front(t + 1)
            back(t)
            if t % 4 == 3:
                fmaj(t // 4)
        P.emit()


def phase_B(P, l, D):
    with ExitStack() as s:
        ws32 = P.sb(s, 'b_ws32', [128, 4, 128], F32)
        idf = P.sb(s, 'b_idf', [128, 128], F32)
        wsT = P.sb(s, 'b_wsT', [128, 4, 128], BF16)
        bias = P.sb(s, 'b_bias', [64, 4, 128], F32)
        vn = [P.sb(s, 'b_vn%d' % i, [128, 4, 256], BF16) for i in range(2)]
        ut = [P.sb(s, 'b_ut%d' % i, [64, 4, 512], F32) for i in range(2)]
        mx = P.sb(s, 'b_mx', [64, 4, 128], F32)
        yb = [P.sb(s, 'b_yb%d' % i, [64, 4, 512], BF16) for i in range(2)]
        ptr = P.ps(s, 'b_ptr', [128, 512])
        pm = [P.ps(s, 'b_pm%d' % i, [64, 512]) for i in range(4)]
        P.dma('sp', ws32[:], D['w_s'][l].rearrange('g i j -> i g j'), writes=['ws32'])
        P.dma('sp', idf[:], D['ident'], writes=['idf'])
        P.dma('sp', bias[:].rearrange('p g i -> p (g i)'), D['b_s'][l].rearrange('g i -> (g i)').partition_broadcast(64), writes=['bias'])
        for g in range(4):
            P.op('pe', lambda e, g=g: e.transpose(ptr[:, g * 128:(g + 1) * 128], ws32[:, g, :], idf[:]), reads=['ws32', 'idf'], writes=['ptr'])
        P.op('dve', lambda e: e.tensor_copy(wsT[:].rearrange('p g i -> p (g i)'), ptr[:]), reads=['ptr'], writes=['wsT'])
        for it in range(8):
            b = it % 2
            P.dma('sp', vn[b][:], D['vn'][it * 512:(it + 1) * 512, :].rearrange('(c p) n -> p c n', p=128), reads=['d_vn'], writes=[('vn', b)])
            P.dma('sp', ut[b][:], D['uT'][:, it * 512:(it + 1) * 512].rearrange('(g d) t -> d g t', d=64), reads=['d_uT'], writes=[('ut', b)])
            for g in range(4):
                for c in range(4):
                    P.op('pe', lambda e, g=g, c=c, b=b: e.matmul(pm[g][:, c * 128:(c + 1) * 128], vn[b][:, c, g * 64:(g + 1) * 64], wsT[:, g, :], start=True, stop=True),
                         reads=[('vn', b), 'wsT'], writes=[('pm', g)])
                P.op('dve', lambda e, g=g: e.tensor_tensor(mx[:], pm[g][:].rearrange('p (c i) -> p c i', i=128), bc(bias[:, g:g + 1, :], [64, 4, 128]), ALU.add),
                     reads=[('pm', g), 'bias'], writes=['mx'])
                P.op('dve', lambda e, g=g, b=b: e.tensor_tensor(yb[b][:, g, :], mx[:].rearrange('p c i -> p (c i)'), ut[b][:, g, :], ALU.mult),
                     reads=['mx', ('ut', b)], writes=[('yb', b)])
            P.dma('sp', D['yT'][0:256, it * 512:(it + 1) * 512].rearrange('(g d) t -> d g t', d=64), yb[b][:], reads=[('yb', b)], writes=['d_yT'])
        P.emit()


PATS = (1, 4, 16)
KPAD = 1024


def phase_C(P, l, D):
    with ExitStack() as s:
        vs = {}
        for d in PATS:
            nt = d * (S // d // 128 + 1)
            vs[d] = P.sb(s, 'c_vs%d' % d, [128, nt, 390], BF16)
        mab = P.sb(s, 'c_mab', [128, 512], BF16)
        sel = P.sb(s, 'c_sel', [65, 64], F32)
        qh = [P.sb(s, 'c_qh%d' % i, [64, S], BF16) for i in range(2)]
        kh = [P.sb(s, 'c_kh%d' % i, [64, S + 2 * KPAD], BF16) for i in range(2)]
        pex = [P.sb(s, 'c_pex%d' % i, [128, 512], BF16) for i in range(3)]
        acc = P.sb(s, 'c_acc', [65, S], F32)
        rd = P.sb(s, 'c_rd', [64, 512], F32)
        yb = [P.sb(s, 'c_yb%d' % i, [64, 512], BF16) for i in range(2)]
        pss = [P.ps(s, 'c_ps%d' % i, [128, 512]) for i in range(3)]
        ppv = [P.ps(s, 'c_pv%d' % i, [65, 512]) for i in range(3)]
        pd = P.ps(s, 'c_pd', [64, 512])
        P.dma('pool', mab[:], D['mab'], writes=['mab'])
        P.dma('sp', sel[:], D['sel65'], writes=['sel'])
        for i in range(2):
            P.op('pool', lambda e, i=i: e.memset(kh[i][:, 0:KPAD], 0.0), writes=[('kh', i)])
            P.op('pool', lambda e, i=i: e.memset(kh[i][:, KPAD + S:], 0.0), writes=[('kh', i)])
        for d in PATS:
            L = S // d
            nqb = L // 128
            P.op('pool', lambda e, d=d: e.memset(vs[d][:].rearrange('p a b -> p (a b)'), 0.0), writes=[('vs', d)])
            vsrc = D['vaug'].rearrange('(j r) c -> r j c', r=d)
            for r in range(d):
                tb = r * (nqb + 1)
                if nqb > 1:
                    P.dma('sp', vs[d][:, tb + 1:tb + nqb, :], vsrc[r, 64:64 + (nqb - 1) * 128, :].rearrange('(k p) c -> p k c', p=128),
                          reads=['d_vaug', ('vs', d)], writes=[('vs', d)])
                P.dma('sp', vs[d][64:128, tb, :], vsrc[r, 0:64, :], reads=['d_vaug', ('vs', d)], writes=[('vs', d)])
                P.dma('sp', vs[d][0:64, tb + nqb, :], vsrc[r, L - 64:L, :], reads=['d_vaug', ('vs', d)], writes=[('vs', d)])
        for h in range(6):
            hb = h % 2
            P.dma('sp', qh[hb][:], D['qkT'][h // 2, (h % 2) * 64:(h % 2) * 64 + 64, :], reads=['d_qkT'], writes=[('qh', hb)])
            P.dma('sp', kh[hb][:, KPAD:KPAD + S], D['qkT'][3 + h // 2, (h % 2) * 64:(h % 2) * 64 + 64, :], reads=['d_qkT'], writes=[('kh', hb)])
            its = []
            for pi, d in enumerate(PATS):
                L = S // d
                nqb = L // 128
                for r in range(d):
                    tb = r * (nqb + 1)
                    for qb0 in range(0, nqb, 2):
                        its.append((pi, d, r, tb, qb0))

            def stage1(n):
                pi, d, r, tb, qb0 = its[n]
                ib = n % 3
                combos = ((qb0, qb0), (qb0 + 1, qb0), (qb0 + 1, qb0 + 1), (qb0 + 2, qb0 + 1))
                for ci, (kt, qb) in enumerate(combos):
                    k0 = KPAD + r + d * (kt * 128 - 64)
                    q0 = r + d * (qb * 128)
                    P.op('pe', lambda e, ib=ib, ci=ci, k0=k0, q0=q0, d=d, hb=hb: e.matmul(
                        pss[ib][:, ci * 128:(ci + 1) * 128], kh[hb][:, ssl(k0, 128, d)], qh[hb][:, ssl(q0, 128, d)], start=True, stop=True),
                        reads=[('kh', hb), ('qh', hb)], writes=[('pss', ib)])
                P.op('act', lambda e, ib=ib: e.activation(pex[ib][:], pss[ib][:], AF.Exp, scale=0.125), reads=[('pss', ib)], writes=[('pex', ib)])
                P.op('dve', lambda e, ib=ib: e.tensor_tensor(pex[ib][:], pex[ib][:], mab[:], ALU.mult), reads=[('pex', ib), 'mab'], writes=[('pex', ib)])

            def stage2(n):
                pi, d, r, tb, qb0 = its[n]
                ib = n % 3
                combos = ((qb0, qb0), (qb0 + 1, qb0), (qb0 + 1, qb0 + 1), (qb0 + 2, qb0 + 1))
                for ci, (kt, qb) in enumerate(combos):
                    qi = qb - qb0
                    P.op('pe', lambda e, ib=ib, ci=ci, kt=kt, qi=qi, d=d, tb=tb, h=h: e.matmul(
                        ppv[ib][:, qi * 128:(qi + 1) * 128], vs[d][:, tb + kt, h * 65:(h + 1) * 65], pex[ib][:, ci * 128:(ci + 1) * 128],
                        start=(ci % 2 == 0), stop=(ci % 2 == 1)), reads=[('vs', d), ('pex', ib)], writes=[('ppv', ib)])
                a0 = r + d * (qb0 * 128)
                av = acc[:, ssl(a0, 256, d)]
                if pi == 0:
                    P.op('dve', lambda e, av=av, ib=ib: e.tensor_copy(av, ppv[ib][:, 0:256]), reads=[('ppv', ib)], writes=['acc'])
                else:
                    P.op('dve', lambda e, av=av, ib=ib: e.tensor_tensor(av, av, ppv[ib][:, 0:256], ALU.add), reads=[('ppv', ib), 'acc'], writes=['acc'])

            stage1(0)
            for n in range(len(its)):
                if n + 1 < len(its):
                    stage1(n + 1)
                stage2(n)
            for c4 in range(8):
                yb_ = yb[c4 % 2]
                P.op('pe', lambda e, c4=c4: e.matmul(pd[:], sel[:], acc[:, c4 * 512:(c4 + 1) * 512], start=True, stop=True), reads=['sel', 'acc'], writes=['pd'])
                P.op('dve', lambda e: e.reciprocal(rd[:], pd[:]), reads=['pd'], writes=['rd'])
                P.op('dve', lambda e, c4=c4, yb_=yb_: e.tensor_tensor(yb_[:], acc[0:64, c4 * 512:(c4 + 1) * 512], rd[:], ALU.mult), reads=['acc', 'rd'], writes=[('yb', c4 % 2)])
                P.dma('sp', D['yT'][256 + h * 64:256 + (h + 1) * 64, c4 * 512:(c4 + 1) * 512], yb_[:], reads=[('yb', c4 % 2)], writes=['d_yT'])
        P.emit()


def phase_D1(P, l, D):
    with ExitStack() as s:
        cw = P.sb(s, 'd_cw', [128, 5, 9], F32)
        idf = P.sb(s, 'd_idf', [128, 128], F32)
        raw = [P.sb(s, 'd_raw%d' % i, [128, S + 4], F32) for i in range(2)]
        cv = P.sb(s, 'd_cv', [128, S], F32)
        tm = [P.sb(s, 'd_tm%d' % i, [128, 4, 128], F32) for i in range(3)]
        sq = P.sb(s, 'd_sq', [128, 8, 64], F32)
        r8 = P.sb(s, 'd_r8', [128, 8], F32)
        fT = [P.sb(s, 'd_fT%d' % i, [128, 512], BF16) for i in range(3)]
        abt = P.sb(s, 'd_abt', [128, NT, 24], F32)
        gbt = P.sb(s, 'd_gbt', [128, NT, 24], F32)
        dtb = P.sb(s, 'd_dtb', [128, 12], F32)
        nA = P.sb(s, 'd_nA', [128, 12], F32)
        ptr = [P.ps(s, 'd_ptr%d' % i, [128, 512]) for i in range(3)]
        ptb = [P.ps(s, 'd_ptb%d' % i, [128, 512]) for i in range(3)]
        for k in range(5):
            P.dma('sp', cw[:, k, :], D['conv_w'][l, k].rearrange('(c p) -> p c', p=128), writes=['cw'], allow_slow_non_contiguous=True)
        P.dma('sp', idf[:], D['ident'], writes=['idf'])
        for i in range(2):
            P.op('pool', lambda e, i=i: e.memset(raw[i][:, 0:2], 0.0), writes=[('raw', i)])
            P.op('pool', lambda e, i=i: e.memset(raw[i][:, S + 2:S + 4], 0.0), writes=[('raw', i)])
        P.dma('sp', abt[:], D['ab'].rearrange('(t p) c -> p t c', p=128), reads=['d_ab'], writes=['abt'])
        P.dma('sp', dtb[:], D['dt_bias'][l].rearrange('a h -> (a h)').partition_broadcast(128), writes=['dtb'])
        P.dma('sp', nA[:], D['a_log'][l].rearrange('a h -> (a h)').partition_broadcast(128), writes=['nA'])
        P.op('act', lambda e: e.activation(nA[:], nA[:], AF.Exp), reads=['nA'], writes=['nA'])
        P.op('dve', lambda e: e.tensor_scalar(nA[:], nA[:], -1.0, None, ALU.mult), reads=['nA'], writes=['nA'])
        P.op('dve', lambda e: e.tensor_tensor(gbt[:, :, 0:12], abt[:, :, 0:12], bc(dtb[:].unsqueeze(1), [128, NT, 12]), ALU.add), reads=['abt', 'dtb'], writes=['gbt0'])
        P.op('act', lambda e: e.activation(gbt[:, :, 0:12], gbt[:, :, 0:12], AF.Exp), reads=['gbt0'], writes=['gbt0'])
        P.op('act', lambda e: e.activation(gbt[:, :, 0:12], gbt[:, :, 0:12], AF.Ln, bias=1.0), reads=['gbt0'], writes=['gbt0'])
        P.op('dve', lambda e: e.tensor_tensor(gbt[:, :, 0:12], gbt[:, :, 0:12], bc(nA[:].unsqueeze(1), [128, NT, 12]), ALU.mult), reads=['gbt0', 'nA'], writes=['gbt0'])
        P.op('act', lambda e: e.activation(gbt[:, :, 12:24], abt[:, :, 12:24], AF.Sigmoid), reads=['abt'], writes=['gbt1'])
        P.dma('sp', D['gb'].rearrange('(t p) c -> p t c', p=128), gbt[:], reads=['gbt0', 'gbt1'], writes=['d_gb'])
        n4 = 0
        import os
        CUT = int(os.environ.get('D1CUT', '99'))
        for c in range(int(os.environ.get('D1C0', '0')), int(os.environ.get('D1C1', '9'))):
            rb = c % 2
            R = raw[rb]
            P.dma('sp', R[:, 2:S + 2], D['cT'][c * 128:(c + 1) * 128, :], reads=['d_cT'], writes=[('raw', rb)])
            P.op('dve', lambda e, R=R, c=c: e.tensor_scalar(cv[:], R[:, 0:S], cw[:, 0, c:c + 1], None, ALU.mult), reads=[('raw', rb), 'cw'], writes=['cv'])
            for k in range(1, 5):
                eng = 'dve'
                P.op(eng, lambda e, R=R, c=c, k=k: e.scalar_tensor_tensor(cv[:], R[:, k:k + S], cw[:, k, c:c + 1], cv[:], ALU.mult, ALU.add),
                     reads=[('raw', rb), 'cw', 'cv'], writes=['cv'])
            P.op('act', lambda e: e.activation(cv[:], cv[:], AF.Silu), reads=['cv'], writes=['cv'])
            def stA(t4, c=c):
                pb = (c * 8 + t4) % 3
                for j in range(4):
                    t = t4 * 4 + j
                    P.op('pe', lambda e, pb=pb, j=j, t=t: e.transpose(ptr[pb][:, j * 128:(j + 1) * 128], cv[:, t * 128:(t + 1) * 128], idf[:]),
                         reads=['cv', 'idf'], writes=[('ptr', pb)])
                TM = tm[pb]
                PV = ptr[pb][:].rearrange('p (j h d) -> p (j h) d', h=2, d=64)
                if c >= 6:
                    P.op('act', lambda e, pb=pb, TM=TM: e.copy(TM[:].rearrange('p j c -> p (j c)'), ptr[pb][:]), reads=[('ptr', pb)], writes=[('tm', pb)])
                    P.dma('sp', D['v_tm'][t4 * 512:(t4 + 1) * 512, (c - 6) * 128:(c - 5) * 128].rearrange('(j p) c -> p j c', p=128), TM[:], reads=[('tm', pb)], writes=['d_vtm'])
                else:
                    P.op('act', lambda e, pb=pb: e.activation(sq[:].rearrange('p a b -> p (a b)'), ptr[pb][:], AF.Square), reads=[('ptr', pb)], writes=['sq'])
                    P.op('dve', lambda e: e.tensor_reduce(r8[:], sq[:], AX.X, ALU.add), reads=['sq'], writes=['r8'])
                    P.op('act', lambda e: e.activation(r8[:], r8[:], AF.Sqrt, bias=EPS, scale=1.0), reads=['r8'], writes=['r8'])
                    P.op('dve', lambda e: e.reciprocal(r8[:], r8[:]), reads=['r8'], writes=['r8'])
                    if c < 3:
                        P.op('dve', lambda e: e.tensor_scalar(r8[:], r8[:], 0.125, None, ALU.mult), reads=['r8'], writes=['r8'])
                    P.op('dve', lambda e, TM=TM, PV=PV: e.tensor_tensor(TM[:].rearrange('p j (h d) -> p (j h) d', d=64), PV, bc(r8[:].unsqueeze(2), [128, 8, 64]), ALU.mult),
                         reads=[('ptr', pb), 'r8'], writes=[('tm', pb)])
                    if c >= 3:
                        P.dma('sp', D['k_tm'][t4 * 512:(t4 + 1) * 512, (c - 3) * 128:(c - 2) * 128].rearrange('(j p) c -> p j c', p=128), TM[:], reads=[('tm', pb)], writes=['d_ktm'])

            def stB(t4, c=c):
                pb = (c * 8 + t4) % 3
                TM = tm[pb]
                if c < 6:
                    for j in range(4):
                        P.op('pe', lambda e, pb=pb, j=j, TM=TM: e.transpose(ptb[pb][:, j * 128:(j + 1) * 128], TM[:, j, :], idf[:]), reads=[('tm', pb), 'idf'], writes=[('ptb', pb)])
                    P.op('act', lambda e, pb=pb: e.copy(fT[pb][:], ptb[pb][:]), reads=[('ptb', pb)], writes=[('fT', pb)])
                    dst = D['qT_g'] if c < 3 else D['kT_g']
                    cc = c if c < 3 else c - 3
                    P.dma('sp', dst[cc * 128:(cc + 1) * 128, t4 * 512:(t4 + 1) * 512], fT[pb][:], reads=[('fT', pb)], writes=['d_qkTg'])

            stA(0)
            stA(1)
            for t4 in range(8):
                if t4 + 2 < 8:
                    stA(t4 + 2)
                stB(t4)
        P.emit()


def phase_D2(P, l, D):
    import os
    C = 64
    NCH = S // C
    NST = int(os.environ.get('D2N', str(NCH)))
    MD = BF16 if os.environ.get('D2BF', '1') == '1' else F32
    with ExitStack() as s:
        def T12(name, dt=F32):
            return P.sb(s, 'e_' + name, [64, 12, 64], dt)
        ones = P.sb(s, 'e_ones', [64, 64], F32)
        idf = P.sb(s, 'e_idf', [64, 64], F32)
        idm = P.sb(s, 'e_idm', [64, 64], MD)
        idbc = T12('idbc')
        triF = P.sb(s, 'e_triF', [64, 64], F32)
        triB = P.sb(s, 'e_triB', [64, 64], F32)
        mW, mWt, mI = T12('mW'), T12('mWt'), T12('mI')
        St, St2, Sm = T12('S'), T12('S2'), T12('Sm', MD)
        ktm = [T12('ktm%d' % i) for i in range(2)]
        vtm = [T12('vtm%d' % i) for i in range(2)]
        kT = [T12('kT%d' % i, MD) for i in range(2)]
        qT = [T12('qT%d' % i, MD) for i in range(2)]
        gbv = [P.sb(s, 'e_gb%d' % i, [64, 24], F32) for i in range(2)]
        gc = P.sb(s, 'e_gc', [64, 12], F32)
        egc = [P.sb(s, 'e_egc%d' % i, [64, 12], F32) for i in range(2)]
        egl = [P.sb(s, 'e_egl%d' % i, [64, 12], F32) for i in range(2)]
        egd = P.sb(s, 'e_egd', [64, 12], F32)
        Dg = P.sb(s, 'e_Dg', [64, 24, 64], F32)
        diff, Ea, Eb = T12('diff'), T12('Ea'), T12('Eb')
        W, Wt = T12('W', MD), T12('Wt', MD)
        A1, A1t, A2, A2t = T12('A1', MD), T12('A1t', MD), T12('A2', MD), T12('A2t', MD)
        nxTI = T12('nxTI', MD)
        Yt = [T12('Yt0', MD), T12('Yt1', MD)]
        Yf = [T12('Yf0', MD), T12('Yf1', MD)]
        QKm = [T12('QKm0', MD), T12('QKm1', MD)]
        kd = [T12('kd0', MD), T12('kd1', MD)]
        Rr, Rm, vnew, o1 = T12('R'), T12('Rm', MD), T12('vnew', MD), T12('o1')
        ob = [T12('ob0'), T12('ob1')]
        pA = P.ps(s, 'e_pA', [64, 1024])
        pB = P.ps(s, 'e_pB', [64, 1024])
        pC = P.ps(s, 'e_pC', [64, 1024])
        pS = P.ps(s, 'e_pS', [64, 1024])

        def pv(p, h):
            return p[:, h * 512:h * 512 + 384].rearrange('p (j t) -> p j t', t=64)

        def sv(t, h):
            return t[:, h * 6:(h + 1) * 6, :]

        def pcol(p, j):
            c0 = (j // 6) * 512 + (j % 6) * 64
            return p[:, c0:c0 + 64]

        def mm12(pt, pn, lfn, rfn, rfun):
            for j in range(12):
                o_, l_, r_ = pcol(pt, j), lfn(j), rfn(j)
                P.op('pe', lambda e, o_=o_, l_=l_, r_=r_: e.matmul(o_, l_, r_, start=True, stop=True), reads=rfun(j // 6), writes=[(pn, j // 6)])

        def bcol(ap12, h):
            return bc(ap12[:, h * 6:(h + 1) * 6].unsqueeze(2), [64, 6, 64])

        P.dma('sp', ones[:], D['ones64'], writes=['ones'])
        P.dma('sp', idf[:], D['ident'][0:64, 0:64], writes=['idf'])
        P.dma('sp', triF[:], D['triF'], writes=['triF'])
        P.dma('sp', triB[:], D['triB'], writes=['triB'])
        P.dma('sp', mW[:], D['mW'], writes=['mW'])
        P.dma('sp', mWt[:], D['mWt'], writes=['mWt'])
        P.dma('sp', mI[:], D['mI'], writes=['mI'])
        P.op('dve', lambda e: e.tensor_copy(idbc[:], bc(idf[:].unsqueeze(1), [64, 12, 64])), reads=['idf'], writes=['idbc'])
        P.op('dve', lambda e: e.tensor_copy(idm[:], idf[:]), reads=['idf'], writes=['idm'])
        P.op('dve', lambda e: e.memset(St[:].rearrange('p a b -> p (a b)'), 0.0), writes=[('S', 0), ('S', 1)])
        P.op('pool', lambda e: e.memset(Sm[:].rearrange('p a b -> p (a b)'), 0.0), writes=[('Sm', 0), ('Sm', 1)])

        def prep(i):
            b = i % 2
            cf = i
            cb = NCH - 1 - i
            K_, V_, KT_, QT_, GB_ = ktm[b], vtm[b], kT[b], qT[b], gbv[b]
            EGC, EGL, QKM, KD = egc[b], egl[b], QKm[b], kd[b]
            for h, cc in ((0, cf), (1, cb)):
                sl = slice(h * 6, h * 6 + 6)
                tk = slice(cc * C, (cc + 1) * C)
                P.dma('sp', K_[:, sl, :], D['k_tm'][tk, :].rearrange('t (h d) -> t h d', d=64), reads=['d_ktm'], writes=[('ktm', b, h)])
                P.dma('sp', V_[:, sl, :], D['v_tm'][tk, :].rearrange('t (h d) -> t h d', d=64), reads=['d_vtm'], writes=[('vtm', b, h)])
                P.dma('sp', KT_[:, sl, :], D['kT_g'][:, tk].rearrange('(h d) t -> d h t', d=64), reads=['d_qkTg'], writes=[('kT', b, h)])
                P.dma('sp', QT_[:, sl, :], D['qT_g'][:, tk].rearrange('(h d) t -> d h t', d=64), reads=['d_qkTg'], writes=[('qT', b, h)])
                P.dma('sp', GB_[:, h * 6:h * 6 + 6], D['gb'][tk, h * 6:h * 6 + 6], reads=['d_gb'], writes=[('gb', b, h)])
                P.dma('sp', GB_[:, 12 + h * 6:12 + h * 6 + 6], D['gb'][tk, 12 + h * 6:12 + h * 6 + 6], reads=['d_gb'], writes=[('gbb', b, h)])
            beta = GB_[:, 12:24]
            DgF = Dg[:].rearrange('p a b -> p (a b)')
            for h in range(2):
                tri = triF if h == 0 else triB
                trin = 'triF' if h == 0 else 'triB'
                rG, rBt = ('gb', b, h), ('gbb', b, h)
                P.op('pe', lambda e, h=h, tri=tri, GB_=GB_: e.matmul(pA[:, h * 512:h * 512 + 6], tri[:], GB_[:, h * 6:h * 6 + 6], start=True, stop=True), reads=[trin, rG], writes=[('pA', h)])
                P.op('dve', lambda e, h=h: e.tensor_copy(gc[:, h * 6:h * 6 + 6], pA[:, h * 512:h * 512 + 6]), reads=[('pA', h)], writes=[('gc', h)])
                P.op('act', lambda e, h=h, EGC=EGC: e.activation(EGC[:, h * 6:h * 6 + 6], pA[:, h * 512:h * 512 + 6], AF.Exp), reads=[('pA', h)], writes=[('egc', b, h)])
                P.op('dve', lambda e, h=h: e.tensor_tensor(Dg[:, h * 6:h * 6 + 6, :], sv(idbc, 0), bcol(gc, h), ALU.mult), reads=['idbc', ('gc', h)], writes=[('Dg', h)])
                P.op('pool', lambda e, h=h, beta=beta: e.tensor_tensor(Dg[:, 12 + h * 6:12 + h * 6 + 6, :], sv(idbc, 0), bcol(beta, h), ALU.mult), reads=['idbc', rBt], writes=[('Dgb', h)])
                P.op('pe', lambda e, h=h: e.matmul(pB[:, h * 512:h * 512 + 384], ones[:], DgF[:, h * 384:(h + 1) * 384], start=True, stop=True), reads=['ones', ('Dg', h)], writes=[('pB', h)])
                P.op('pe', lambda e, h=h: e.matmul(pC[:, h * 512:h * 512 + 384], ones[:], DgF[:, 768 + h * 384:768 + (h + 1) * 384], start=True, stop=True), reads=['ones', ('Dgb', h)], writes=[('pC', h)])
                P.op('dve', lambda e, h=h: e.tensor_tensor(sv(diff, h), pv(pB, h), bcol(gc, h), ALU.subtract), reads=[('pB', h), ('gc', h)], writes=[('diff', h)])
                lc = h * 512 + (63 if h == 0 else 0)
                lastv = pB[:, lc:lc + 64 * 5 + 1:64]
                P.op('act', lambda e, h=h, lastv=lastv, EGL=EGL: e.activation(EGL[:, h * 6:h * 6 + 6], lastv, AF.Exp), reads=[('pB', h)], writes=[('egl', b, h)])
                P.op('dve', lambda e, h=h, lastv=lastv: e.tensor_tensor(egd[:, h * 6:h * 6 + 6], lastv, gc[:, h * 6:h * 6 + 6], ALU.subtract), reads=[('pB', h), ('gc', h)], writes=[('egd', h)])
                P.op('act', lambda e, h=h: e.activation(egd[:, h * 6:h * 6 + 6], egd[:, h * 6:h * 6 + 6], AF.Exp), reads=[('egd', h)], writes=[('egd', h)])
                P.op('pool', lambda e, h=h, K_=K_, KD=KD: e.tensor_tensor(sv(KD, h), sv(K_, h), bcol(egd, h), ALU.mult), reads=[('ktm', b, h), ('egd', h)], writes=[('kd', b, h)])
                P.op('act', lambda e, h=h: e.activation(sv(Ea, h), sv(diff, h), AF.Exp), reads=[('diff', h)], writes=[('Ea', h)])
                P.op('act', lambda e, h=h: e.activation(sv(Eb, h), sv(diff, h), AF.Exp, scale=-1.0), reads=[('diff', h)], writes=[('Eb', h)])
            yield
            mm12(pA, 'pA', lambda j: KT_[:, j, :], lambda j: KT_[:, j, :], lambda h: [('kT', b, h)])
            for h in range(2):
                rBt = ('gbb', b, h)
                P.op('dve', lambda e, h=h: e.scalar_tensor_tensor(sv(Eb, h), sv(Eb, h), 1.0, sv(mWt, h), ALU.min, ALU.mult), reads=[('Eb', h), 'mWt'], writes=[('Eb', h)])
                P.op('dve', lambda e, h=h: e.tensor_tensor(sv(Eb, h), sv(Eb, h), pv(pC, h), ALU.mult), reads=[('Eb', h), ('pC', h)], writes=[('Eb', h)])
                P.op('dve', lambda e, h=h: e.tensor_tensor(sv(Wt, h), sv(Eb, h), pv(pA, h), ALU.mult), reads=[('Eb', h), ('pA', h)], writes=[('Wt', h)])
            yield
            mm12(pC, 'pC', lambda j: KT_[:, j, :], lambda j: QT_[:, j, :], lambda h: [('kT', b, h), ('qT', b, h)])
            for h in range(2):
                rBt = ('gbb', b, h)
                P.op('dve', lambda e, h=h: e.scalar_tensor_tensor(sv(diff, h), sv(Ea, h), 1.0, sv(mI, h), ALU.min, ALU.mult), reads=[('Ea', h), 'mI'], writes=[('diff', h)])
                P.op('dve', lambda e, h=h, QKM=QKM: e.tensor_tensor(sv(QKM, h), sv(diff, h), pv(pC, h), ALU.mult), reads=[('diff', h), ('pC', h)], writes=[('QKm', b, h)])
                P.op('dve', lambda e, h=h: e.scalar_tensor_tensor(sv(Ea, h), sv(Ea, h), 1.0, sv(mW, h), ALU.min, ALU.mult), reads=[('Ea', h), 'mW', ('diff', h)], writes=[('Ea', h)])
                P.op('pool', lambda e, h=h, beta=beta: e.tensor_tensor(sv(Ea, h), sv(Ea, h), bcol(beta, h), ALU.mult), reads=[('Ea', h), rBt], writes=[('Ea', h)])
                P.op('dve', lambda e, h=h: e.tensor_tensor(sv(W, h), sv(Ea, h), pv(pA, h), ALU.mult), reads=[('Ea', h), ('pA', h)], writes=[('W', h)])
                P.op('pool', lambda e, h=h: e.tensor_tensor(sv(Yt[0], h), sv(idbc, h), sv(W, h), ALU.subtract), reads=['idbc', ('W', h)], writes=[('Yt0', h)])
            yield
            cur, curT, cn, cnT = W, Wt, 'W', 'Wt'
            yi = 0
            bufs = [(A1, A1t, 'A1', 'A1t'), (A2, A2t, 'A2', 'A2t')]
            for lev in range(5):
                nx, nxT, nn, nnT = bufs[lev % 2]
                mm12(pB, 'pB', lambda j, cur=cur: cur[:, j, :], lambda j, curT=curT: curT[:, j, :], lambda h, cn=cn, cnT=cnT: [(cn, h), (cnT, h)])
                for h in range(2):
                    if lev < 4:
                        P.op('act', lambda e, h=h, nxT=nxT: e.copy(sv(nxT, h), pv(pB, h)), reads=[('pB', h)], writes=[(nnT, h)])
                    P.op('dve', lambda e, h=h: e.tensor_tensor(sv(nxTI, h), pv(pB, h), sv(idbc, h), ALU.add), reads=[('pB', h), 'idbc'], writes=[('nxTI', h)])
                if lev < 4:
                    mm12(pC, 'pC', lambda j, curT=curT: curT[:, j, :], lambda j, cur=cur: cur[:, j, :], lambda h, cn=cn, cnT=cnT: [(cn, h), (cnT, h)])
                    for h in range(2):
                        P.op('dve', lambda e, h=h, nx=nx: e.tensor_copy(sv(nx, h), pv(pC, h)), reads=[('pC', h)], writes=[(nn, h)])
                Yc = Yt[yi]
                yc_ = 'Yt%d' % yi
                if lev < 4:
                    Yn, yn_ = Yt[1 - yi], ('Yt%d' % (1 - yi),)
                else:
                    Yn, yn_ = Yf[b], ('Yf', b)
                for j in range(12):
                    h = j // 6
                    P.op('pe', lambda e, j=j, Yc=Yc: e.matmul(pcol(pA, j), nxTI[:, j, :], Yc[:, j, :], start=True, stop=True), reads=[('nxTI', h), (yc_, h)], writes=[('pA', h)])
                for h in range(2):
                    P.op('dve', lambda e, h=h, Yn=Yn: e.tensor_copy(sv(Yn, h), pv(pA, h)), reads=[('pA', h)], writes=[yn_ + (h,)])
                yi = 1 - yi
                cur, curT, cn, cnT = nx, nxT, nn, nnT
                yield

        def scan(i):
            b = i % 2
            cf = i
            cb = NCH - 1 - i
            V_, KT_, QT_, GB_ = vtm[b], kT[b], qT[b], gbv[b]
            EGC, EGL, QKM, KD, YF = egc[b], egl[b], QKm[b], kd[b], Yf[b]
            beta = GB_[:, 12:24]
            mm12(pS, 'pS', lambda j: KT_[:, j, :], lambda j: Sm[:, j, :], lambda h: [('kT', b, h), ('Sm', h)])
            for h in range(2):
                P.op('dve', lambda e, h=h, EGC=EGC: e.tensor_tensor(sv(Rr, h), pv(pS, h), bcol(EGC, h), ALU.mult), reads=[('pS', h), ('egc', b, h)], writes=[('R', h)])
                P.op('dve', lambda e, h=h, V_=V_: e.tensor_tensor(sv(Rm, h), sv(V_, h), sv(Rr, h), ALU.subtract), reads=[('R', h), ('vtm', b, h)], writes=[('Rm', h)])
            yield
            mm12(pS, 'pS', lambda j: QT_[:, j, :], lambda j: Sm[:, j, :], lambda h: [('qT', b, h), ('Sm', h)])
            for h in range(2):
                P.op('act', lambda e, h=h: e.copy(sv(o1, h), pv(pS, h)), reads=[('pS', h)], writes=[('o1', h)])
                P.op('pool', lambda e, h=h, EGC=EGC: e.tensor_tensor(sv(o1, h), sv(o1, h), bcol(EGC, h), ALU.mult), reads=[('o1', h), ('egc', b, h)], writes=[('o1', h)])
            yield
            yield
            mm12(pS, 'pS', lambda j: YF[:, j, :], lambda j: Rm[:, j, :], lambda h: [('Yf', b, h), ('Rm', h)])
            for h in range(2):
                P.op('dve', lambda e, h=h, beta=beta: e.tensor_tensor(sv(vnew, h), pv(pS, h), bcol(beta, h), ALU.mult), reads=[('pS', h), ('gbb', b, h)], writes=[('vnew', h)])
            yield
            mm12(pS, 'pS', lambda j: KD[:, j, :], lambda j: vnew[:, j, :], lambda h: [('kd', b, h), ('vnew', h)])
            OB = ob[b]
            for h in range(2):
                P.op('pool', lambda e, h=h, EGL=EGL: e.tensor_tensor(sv(St2, h), sv(St, h), bcol(EGL, h), ALU.mult), reads=[('S', h), ('egl', b, h)], writes=[('S2', h)])
                P.op('dve', lambda e, h=h: e.tensor_tensor(sv(St, h), sv(St2, h), pv(pS, h), ALU.add), reads=[('S2', h), ('pS', h)], writes=[('S', h)])
                P.op('act', lambda e, h=h: e.copy(sv(Sm, h), sv(St, h)), reads=[('S', h)], writes=[('Sm', h)])
            yield
            mm12(pS, 'pS', lambda j: QKM[:, j, :], lambda j: vnew[:, j, :], lambda h: [('QKm', b, h), ('vnew', h)])
            for h in range(2):
                cc = cf if h == 0 else cb
                P.op('dve', lambda e, h=h, OB=OB: e.tensor_tensor(sv(OB, h), sv(o1, h), pv(pS, h), ALU.add), reads=[('o1', h), ('pS', h)], writes=[('ob', b, h)])
                P.dma('sp', D['o_fb'][h, cc * C:(cc + 1) * C, :].rearrange('t (h d) -> t h d', d=64), sv(OB, h), reads=[('ob', b, h)], writes=['d_ofb'])

        def run(gens):
            gens = [g for g in gens if g is not None]
            while gens:
                for g in list(gens):
                    try:
                        next(g)
                    except StopIteration:
                        gens.remove(g)

        run([prep(0)])
        for i in range(NST):
            run([prep(i + 1) if i + 1 < NST else None, scan(i)])
        P.emit()


def phase_E(P, l, D, first):
    xsrc = D['x'] if first else D['xw']
    with ExitStack() as s:
        wo = P.sb(s, 'f_wo', [128, 8, 1024], BF16)
        ong = P.sb(s, 'f_ong', [128, 64], F32)
        idb = P.sb(s, 'f_idb', [128, 128], BF16)
        of_ = [P.sb(s, 'f_of%d' % i, [128, 2, 384], F32) for i in range(3)]
        gt = [P.sb(s, 'f_gt%d' % i, [128, 384], F32) for i in range(3)]
        o = P.sb(s, 'f_o', [128, 6, 64], F32)
        sq = P.sb(s, 'f_sq', [128, 6, 64], F32)
        r6 = P.sb(s, 'f_r6', [128, 6], F32)
        ycb = P.sb(s, 'f_ycb', [128, 384], BF16)
        yT = [P.sb(s, 'f_yT%d' % i, [128, 8, 128], BF16) for i in range(3)]
        xt = [P.sb(s, 'f_xt%d' % i, [128, 1024], F32) for i in range(3)]
        pT = P.ps(s, 'f_pT', [128, 512], BF16)
        po = [P.ps(s, 'f_po%d' % i, [128, 512]) for i in range(2)]
        for k in range(8):
            P.dma('pool', wo[:, k, :], D['w_out'][l, k * 128:(k + 1) * 128, :], writes=[('wo', k)])
        P.dma('sp', ong[:], D['o_norm_g'][l].partition_broadcast(128), writes=['ong'])
        P.dma('pool', idb[:], D['ident'], writes=['idb'])
        def front(t):
            b = t % 3
            tk = slice(t * 128, (t + 1) * 128)
            P.dma('sp', of_[b][:], D['o_fb'][:, tk, :].rearrange('a t c -> t a c'), reads=['d_ofb'], writes=[('of', b)])
            P.dma('sp', gt[b][:], D['gate_s'][tk, :], reads=['d_gate'], writes=[('gt', b)])
            P.dma('sp', xt[b][:], xsrc[tk, :], writes=[('xt', b)])
            P.dma('sp', yT[b][:, 0:5, :], D['yT'][0:640, tk].rearrange('(k p) t -> p k t', p=128), reads=['d_yT'], writes=[('yT', b, 0)])
            OF = o[:].rearrange('p a b -> p (a b)')
            P.op('dve', lambda e, b=b: e.tensor_tensor(OF, of_[b][:, 0, :], of_[b][:, 1, :], ALU.add), reads=[('of', b)], writes=['o'])
            P.op('act', lambda e: e.activation(sq[:], o[:], AF.Square), reads=['o'], writes=['sq'])
            P.op('dve', lambda e: e.tensor_reduce(r6[:], sq[:], AX.X, ALU.add), reads=['sq'], writes=['r6'])
            P.op('act', lambda e: e.activation(r6[:], r6[:], AF.Sqrt, bias=EPS, scale=1.0 / 64), reads=['r6'], writes=['r6'])
            P.op('dve', lambda e: e.reciprocal(r6[:], r6[:]), reads=['r6'], writes=['r6'])
            P.op('dve', lambda e: e.tensor_tensor(o[:], o[:], bc(r6[:].unsqueeze(2), [128, 6, 64]), ALU.mult), reads=['o', 'r6'], writes=['o'])
            P.op('pool', lambda e: e.tensor_tensor(o[:], o[:], bc(ong[:].unsqueeze(1), [128, 6, 64]), ALU.mult), reads=['o', 'ong'], writes=['o'])
            P.op('pool', lambda e, b=b: e.tensor_tensor(ycb[:], OF, gt[b][:], ALU.mult), reads=['o', ('gt', b)], writes=['ycb'])
            for k in range(3):
                P.op('pe', lambda e, k=k: e.transpose(pT[:, k * 128:(k + 1) * 128], ycb[:, k * 128:(k + 1) * 128], idb[:]), reads=['ycb', 'idb'], writes=['pT'])
            P.op('act', lambda e, b=b: e.copy(yT[b][:, 5:8, :], pT[:, 0:384].rearrange('p (k t) -> p k t', t=128)), reads=['pT'], writes=[('yT', b, 1)])
        def back(t):
            b = t % 3
            tk = slice(t * 128, (t + 1) * 128)
            for hf in range(2):
                for k in range(8):
                    P.op('pe', lambda e, hf=hf, k=k, b=b: e.matmul(po[hf][:], yT[b][:, k, :], wo[:, k, hf * 512:(hf + 1) * 512], start=(k == 0), stop=(k == 7)),
                         reads=[('yT', b, 0), ('yT', b, 1), ('wo', k)], writes=[('po', hf)])
                P.op('dve', lambda e, hf=hf, b=b: e.tensor_tensor(xt[b][:, hf * 512:(hf + 1) * 512], xt[b][:, hf * 512:(hf + 1) * 512], po[hf][:], ALU.add),
                     reads=[('po', hf), ('xt', b)], writes=[('xt', b)])
            P.dma('sp', D['xw'][tk, :], xt[b][:], reads=[('xt', b)], writes=[('d_xw', t)])

        front(0)
        front(1)
        for t in range(NT):
            if t + 2 < NT:
                front(t + 2)
            back(t)
        P.emit()


def phase_F(P, l, D):
    import os
    NE = int(os.environ.get('FNE', '16'))
    with ExitStack() as s:
        gff = P.sb(s, 'g_gff', [128, 1024], F32)
        idf = P.sb(s, 'g_idf', [128, 128], F32)
        idb = P.sb(s, 'g_idb', [128, 128], BF16)
        wr = P.sb(s, 'g_wr', [128, 8, 16], F32)
        aff = P.sb(s, 'g_aff', [128, NT, 16], F32)
        sel = P.sb(s, 'g_sel', [128, NT, 16], F32)
        rank = P.sb(s, 'g_rank', [128, NT, 16], F32)
        cA = P.sb(s, 'g_cA', [128, NT, 16], F32)
        cB = P.sb(s, 'g_cB', [128, NT, 16], F32)
        selb = P.sb(s, 'g_selb', [128, NT * 16], BF16)
        triS = P.sb(s, 'g_triS', [128, 128], BF16)
        onesb = P.sb(s, 'g_onesb', [128, 128], BF16)
        tg = P.sb(s, 'g_tg', [128, NT, 16, 5], BF16)
        tp = P.sb(s, 'g_tp', [128, NT, 2], F32)
        iota = P.sb(s, 'g_iota', [128, 512], F32)
        Selt = [P.sb(s, 'g_Selt%d' % i, [128, 512], BF16) for i in range(2)]
        idxf = P.sb(s, 'g_idxf', [128, 4, 8], F32)
        row5 = P.sb(s, 'g_row5', [5, 512], F32)
        idxv = P.sb(s, 'g_idxv', [128, 4], F32)
        idxi = [P.sb(s, 'g_idxi%d' % i, [128, 4], I32) for i in range(2)]
        gate = [P.sb(s, 'g_gate%d' % i, [128, 4], F32) for i in range(2)]
        affT2 = P.sb(s, 'g_affT2', [16, S], F32)
        bs = P.sb(s, 'g_bs', [16, 8], F32)
        ones16 = P.sb(s, 'g_ones16', [16, 128], F32)
        dthr = P.sb(s, 'g_dthr', [16, 16], F32)
        thrb = P.sb(s, 'g_thrb', [128, 16], F32)
        xt = P.sb(s, 'g_xt', [128, 1024], F32)
        junk = P.sb(s, 'g_junk', [128, 1024], BF16)
        ss = P.sb(s, 'g_ss', [128, 4], F32)
        h32s = [P.sb(s, 'g_h32_%d' % i, [128, 1024], F32) for i in range(2)]
        hb16 = P.sb(s, 'g_hb16', [128, 1024], BF16)
        hT32 = P.sb(s, 'g_hT32', [128, 8, 128], F32)
        sm = P.sb(s, 'g_sm', [128, 4], F32)
        ex = P.sb(s, 'g_ex', [128, 16], F32)
        wg = [P.sb(s, 'g_wg%d' % i, [128, 8, 1024], BF16) for i in range(2)]
        wu = [P.sb(s, 'g_wu%d' % i, [128, 8, 1024], BF16) for i in range(2)]
        wd = [P.sb(s, 'g_wd%d' % i, [128, 8, 1024], BF16) for i in range(2)]
        xe = P.sb(s, 'g_xe', [128, 4, 1024], BF16)
        bjv = xe[0:16, :, :].rearrange('p g c -> p (g c)')
        xeTs = [P.sb(s, 'g_xeT%d' % i, [128, 8, 512], BF16) for i in range(2)]
        hid = P.sb(s, 'g_hid', [128, 8, 512], BF16)
        sg = [P.sb(s, 'g_sg%d' % i, [128, 512], BF16) for i in range(2)]
        ye = [P.sb(s, 'g_ye%d' % i, [128, 1024], F32) for i in range(2)]
        pbig = P.ps(s, 'g_pbig', [128, 1024])
        pl = P.ps(s, 'g_pl', [128, 512])
        pT = P.ps(s, 'g_pT', [128, 1024], BF16)
        pg_ = P.ps(s, 'g_pg', [128, 512])
        pu_ = P.ps(s, 'g_pu', [128, 512])
        py = [P.ps(s, 'g_py%d' % i, [128, 512]) for i in range(2)]

        def load_w(ex_):
            eb = ex_ % 2
            for k in range(8):
                P.dma('pool', wg[eb][:, k, :], D['w_e_gate'][l, ex_, k * 128:(k + 1) * 128, :], writes=[('wg', eb, k)])
                P.dma('pool', wu[eb][:, k, :], D['w_e_up'][l, ex_, k * 128:(k + 1) * 128, :], writes=[('wu', eb, k)])
            for k in range(8):
                P.dma('pool', wd[eb][:, k, :], D['w_e_down'][l, ex_, k * 128:(k + 1) * 128, :], writes=[('wd', eb, k)])

        P.dma('sp', gff[:], D['g_ffn'][l].partition_broadcast(128), writes=['gff'])
        P.dma('sp', idf[:], D['ident'], writes=['idf'])
        P.dma('pool', idb[:], D['ident'], writes=['idb'])
        P.dma('pool', triS[:], D['triS'], writes=['triS'])
        P.dma('pool', onesb[:], D['ones128'], writes=['onesb'])
        P.dma('sp', wr[:], D['w_router'][l].rearrange('(k p) e -> p k e', p=128), writes=['wr'])
        P.dma('sp', ones16[:], D['ones128'][0:16, :], writes=['ones16'])
        P.dma('sp', tp[:], D['tp'], writes=['tp'])
        P.dma('sp', iota[:], D['iota512'], writes=['iota'])
        load_w(0)
        def frontF(t):
            tk = slice(t * 128, (t + 1) * 128)
            h32 = h32s[t % 2]
            kh = ('h32', t % 2)
            P.dma('sp', xt[:], D['xw'][tk, :], reads=['d_xw'], writes=['xt'])
            P.op('dve', lambda e: e.memset(ss[:, 0:1], 0.0), writes=['ss'])
            P.op('act', lambda e: e.activation(junk[:], xt[:], AF.Square, accum_out=ss[:, 0:1]), reads=['xt', 'ss'], writes=['junk', 'ss'])
            P.op('act', lambda e: e.activation(ss[:, 0:1], ss[:, 0:1], AF.Sqrt, bias=EPS, scale=1.0 / 1024), reads=['ss'], writes=['ss'])
            P.op('dve', lambda e: e.reciprocal(ss[:, 0:1], ss[:, 0:1]), reads=['ss'], writes=['ss'])
            P.op('dve', lambda e, h32=h32: e.scalar_tensor_tensor(h32[:], xt[:], ss[:, 0:1], gff[:], ALU.mult, ALU.mult), reads=['xt', 'ss', 'gff'], writes=[kh])
            P.op('act', lambda e, h32=h32: e.copy(hb16[:], h32[:]), reads=[kh], writes=['hb16'])
            P.dma('sp', D['hb'][tk, :], hb16[:], reads=['hb16'], writes=['d_hb'])

        def backF(t):
            h32 = h32s[t % 2]
            kh = ('h32', t % 2)
            for k in range(8):
                P.op('pe', lambda e, k=k, h32=h32: e.transpose(pbig[:, k * 128:(k + 1) * 128], h32[:, k * 128:(k + 1) * 128], idf[:]), reads=[kh, 'idf'], writes=[('pbig', k // 4)])
            P.op('act', lambda e: e.copy(hT32[:, 0:4, :], pbig[:, 0:512].rearrange('p (k t) -> p k t', t=128)), reads=[('pbig', 0)], writes=['hT32a'])
            P.op('dve', lambda e: e.tensor_copy(hT32[:, 4:8, :], pbig[:, 512:1024].rearrange('p (k t) -> p k t', t=128)), reads=[('pbig', 1)], writes=['hT32b'])
            for k in range(8):
                P.op('pe', lambda e, k=k: e.matmul(pl[:, 0:16], hT32[:, k, :], wr[:, k, :], start=(k == 0), stop=(k == 7)), reads=['hT32a', 'hT32b', 'wr'], writes=['pl'])
            P.op('dve', lambda e: e.tensor_reduce(sm[:, 0:1], pl[:, 0:16], AX.X, ALU.max), reads=['pl'], writes=['sm'])
            P.op('dve', lambda e: e.tensor_scalar(sm[:, 1:2], sm[:, 0:1], -1.0, None, ALU.mult), reads=['sm'], writes=['sm'])
            P.op('dve', lambda e: e.memset(sm[:, 2:3], 0.0), reads=['sm'], writes=['sm'])
            P.op('act', lambda e: e.activation(ex[:], pl[:, 0:16], AF.Exp, bias=sm[:, 1:2], accum_out=sm[:, 2:3]), reads=['pl', 'sm'], writes=['ex', 'sm'])
            P.op('dve', lambda e: e.reciprocal(sm[:, 3:4], sm[:, 2:3]), reads=['sm'], writes=['sm'])
            P.op('dve', lambda e, t=t: e.tensor_scalar(aff[:, t, :], ex[:], sm[:, 3:4], None, ALU.mult), reads=['ex', 'sm'], writes=[('aff', t)])
            P.op('pe', lambda e, t=t: e.transpose(pl[0:16, 128:256], aff[:, t, :], idf[:]), reads=[('aff', t), 'idf'], writes=['pl'])
            P.op('act', lambda e, t=t: e.mul(affT2[:, t * 128:(t + 1) * 128], pl[0:16, 128:256], 2.0), reads=['pl'], writes=['affT2'])

        frontF(0)
        for t in range(NT):
            if t + 1 < NT:
                frontF(t + 1)
            backF(t)
        lo, hi, half, mid2, cnt, gef, tt = (bs[:, i:i + 1] for i in range(7))
        P.op('dve', lambda e: e.memset(bs[:], 0.0), writes=['bs'])
        P.op('dve', lambda e: e.memset(hi, 1.0), reads=['bs'], writes=['bs'])
        for itn in range(27):
            P.op('dve', lambda e: e.tensor_tensor(mid2, lo, hi, ALU.add), reads=['bs'], writes=['bs'])
            P.op('dve', lambda e: e.tensor_scalar(half, mid2, 0.5, None, ALU.mult), reads=['bs'], writes=['bs'])
            P.op('dve', lambda e: e.memset(cnt, 0.0), reads=['bs'], writes=['bs'])
            P.op('dve', lambda e: e.tensor_scalar(bjv, affT2[:], mid2, 0.0, ALU.is_ge, ALU.add, accum_out=cnt), reads=['affT2', 'bs'], writes=['bj', 'bs'])
            P.op('dve', lambda e: e.tensor_scalar(gef, cnt, 511.5, None, ALU.is_ge), reads=['bs'], writes=['bs'])
            P.op('dve', lambda e: e.tensor_tensor(tt, half, lo, ALU.subtract), reads=['bs'], writes=['bs'])
            P.op('dve', lambda e: e.tensor_tensor(tt, tt, gef, ALU.mult), reads=['bs'], writes=['bs'])
            P.op('dve', lambda e: e.tensor_tensor(lo, lo, tt, ALU.add), reads=['bs'], writes=['bs'])
            P.op('dve', lambda e: e.tensor_tensor(tt, hi, half, ALU.subtract), reads=['bs'], writes=['bs'])
            P.op('dve', lambda e: e.tensor_tensor(tt, tt, gef, ALU.mult), reads=['bs'], writes=['bs'])
            P.op('dve', lambda e: e.tensor_tensor(hi, half, tt, ALU.add), reads=['bs'], writes=['bs'])
        P.op('dve', lambda e: e.tensor_scalar(dthr[:], idf[0:16, 0:16], lo, None, ALU.mult), reads=['idf', 'bs'], writes=['dthr'])
        P.op('pe', lambda e: e.matmul(pl[:, 256:272], ones16[:], dthr[:], start=True, stop=True), reads=['ones16', 'dthr'], writes=['pl'])
        P.op('dve', lambda e: e.tensor_copy(thrb[:], pl[:, 256:272]), reads=['pl'], writes=['thrb'])
        AFF = [('aff', t) for t in range(NT)]
        P.op('dve', lambda e: e.tensor_tensor(sel[:], aff[:], bc(thrb[:].unsqueeze(1), [128, NT, 16]), ALU.is_ge), reads=AFF + ['thrb'], writes=['sel'])
        P.op('dve', lambda e: e.tensor_copy(selb[:], sel[:].rearrange('p t e -> p (t e)')), reads=['sel'], writes=['selb'])
        P.op('pe', lambda e: e.matmul(pg_[:], triS[:], selb[:], start=True, stop=True), reads=['triS', 'selb'], writes=['pg'])
        P.op('pe', lambda e: e.matmul(pu_[:], onesb[:], selb[:], start=True, stop=True), reads=['onesb', 'selb'], writes=['pu'])
        P.op('dve', lambda e: e.tensor_copy(cA[:].rearrange('p t e -> p (t e)'), pu_[:]), reads=['pu'], writes=['cA'])
        src, dst, sn, dn = cA, cB, 'cA', 'cB'
        for sft in (1, 2, 4, 8, 16):
            P.op('pool', lambda e, src=src, dst=dst, sft=sft: e.tensor_copy(dst[:, 0:sft, :], src[:, 0:sft, :]), reads=[sn], writes=[dn])
            P.op('dve', lambda e, src=src, dst=dst, sft=sft: e.tensor_tensor(dst[:, sft:NT, :], src[:, sft:NT, :], src[:, 0:NT - sft, :], ALU.add), reads=[sn], writes=[dn])
            src, dst, sn, dn = dst, src, dn, sn
        P.op('dve', lambda e, src=src: e.tensor_tensor(rank[:].rearrange('p t e -> p (t e)'), src[:].rearrange('p t e -> p (t e)'), pu_[:], ALU.subtract), reads=[sn, 'pu'], writes=['rank'])
        P.op('dve', lambda e: e.tensor_tensor(rank[:].rearrange('p t e -> p (t e)'), rank[:].rearrange('p t e -> p (t e)'), pg_[:], ALU.add), reads=['rank', 'pg'], writes=['rank'])
        P.op('dve', lambda e: e.scalar_tensor_tensor(rank[:], rank[:], 1.0, sel[:], ALU.add, ALU.mult), reads=['rank', 'sel'], writes=['rank'])
        P.op('dve', lambda e: e.tensor_scalar(rank[:], rank[:], -1.0, None, ALU.add), reads=['rank'], writes=['rank'])
        P.op('dve', lambda e: e.tensor_copy(tg[:, :, :, 0:2], bc(tp[:].unsqueeze(2), [128, NT, 16, 2])), reads=['tp'], writes=['tg0'])
        P.op('dve', lambda e: e.tensor_copy(tg[:, :, :, 2], aff[:]), reads=AFF, writes=['tg1'])
        P.op('dve', lambda e: e.tensor_tensor(cA[:], aff[:], tg[:, :, :, 2], ALU.subtract), reads=AFF + ['tg1', 'cA', 'cB'], writes=['cA'])
        P.op('dve', lambda e: e.tensor_copy(tg[:, :, :, 3], cA[:]), reads=['cA'], writes=['tg2'])
        P.op('dve', lambda e: e.tensor_tensor(cB[:], cA[:], tg[:, :, :, 3], ALU.subtract), reads=['cA', 'tg2', 'cB'], writes=['cB'])
        P.op('dve', lambda e: e.tensor_copy(tg[:, :, :, 4], cB[:]), reads=['cB'], writes=['tg3'])
        TG = ['tg0', 'tg1', 'tg2', 'tg3']
        nsel = 0
        npy = 0
        nsg = 0
        def stage1(ex_):
            nonlocal nsel
            eb = ex_ % 2
            xeT = xeTs[eb]
            for t in range(NT):
                sb_ = nsel % 2
                nsel += 1
                P.op('dve', lambda e, sb_=sb_, t=t, ex_=ex_: e.tensor_scalar(Selt[sb_][:], iota[:], rank[:, t, ex_:ex_ + 1], None, ALU.is_equal), reads=['iota', 'rank'], writes=[('Selt', sb_)])
                P.op('pe', lambda e, sb_=sb_, t=t, ex_=ex_: e.matmul(pl[0:5, 0:512], tg[:, t, ex_, :], Selt[sb_][:], start=(t == 0), stop=(t == NT - 1)),
                     reads=[('Selt', sb_)] + TG, writes=['pl'])
            P.op('act', lambda e: e.copy(row5[:], pl[0:5, 0:512]), reads=['pl'], writes=['row5'])
            for g in range(4):
                P.op('pe', lambda e, g=g: e.transpose(pl[:, g * 8:g * 8 + 5], row5[0:5, g * 128:(g + 1) * 128], idf[0:5, 0:5]), reads=['row5', 'idf'], writes=['pl'])
            P.op('dve', lambda e: e.tensor_copy(idxf[:, :, 0:5], pl[:, 0:32].rearrange('p (g c) -> p g c', c=8)[:, :, 0:5]), reads=['pl'], writes=['idxf'])
            P.op('dve', lambda e: e.scalar_tensor_tensor(idxv[:], idxf[:, :, 0], 128.0, idxf[:, :, 1], ALU.mult, ALU.add), reads=['idxf'], writes=['idxv'])
            P.op('dve', lambda e, eb=eb: e.tensor_copy(idxi[eb][:], idxv[:]), reads=['idxv'], writes=[('idxi', eb)])
            P.op('dve', lambda e, eb=eb: e.tensor_tensor(gate[eb][:], idxf[:, :, 2], idxf[:, :, 3], ALU.add), reads=['idxf'], writes=[('gate', eb)])
            P.op('dve', lambda e, eb=eb: e.tensor_tensor(gate[eb][:], gate[eb][:], idxf[:, :, 4], ALU.add), reads=['idxf', ('gate', eb)], writes=[('gate', eb)])
            for g in range(4):
                P.idma(lambda e, g=g, eb=eb: e.indirect_dma_start(out=xe[:, g, :], out_offset=None, in_=D['hb'][:, :],
                                                                   in_offset=bass.IndirectOffsetOnAxis(ap=idxi[eb][:, g:g + 1], axis=0), bounds_check=P.breg(e), oob_is_err=False),
                       reads=['d_hb', ('idxi', eb)], writes=[('xe', g)])
            for g in range(4):
                for k in range(8):
                    P.op('pe', lambda e, g=g, k=k: e.transpose(pT[:, k * 128:(k + 1) * 128], xe[:, g, k * 128:(k + 1) * 128], idb[:]), reads=[('xe', g), 'idb'], writes=['pT'])
                eng = 'act' if g % 2 == 0 else 'dve'
                if eng == 'act':
                    P.op('act', lambda e, g=g, xeT=xeT: e.copy(xeT[:, :, g * 128:(g + 1) * 128], pT[:].rearrange('p (k t) -> p k t', t=128)), reads=['pT'], writes=[('xeT', eb, g)])
                else:
                    P.op('dve', lambda e, g=g, xeT=xeT: e.tensor_copy(xeT[:, :, g * 128:(g + 1) * 128], pT[:].rearrange('p (k t) -> p k t', t=128)), reads=['pT'], writes=[('xeT', eb, g)])

        def stage2(ex_):
            nonlocal npy, nsg
            eb = ex_ % 2
            xeT = xeTs[eb]
            XET = [('xeT', eb, g) for g in range(4)]
            for fc in range(8):
                for k in range(8):
                    P.op('pe', lambda e, fc=fc, k=k, eb=eb, xeT=xeT: e.matmul(pg_[:], wg[eb][:, k, fc * 128:(fc + 1) * 128], xeT[:, k, :], start=(k == 0), stop=(k == 7)),
                         reads=XET + [('wg', eb, k)], writes=['pg'])
                for k in range(8):
                    P.op('pe', lambda e, fc=fc, k=k, eb=eb, xeT=xeT: e.matmul(pu_[:], wu[eb][:, k, fc * 128:(fc + 1) * 128], xeT[:, k, :], start=(k == 0), stop=(k == 7)),
                         reads=XET + [('wu', eb, k)], writes=['pu'])
                sb2 = nsg % 2
                nsg += 1
                P.op('act', lambda e, sb2=sb2: e.activation(sg[sb2][:], pg_[:], AF.Silu), reads=['pg'], writes=[('sg', sb2)])
                P.op('dve', lambda e, sb2=sb2, fc=fc: e.tensor_tensor(hid[:, fc, :], sg[sb2][:], pu_[:], ALU.mult), reads=[('sg', sb2), 'pu'], writes=[('hid', fc)])
            HID = [('hid', fc) for fc in range(8)]
            for g in range(4):
                yb = g % 2
                for hf in range(2):
                    pb = npy % 2
                    npy += 1
                    for fc in range(8):
                        P.op('pe', lambda e, pb=pb, fc=fc, g=g, hf=hf, eb=eb: e.matmul(py[pb][:], hid[:, fc, g * 128:(g + 1) * 128], wd[eb][:, fc, hf * 512:(hf + 1) * 512], start=(fc == 0), stop=(fc == 7)),
                             reads=HID + [('wd', eb, fc)], writes=[('py', pb)])
                    if hf == 0:
                        P.op('act', lambda e, pb=pb, yb=yb, g=g, eb=eb: e.activation(ye[yb][:, 0:512], py[pb][:], AF.Copy, scale=gate[eb][:, g:g + 1]), reads=[('py', pb), ('gate', eb)], writes=[('ye', yb, 0)])
                    else:
                        P.op('dve', lambda e, pb=pb, yb=yb, g=g, eb=eb: e.tensor_scalar(ye[yb][:, 512:1024], py[pb][:], gate[eb][:, g:g + 1], None, ALU.mult), reads=[('py', pb), ('gate', eb)], writes=[('ye', yb, 1)])
                P.idma(lambda e, g=g, eb=eb, yb=yb: e.indirect_dma_start(out=D['xw'][:, :], out_offset=bass.IndirectOffsetOnAxis(ap=idxi[eb][:, g:g + 1], axis=0), in_=ye[yb][:],
                                                                          in_offset=None, bounds_check=P.breg(e), oob_is_err=False, compute_op=ALU.add),
                       reads=[('ye', yb, 0), ('ye', yb, 1), ('idxi', eb), 'd_xw'], writes=['d_xw'])

        stage1(0)
        for ex_ in range(NE):
            if ex_ + 1 < NE:
                load_w(ex_ + 1)
                stage1(ex_ + 1)
            stage2(ex_)
        P.emit()


def phase_G(P, l, D, last):
    xdst = D['out'] if last else D['xw']
    with ExitStack() as s:
        wp = P.sb(s, 'h_wp', [128, 2, 1024], BF16)
        wgt = P.sb(s, 'h_wgt', [128, 8, 1024], BF16)
        gpl = P.sb(s, 'h_gpl', [128, 1024], F32)
        gpg = P.sb(s, 'h_gpg', [128, 1024], F32)
        idb = P.sb(s, 'h_idb', [128, 128], BF16)
        xt = [P.sb(s, 'h_xt%d' % i, [128, 1024], F32) for i in range(3)]
        pb_ = [P.sb(s, 'h_pb%d' % i, [128, 256], BF16) for i in range(3)]
        junk = P.sb(s, 'h_junk', [128, 1024], BF16)
        ss = P.sb(s, 'h_ss', [128, 4], F32)
        ssf = P.sb(s, 'h_ssf', [128, 2], F32)
        junkf = P.sb(s, 'h_junkf', [128, 1024], BF16)
        xn = P.sb(s, 'h_xn', [128, 1024], BF16)
        xTs = [P.sb(s, 'h_xT%d' % i, [128, 10, 128], BF16) for i in range(3)]
        er = P.sb(s, 'h_er', [128, 1024], F32)
        gt = P.sb(s, 'h_gt', [128, 1024], F32)
        pT = P.ps(s, 'h_pT', [128, 2048], BF16)
        pe_ = P.ps(s, 'h_pe', [128, 1024])
        pg_ = P.ps(s, 'h_pg', [128, 1024])
        for k in range(2):
            P.dma('pool', wp[:, k, :], D['w_ple'][l, k * 128:(k + 1) * 128, :], writes=[('wp', k)])
        for k in range(8):
            P.dma('pool', wgt[:, k, :], D['w_ple_gate'][l, k * 128:(k + 1) * 128, :], writes=[('wgt', k)])
        P.dma('sp', gpl[:], D['g_ple'][l].partition_broadcast(128), writes=['gpl'])
        P.dma('sp', gpg[:], D['g_ple_gate'][l].partition_broadcast(128), writes=['gpg'])
        P.dma('pool', idb[:], D['ident'], writes=['idb'])
        def front(t):
            b = t % 3
            xT = xTs[b]
            tk = slice(t * 128, (t + 1) * 128)
            P.dma('sp', xt[b][:], D['xw'][tk, :], reads=[('d_xw', t)], writes=[('xt', b)])
            P.dma('pool', pb_[b][:], D['p'][l, tk, :], writes=[('pb', b)])
            P.op('dve', lambda e: e.memset(ssf[:, 0:1], 0.0), writes=['ssf'])
            P.op('act', lambda e, b=b: e.activation(junkf[:], xt[b][:], AF.Square, accum_out=ssf[:, 0:1]), reads=[('xt', b), 'ssf'], writes=['junkf', 'ssf'])
            P.op('act', lambda e: e.activation(ssf[:, 0:1], ssf[:, 0:1], AF.Sqrt, bias=EPS, scale=1.0 / 1024), reads=['ssf'], writes=['ssf'])
            P.op('dve', lambda e: e.reciprocal(ssf[:, 0:1], ssf[:, 0:1]), reads=['ssf'], writes=['ssf'])
            P.op('dve', lambda e, b=b: e.scalar_tensor_tensor(xn[:], xt[b][:], ssf[:, 0:1], gpg[:], ALU.mult, ALU.mult), reads=[('xt', b), 'ssf', 'gpg'], writes=['xn'])
            for k in range(8):
                P.op('pe', lambda e, k=k: e.transpose(pT[:, k * 128:(k + 1) * 128], xn[:, k * 128:(k + 1) * 128], idb[:]), reads=['xn', 'idb'], writes=[('pT', 0)])
            for k in range(2):
                P.op('pe', lambda e, k=k, b=b: e.transpose(pT[:, (8 + k) * 128:(9 + k) * 128], pb_[b][:, k * 128:(k + 1) * 128], idb[:]), reads=[('pb', b), 'idb'], writes=[('pT', 1)])
            P.op('act', lambda e, xT=xT: e.copy(xT[:, 0:8, :].rearrange('p k t -> p (k t)'), pT[:, 0:1024]), reads=[('pT', 0)], writes=[('xTa', b)])
            P.op('dve', lambda e, xT=xT: e.tensor_copy(xT[:, 8:10, :].rearrange('p k t -> p (k t)'), pT[:, 1024:1280]), reads=[('pT', 1)], writes=[('xTb', b)])

        def back(t):
            b = t % 3
            xT = xTs[b]
            tk = slice(t * 128, (t + 1) * 128)
            for hf in range(2):
                for k in range(2):
                    P.op('pe', lambda e, hf=hf, k=k, xT=xT: e.matmul(pe_[:, hf * 512:(hf + 1) * 512], xT[:, 8 + k, :], wp[:, k, hf * 512:(hf + 1) * 512], start=(k == 0), stop=(k == 1)),
                         reads=[('xTb', b), ('wp', k)], writes=[('pe', hf)])
                for k in range(8):
                    P.op('pe', lambda e, hf=hf, k=k, xT=xT: e.matmul(pg_[:, hf * 512:(hf + 1) * 512], xT[:, k, :], wgt[:, k, hf * 512:(hf + 1) * 512], start=(k == 0), stop=(k == 7)),
                         reads=[('xTa', b), ('wgt', k)], writes=[('pg', hf)])
            P.op('dve', lambda e: e.memset(ss[:, 1:3], 0.0), reads=['ss'], writes=['ss'])
            for hf in range(2):
                P.op('act', lambda e, hf=hf: e.activation(junk[:, hf * 512:(hf + 1) * 512], pe_[:, hf * 512:(hf + 1) * 512], AF.Square, accum_out=ss[:, 1 + hf:2 + hf]), reads=[('pe', hf), 'ss'], writes=['junk', 'ss'])
            P.op('dve', lambda e: e.tensor_tensor(ss[:, 1:2], ss[:, 1:2], ss[:, 2:3], ALU.add), reads=['ss'], writes=['ss'])
            P.op('act', lambda e: e.activation(ss[:, 1:2], ss[:, 1:2], AF.Sqrt, bias=EPS, scale=1.0 / 1024), reads=['ss'], writes=['ss'])
            P.op('dve', lambda e: e.reciprocal(ss[:, 1:2], ss[:, 1:2]), reads=['ss'], writes=['ss'])
            for hf in range(2):
                hs = slice(hf * 512, (hf + 1) * 512)
                P.op('dve', lambda e, hs=hs: e.scalar_tensor_tensor(er[:, hs], pe_[:, hs], ss[:, 1:2], gpl[:, hs], ALU.mult, ALU.mult), reads=[('pe', hf), 'ss', 'gpl'], writes=['er'])
                P.op('act', lambda e, hs=hs: e.activation(gt[:, hs], pg_[:, hs], AF.Sigmoid), reads=[('pg', hf)], writes=['gt'])
            P.op('dve', lambda e: e.tensor_tensor(er[:], er[:], gt[:], ALU.mult), reads=['er', 'gt'], writes=['er'])
            P.op('dve', lambda e, b=b: e.tensor_tensor(xt[b][:], xt[b][:], er[:], ALU.add), reads=['er', ('xt', b)], writes=[('xt', b)])
            P.dma('sp', xdst[tk, :], xt[b][:], reads=[('xt', b)], writes=[('d_xw', t)])

        front(0)
        front(1)
        for t in range(NT):
            if t + 2 < NT:
                front(t + 2)
            back(t)
        P.emit()


WEIGHTS = [('g_mix', [4, 1024]), ('w_in', [4, 1024, 3224]), ('ln_v_g', [4, 4, 64]), ('ln_v_b', [4, 4, 64]), ('w_s', [4, 4, 128, 128]),
           ('b_s', [4, 4, 128]), ('q_norm_g', [4, 64]), ('k_norm_g', [4, 64]), ('conv_w', [4, 5, 1152]), ('a_log', [4, 2, 6]),
           ('dt_bias', [4, 2, 6]), ('o_norm_g', [4, 64]), ('w_out', [4, 1024, 1024]), ('g_ffn', [4, 1024]), ('w_router', [4, 1024, 16]),
           ('w_e_gate', [4, 16, 1024, 1024]), ('w_e_up', [4, 16, 1024, 1024]), ('w_e_down', [4, 16, 1024, 1024]), ('w_ple', [4, 256, 1024]),
           ('g_ple', [4, 1024]), ('g_ple_gate', [4, 1024]), ('w_ple_gate', [4, 1024, 1024])]


def make_consts():
    c = {}
    c['ident'] = np.eye(128, dtype=np.float32)
    c['ones64'] = np.ones((64, 64), np.float32)
    c['ones128'] = np.ones((128, 128), np.float32)
    half = 8
    c['invf'] = (np.float32(500000.0) ** (-np.arange(half, dtype=np.float32) * np.float32(2.0) / np.float32(16))).astype(np.float32)
    a = np.arange(128)[:, None]
    b = np.arange(128)[None, :]
    mA = (a >= b).astype(np.float32)
    mB = (a <= b).astype(np.float32)
    c['mab'] = np.concatenate([mA, mB, mA, mB], axis=1)
    sel = np.zeros((65, 64), np.float32)
    sel[64, :] = 1.0
    c['sel65'] = sel
    p = np.arange(64)[:, None]
    f = np.arange(64)[None, :]
    c['triF'] = (p <= f).astype(np.float32)
    c['triB'] = (p >= f).astype(np.float32)

    def m12(fw, bw):
        return np.ascontiguousarray(np.stack([fw] * 6 + [bw] * 6, axis=1).astype(np.float32))
    c['mW'] = m12(f > p, f < p)
    c['mWt'] = m12(p > f, p < f)
    c['mI'] = m12(f >= p, f <= p)
    c['triS'] = (a < b).astype(np.float32)
    c['iota512'] = np.ascontiguousarray(np.broadcast_to(np.arange(512, dtype=np.float32)[None, :], (128, 512)))
    tpv = np.zeros((128, NT, 2), np.float32)
    tpv[:, :, 0] = np.arange(NT)[None, :]
    tpv[:, :, 1] = np.arange(128)[:, None]
    c['tp'] = tpv
    return c


SCRATCH = [('cs', [S, 16], F32), ('vn', [S, 256], BF16), ('qkT', [6, 128, S], BF16), ('vaug', [S, 390], BF16), ('gate_s', [S, 384], F32),
           ('ab', [S, 24], F32), ('uT', [256, S], F32), ('cT', [1152, S], F32), ('yT', [1024, S], BF16), ('v_tm', [S, 384], F32),
           ('k_tm', [S, 384], F32), ('qT_g', [384, S], BF16), ('kT_g', [384, S], BF16), ('gb', [S, 24], F32), ('o_fb', [2, S, 384], F32),
           ('xw', [S, 1024], F32), ('hb', [S, 1024], BF16)]


def build(n_layers=4, phases=None, dbg=()):
    P = Prog()
    D = {}
    D['x'] = P.dram('x', [S, 1024], F32, 'ExternalInput')
    D['p'] = P.dram('p', [n_layers, S, 256], F32, 'ExternalInput')
    D['positions'] = P.dram('positions', [128, NT], I32, 'ExternalInput')
    for n, shp in WEIGHTS:
        D[n] = P.dram(n, [n_layers] + list(shp[1:]), F32, 'ExternalInput')
    for n, v in make_consts().items():
        D[n] = P.dram(n, list(v.shape), F32, 'ExternalInput')
    for n, shp, dt in SCRATCH:
        D[n] = P.dram(n, shp, dt, 'ExternalOutput' if n in dbg else 'Internal')
    D['out'] = P.dram('out', [S, 1024], F32, 'ExternalOutput')
    allp = phases is None
    if allp or 'R' in phases:
        phase_rope(P, D)
    for l in range(n_layers):
        first = (l == 0)
        last = (l == n_layers - 1)
        if allp or 'A' in phases:
            phase_A(P, l, D, first)
        if allp or 'B' in phases:
            phase_B(P, l, D)
        if allp or 'C' in phases:
            phase_C(P, l, D)
        if allp or 'D1' in phases:
            phase_D1(P, l, D)
        if allp or 'D2' in phases:
            phase_D2(P, l, D)
        if allp or 'E' in phases:
            phase_E(P, l, D, first)
        if allp or 'F' in phases:
            phase_F(P, l, D)
        if allp or 'G' in phases:
            phase_G(P, l, D, last and allp)
    return P


def kernel(**inputs):
    n = 8
    P = build(4)
    consts = make_consts()
    shared = {k: np.ascontiguousarray(np.asarray(inputs[k], dtype=np.float32)) for k, _ in WEIGHTS}
    shared.update(consts)
    x = np.asarray(inputs['x'], dtype=np.float32)
    p = np.asarray(inputs['p'], dtype=np.float32)
    pos = np.asarray(inputs['positions']).astype(np.int32)
    in_maps = []
    for c in range(n):
        m = dict(shared)
        m['x'] = np.ascontiguousarray(x[c])
        m['p'] = np.ascontiguousarray(p[:, c])
        m['positions'] = np.ascontiguousarray(pos[c].reshape(NT, 128).T)
        in_maps.append(m)
    res = run_bass_kernel_spmd(P.nc, in_maps, core_ids=list(range(n)))
    return np.stack([np.asarray(res.results[c]['out'], dtype=np.float32) for c in range(n)], axis=0)
```
